# Optimizing a Trainium2 kernel written in Bass

```python
import jax, jax.numpy as jnp
from jax import lax
import numpy as np

D_MODEL = 1024
BATCH = 8
SEQ = 4096
DEPTH = 1

MEM_TOKENS = 256
MEM_HEADS = 4
MEM_HEAD_DIM = D_MODEL // MEM_HEADS
POOL_WIDTH = D_MODEL // 2
POOL_WINDOWS = (2, 4, 8, 16)
POOL_GROUPS = len(POOL_WINDOWS)
POOL_GROUP_DIM = POOL_WIDTH // POOL_GROUPS
ATT_HEADS = 8
ATT_HEAD_DIM = 64
ATT_WIDTH = ATT_HEADS * ATT_HEAD_DIM
KV_RANK = 128
IDX_HEADS = 8
IDX_HEAD_DIM = 64
TOPK_MAX = 256
Q_BLOCK = 128
MIX_WIDTH = POOL_WIDTH + ATT_WIDTH
IN_SPLITS = (POOL_WIDTH, ATT_WIDTH, KV_RANK, IDX_HEADS * IDX_HEAD_DIM, IDX_HEAD_DIM, IDX_HEADS)
IN_WIDTH = sum(IN_SPLITS)
N_EXPERTS = 32
TOP_K_EXPERTS = 4
D_FF = D_MODEL
SWIGLU_LIMIT = 7.0
SWIGLU_ALPHA = 1.702
MOE_BLOCK = 128
LN_EPS = 1e-5
RMS_EPS = 1e-6
DN_ALPHA = (2 * DEPTH) ** 0.25
DN_BETA = (8 * DEPTH) ** -0.25

kernel_name = 'hybrid_pool_dsa_moe_deepnorm'


def layer_norm(x, g, b):
    xf = x.astype(jnp.float32)
    mu = jnp.mean(xf, axis=-1, keepdims=True)
    var = jnp.mean(jnp.square(xf - mu), axis=-1, keepdims=True)
    y = (xf - mu) * lax.rsqrt(var + LN_EPS) * g.astype(jnp.float32) + b.astype(jnp.float32)
    return y.astype(x.dtype)


def rms_norm(x, g):
    xf = x.astype(jnp.float32)
    y = xf * lax.rsqrt(jnp.mean(jnp.square(xf), axis=-1, keepdims=True) + RMS_EPS) * g.astype(jnp.float32)
    return y.astype(x.dtype)


def pool_mixer(u, w_pool, pool_scale):
    B, L, _ = u.shape
    uf = u.astype(jnp.float32).reshape(B, L, POOL_GROUPS, POOL_GROUP_DIM)
    csum = jnp.concatenate([jnp.zeros_like(uf[:, :1]), jnp.cumsum(uf, axis=1)], axis=1)
    pos = jnp.arange(L)
    outs = []
    for g, w in enumerate(POOL_WINDOWS):
        lo = jnp.maximum(pos + 1 - w, 0)
        cnt = jnp.minimum(pos + 1, w).astype(jnp.float32)
        mean = (csum[:, 1:, g] - csum[:, lo, g]) / cnt[None, :, None]
        outs.append(mean - uf[:, :, g])
    d = jnp.stack(outs, axis=2).astype(u.dtype)
    y = jnp.einsum('blgc,gcd->blgd', d, w_pool).reshape(B, L, POOL_WIDTH)
    return y * pool_scale


def dsa_attention(q, c_kv, q_idx, k_idx, w_idx, w_uk, w_uv):
    B, L = q.shape[0], q.shape[1]
    top_k = min(TOPK_MAX, L // 4)
    nb = L // Q_BLOCK
    q_lat = jnp.einsum('blhd,hdr->blhr', q, w_uk)
    idx_scale = (IDX_HEAD_DIM ** -0.5) * (IDX_HEADS ** -0.5)
    att_scale = ATT_HEAD_DIM ** -0.5
    key_pos = jnp.arange(L)
    k_idx_f = k_idx.astype(jnp.float32)

    def to_blocks(a):
        return a.reshape((B, nb, Q_BLOCK) + a.shape[2:]).swapaxes(0, 1)

    def block(args):
        qi, wi, ql, start = args
        qpos = start + jnp.arange(Q_BLOCK)
        s = jnp.einsum('bqhd,bkd->bqhk', qi.astype(jnp.float32), k_idx_f)
        score = jnp.einsum('bqhk,bqh->bqk', jax.nn.relu(s), wi.astype(jnp.float32)) * idx_scale
        causal = key_pos[None, :] <= qpos[:, None]
        score = jnp.where(causal[None], score, -jnp.inf)
        _, sel = lax.top_k(score, top_k)
        valid = sel <= qpos[None, :, None]
        c_sel = jax.vmap(lambda c, i: c[i])(c_kv, sel)
        logits = jnp.einsum('bqhr,bqkr->bqhk', ql, c_sel).astype(jnp.float32) * att_scale
        logits = jnp.where(valid[:, :, None, :], logits, -jnp.inf)
        p = jax.nn.softmax(logits, axis=-1).astype(c_sel.dtype)
        return jnp.einsum('bqhk,bqkr->bqhr', p, c_sel)

    starts = jnp.arange(nb) * Q_BLOCK
    o_lat = lax.map(block, (to_blocks(q_idx), to_blocks(w_idx), to_blocks(q_lat), starts))
    o_lat = o_lat.swapaxes(0, 1).reshape(B, L, ATT_HEADS, KV_RANK)
    return jnp.einsum('blhr,hrd->blhd', o_lat, w_uv).reshape(B, L, ATT_WIDTH)


def memory_attention(h, mem, w_mq, w_mkv, w_mo):
    B, L, _ = h.shape
    M = mem.shape[1]
    q = (h @ w_mq).reshape(B, L, MEM_HEADS, MEM_HEAD_DIM)
    kv = (mem @ w_mkv).reshape(B, M, 2, MEM_HEADS, MEM_HEAD_DIM)
    k, v = kv[:, :, 0], kv[:, :, 1]
    logits = jnp.einsum('blhd,bmhd->bhlm', q, k).astype(jnp.float32) * (MEM_HEAD_DIM ** -0.5)
    p = jax.nn.softmax(logits, axis=-1).astype(v.dtype)
    o = jnp.einsum('bhlm,bmhd->blhd', p, v).reshape(B, L, D_MODEL)
    return o @ w_mo


def moe_ffn(h, w_router, b_router, w_gate_up, b_gate_up, w_down, b_down):
    B, L, D = h.shape
    hf = h.reshape(-1, D)
    T = hf.shape[0]
    logits = hf.astype(jnp.float32) @ w_router.astype(jnp.float32) + b_router.astype(jnp.float32)
    top_val, top_exp = lax.top_k(logits, TOP_K_EXPERTS)
    gates = jax.nn.softmax(top_val, axis=-1)
    M = T * TOP_K_EXPERTS
    flat_exp = top_exp.reshape(M)
    flat_tok = jnp.arange(M) // TOP_K_EXPERTS
    flat_gate = gates.reshape(M)
    order = jnp.argsort(flat_exp)
    sorted_exp = flat_exp[order]
    counts = jnp.zeros((N_EXPERTS,), jnp.int32).at[flat_exp].add(1)
    padded = (counts + MOE_BLOCK - 1) // MOE_BLOCK * MOE_BLOCK
    pad_end = jnp.cumsum(padded)
    pad_start = pad_end - padded
    start = jnp.cumsum(counts) - counts
    dest = pad_start[sorted_exp] + jnp.arange(M) - start[sorted_exp]
    P = M + N_EXPERTS * MOE_BLOCK
    n_blocks = P // MOE_BLOCK
    row_tok = jnp.zeros((P,), jnp.int32).at[dest].set(flat_tok[order])
    row_gate = jnp.zeros((P,), jnp.float32).at[dest].set(flat_gate[order])
    block_exp = jnp.minimum(jnp.searchsorted(pad_end, jnp.arange(n_blocks) * MOE_BLOCK, side='right'), N_EXPERTS - 1)

    def expert_block(args):
        tok, gate, e = args
        xb = hf[tok]
        gu = xb @ w_gate_up[e] + b_gate_up[e]
        g, u = jnp.split(gu, 2, axis=-1)
        g = jnp.minimum(g, SWIGLU_LIMIT)
        u = jnp.clip(u, -SWIGLU_LIMIT, SWIGLU_LIMIT)
        act = (u + 1.0) * g * jax.nn.sigmoid(SWIGLU_ALPHA * g)
        y = act @ w_down[e] + b_down[e]
        return y.astype(jnp.float32) * gate[:, None]

    y = lax.map(expert_block, (row_tok.reshape(n_blocks, MOE_BLOCK), row_gate.reshape(n_blocks, MOE_BLOCK), block_exp))
    out = jnp.zeros((T, D), jnp.float32).at[row_tok].add(y.reshape(P, D))
    return out.astype(h.dtype).reshape(B, L, D)


def setup_inputs(seed: int = 0) -> dict:
    key = jax.random.key(seed)
    ks = jax.random.split(key, 26)
    f32 = jnp.float32

    def nrm(k, shape, scale):
        return jax.random.normal(k, shape, f32) * scale

    def gain(k, shape):
        return 1.0 + 0.02 * jax.random.normal(k, shape, f32)

    Ly = DEPTH
    return {
        'x': nrm(ks[0], (BATCH, SEQ, D_MODEL), 1.0),
        'mem': nrm(ks[1], (BATCH, MEM_TOKENS, D_MODEL), 1.0),
        'w_in': nrm(ks[2], (Ly, D_MODEL, IN_WIDTH), D_MODEL ** -0.5),
        'w_pool': nrm(ks[3], (Ly, POOL_GROUPS, POOL_GROUP_DIM, POOL_GROUP_DIM), POOL_GROUP_DIM ** -0.5),
        'pool_scale': gain(ks[4], (Ly, POOL_WIDTH)),
        'idx_k_norm_g': gain(ks[5], (Ly, IDX_HEAD_DIM)),
        'idx_k_norm_b': nrm(ks[6], (Ly, IDX_HEAD_DIM), 0.02),
        'kv_norm_g': gain(ks[7], (Ly, KV_RANK)),
        'w_uk': nrm(ks[8], (Ly, ATT_HEADS, ATT_HEAD_DIM, KV_RANK), ATT_HEAD_DIM ** -0.5),
        'w_uv': nrm(ks[9], (Ly, ATT_HEADS, KV_RANK, ATT_HEAD_DIM), KV_RANK ** -0.5),
        'w_o': nrm(ks[10], (Ly, MIX_WIDTH, D_MODEL), DN_BETA * MIX_WIDTH ** -0.5),
        'ln1_g': gain(ks[11], (Ly, D_MODEL)),
        'ln1_b': nrm(ks[12], (Ly, D_MODEL), 0.02),
        'w_mq': nrm(ks[13], (Ly, D_MODEL, D_MODEL), D_MODEL ** -0.5),
        'w_mkv': nrm(ks[14], (Ly, D_MODEL, 2 * D_MODEL), D_MODEL ** -0.5),
        'w_mo': nrm(ks[15], (Ly, D_MODEL, D_MODEL), DN_BETA * D_MODEL ** -0.5),
        'ln2_g': gain(ks[16], (Ly, D_MODEL)),
        'ln2_b': nrm(ks[17], (Ly, D_MODEL), 0.02),
        'w_router': nrm(ks[18], (Ly, D_MODEL, N_EXPERTS), D_MODEL ** -0.5),
        'b_router': nrm(ks[19], (Ly, N_EXPERTS), 0.01),
        'w_gate_up': nrm(ks[20], (Ly, N_EXPERTS, D_MODEL, 2 * D_FF), D_MODEL ** -0.5),
        'b_gate_up': nrm(ks[21], (Ly, N_EXPERTS, 2 * D_FF), 0.01),
        'w_down': nrm(ks[22], (Ly, N_EXPERTS, D_FF, D_MODEL), DN_BETA * D_FF ** -0.5),
        'b_down': nrm(ks[23], (Ly, N_EXPERTS, D_MODEL), 0.01),
        'ln3_g': gain(ks[24], (Ly, D_MODEL)),
        'ln3_b': nrm(ks[25], (Ly, D_MODEL), 0.02),
    }


def reference(x, mem, w_in, w_pool, pool_scale, idx_k_norm_g, idx_k_norm_b, kv_norm_g, w_uk, w_uv, w_o,
              ln1_g, ln1_b, w_mq, w_mkv, w_mo, ln2_g, ln2_b, w_router, b_router, w_gate_up, b_gate_up,
              w_down, b_down, ln3_g, ln3_b):
    B, L, _ = x.shape
    split_points = np.cumsum(IN_SPLITS)[:-1].tolist()
    for l in range(DEPTH):
        proj = x @ w_in[l]
        u_pool, q, c_kv, q_idx, k_idx, w_idx = jnp.split(proj, split_points, axis=-1)
        q = q.reshape(B, L, ATT_HEADS, ATT_HEAD_DIM)
        c_kv = rms_norm(c_kv, kv_norm_g[l])
        q_idx = q_idx.reshape(B, L, IDX_HEADS, IDX_HEAD_DIM)
        k_idx = layer_norm(k_idx, idx_k_norm_g[l], idx_k_norm_b[l])
        y_pool = pool_mixer(u_pool, w_pool[l], pool_scale[l])
        y_att = dsa_attention(q, c_kv, q_idx, k_idx, w_idx, w_uk[l], w_uv[l])
        mix = jnp.concatenate([y_pool, y_att], axis=-1) @ w_o[l]
        x = layer_norm(DN_ALPHA * x + mix, ln1_g[l], ln1_b[l])
        x = layer_norm(DN_ALPHA * x + memory_attention(x, mem, w_mq[l], w_mkv[l], w_mo[l]), ln2_g[l], ln2_b[l])
        y_moe = moe_ffn(x, w_router[l], b_router[l], w_gate_up[l], b_gate_up[l], w_down[l], b_down[l])
        x = layer_norm(DN_ALPHA * x + y_moe, ln3_g[l], ln3_b[l])
    return x
```

```python
import numpy as np
from contextlib import ExitStack
import concourse.bass as bass
import concourse.mybir as mybir
from concourse.bass_utils import run_bass_kernel_spmd

F32 = mybir.dt.float32
BF16 = mybir.dt.bfloat16
I32 = mybir.dt.int32
AF = mybir.ActivationFunctionType
ALU = mybir.AluOpType

T = 4096
NT = 32
D = 1024
CH = 512
NCH = 8
INW = 1736
CAP = 768
NE = 32
ALPHA = 2.0 ** 0.25
NEG = -1.0e30
NEG2 = -3.0e30
KBIS = 25
SIGMAX = float(1.0 / (1.0 + np.exp(-1.702 * 7.0)))

DEBUG = None
NE_RUN = NE
P2_STEPS = 9
P2_EW = 63


class Sched:
    EPOCH = 30000
    ENG = ("sync", "scalar", "vector", "gpsimd", "tensor")

    def __init__(self, sem_pool):
        self.ops = {e: [] for e in self.ENG}
        self.cnt = {}
        self.lastw = {}
        self.readers = {}
        self.seen = {e: {} for e in self.ENG}
        self.sem_pool = list(sem_pool)
        self.sems = {}

    def _sem(self, counter, val):
        if counter.startswith("E:"):
            ep = (val - 1) // self.EPOCH
            name, lv = f"{counter}#{ep}", val - ep * self.EPOCH
        else:
            name, lv = counter, val
        if name not in self.sems:
            self.sems[name] = self.sem_pool.pop()
        return self.sems[name], lv

    def _waits(self, eng, reads, writes, extra=()):
        need = {}
        def add(cv):
            c, v = cv
            if v > need.get(c, 0):
                need[c] = v
        for r in reads:
            if r in self.lastw:
                add(self.lastw[r])
        for w in writes:
            if w in self.lastw:
                add(self.lastw[w])
            for cv in self.readers.get(w, {}).items():
                add(cv)
        for cv in extra:
            add(cv)
        waits = []
        for c, v in need.items():
            if eng == "tensor" and c == "E:tensor":
                continue
            if self.seen[eng].get(c, 0) < v:
                self.seen[eng][c] = v
                waits.append(self._sem(c, v))
        return waits

    def _book(self, c, v, reads, writes):
        for r in reads:
            d = self.readers.setdefault(r, {})
            if d.get(c, 0) < v:
                d[c] = v
        for w in writes:
            self.lastw[w] = (c, v)
            self.readers[w] = {}

    def op(self, eng, fn, reads=(), writes=()):
        waits = self._waits(eng, reads, writes)
        c = "E:" + eng
        v = self.cnt.get(c, 0) + 1
        self.cnt[c] = v
        sem, _ = self._sem(c, v)
        self.ops[eng].append((waits, fn, sem, 1))
        self._book(c, v, reads, writes)

    def dma(self, eng, fn, reads=(), writes=(), key="d", serialize=True):
        c = "D:" + key
        prev = self.cnt.get(c, 0)
        waits = self._waits(eng, reads, writes, extra=[(c, prev)] if (prev and serialize) else [])
        v = prev + 16
        self.cnt[c] = v
        sem, _ = self._sem(c, v)
        self.ops[eng].append((waits, fn, sem, 16))
        self._book(c, v, reads, writes)

    def barrier_all(self, eng="sync"):
        waits = []
        for c, v in self.cnt.items():
            if v and self.seen[eng].get(c, 0) < v:
                self.seen[eng][c] = v
                waits.append(self._sem(c, v))
        self.ops[eng].append((waits, None, None, 0))

    def full_barrier(self):
        for e in self.ENG:
            self.barrier_all(e)

    def flush(self, nc):
        with nc.Block() as blk:
            for eng in self.ENG:
                lst = self.ops[eng]
                if not lst:
                    continue
                def body(e, lst=lst):
                    for waits, fn, sem, inc in lst:
                        for s, v in waits:
                            e.wait_ge(s, v)
                        if fn is not None:
                            ins = fn(e)
                            ins.then_inc(sem, inc)
                getattr(blk, eng)(body)
        self.ops = {e: [] for e in self.ENG}


class Rot:
    def __init__(self, items):
        self.items = items
        self.i = 0
    def next(self):
        it = self.items[self.i % len(self.items)]
        self.i += 1
        return it


def build_program():
    nc = bass.Bass("TRN2", target_bir_lowering=False)
    dt = lambda name, shape, dtype=F32, kind="ExternalInput": nc.dram_tensor(name, shape, dtype, kind=kind).ap()
    x_d = dt("x", [T, D])
    mem_d = dt("mem", [256, D])
    w_in_d = dt("w_in", [D, INW])
    w_pool_d = dt("w_pool", [4, 128, 128])
    pool_scale_d = dt("pool_scale", [512])
    kig_d = dt("idx_k_norm_g", [64])
    kib_d = dt("idx_k_norm_b", [64])
    kvg_d = dt("kv_norm_g", [128])
    w_uk_d = dt("w_uk", [8, 64, 128])
    w_uv_d = dt("w_uv", [8, 128, 64])
    w_o_d = dt("w_o", [D, D])
    ln1g_d = dt("ln1_g", [D]); ln1b_d = dt("ln1_b", [D])
    w_mq_d = dt("w_mq", [D, D])
    w_mkv_d = dt("w_mkv", [D, 2 * D])
    w_mo_d = dt("w_mo", [D, D])
    ln2g_d = dt("ln2_g", [D]); ln2b_d = dt("ln2_b", [D])
    w_r_d = dt("w_router", [D, NE])
    b_r_d = dt("b_router", [NE])
    w_gu_d = dt("w_gate_up", [NE, D, 2 * D])
    b_gu_d = dt("b_gate_up", [NE, 2 * D])
    w_dn_d = dt("w_down", [NE, D, D])
    b_dn_d = dt("b_down", [NE, D])
    ln3g_d = dt("ln3_g", [D]); ln3b_d = dt("ln3_b", [D])
    out_d = dt("out", [T, D], F32, "ExternalOutput")
    h1_d = dt("h1_scr", [T, D], F32, "Internal")
    h2_d = dt("h2_scr", [T, D], F32, "Internal")
    xg_d = dt("xg_scr", [NE * CAP, D], BF16, "Internal")
    ys_d = dt("ys_scr", [NE * CAP, D], F32, "Internal")
    dbg = {}
    if DEBUG:
        dbg["h"] = dt("dbg_h", [T, D], F32, "ExternalOutput")
        dbg["mixT"] = dt("dbg_mixT", [NCH, 128, 8 * CH], BF16, "ExternalOutput")

    dumps = []
    def dump(S, name, ap2d, shape, dtype, reads):
        if not DEBUG:
            return
        d = dt("dbg_" + name, shape, dtype, "ExternalOutput")
        S.dma("sync", lambda e: e.dma_start(out=d, in_=ap2d), reads=reads, key="dbgd")

    def bc(ap1d, n):
        return ap1d.rearrange("(o n) -> o n", o=1).to_broadcast([128, n])

    with ExitStack() as top:
        sem_pool = [top.enter_context(nc.semaphore(f"s{i}")) for i in range(96)]
        S = Sched(sem_pool)
        sb = lambda es, name, shape, dtype=F32: es.enter_context(nc.sbuf_tensor(name, shape, dtype))
        ps = lambda es, name, shape, dtype=F32: es.enter_context(nc.psum_tensor(name, shape, dtype))

        ident_b = sb(top, "ident_b", [128, 128], BF16)
        ident_f = sb(top, "ident_f", [128, 128], F32)
        ones_b = sb(top, "ones_b", [128, 128], BF16)
        slots_all = sb(top, "slots_all", [128, NT, 4], I32)
        gates_all = sb(top, "gates_all", [128, NT, 4], F32)

        S.op("gpsimd", lambda e: e.memset(ident_f[:], 0.0), writes=["ident_f"])
        S.op("gpsimd", lambda e: e.affine_select(out=ident_f[:], in_=ident_f[:], pattern=[[-1, 128]],
                                                   compare_op=ALU.not_equal, fill=1.0, base=0, channel_multiplier=1),
             reads=["ident_f"], writes=["ident_f"])
        S.op("vector", lambda e: e.tensor_copy(out=ident_b[:], in_=ident_f[:]), reads=["ident_f"], writes=["ident_b"])
        S.op("vector", lambda e: e.memset(ones_b[:], 1.0), writes=["ones_b"])

        def ln_rstd(var_ap, rstd_ap, tag, scale, rname, wname):
            S.op("scalar", lambda e: e.activation(out=rstd_ap, in_=var_ap, func=AF.Ln, bias=eps_tile[:, tag:tag + 1], scale=scale),
                 reads=[rname, "eps", wname], writes=[wname])
            S.op("scalar", lambda e: e.activation(out=rstd_ap, in_=rstd_ap, func=AF.Exp, scale=-0.5),
                 reads=[wname], writes=[wname])

        eps_tile = sb(top, "eps", [128, 2], F32)
        S.op("vector", lambda e: e.memset(eps_tile[:, 0:1], 1e-5), writes=["eps"])
        S.op("vector", lambda e: e.memset(eps_tile[:, 1:2], 1e-6), reads=["eps"], writes=["eps"])

        def layer_norm_tile(r_ap, out_ap, g_bc, b_bc, stats, mv, rstd, tmp_ap, names):
            rn, on, tn = names
            for hf in range(2):
                S.op("vector", lambda e, hf=hf: e.bn_stats(out=stats[:, hf, :], in_=r_ap[:, hf * 512:(hf + 1) * 512]),
                     reads=[rn], writes=[stats.name])
            S.op("vector", lambda e: e.bn_aggr(out=mv[:], in_=stats[:].rearrange("p a b -> p (a b)")),
                 reads=[stats.name], writes=[mv.name])
            ln_rstd(mv[:, 1:2], rstd[:, 0:1], 0, 1.0, mv.name, rstd.name)
            S.op("vector", lambda e: e.tensor_scalar(out=tmp_ap, in0=r_ap, scalar1=mv[:, 0:1], scalar2=rstd[:, 0:1],
                                                      op0=ALU.subtract, op1=ALU.mult),
                 reads=[rn, mv.name, rstd.name], writes=[tn])
            S.op("vector", lambda e: e.tensor_tensor(out=tmp_ap, in0=tmp_ap, in1=g_bc[:], op=ALU.mult),
                 reads=[tn, g_bc.name], writes=[tn])
            S.op("vector", lambda e: e.tensor_tensor(out=out_ap, in0=tmp_ap, in1=b_bc[:], op=ALU.add),
                 reads=[tn, b_bc.name], writes=[on])

        with ExitStack() as p1:
            w_in_sb = sb(p1, "w_in_sb", [128, 8, INW], BF16)
            w_o_sb = sb(p1, "w_o_sb", [128, 8, D], BF16)
            wpool_sb = sb(p1, "wpool_sb", [128, 4, 128], BF16)
            wuk_sb = sb(p1, "wuk_sb", [128, 4, 128], BF16)
            wuv_sb = sb(p1, "wuv_sb", [128, 4, 2, 128], BF16)
            pscale_sb = sb(p1, "pscale_sb", [128, 4], F32)
            g1_bc = sb(p1, "g1_bc", [128, D], F32)
            b1_bc = sb(p1, "b1_bc", [128, D], F32)
            kvg_bc = sb(p1, "kvg_bc", [128, 128], F32)
            kig_bc = sb(p1, "kig_bc", [128, 64], F32)
            kib_bc = sb(p1, "kib_bc", [128, 64], F32)
            ic16 = sb(p1, "ic16", [128, 4, 16], F32)
            ckv1 = sb(p1, "ckv1", [128, NT, 130], BF16)
            ckvT = sb(p1, "ckvT", [128, T], BF16)
            kiT = sb(p1, "kiT", [128, T], BF16)
            widx = sb(p1, "widx", [128, NT, 8], F32)
            xt = [sb(p1, f"xt{i}", [128, D], F32) for i in range(2)]
            xb = [sb(p1, f"xb{i}", [128, D], BF16) for i in range(2)]
            xT = sb(p1, "xT", [128, 8, CH], BF16)
            ug = sb(p1, "ug", [128, 528], F32)
            halo = sb(p1, "halo", [128, 4, 16], F32)
            pA = sb(p1, "pA", [128, 528], F32)
            pB = sb(p1, "pB", [128, 528], F32)
            dT = sb(p1, "dT", [128, CH], BF16)
            qTs = [sb(p1, f"qT{i}", [128, 4, CH], BF16) for i in range(2)]
            qiT = sb(p1, "qiT", [128, 4, CH], BF16)
            mixTs = [sb(p1, f"mixT{i}", [128, 8, CH], BF16) for i in range(2)]
            SC = sb(p1, "SC", [128, T], F32)
            bmax = sb(p1, "bmax", [128, 1], F32)
            bmid = sb(p1, "bmid", [128, 1], F32)
            bcnt = sb(p1, "bcnt", [128, 1], F32)
            bd = sb(p1, "bd", [128, 1], F32)
            bnegl = sb(p1, "bnegl", [128, 1], F32)
            c256 = sb(p1, "c256", [128, 1], F32)
            pw2 = sb(p1, "pw2", [128, KBIS], F32)
            bsteps = sb(p1, "bsteps", [128, KBIS], F32)
            bnegh = sb(p1, "bnegh", [128, KBIS], F32)
            m128 = [sb(p1, f"m128_{i}", [128, 128], BF16) for i in range(2)]
            maskTs = [sb(p1, f"maskT{i}", [128, NT, CH], mybir.dt.uint8) for i in range(2)]
            rl = [sb(p1, f"rl{i}", [128, CH], F32) for i in range(2)]
            Eb = [sb(p1, f"Eb{i}", [128, CH], BF16) for i in range(2)]
            Pb = [sb(p1, f"Pb{i}", [128, CH], BF16) for i in range(2)]
            qlat = [sb(p1, f"qlat{i}", [128, CH], BF16) for i in range(2)]
            olat = [sb(p1, f"olat{i}", [128, 128], BF16) for i in range(2)]
            rden = sb(p1, "rden", [128, 4], F32)
            ckn = sb(p1, "ckn", [128, 128], BF16)
            kn32 = sb(p1, "kn32", [128, 64], F32)
            kn2 = sb(p1, "kn2", [128, 128], BF16)
            tm = sb(p1, "tm", [128, 200], F32)
            olT2 = sb(p1, "olT2", [128, 2, CH], BF16)
            junk = sb(p1, "junk", [128, 128], F32)
            st1 = sb(p1, "st1", [128, 8], F32)
            bst = sb(p1, "bst", [128, 2, 6], F32)
            bmv = sb(p1, "bmv", [128, 2], F32)
            brs = sb(p1, "brs", [128, 1], F32)
            r1 = sb(p1, "r1", [128, D], F32)
            h1o = [r1, r1]
            mm = [ps(p1, f"mm{i}", [128, 512], F32) for i in range(2)]
            oacc = [ps(p1, f"oacc{i}", [128, 2, 512], F32) for i in range(2)]
            tp = [ps(p1, f"tp{i}", [128, 1024], BF16) for i in range(2)]
            mmr = Rot([0, 1]); tpr = Rot([0, 1])

            S.op("gpsimd", lambda e: e.memset(maskTs[1][:], 0), writes=["maskT1"])
            zsrc = maskTs[1][:].rearrange("p a b -> p (a b)").bitcast(BF16).rearrange("p (t d) -> p t d", d=D)
            for zi in range(NE * CAP // 1024):
                S.dma("scalar", lambda e, zi=zi: e.dma_start(out=xg_d[zi * 1024:(zi + 1) * 1024, :].rearrange("(t p) d -> p t d", p=128), in_=zsrc),
                      reads=["maskT1"], key="zf", serialize=False)
            S.lastw["xg_d"] = ("D:zf", S.cnt["D:zf"])
            S.dma("gpsimd", lambda e: e.dma_start(out=w_in_sb[:], in_=w_in_d.rearrange("(k p) n -> p k n", p=128)),
                  writes=["w_in_sb"], key="w0")
            S.dma("gpsimd", lambda e: e.dma_start(out=wpool_sb[:], in_=w_pool_d.rearrange("g c d -> c g d")),
                  writes=["wpool_sb"], key="w1")
            S.dma("gpsimd", lambda e: e.dma_start(out=wuk_sb[:], in_=w_uk_d.rearrange("(hp h2) d r -> (h2 d) hp r", h2=2)),
                  writes=["wuk_sb"], key="w2")
            S.op("vector", lambda e: e.memset(wuv_sb[:], 0.0), writes=["wuv_sb"])
            for h2 in range(2):
                S.dma("gpsimd", lambda e, h2=h2: e.dma_start(out=wuv_sb[:, :, h2, h2 * 64:(h2 + 1) * 64],
                                                             in_=w_uv_d.rearrange("(hp h2) r d -> h2 r hp d", h2=2)[h2]),
                      reads=["wuv_sb"], writes=["wuv_sb"], key=f"w3{h2}")
            S.dma("gpsimd", lambda e: e.dma_start(out=w_o_sb[:], in_=w_o_d.rearrange("(k p) n -> p k n", p=128)),
                  writes=["w_o_sb"], key="w4")
            S.dma("sync", lambda e: e.dma_start(out=pscale_sb[:], in_=pool_scale_d.rearrange("(g d) -> d g", d=128),
                                                allow_slow_non_contiguous=True),
                  writes=["pscale_sb"], key="c0")
            S.dma("sync", lambda e: e.dma_start(out=g1_bc[:], in_=bc(ln1g_d, D)), writes=["g1_bc"], key="c1")
            S.dma("sync", lambda e: e.dma_start(out=b1_bc[:], in_=bc(ln1b_d, D)), writes=["b1_bc"], key="c2")
            S.dma("sync", lambda e: e.dma_start(out=kvg_bc[:], in_=bc(kvg_d, 128)), writes=["kvg_bc"], key="c3")
            S.dma("sync", lambda e: e.dma_start(out=kig_bc[:], in_=bc(kig_d, 64)), writes=["kig_bc"], key="c4")
            S.dma("sync", lambda e: e.dma_start(out=kib_bc[:], in_=bc(kib_d, 64)), writes=["kib_bc"], key="c5")
            for g in range(4):
                w = 2 ** (g + 1)
                S.op("gpsimd", lambda e, g=g, w=w: e.memset(ic16[:, g, :], 1.0 / w), reads=["ic16"], writes=["ic16"])
                for t in range(w - 1):
                    S.op("gpsimd", lambda e, g=g, t=t: e.memset(ic16[:, g, t:t + 1], 1.0 / (t + 1)), reads=["ic16"], writes=["ic16"])
            S.op("gpsimd", lambda e: e.memset(halo[:], 0.0), writes=["halo"])
            S.op("gpsimd", lambda e: e.memset(c256[:], 256.0), writes=["c256"])
            for i_ in range(KBIS):
                S.op("gpsimd", lambda e, i_=i_: e.memset(pw2[:, i_:i_ + 1], 2.0 ** (-i_)), reads=["pw2"], writes=["pw2"])
            S.op("gpsimd", lambda e: e.memset(ckv1[:, :, 128:130], 1.0), writes=["ckv1"])

            def front(c):
                for t in range(4):
                    tg = 4 * c + t
                    b = tg % 2
                    S.dma("sync", lambda e, tg=tg, b=b: e.dma_start(out=xt[b][:], in_=x_d[tg * 128:(tg + 1) * 128, :]),
                          writes=[f"xt{b}"], key=f"x{b}")
                    S.op("scalar", lambda e, b=b: e.activation(out=xb[b][:], in_=xt[b][:], func=AF.Copy),
                         reads=[f"xt{b}"], writes=[f"xb{b}"])
                    for kh in range(2):
                        ti = tpr.next()
                        def tr(e, b=b, kh=kh, ti=ti):
                            ins = None
                            for kk in range(4):
                                k = kh * 4 + kk
                                ins = e.transpose(out=tp[ti][:, kk * 128:(kk + 1) * 128], in_=xb[b][:, k * 128:(k + 1) * 128],
                                                  identity=ident_b[:])
                            return ins
                        S.op("tensor", tr, reads=[f"xb{b}", "ident_b"], writes=[f"tp{ti}"])
                        S.op("vector", lambda e, kh=kh, ti=ti, t=t: e.tensor_copy(
                            out=xT[:, kh * 4:(kh + 1) * 4, t * 128:(t + 1) * 128],
                            in_=tp[ti][:, 0:512].rearrange("p (k n) -> p k n", k=4)),
                            reads=[f"tp{ti}"], writes=["xT"])

                def inproj_fm(col0):
                    bi = mmr.next()
                    def f(e, col0=col0, bi=bi):
                        ins = None
                        for k in range(8):
                            ins = e.matmul(mm[bi][:], lhsT=w_in_sb[:, k, col0:col0 + 128], rhs=xT[:, k, :],
                                           start=(k == 0), stop=(k == 7))
                        return ins
                    S.op("tensor", f, reads=["w_in_sb", "xT"], writes=[f"mm{bi}"])
                    return bi

                for g in range(4):
                    bi = inproj_fm(g * 128)
                    S.op("gpsimd", lambda e, g=g: e.tensor_copy(out=ug[:, 0:16], in_=halo[:, g, :]),
                         reads=["halo", "ug"], writes=["ug"])
                    S.op("scalar", lambda e, bi=bi: e.activation(out=ug[:, 16:528], in_=mm[bi][:], func=AF.Copy),
                         reads=[f"mm{bi}", "ug"], writes=["ug"])
                    S.op("gpsimd", lambda e, g=g: e.tensor_copy(out=halo[:, g, :], in_=ug[:, 512:528]),
                         reads=["ug"], writes=["halo"])
                    src, srcn = ug, "ug"
                    bufs = [(pA, "pA"), (pB, "pB")]
                    for lv in range(g + 1):
                        s = 2 ** lv
                        lo = 2 ** (lv + 1) - 1
                        dst, dstn = bufs[lv % 2]
                        S.op("gpsimd", lambda e, src=src, dst=dst, s=s, lo=lo: e.tensor_tensor(
                            out=dst[:, lo:528], in0=src[:, lo:528], in1=src[:, lo - s:528 - s], op=ALU.add),
                            reads=[srcn, dstn], writes=[dstn])
                        src, srcn = dst, dstn
                    w = 2 ** (g + 1)
                    S.op("vector", lambda e, src=src, w=w: e.scalar_tensor_tensor(
                        out=dT[:], in0=src[:, 16:528], scalar=1.0 / w, in1=ug[:, 16:528], op0=ALU.mult, op1=ALU.subtract),
                        reads=[srcn, "ug"], writes=["dT"])
                    if c == 0:
                        S.op("vector", lambda e, src=src, g=g: e.tensor_tensor(out=junk[:, 0:16], in0=src[:, 16:32], in1=ic16[:, g, :], op=ALU.mult),
                             reads=[srcn, "ic16"], writes=["junk"])
                        S.op("vector", lambda e: e.tensor_tensor(out=dT[:, 0:16], in0=junk[:, 0:16], in1=ug[:, 16:32], op=ALU.subtract),
                             reads=["junk", "ug", "dT"], writes=["dT"])
                    bi2 = mmr.next()
                    S.op("tensor", lambda e, g=g, bi2=bi2: e.matmul(mm[bi2][:], lhsT=wpool_sb[:, g, :], rhs=dT[:], start=True, stop=True),
                         reads=["wpool_sb", "dT"], writes=[f"mm{bi2}"])
                    S.op("scalar", lambda e, g=g, bi2=bi2: e.activation(out=mixTs[c % 2][:, g, :], in_=mm[bi2][:], func=AF.Copy, scale=pscale_sb[:, g:g + 1]),
                         reads=[f"mm{bi2}", "pscale_sb", f"mixT{c % 2}"], writes=[f"mixT{c % 2}"])
                for j in range(4):
                    bi = inproj_fm(512 + j * 128)
                    S.op("scalar", lambda e, j=j, bi=bi: e.activation(out=qTs[c % 2][:, j, :], in_=mm[bi][:], func=AF.Copy),
                         reads=[f"mm{bi}"], writes=[f"qT{c % 2}"])
                for j in range(4):
                    bi = inproj_fm(1152 + j * 128)
                    S.op("vector", lambda e, j=j, bi=bi: e.tensor_copy(out=qiT[:, j, :], in_=mm[bi][:]),
                         reads=[f"mm{bi}"], writes=["qiT"])
                tiA = tpr.next(); tiB = tpr.next()
                for t in range(4):
                    tg = 4 * c + t
                    bi = mmr.next()
                    def f(e, t=t, bi=bi):
                        ins = None
                        for k in range(8):
                            ins = e.matmul(mm[bi][:, 0:128], lhsT=xT[:, k, t * 128:(t + 1) * 128], rhs=w_in_sb[:, k, 1024:1152],
                                           start=(k == 0), stop=(k == 7))
                        for k in range(8):
                            ins = e.matmul(mm[bi][:, 128:200], lhsT=xT[:, k, t * 128:(t + 1) * 128], rhs=w_in_sb[:, k, 1664:1736],
                                           start=(k == 0), stop=(k == 7))
                        return ins
                    S.op("tensor", f, reads=["w_in_sb", "xT"], writes=[f"mm{bi}"])
                    S.op("scalar", lambda e, bi=bi: e.activation(out=tm[:], in_=mm[bi][:, 0:200], func=AF.Copy),
                         reads=[f"mm{bi}"], writes=["tm"])
                    S.op("scalar", lambda e: e.activation(out=junk[:], in_=tm[:, 0:128], func=AF.Square, accum_out=st1[:, 0:1]),
                         reads=["tm", "st1"], writes=["junk", "st1"])
                    ln_rstd(st1[:, 0:1], st1[:, 1:2], 1, 1.0 / 128, "st1", "st1")
                    S.op("vector", lambda e: e.scalar_tensor_tensor(out=ckn[:], in0=tm[:, 0:128], scalar=st1[:, 1:2], in1=kvg_bc[:],
                                                                     op0=ALU.mult, op1=ALU.mult),
                         reads=["tm", "st1", "kvg_bc"], writes=["ckn"])
                    S.op("gpsimd", lambda e, tg=tg: e.tensor_copy(out=ckv1[:, tg, 0:128], in_=ckn[:]), reads=["ckn", "ckv1"], writes=["ckv1"])
                    S.op("vector", lambda e: e.bn_stats(out=bst[:, 0, :], in_=tm[:, 128:192]), reads=["tm"], writes=["bst"])
                    S.op("vector", lambda e: e.bn_aggr(out=bmv[:], in_=bst[:, 0, :]), reads=["bst"], writes=["bmv"])
                    ln_rstd(bmv[:, 1:2], brs[:, 0:1], 0, 1.0, "bmv", "brs")
                    S.op("vector", lambda e: e.tensor_scalar(out=kn32[:], in0=tm[:, 128:192], scalar1=bmv[:, 0:1], scalar2=brs[:, 0:1],
                                                              op0=ALU.subtract, op1=ALU.mult),
                         reads=["tm", "bmv", "brs"], writes=["kn32"])
                    S.op("vector", lambda e, tg=tg: e.tensor_copy(out=widx[:, tg, :], in_=tm[:, 192:200]),
                         reads=["tm", "widx"], writes=["widx"])
                    S.op("gpsimd", lambda e: e.tensor_tensor(out=kn32[:], in0=kn32[:], in1=kig_bc[:], op=ALU.mult),
                         reads=["kn32", "kig_bc"], writes=["kn32"])
                    S.op("gpsimd", lambda e: e.tensor_tensor(out=kn2[:, 0:64], in0=kn32[:], in1=kib_bc[:], op=ALU.add),
                         reads=["kn32", "kib_bc", "kn2"], writes=["kn2"])
                    S.op("gpsimd", lambda e: e.tensor_copy(out=kn2[:, 64:128], in_=kn2[:, 0:64]), reads=["kn2"], writes=["kn2"])
                    S.op("tensor", lambda e, t=t, tiA=tiA: e.transpose(out=tp[tiA][:, t * 128:(t + 1) * 128], in_=ckn[:], identity=ident_b[:]),
                         reads=["ckn", "ident_b"], writes=[f"tp{tiA}"])
                    S.op("tensor", lambda e, t=t, tiB=tiB: e.transpose(out=tp[tiB][:, t * 128:(t + 1) * 128], in_=kn2[:], identity=ident_b[:]),
                         reads=["kn2", "ident_b"], writes=[f"tp{tiB}"])
                S.op("scalar", lambda e, c=c, tiA=tiA: e.activation(out=ckvT[:, c * CH:(c + 1) * CH], in_=tp[tiA][:, 0:512], func=AF.Copy),
                     reads=[f"tp{tiA}", "ckvT"], writes=["ckvT"])
                S.op("scalar", lambda e, c=c, tiB=tiB: e.activation(out=kiT[:, c * CH:(c + 1) * CH], in_=tp[tiB][:, 0:512], func=AF.Copy),
                     reads=[f"tp{tiB}", "kiT"], writes=["kiT"])


            def tile_sel(c, t):
                qt = 4 * c + t
                qt = 4 * c + t
                N = 128 * (qt + 1)
                nkc = (N + 511) // 512
                for kc in range(nkc):
                    k0 = kc * 512
                    kw = min(512, N - k0)
                    for h in range(8):
                        hp, h2 = h // 2, h % 2
                        bi = mmr.next()
                        S.op("tensor", lambda e, hp=hp, h2=h2, t=t, bi=bi, k0=k0, kw=kw: e.matmul(
                            mm[bi][:, 0:kw], lhsT=qiT[h2 * 64:(h2 + 1) * 64, hp, t * 128:(t + 1) * 128],
                            rhs=kiT[h2 * 64:(h2 + 1) * 64, k0:k0 + kw], start=True, stop=True),
                            reads=["qiT", "kiT"], writes=[f"mm{bi}"])
                        ri = h % 2
                        S.op("scalar", lambda e, bi=bi, ri=ri, kw=kw: e.activation(out=rl[ri][:, 0:kw], in_=mm[bi][:, 0:kw], func=AF.Relu),
                             reads=[f"mm{bi}"], writes=[f"rl{ri}"])
                        if h == 0:
                            S.op("vector", lambda e, ri=ri, k0=k0, kw=kw, qt=qt: e.tensor_scalar(
                                out=SC[:, k0:k0 + kw], in0=rl[ri][:, 0:kw], scalar1=widx[:, qt, 0:1], scalar2=None, op0=ALU.mult),
                                reads=[f"rl{ri}", "widx", "SC"], writes=["SC"])
                        else:
                            S.op("vector", lambda e, ri=ri, k0=k0, kw=kw, qt=qt, h=h: e.scalar_tensor_tensor(
                                out=SC[:, k0:k0 + kw], in0=rl[ri][:, 0:kw], scalar=widx[:, qt, h:h + 1], in1=SC[:, k0:k0 + kw],
                                op0=ALU.mult, op1=ALU.add),
                                reads=[f"rl{ri}", "widx", "SC"], writes=["SC"])
                if N > 256:
                    S.op("vector", lambda e, N=N: e.tensor_reduce(out=bmax[:], in_=SC[:, 0:N], axis=mybir.AxisListType.X, op=ALU.max,
                                                                   apply_absolute_value=True), reads=["SC"], writes=["bmax"])
                    S.op("vector", lambda e: e.tensor_scalar(out=bmax[:], in0=bmax[:], scalar1=1.0001, scalar2=1e-20, op0=ALU.mult, op1=ALU.add),
                         reads=["bmax"], writes=["bmax"])
                S.op("gpsimd", lambda e, qt=qt: e.affine_select(out=SC[:, qt * 128:(qt + 1) * 128], in_=SC[:, qt * 128:(qt + 1) * 128],
                                                                 pattern=[[-1, 128]], compare_op=ALU.is_ge, fill=NEG, base=0, channel_multiplier=1),
                     reads=["SC"], writes=["SC"])
                if N > 256:
                    S.op("vector", lambda e, N=N: e.tensor_scalar(out=bmid[:], in0=bmax[:], scalar1=0.0, scalar2=None, op0=ALU.mult),
                         reads=["bmax"], writes=["bmid"])
                    S.op("vector", lambda e: e.tensor_scalar(out=bsteps[:], in0=pw2[:], scalar1=bmax[:, 0:1], scalar2=None, op0=ALU.mult),
                         reads=["pw2", "bmax"], writes=["bsteps"])
                    S.op("vector", lambda e: e.tensor_scalar(out=bnegh[:], in0=bsteps[:], scalar1=-0.5, scalar2=None, op0=ALU.mult),
                         reads=["bsteps"], writes=["bnegh"])
                    for it_ in range(KBIS):
                        S.op("vector", lambda e, N=N: e.tensor_scalar(out=xT[:].rearrange("p k n -> p (k n)").bitcast(mybir.dt.uint8)[:, 0:N], in0=SC[:, 0:N], scalar1=bmid[:, 0:1], scalar2=0.0,
                                                                       op0=ALU.is_ge, op1=ALU.add, accum_out=bcnt[:, 0:1]),
                             reads=["SC", "bmid"], writes=["xT", "bcnt"])
                        S.op("vector", lambda e, it_=it_: e.tensor_scalar(out=bd[:], in0=bcnt[:], scalar1=c256[:, 0:1], scalar2=bsteps[:, it_:it_ + 1],
                                                                          op0=ALU.is_ge, op1=ALU.mult),
                             reads=["bcnt", "c256", "bsteps"], writes=["bd"])
                        sc2 = bnegh[:, it_:it_ + 1] if it_ < KBIS - 1 else bnegl[:, 0:1]
                        if it_ == KBIS - 1:
                            S.op("vector", lambda e, it_=it_: e.tensor_scalar(out=bnegl[:], in0=bsteps[:, it_:it_ + 1], scalar1=-1.0, scalar2=None, op0=ALU.mult),
                                 reads=["bsteps"], writes=["bnegl"])
                        S.op("vector", lambda e, sc2=sc2: e.tensor_scalar(out=bmid[:], in0=bmid[:], scalar1=bd[:, 0:1], scalar2=sc2,
                                                                          op0=ALU.add, op1=ALU.add),
                             reads=["bmid", "bd", "bnegh", "bnegl"], writes=["bmid"])

            def tile_mask(c, t):
                qt = 4 * c + t
                N = 128 * (qt + 1)
                for kt in range(qt + 1):
                    mi = kt % 2
                    if N > 256:
                        S.op("vector", lambda e, kt=kt, mi=mi: e.tensor_scalar(out=m128[mi][:], in0=SC[:, kt * 128:(kt + 1) * 128],
                                                                               scalar1=bmid[:, 0:1], scalar2=None, op0=ALU.is_ge),
                             reads=["SC", "bmid"], writes=[f"m128_{mi}"])
                    else:
                        S.op("vector", lambda e, kt=kt, mi=mi: e.tensor_scalar(out=m128[mi][:], in0=SC[:, kt * 128:(kt + 1) * 128],
                                                                               scalar1=-0.5e30, scalar2=None, op0=ALU.is_ge),
                             reads=["SC"], writes=[f"m128_{mi}"])
                    ti = tpr.next()
                    S.op("tensor", lambda e, mi=mi, ti=ti: e.transpose(out=tp[ti][:, 0:128], in_=m128[mi][:], identity=ident_b[:]),
                         reads=[f"m128_{mi}", "ident_b"], writes=[f"tp{ti}"])
                    S.op("scalar", lambda e, kt=kt, t=t, ti=ti: e.activation(out=maskTs[c % 2][:, kt, t * 128:(t + 1) * 128], in_=tp[ti][:, 0:128], func=AF.Copy),
                         reads=[f"tp{ti}", f"maskT{c % 2}"], writes=[f"maskT{c % 2}"])


            def att_head(c, h, use_dve=False):
                nkt = 4 * c + 4
                hp, h2 = h // 2, h % 2
                qi = h % 2
                oi = h % 2
                bi = mmr.next()
                S.op("tensor", lambda e, hp=hp, h2=h2, bi=bi: e.matmul(mm[bi][:], lhsT=wuk_sb[h2 * 64:(h2 + 1) * 64, hp, :],
                                                                    rhs=qTs[c % 2][h2 * 64:(h2 + 1) * 64, hp, :], start=True, stop=True),
                     reads=["wuk_sb", f"qT{c % 2}"], writes=[f"mm{bi}"])
                S.op("scalar", lambda e, bi=bi, qi=qi: e.activation(out=qlat[qi][:], in_=mm[bi][:], func=AF.Copy, scale=0.125),
                     reads=[f"mm{bi}"], writes=[f"qlat{qi}"])
                def qk(kt):
                    j0 = max(0, kt - 4 * c)
                    ncol = (4 - j0) * 128
                    c0 = j0 * 128
                    bi = mmr.next()
                    S.op("tensor", lambda e, kt=kt, bi=bi, qi=qi, c0=c0, ncol=ncol: e.matmul(
                        mm[bi][:, 0:ncol], lhsT=ckvT[:, kt * 128:(kt + 1) * 128], rhs=qlat[qi][:, c0:c0 + ncol], start=True, stop=True),
                        reads=["ckvT", f"qlat{qi}"], writes=[f"mm{bi}"])
                    return bi, j0, ncol, c0
                pend = qk(0)
                for kt in range(nkt):
                    bi, j0, ncol, c0 = pend
                    if kt + 1 < nkt:
                        pend = qk(kt + 1)
                    ei = kt % 2
                    S.op("scalar", lambda e, bi=bi, ei=ei, ncol=ncol: e.activation(out=Eb[ei][:, 0:ncol], in_=mm[bi][:, 0:ncol], func=AF.Exp),
                         reads=[f"mm{bi}"], writes=[f"Eb{ei}"])
                    S.op("vector" if (use_dve and kt % 2 == 0) else "gpsimd", lambda e, ei=ei, kt=kt, c0=c0, ncol=ncol, c=c: e.tensor_tensor(
                        out=Pb[ei][:, 0:ncol], in0=Eb[ei][:, 0:ncol], in1=maskTs[c % 2][:, kt, c0:c0 + ncol], op=ALU.mult),
                        reads=[f"Eb{ei}", f"maskT{c % 2}"], writes=[f"Pb{ei}"])
                    def pv(e, kt=kt, j0=j0, ei=ei, oi=oi, c=c):
                        ins = None
                        for j in range(j0, 4):
                            bank, off = (0, j * 129) if j < 3 else (1, 0)
                            ins = e.matmul(oacc[oi][:, bank, off:off + 129], lhsT=Pb[ei][:, (j - j0) * 128:(j - j0 + 1) * 128],
                                           rhs=ckv1[:, kt, 0:129], start=(kt == 0 and j in (0, 3)), stop=(kt == 4 * c + j),
                                           skip_group_check=True)
                        return ins
                    S.op("tensor", pv, reads=[f"Pb{ei}", "ckv1"], writes=[f"oacc{oi}"])
                ti = tpr.next()
                for j in range(4):
                    bank, off = (0, j * 129) if j < 3 else (1, 0)
                    li = j % 2
                    S.op("scalar", lambda e, oi=oi, bank=bank, off=off, j=j: e.activation(out=rden[:, j:j + 1], in_=oacc[oi][:, bank, off + 128:off + 129], func=AF.Ln),
                         reads=[f"oacc{oi}", "rden"], writes=["rden"])
                    S.op("scalar", lambda e, j=j: e.activation(out=rden[:, j:j + 1], in_=rden[:, j:j + 1], func=AF.Exp, scale=-1.0),
                         reads=["rden"], writes=["rden"])
                    S.op("scalar", lambda e, oi=oi, bank=bank, off=off, j=j, li=li: e.activation(
                        out=olat[li][:], in_=oacc[oi][:, bank, off:off + 128], func=AF.Copy, scale=rden[:, j:j + 1]),
                        reads=[f"oacc{oi}", "rden"], writes=[f"olat{li}"])
                    S.op("tensor", lambda e, li=li, ti=ti, j=j: e.transpose(out=tp[ti][:, j * 128:(j + 1) * 128], in_=olat[li][:], identity=ident_b[:]),
                         reads=[f"olat{li}", "ident_b"], writes=[f"tp{ti}"])
                S.op("scalar", lambda e, ti=ti, h2=h2: e.activation(out=olT2[:, h2, :], in_=tp[ti][:, 0:512], func=AF.Copy),
                     reads=[f"tp{ti}", "olT2"], writes=["olT2"])
                if h2 == 1:
                    bi = mmr.next()
                    def f(e, hp=hp, bi=bi):
                        e.matmul(mm[bi][:], lhsT=wuv_sb[:, hp, 0, :], rhs=olT2[:, 0, :], start=True, stop=False)
                        return e.matmul(mm[bi][:], lhsT=wuv_sb[:, hp, 1, :], rhs=olT2[:, 1, :], start=False, stop=True)
                    S.op("tensor", f, reads=["wuv_sb", "olT2"], writes=[f"mm{bi}"])
                    S.op("scalar", lambda e, hp=hp, bi=bi, c=c: e.activation(out=mixTs[c % 2][:, 4 + hp, :], in_=mm[bi][:], func=AF.Copy),
                         reads=[f"mm{bi}", f"mixT{c % 2}"], writes=[f"mixT{c % 2}"])


            def out_ln(c):
                if DEBUG == "1a" and c == 0:
                    dump(S, "qT", qTs[0][:].rearrange("p k n -> p (k n)"), [128, 4 * CH], BF16, ["qT0"])
                    dump(S, "qiT", qiT[:].rearrange("p k n -> p (k n)"), [128, 4 * CH], BF16, ["qiT"])
                    dump(S, "maskT", maskTs[0][:, 0:4, :].rearrange("p k n -> p (k n)"), [128, 4 * CH], mybir.dt.uint8, ["maskT0"])
                    dump(S, "olT2", olT2[:].rearrange("p k n -> p (k n)"), [128, 2 * CH], BF16, ["olT2"])
                    dump(S, "SC", SC[:, 0:512], [128, 512], F32, ["SC"])
                if DEBUG == "1a" and c == 7:
                    dump(S, "ckv1", ckv1[:].rearrange("p k n -> p (k n)"), [128, NT * 130], BF16, ["ckv1"])
                    dump(S, "ckvT", ckvT[:], [128, T], BF16, ["ckvT"])
                    dump(S, "kiT", kiT[:], [128, T], BF16, ["kiT"])
                    dump(S, "widx", widx[:].rearrange("p k n -> p (k n)"), [128, NT * 8], F32, ["widx"])
                if DEBUG == "1a":
                    S.dma("sync", lambda e, c=c: e.dma_start(out=dbg["mixT"][c], in_=mixTs[c % 2][:].rearrange("p k n -> p (k n)")),
                          reads=[f"mixT{c % 2}"], key="dbgm")

                for t in range(4):
                    tg = 4 * c + t
                    b = tg % 2
                    S.dma("sync", lambda e, tg=tg, b=b: e.dma_start(out=xt[b][:], in_=x_d[tg * 128:(tg + 1) * 128, :]),
                          writes=[f"xt{b}"], key=f"x{b}")
                    for hf in range(2):
                        bi = mmr.next()
                        def f(e, t=t, hf=hf, bi=bi):
                            ins = None
                            for k in range(8):
                                ins = e.matmul(mm[bi][:], lhsT=mixTs[c % 2][:, k, t * 128:(t + 1) * 128], rhs=w_o_sb[:, k, hf * 512:(hf + 1) * 512],
                                               start=(k == 0), stop=(k == 7))
                            return ins
                        S.op("tensor", f, reads=[f"mixT{c % 2}", "w_o_sb"], writes=[f"mm{bi}"])
                        S.op("vector", lambda e, b=b, hf=hf, bi=bi: e.scalar_tensor_tensor(
                            out=r1[:, hf * 512:(hf + 1) * 512], in0=xt[b][:, hf * 512:(hf + 1) * 512], scalar=ALPHA, in1=mm[bi][:],
                            op0=ALU.mult, op1=ALU.add),
                            reads=[f"xt{b}", f"mm{bi}", "r1"], writes=["r1"])
                    layer_norm_tile(r1[:], h1o[b][:], g1_bc, b1_bc, bst, bmv, brs, r1[:], ("r1", "r1", "r1"))
                    S.dma("sync", lambda e, tg=tg, b=b: e.dma_start(out=h1_d[tg * 128:(tg + 1) * 128, :], in_=h1o[b][:]),
                          reads=["r1"], key="h1s0")
                    if DEBUG == "1a":
                        S.dma("sync", lambda e, tg=tg, b=b: e.dma_start(out=dbg["h"][tg * 128:(tg + 1) * 128, :], in_=h1o[b][:]),
                              reads=["r1"], key="dbgh0")

            for c in range(NCH):
                front(c)
                for t in range(4):
                    tile_sel(c, t)
                    if c > 0:
                        att_head(c - 1, 2 * t)
                        att_head(c - 1, 2 * t + 1)
                    tile_mask(c, t)
                if c > 0:
                    out_ln(c - 1)
            for h in range(8):
                att_head(NCH - 1, h, use_dve=True)
            out_ln(NCH - 1)
            S.full_barrier()
            S.flush(nc)

        if DEBUG == "1a":
            return nc

        with ExitStack() as p2:
            w_mq_sb = sb(p2, "w_mq_sb", [128, 8, D], BF16)
            w_mo_sb = sb(p2, "w_mo_sb", [128, 8, D], BF16)
            w_mkv_sb = sb(p2, "w_mkv_sb", [128, 8, 2 * D], BF16)
            mb = sb(p2, "mb", [128, 2, D], BF16)
            memT = sb(p2, "memT", [128, 8, 256], BF16)
            KmT = sb(p2, "KmT", [128, 8, 256], BF16)
            Vm = sb(p2, "Vm", [128, 2, D], BF16)
            g2_bc = sb(p2, "g2_bc", [128, D], F32)
            b2_bc = sb(p2, "b2_bc", [128, D], F32)
            wr_sb = sb(p2, "wr_sb", [128, 8, NE], F32)
            br_bc = sb(p2, "br_bc", [128, NE], F32)
            ustr_f = sb(p2, "ustr_f", [128, 128], F32)
            ustr_b = sb(p2, "ustr_b", [128, 128], BF16)
            cb_i = sb(p2, "cb_i", [128, NE], I32)
            cbase = sb(p2, "cbase", [128, NE], F32)
            caphi = sb(p2, "caphi", [128, NE], F32)
            h1c = sb(p2, "h1c", [128, 4, D], F32)
            h1b = [sb(p2, f"h1b{i}", [128, D], BF16) for i in range(2)]
            hT = sb(p2, "hT", [128, 8, CH], BF16)
            qmT = sb(p2, "qmT", [128, 8, CH], BF16)
            Pm = [sb(p2, f"Pm{i}", [128, CH], BF16) for i in range(2)]
            rdn = sb(p2, "rdn", [128, CH], F32)
            omT = sb(p2, "omT", [128, 8, CH], BF16)
            r2 = sb(p2, "r2", [128, D], F32)
            tmpn2 = sb(p2, "tmpn2", [128, D], F32)
            h2o = [sb(p2, f"h2o{i}", [128, D], F32) for i in range(2)]
            h2b = [sb(p2, f"h2b{i}", [128, D], BF16) for i in range(2)]
            h2T = sb(p2, "h2T", [128, 8, 128], F32)
            lg = sb(p2, "lg", [128, NE], F32)
            mx8r = sb(p2, "mx8r", [128, 8], F32)
            negm = sb(p2, "negm", [128, 1], F32)
            ex4 = sb(p2, "ex4", [128, 4], F32)
            gsum = sb(p2, "gsum", [128, 1], F32)
            selb = sb(p2, "selb", [128, NE], BF16)
            slotm = sb(p2, "slotm", [128, NE], F32)
            ohp = sb(p2, "ohp", [128, NE], F32)
            slotf = sb(p2, "slotf", [128, 4], F32)
            bst2 = sb(p2, "bst2", [128, 2, 6], F32)
            bmv2 = sb(p2, "bmv2", [128, 2], F32)
            brs2 = sb(p2, "brs2", [128, 1], F32)
            mm = [ps(p2, f"mmB{i}", [128, 512], F32) for i in range(6)]
            tp = [ps(p2, f"tpB{i}", [128, 1024], BF16) for i in range(2)]
            mmr = Rot(list(range(6))); tpr = Rot([0, 1])

            S.dma("gpsimd", lambda e: e.dma_start(out=w_mkv_sb[:], in_=w_mkv_d.rearrange("(k p) n -> p k n", p=128)), writes=["w_mkv_sb"], key="w0")
            S.dma("gpsimd", lambda e: e.dma_start(out=mb[:], in_=mem_d.rearrange("(t p) d -> p t d", p=128)), writes=["mb"], key="w1")
            S.dma("gpsimd", lambda e: e.dma_start(out=w_mq_sb[:], in_=w_mq_d.rearrange("(k p) n -> p k n", p=128)), writes=["w_mq_sb"], key="w2")
            S.dma("gpsimd", lambda e: e.dma_start(out=w_mo_sb[:], in_=w_mo_d.rearrange("(k p) n -> p k n", p=128)), writes=["w_mo_sb"], key="w4")
            S.dma("sync", lambda e: e.dma_start(out=g2_bc[:], in_=bc(ln2g_d, D)), writes=["g2_bc"], key="c1")
            S.dma("sync", lambda e: e.dma_start(out=b2_bc[:], in_=bc(ln2b_d, D)), writes=["b2_bc"], key="c2")
            S.dma("sync", lambda e: e.dma_start(out=wr_sb[:], in_=w_r_d.rearrange("(k p) n -> p k n", p=128)), writes=["wr_sb"], key="c3")
            S.dma("sync", lambda e: e.dma_start(out=br_bc[:], in_=bc(b_r_d, NE)), writes=["br_bc"], key="c4")
            S.op("gpsimd", lambda e: e.memset(ustr_f[:], 1.0), writes=["ustr_f"])
            S.op("gpsimd", lambda e: e.affine_select(out=ustr_f[:], in_=ustr_f[:], pattern=[[1, 128]], compare_op=ALU.is_gt, fill=0.0,
                                                       base=0, channel_multiplier=-1), reads=["ustr_f"], writes=["ustr_f"])
            S.op("vector", lambda e: e.tensor_copy(out=ustr_b[:], in_=ustr_f[:]), reads=["ustr_f"], writes=["ustr_b"])
            S.op("gpsimd", lambda e: e.iota(out=cb_i[:], pattern=[[CAP, NE]], base=0, channel_multiplier=0), writes=["cb_i"])
            S.op("vector", lambda e: e.tensor_copy(out=cbase[:], in_=cb_i[:]), reads=["cb_i"], writes=["cbase"])
            S.op("vector", lambda e: e.tensor_scalar(out=caphi[:], in0=cbase[:], scalar1=float(CAP - 1), scalar2=None, op0=ALU.add),
                 reads=["cbase"], writes=["caphi"])
            for mt in range(2):
                for kh in range(2):
                    ti = tpr.next()
                    def tr(e, mt=mt, kh=kh, ti=ti):
                        ins = None
                        for kk in range(4):
                            k = kh * 4 + kk
                            ins = e.transpose(out=tp[ti][:, kk * 128:(kk + 1) * 128], in_=mb[:, mt, k * 128:(k + 1) * 128], identity=ident_b[:])
                        return ins
                    S.op("tensor", tr, reads=["mb", "ident_b"], writes=[f"tp{ti}"])
                    S.op("vector", lambda e, mt=mt, kh=kh, ti=ti: e.tensor_copy(
                        out=memT[:, kh * 4:(kh + 1) * 4, mt * 128:(mt + 1) * 128], in_=tp[ti][:, 0:512].rearrange("p (k n) -> p k n", k=4)),
                        reads=[f"tp{ti}", "memT"], writes=["memT"])
            for cc in range(8):
                bi = mmr.next()
                def f(e, cc=cc, bi=bi):
                    ins = None
                    for k in range(8):
                        ins = e.matmul(mm[bi][:, 0:256], lhsT=w_mkv_sb[:, k, cc * 128:(cc + 1) * 128], rhs=memT[:, k, :], start=(k == 0), stop=(k == 7))
                    return ins
                S.op("tensor", f, reads=["w_mkv_sb", "memT"], writes=[f"mm{bi}"])
                S.op("vector", lambda e, cc=cc, bi=bi: e.tensor_copy(out=KmT[:, cc, :], in_=mm[bi][:, 0:256]), reads=[f"mm{bi}", "KmT"], writes=["KmT"])
            for mt in range(2):
                for hf in range(2):
                    bi = mmr.next()
                    def f(e, mt=mt, hf=hf, bi=bi):
                        ins = None
                        for k in range(8):
                            ins = e.matmul(mm[bi][:], lhsT=memT[:, k, mt * 128:(mt + 1) * 128], rhs=w_mkv_sb[:, k, D + hf * 512:D + (hf + 1) * 512],
                                           start=(k == 0), stop=(k == 7))
                        return ins
                    S.op("tensor", f, reads=["w_mkv_sb", "memT"], writes=[f"mm{bi}"])
                    S.op("scalar", lambda e, mt=mt, hf=hf, bi=bi: e.activation(out=Vm[:, mt, hf * 512:(hf + 1) * 512], in_=mm[bi][:], func=AF.Copy),
                         reads=[f"mm{bi}", "Vm"], writes=["Vm"])

            for c in range(NCH):
                S.dma("sync", lambda e, c=c: e.dma_start(out=h1c[:], in_=h1_d[c * CH:(c + 1) * CH, :].rearrange("(t p) d -> p t d", p=128)),
                      writes=["h1c"], key="h1l")
                for t in range(4):
                    b = t % 2
                    S.op("scalar", lambda e, b=b, t=t: e.activation(out=h1b[b][:], in_=h1c[:, t, :], func=AF.Copy),
                         reads=["h1c"], writes=[f"h1b{b}"])
                    for kh in range(2):
                        ti = tpr.next()
                        def tr(e, b=b, kh=kh, ti=ti):
                            ins = None
                            for kk in range(4):
                                k = kh * 4 + kk
                                ins = e.transpose(out=tp[ti][:, kk * 128:(kk + 1) * 128], in_=h1b[b][:, k * 128:(k + 1) * 128], identity=ident_b[:])
                            return ins
                        S.op("tensor", tr, reads=[f"h1b{b}", "ident_b"], writes=[f"tp{ti}"])
                        S.op("vector", lambda e, kh=kh, ti=ti, t=t: e.tensor_copy(
                            out=hT[:, kh * 4:(kh + 1) * 4, t * 128:(t + 1) * 128], in_=tp[ti][:, 0:512].rearrange("p (k n) -> p k n", k=4)),
                            reads=[f"tp{ti}", "hT"], writes=["hT"])
                for cc in range(8):
                    bi = mmr.next()
                    def f(e, cc=cc, bi=bi):
                        ins = None
                        for k in range(8):
                            ins = e.matmul(mm[bi][:], lhsT=w_mq_sb[:, k, cc * 128:(cc + 1) * 128], rhs=hT[:, k, :], start=(k == 0), stop=(k == 7))
                        return ins
                    S.op("tensor", f, reads=["w_mq_sb", "hT"], writes=[f"mm{bi}"])
                    S.op("scalar", lambda e, cc=cc, bi=bi: e.activation(out=qmT[:, cc, :], in_=mm[bi][:], func=AF.Copy, scale=1.0 / 16),
                         reads=[f"mm{bi}", "qmT"], writes=["qmT"])
                for h in range(4):
                    for mt in range(2):
                        bi = mmr.next()
                        def f(e, h=h, mt=mt, bi=bi):
                            e.matmul(mm[bi][:], lhsT=KmT[:, 2 * h, mt * 128:(mt + 1) * 128], rhs=qmT[:, 2 * h, :], start=True, stop=False)
                            return e.matmul(mm[bi][:], lhsT=KmT[:, 2 * h + 1, mt * 128:(mt + 1) * 128], rhs=qmT[:, 2 * h + 1, :], start=False, stop=True)
                        S.op("tensor", f, reads=["KmT", "qmT"], writes=[f"mm{bi}"])
                        S.op("scalar", lambda e, mt=mt, bi=bi: e.activation(out=Pm[mt][:], in_=mm[bi][:], func=AF.Exp),
                             reads=[f"mm{bi}"], writes=[f"Pm{mt}"])
                    bi = mmr.next()
                    def f(e, bi=bi):
                        e.matmul(mm[bi][:], lhsT=ones_b[:], rhs=Pm[0][:], start=True, stop=False)
                        return e.matmul(mm[bi][:], lhsT=ones_b[:], rhs=Pm[1][:], start=False, stop=True)
                    S.op("tensor", f, reads=["ones_b", "Pm0", "Pm1"], writes=[f"mm{bi}"])
                    S.op("vector", lambda e, bi=bi: e.reciprocal(out=rdn[:], in_=mm[bi][:]), reads=[f"mm{bi}"], writes=["rdn"])
                    for dvc in range(2):
                        bi = mmr.next()
                        def f(e, h=h, dvc=dvc, bi=bi):
                            c0 = h * 256 + dvc * 128
                            e.matmul(mm[bi][:], lhsT=Vm[:, 0, c0:c0 + 128], rhs=Pm[0][:], start=True, stop=False)
                            return e.matmul(mm[bi][:], lhsT=Vm[:, 1, c0:c0 + 128], rhs=Pm[1][:], start=False, stop=True)
                        S.op("tensor", f, reads=["Vm", "Pm0", "Pm1"], writes=[f"mm{bi}"])
                        S.op("vector", lambda e, h=h, dvc=dvc, bi=bi: e.tensor_tensor(out=omT[:, 2 * h + dvc, :], in0=mm[bi][:], in1=rdn[:], op=ALU.mult),
                             reads=[f"mm{bi}", "rdn", "omT"], writes=["omT"])
                def outproj_ln(t):
                    tg = 4 * c + t
                    b = tg % 2
                    for hf in range(2):
                        bi = mmr.next()
                        def f(e, t=t, hf=hf, bi=bi):
                            ins = None
                            for k in range(8):
                                ins = e.matmul(mm[bi][:], lhsT=omT[:, k, t * 128:(t + 1) * 128], rhs=w_mo_sb[:, k, hf * 512:(hf + 1) * 512],
                                               start=(k == 0), stop=(k == 7))
                            return ins
                        S.op("tensor", f, reads=["omT", "w_mo_sb"], writes=[f"mm{bi}"])
                        S.op("vector", lambda e, t=t, hf=hf, bi=bi: e.scalar_tensor_tensor(
                            out=r2[:, hf * 512:(hf + 1) * 512], in0=h1c[:, t, hf * 512:(hf + 1) * 512], scalar=ALPHA, in1=mm[bi][:],
                            op0=ALU.mult, op1=ALU.add), reads=["h1c", f"mm{bi}", "r2"], writes=["r2"])
                    layer_norm_tile(r2[:], h2o[b][:], g2_bc, b2_bc, bst2, bmv2, brs2, tmpn2[:], ("r2", f"h2o{b}", "tmpn2"))
                    S.dma("sync", lambda e, tg=tg, b=b: e.dma_start(out=h2_d[tg * 128:(tg + 1) * 128, :], in_=h2o[b][:]),
                          reads=[f"h2o{b}"], key=f"h2s{b}")
                    if DEBUG == "1b":
                        S.dma("sync", lambda e, tg=tg, b=b: e.dma_start(out=dbg["h"][tg * 128:(tg + 1) * 128, :], in_=h2o[b][:]),
                              reads=[f"h2o{b}"], key=f"dbgh{b}")
                    S.op("scalar", lambda e, b=b: e.activation(out=h2b[b][:], in_=h2o[b][:], func=AF.Copy), reads=[f"h2o{b}"], writes=[f"h2b{b}"])
                def router(t):
                    tg = 4 * c + t
                    b = tg % 2
                    for kh in range(2):
                        bi = mmr.next()
                        def tr(e, b=b, kh=kh, bi=bi):
                            ins = None
                            for kk in range(4):
                                k = kh * 4 + kk
                                ins = e.transpose(out=mm[bi][:, kk * 128:(kk + 1) * 128], in_=h2o[b][:, k * 128:(k + 1) * 128], identity=ident_f[:])
                            return ins
                        S.op("tensor", tr, reads=[f"h2o{b}", "ident_f"], writes=[f"mm{bi}"])
                        S.op("vector", lambda e, kh=kh, bi=bi: e.tensor_copy(out=h2T[:, kh * 4:(kh + 1) * 4, :],
                                                                              in_=mm[bi][:].rearrange("p (k n) -> p k n", k=4)),
                             reads=[f"mm{bi}", "h2T"], writes=["h2T"])
                    bi = mmr.next()
                    def f(e, bi=bi):
                        ins = None
                        for k in range(8):
                            ins = e.matmul(mm[bi][:, 0:NE], lhsT=h2T[:, k, :], rhs=wr_sb[:, k, :], start=(k == 0), stop=(k == 7))
                        return ins
                    S.op("tensor", f, reads=["h2T", "wr_sb"], writes=[f"mm{bi}"])
                    S.op("vector", lambda e, bi=bi: e.tensor_tensor(out=lg[:], in0=mm[bi][:, 0:NE], in1=br_bc[:], op=ALU.add),
                         reads=[f"mm{bi}", "br_bc"], writes=["lg"])
                    S.op("vector", lambda e: e.max(out=mx8r[:], in_=lg[:]), reads=["lg"], writes=["mx8r"])
                    S.op("vector", lambda e: e.tensor_scalar(out=negm[:], in0=mx8r[:, 0:1], scalar1=-1.0, scalar2=None, op0=ALU.mult),
                         reads=["mx8r"], writes=["negm"])
                    S.op("scalar", lambda e: e.activation(out=ex4[:], in_=mx8r[:, 0:4], func=AF.Exp, bias=negm[:, 0:1], accum_out=gsum[:, 0:1]),
                         reads=["mx8r", "negm", "gsum"], writes=["ex4", "gsum"])
                    S.op("vector", lambda e: e.reciprocal(out=gsum[:], in_=gsum[:]), reads=["gsum"], writes=["gsum"])
                    S.op("vector", lambda e, tg=tg: e.tensor_scalar(out=gates_all[:, tg, :], in0=ex4[:], scalar1=gsum[:, 0:1], scalar2=None, op0=ALU.mult),
                         reads=["ex4", "gsum", "gates_all"], writes=["gates_all"])
                    S.op("vector", lambda e: e.tensor_scalar(out=selb[:], in0=lg[:], scalar1=mx8r[:, 3:4], scalar2=None, op0=ALU.is_ge),
                         reads=["lg", "mx8r"], writes=["selb"])
                    bi = mmr.next()
                    def f(e, bi=bi):
                        e.matmul(mm[bi][:, 0:NE], lhsT=ustr_b[:], rhs=selb[:], start=True, stop=True)
                        return e.matmul(mm[bi][:, 64:64 + NE], lhsT=ones_b[:], rhs=selb[:], start=True, stop=True)
                    S.op("tensor", f, reads=["ustr_b", "ones_b", "selb"], writes=[f"mm{bi}"])
                    S.op("vector", lambda e, bi=bi: e.tensor_tensor(out=slotm[:], in0=mm[bi][:, 0:NE], in1=cbase[:], op=ALU.add),
                         reads=[f"mm{bi}", "cbase"], writes=["slotm"])
                    S.op("vector", lambda e: e.tensor_tensor(out=slotm[:], in0=slotm[:], in1=caphi[:], op=ALU.min),
                         reads=["slotm", "caphi"], writes=["slotm"])
                    S.op("vector", lambda e, bi=bi: e.tensor_tensor(out=cbase[:], in0=mm[bi][:, 64:64 + NE], in1=cbase[:], op=ALU.add),
                         reads=[f"mm{bi}", "cbase"], writes=["cbase"])
                    for k in range(4):
                        S.op("vector", lambda e, k=k: e.scalar_tensor_tensor(out=ohp[:], in0=lg[:], scalar=mx8r[:, k:k + 1], in1=slotm[:],
                                                                             op0=ALU.is_equal, op1=ALU.mult),
                             reads=["lg", "mx8r", "slotm"], writes=["ohp"])
                        S.op("vector", lambda e, k=k: e.reduce_sum(out=slotf[:, k:k + 1], in_=ohp[:], axis=mybir.AxisListType.X),
                             reads=["ohp", "slotf"], writes=["slotf"])
                    S.op("vector", lambda e, tg=tg: e.tensor_copy(out=slots_all[:, tg, :], in_=slotf[:]), reads=["slotf", "slots_all"], writes=["slots_all"])
                    for k in range(4):
                        S.dma("gpsimd", lambda e, tg=tg, k=k, b=b: e.indirect_dma_start(
                            out=xg_d, out_offset=bass.IndirectOffsetOnAxis(ap=slots_all[:, tg, k:k + 1], axis=0), in_=h2b[b][:], in_offset=None),
                            reads=["slots_all", f"h2b{b}"], writes=["xg_d"], key=f"sc{k}")
                for t in range(4):
                    outproj_ln(t)
                    if t > 0:
                        router(t - 1)
                router(3)
            if DEBUG == "1b":
                dump(S, "slots", slots_all[:].rearrange("p a b -> p (a b)"), [128, NT * 4], I32, ["slots_all"])
                dump(S, "gates", gates_all[:].rearrange("p a b -> p (a b)"), [128, NT * 4], F32, ["gates_all"])
            S.full_barrier()
            S.flush(nc)
        if DEBUG == "1b":
            return nc

        BLKS = [(0, 512), (512, CAP - 512)]
        NTE = CAP // 128
        with ExitStack() as p3:
            wgu = [sb(p3, f"wgu{i}", [128, 8, 2 * D], BF16) for i in range(2)]
            wdn = [sb(p3, f"wdn{i}", [128, 8, D], BF16) for i in range(2)]
            bgr = sb(p3, "bgr", [NE, 2 * D], F32)
            bguT = sb(p3, "bguT", [128, 16, NE], F32)
            bgu7 = sb(p3, "bgu7", [128, 8, NE], F32)
            bdn = [sb(p3, f"bdn{i}", [128, D], F32) for i in range(2)]
            xg = [sb(p3, f"xg{i}", [128, NTE, D], BF16) for i in range(2)]
            xgT = sb(p3, "xgT", [128, 8, CAP], BF16)
            actT = sb(p3, "actT", [128, 8, CAP], BF16)
            s0 = [sb(p3, f"s0_{i}", [128, 512], F32) for i in range(2)]
            gcl = [sb(p3, f"gc{i}", [128, 512], F32) for i in range(2)]
            ucl = [sb(p3, f"uc{i}", [128, 512], F32) for i in range(2)]
            ysb = [sb(p3, f"ysb{i}", [128, D], F32) for i in range(2)]
            mm = [ps(p3, f"mmC{i}", [128, 512], F32) for i in range(6)]
            tp = [ps(p3, f"tpC{i}", [128, 1024], BF16) for i in range(2)]
            mmr = Rot(list(range(6))); tpr = Rot([0, 1])

            S.dma("sync", lambda e: e.dma_start(out=bgr[:], in_=b_gu_d), writes=["bgr"], key="c1")
            bi = mmr.next()
            def trb(e, bi=bi):
                ins = None
                for cidx in range(16):
                    ins = e.transpose(out=mm[bi][:, cidx * NE:(cidx + 1) * NE], in_=bgr[0:NE, cidx * 128:(cidx + 1) * 128], identity=ident_f[0:NE, 0:NE])
                return ins
            S.op("tensor", trb, reads=["bgr", "ident_f"], writes=[f"mm{bi}"])
            S.op("vector", lambda e, bi=bi: e.tensor_copy(out=bguT[:].rearrange("p a b -> p (a b)"), in_=mm[bi][:]), reads=[f"mm{bi}"], writes=["bguT"])
            S.op("vector", lambda e: e.tensor_scalar(out=bgu7[:], in0=bguT[:, 8:16, :], scalar1=7.0, scalar2=None, op0=ALU.add),
                 reads=["bguT"], writes=["bgu7"])

            def load_expert(ex):
                bf = ex % 2
                S.dma("gpsimd", lambda e: e.dma_start(out=wgu[bf][:], in_=w_gu_d[ex].rearrange("(k p) n -> p k n", p=128)),
                      writes=[f"wgu{bf}"], key=f"wg{bf}")
                S.dma("gpsimd", lambda e: e.dma_start(out=wdn[bf][:], in_=w_dn_d[ex].rearrange("(k p) n -> p k n", p=128)),
                      writes=[f"wdn{bf}"], key=f"wd{bf}")
                S.dma("sync", lambda e: e.dma_start(out=bdn[bf][:], in_=bc(b_dn_d[ex], D)), writes=[f"bdn{bf}"], key=f"bd{bf}")
                S.dma("sync", lambda e: e.dma_start(out=xg[bf][:], in_=xg_d[ex * CAP:(ex + 1) * CAP, :].rearrange("(t p) d -> p t d", p=128)),
                      reads=["xg_d"], writes=[f"xg{bf}"], key=f"xgl{bf}")

            load_expert(0)
            for ex in range(NE_RUN):
                bf = ex % 2
                if ex + 1 < NE_RUN:
                    load_expert(ex + 1)
                for t in range(NTE):
                    for kh in range(2):
                        ti = tpr.next()
                        def tr(e, bf=bf, t=t, kh=kh, ti=ti):
                            ins = None
                            for kk in range(4):
                                k = kh * 4 + kk
                                ins = e.transpose(out=tp[ti][:, kk * 128:(kk + 1) * 128], in_=xg[bf][:, t, k * 128:(k + 1) * 128], identity=ident_b[:])
                            return ins
                        S.op("tensor", tr, reads=[f"xg{bf}", "ident_b"], writes=[f"tp{ti}"])
                        S.op("scalar", lambda e, kh=kh, ti=ti, t=t: e.activation(
                            out=xgT[:, kh * 4:(kh + 1) * 4, t * 128:(t + 1) * 128], in_=tp[ti][:, 0:512].rearrange("p (k n) -> p k n", k=4), func=AF.Copy),
                            reads=[f"tp{ti}", "xgT"], writes=["xgT"])
                it = 0
                for j in range(8 if P2_STEPS >= 2 else 0):
                    for (b0, bw) in BLKS:
                        big = mmr.next(); biu = mmr.next()
                        def fg(e, bf=bf, j=j, b0=b0, bw=bw, big=big):
                            ins = None
                            for k in range(8):
                                ins = e.matmul(mm[big][:, 0:bw], lhsT=wgu[bf][:, k, j * 128:(j + 1) * 128], rhs=xgT[:, k, b0:b0 + bw],
                                               start=(k == 0), stop=(k == 7))
                            return ins
                        def fu(e, bf=bf, j=j, b0=b0, bw=bw, biu=biu):
                            ins = None
                            for k in range(8):
                                ins = e.matmul(mm[biu][:, 0:bw], lhsT=wgu[bf][:, k, D + j * 128:D + (j + 1) * 128], rhs=xgT[:, k, b0:b0 + bw],
                                               start=(k == 0), stop=(k == 7))
                            return ins
                        S.op("tensor", fg, reads=[f"wgu{bf}", "xgT"], writes=[f"mm{big}"])
                        S.op("tensor", fu, reads=[f"wgu{bf}", "xgT"], writes=[f"mm{biu}"])
                        i2 = it % 2; it += 1
                        S.op("vector", lambda e, big=big, bw=bw, j=j, ex=ex, i2=i2: e.tensor_scalar(
                            out=gcl[i2][:, 0:bw], in0=mm[big][:, 0:bw], scalar1=bguT[:, j, ex:ex + 1], scalar2=7.0, op0=ALU.add, op1=ALU.min),
                            reads=[f"mm{big}", "bguT"], writes=[f"gc{i2}"])
                        S.op("scalar", lambda e, bw=bw, i2=i2: e.activation(out=s0[i2][:, 0:bw], in_=gcl[i2][:, 0:bw], func=AF.Silu, scale=1.702),
                             reads=[f"gc{i2}"], writes=[f"s0_{i2}"])
                        S.op("scalar", lambda e, biu=biu, bw=bw, j=j, ex=ex, i2=i2: e.activation(
                            out=ucl[i2][:, 0:bw], in_=mm[biu][:, 0:bw], func=AF.Relu, bias=bgu7[:, j, ex:ex + 1]),
                            reads=[f"mm{biu}", "bgu7"], writes=[f"uc{i2}"])
                        S.op("vector", lambda e, bw=bw, i2=i2: e.tensor_scalar(
                            out=ucl[i2][:, 0:bw], in0=ucl[i2][:, 0:bw], scalar1=14.0, scalar2=-6.0, op0=ALU.min, op1=ALU.add),
                            reads=[f"uc{i2}"], writes=[f"uc{i2}"])
                        S.op("vector", lambda e, bw=bw, b0=b0, j=j, i2=i2: e.scalar_tensor_tensor(
                            out=actT[:, j, b0:b0 + bw], in0=s0[i2][:, 0:bw], scalar=1.0 / 1.702, in1=ucl[i2][:, 0:bw], op0=ALU.mult, op1=ALU.mult),
                            reads=[f"s0_{i2}", f"uc{i2}", "actT"], writes=["actT"])
                for t in range(NTE if P2_STEPS >= 4 else 0):
                    yb = t % 2
                    for hf in range(2):
                        bi = mmr.next()
                        def fd(e, bf=bf, t=t, hf=hf, bi=bi):
                            ins = None
                            for j in range(8):
                                ins = e.matmul(mm[bi][:], lhsT=actT[:, j, t * 128:(t + 1) * 128], rhs=wdn[bf][:, j, hf * 512:(hf + 1) * 512],
                                               start=(j == 0), stop=(j == 7))
                            return ins
                        S.op("tensor", fd, reads=["actT", f"wdn{bf}"], writes=[f"mm{bi}"])
                        S.op("vector", lambda e, bf=bf, yb=yb, hf=hf, bi=bi: e.tensor_tensor(
                            out=ysb[yb][:, hf * 512:(hf + 1) * 512], in0=mm[bi][:], in1=bdn[bf][:, hf * 512:(hf + 1) * 512], op=ALU.add),
                            reads=[f"mm{bi}", f"bdn{bf}", f"ysb{yb}"], writes=[f"ysb{yb}"])
                    r0 = ex * CAP + t * 128
                    S.dma("sync", lambda e, yb=yb, r0=r0: e.dma_start(out=ys_d[r0:r0 + 128, :], in_=ysb[yb][:]),
                          reads=[f"ysb{yb}"], writes=["ys_d"], key=f"yst{yb}")
            S.full_barrier()
            S.flush(nc)
        if DEBUG == "2":
            return nc

        with ExitStack() as p4:
            g3_bc = sb(p4, "g3_bc", [128, D], F32)
            b3_bc = sb(p4, "b3_bc", [128, D], F32)
            h2t = [sb(p4, f"h2t{i}", [128, D], F32) for i in range(2)]
            yk = [[sb(p4, f"yk{i}_{k}", [128, D], F32) for k in range(4)] for i in range(2)]
            acc = sb(p4, "acc", [128, D], F32)
            tmpn3 = sb(p4, "tmpn3", [128, D], F32)
            outt = [sb(p4, f"outt{i}", [128, D], F32) for i in range(2)]
            bst3 = sb(p4, "bst3", [128, 2, 6], F32)
            bmv3 = sb(p4, "bmv3", [128, 2], F32)
            brs3 = sb(p4, "brs3", [128, 1], F32)
            S.dma("sync", lambda e: e.dma_start(out=g3_bc[:], in_=bc(ln3g_d, D)), writes=["g3_bc"], key="c1")
            S.dma("sync", lambda e: e.dma_start(out=b3_bc[:], in_=bc(ln3b_d, D)), writes=["b3_bc"], key="c2")
            for tg in range(NT):
                b = tg % 2
                S.dma("sync", lambda e, tg=tg, b=b: e.dma_start(out=h2t[b][:], in_=h2_d[tg * 128:(tg + 1) * 128, :]),
                      writes=[f"h2t{b}"], key=f"h2l{b}")
                for k in range(4):
                    S.dma("gpsimd", lambda e, tg=tg, k=k, b=b: e.indirect_dma_start(
                        out=yk[b][k][:], out_offset=None, in_=ys_d, in_offset=bass.IndirectOffsetOnAxis(ap=slots_all[:, tg, k:k + 1], axis=0)),
                        reads=["ys_d", "slots_all"], writes=[f"yk{b}_{k}"], key=f"gk{b}{k}")
                S.op("vector", lambda e, b=b: e.tensor_scalar(out=acc[:], in0=h2t[b][:], scalar1=ALPHA, scalar2=None, op0=ALU.mult),
                     reads=[f"h2t{b}", "acc"], writes=["acc"])
                for k in range(4):
                    S.op("vector", lambda e, b=b, k=k, tg=tg: e.scalar_tensor_tensor(
                        out=acc[:], in0=yk[b][k][:], scalar=gates_all[:, tg, k:k + 1], in1=acc[:], op0=ALU.mult, op1=ALU.add),
                        reads=[f"yk{b}_{k}", "gates_all", "acc"], writes=["acc"])
                layer_norm_tile(acc[:], outt[b][:], g3_bc, b3_bc, bst3, bmv3, brs3, tmpn3[:], ("acc", f"outt{b}", "tmpn3"))
                S.dma("scalar", lambda e, tg=tg, b=b: e.dma_start(out=out_d[tg * 128:(tg + 1) * 128, :], in_=outt[b][:]),
                      reads=[f"outt{b}"], key=f"os{b}")
            S.full_barrier()
            S.flush(nc)
    return nc


_PROG = None


def kernel(**inputs):
    global _PROG
    if _PROG is None:
        _PROG = build_program()
    nc = _PROG
    B = inputs["x"].shape[0]
    in_maps = []
    for b in range(B):
        m = {}
        for k, v in inputs.items():
            a = np.asarray(v)
            if k in ("x", "mem"):
                m[k] = np.ascontiguousarray(a[b])
            else:
                m[k] = np.ascontiguousarray(a[0])
        in_maps.append(m)
    res = run_bass_kernel_spmd(nc, in_maps, core_ids=list(range(B)))
    return np.stack([np.asarray(r["out"]) for r in res.results], axis=0)
```

```python
import numpy as np
from contextlib import ExitStack
import concourse.bass as bass
import concourse.mybir as mybir
from concourse.bass_utils import run_bass_kernel_spmd

F32 = mybir.dt.float32
BF16 = mybir.dt.bfloat16
I32 = mybir.dt.int32
AF = mybir.ActivationFunctionType
ALU = mybir.AluOpType

T = 4096
NT = 32
D = 1024
CH = 512
NCH = 8
INW = 1736
CAP = 768
NE = 32
ALPHA = 2.0 ** 0.25
NEG = -1.0e30
NEG2 = -3.0e30
KBIS = 25
SIGMAX = float(1.0 / (1.0 + np.exp(-1.702 * 7.0)))

DEBUG = None
NE_RUN = NE
P2_STEPS = 9
P2_EW = 63


class Sched:
    EPOCH = 30000
    ENG = ("sync", "scalar", "vector", "gpsimd", "tensor")

    def __init__(self, sem_pool):
        self.ops = {e: [] for e in self.ENG}
        self.cnt = {}
        self.lastw = {}
        self.readers = {}
        self.seen = {e: {} for e in self.ENG}
        self.sem_pool = list(sem_pool)
        self.sems = {}

    def _sem(self, counter, val):
        if counter.startswith("E:"):
            ep = (val - 1) // self.EPOCH
            name, lv = f"{counter}#{ep}", val - ep * self.EPOCH
        else:
            name, lv = counter, val
        if name not in self.sems:
            self.sems[name] = self.sem_pool.pop()
        return self.sems[name], lv

    def _waits(self, eng, reads, writes, extra=()):
        need = {}
        def add(cv):
            c, v = cv
            if v > need.get(c, 0):
                need[c] = v
        for r in reads:
            if r in self.lastw:
                add(self.lastw[r])
        for w in writes:
            if w in self.lastw:
                add(self.lastw[w])
            for cv in self.readers.get(w, {}).items():
                add(cv)
        for cv in extra:
            add(cv)
        waits = []
        for c, v in need.items():
            if eng == "tensor" and c == "E:tensor":
                continue
            if self.seen[eng].get(c, 0) < v:
                self.seen[eng][c] = v
                waits.append(self._sem(c, v))
        return waits

    def _book(self, c, v, reads, writes):
        for r in reads:
            d = self.readers.setdefault(r, {})
            if d.get(c, 0) < v:
                d[c] = v
        for w in writes:
            self.lastw[w] = (c, v)
            self.readers[w] = {}

    def op(self, eng, fn, reads=(), writes=()):
        waits = self._waits(eng, reads, writes)
        c = "E:" + eng
        v = self.cnt.get(c, 0) + 1
        self.cnt[c] = v
        sem, _ = self._sem(c, v)
        self.ops[eng].append((waits, fn, sem, 1))
        self._book(c, v, reads, writes)

    def dma(self, eng, fn, reads=(), writes=(), key="d", serialize=True):
        c = "D:" + key
        prev = self.cnt.get(c, 0)
        waits = self._waits(eng, reads, writes, extra=[(c, prev)] if (prev and serialize) else [])
        v = prev + 16
        self.cnt[c] = v
        sem, _ = self._sem(c, v)
        self.ops[eng].append((waits, fn, sem, 16))
        self._book(c, v, reads, writes)

    def barrier_all(self, eng="sync"):
        waits = []
        for c, v in self.cnt.items():
            if v and self.seen[eng].get(c, 0) < v:
                self.seen[eng][c] = v
                waits.append(self._sem(c, v))
        self.ops[eng].append((waits, None, None, 0))

    def full_barrier(self):
        for e in self.ENG:
            self.barrier_all(e)

    def flush(self, nc):
        with nc.Block() as blk:
            for eng in self.ENG:
                lst = self.ops[eng]
                if not lst:
                    continue
                def body(e, lst=lst):
                    for waits, fn, sem, inc in lst:
                        for s, v in waits:
                            e.wait_ge(s, v)
                        if fn is not None:
                            ins = fn(e)
                            ins.then_inc(sem, inc)
                getattr(blk, eng)(body)
        self.ops = {e: [] for e in self.ENG}


class Rot:
    def __init__(self, items):
        self.items = items
        self.i = 0
    def next(self):
        it = self.items[self.i % len(self.items)]
        self.i += 1
        return it


def build_program():
    nc = bass.Bass("TRN2", target_bir_lowering=False)
    dt = lambda name, shape, dtype=F32, kind="ExternalInput": nc.dram_tensor(name, shape, dtype, kind=kind).ap()
    x_d = dt("x", [T, D])
    mem_d = dt("mem", [256, D])
    w_in_d = dt("w_in", [D, INW])
    w_pool_d = dt("w_pool", [4, 128, 128])
    pool_scale_d = dt("pool_scale", [512])
    kig_d = dt("idx_k_norm_g", [64])
    kib_d = dt("idx_k_norm_b", [64])
    kvg_d = dt("kv_norm_g", [128])
    w_uk_d = dt("w_uk", [8, 64, 128])
    w_uv_d = dt("w_uv", [8, 128, 64])
    w_o_d = dt("w_o", [D, D])
    ln1g_d = dt("ln1_g", [D]); ln1b_d = dt("ln1_b", [D])
    w_mq_d = dt("w_mq", [D, D])
    w_mkv_d = dt("w_mkv", [D, 2 * D])
    w_mo_d = dt("w_mo", [D, D])
    ln2g_d = dt("ln2_g", [D]); ln2b_d = dt("ln2_b", [D])
    w_r_d = dt("w_router", [D, NE])
    b_r_d = dt("b_router", [NE])
    w_gu_d = dt("w_gate_up", [NE, D, 2 * D])
    b_gu_d = dt("b_gate_up", [NE, 2 * D])
    w_dn_d = dt("w_down", [NE, D, D])
    b_dn_d = dt("b_down", [NE, D])
    ln3g_d = dt("ln3_g", [D]); ln3b_d = dt("ln3_b", [D])
    out_d = dt("out", [T, D], F32, "ExternalOutput")
    h1_d = dt("h1_scr", [T, D], F32, "Internal")
    h2_d = dt("h2_scr", [T, D], F32, "Internal")
    xg_d = dt("xg_scr", [NE * CAP, D], BF16, "Internal")
    ys_d = dt("ys_scr", [NE * CAP, D], F32, "Internal")
    dbg = {}
    if DEBUG:
        dbg["h"] = dt("dbg_h", [T, D], F32, "ExternalOutput")
        dbg["mixT"] = dt("dbg_mixT", [NCH, 128, 8 * CH], BF16, "ExternalOutput")

    dumps = []
    def dump(S, name, ap2d, shape, dtype, reads):
        if not DEBUG:
            return
        d = dt("dbg_" + name, shape, dtype, "ExternalOutput")
        S.dma("sync", lambda e: e.dma_start(out=d, in_=ap2d), reads=reads, key="dbgd")

    def bc(ap1d, n):
        return ap1d.rearrange("(o n) -> o n", o=1).to_broadcast([128, n])

    with ExitStack() as top:
        sem_pool = [top.enter_context(nc.semaphore(f"s{i}")) for i in range(96)]
        S = Sched(sem_pool)
        sb = lambda es, name, shape, dtype=F32: es.enter_context(nc.sbuf_tensor(name, shape, dtype))
        ps = lambda es, name, shape, dtype=F32: es.enter_context(nc.psum_tensor(name, shape, dtype))

        ident_b = sb(top, "ident_b", [128, 128], BF16)
        ident_f = sb(top, "ident_f", [128, 128], F32)
        ones_b = sb(top, "ones_b", [128, 128], BF16)
        slots_all = sb(top, "slots_all", [128, NT, 4], I32)
        gates_all = sb(top, "gates_all", [128, NT, 4], F32)

        S.op("gpsimd", lambda e: e.memset(ident_f[:], 0.0), writes=["ident_f"])
        S.op("gpsimd", lambda e: e.affine_select(out=ident_f[:], in_=ident_f[:], pattern=[[-1, 128]],
                                                   compare_op=ALU.not_equal, fill=1.0, base=0, channel_multiplier=1),
             reads=["ident_f"], writes=["ident_f"])
        S.op("vector", lambda e: e.tensor_copy(out=ident_b[:], in_=ident_f[:]), reads=["ident_f"], writes=["ident_b"])
        S.op("vector", lambda e: e.memset(ones_b[:], 1.0), writes=["ones_b"])

        def ln_rstd(var_ap, rstd_ap, tag, scale, rname, wname):
            S.op("scalar", lambda e: e.activation(out=rstd_ap, in_=var_ap, func=AF.Ln, bias=eps_tile[:, tag:tag + 1], scale=scale),
                 reads=[rname, "eps", wname], writes=[wname])
            S.op("scalar", lambda e: e.activation(out=rstd_ap, in_=rstd_ap, func=AF.Exp, scale=-0.5),
                 reads=[wname], writes=[wname])

        eps_tile = sb(top, "eps", [128, 2], F32)
        S.op("vector", lambda e: e.memset(eps_tile[:, 0:1], 1e-5), writes=["eps"])
        S.op("vector", lambda e: e.memset(eps_tile[:, 1:2], 1e-6), reads=["eps"], writes=["eps"])

        def layer_norm_tile(r_ap, out_ap, g_bc, b_bc, stats, mv, rstd, tmp_ap, names):
            rn, on, tn = names
            for hf in range(2):
                S.op("vector", lambda e, hf=hf: e.bn_stats(out=stats[:, hf, :], in_=r_ap[:, hf * 512:(hf + 1) * 512]),
                     reads=[rn], writes=[stats.name])
            S.op("vector", lambda e: e.bn_aggr(out=mv[:], in_=stats[:].rearrange("p a b -> p (a b)")),
                 reads=[stats.name], writes=[mv.name])
            ln_rstd(mv[:, 1:2], rstd[:, 0:1], 0, 1.0, mv.name, rstd.name)
            S.op("vector", lambda e: e.tensor_scalar(out=tmp_ap, in0=r_ap, scalar1=mv[:, 0:1], scalar2=rstd[:, 0:1],
                                                      op0=ALU.subtract, op1=ALU.mult),
                 reads=[rn, mv.name, rstd.name], writes=[tn])
            S.op("vector", lambda e: e.tensor_tensor(out=tmp_ap, in0=tmp_ap, in1=g_bc[:], op=ALU.mult),
                 reads=[tn, g_bc.name], writes=[tn])
            S.op("vector", lambda e: e.tensor_tensor(out=out_ap, in0=tmp_ap, in1=b_bc[:], op=ALU.add),
                 reads=[tn, b_bc.name], writes=[on])

        with ExitStack() as p1:
            w_in_sb = sb(p1, "w_in_sb", [128, 8, INW], BF16)
            w_o_sb = sb(p1, "w_o_sb", [128, 8, D], BF16)
            wpool_sb = sb(p1, "wpool_sb", [128, 4, 128], BF16)
            wuk_sb = sb(p1, "wuk_sb", [128, 4, 128], BF16)
            wuv_sb = sb(p1, "wuv_sb", [128, 4, 2, 128], BF16)
            pscale_sb = sb(p1, "pscale_sb", [128, 4], F32)
            g1_bc = sb(p1, "g1_bc", [128, D], F32)
            b1_bc = sb(p1, "b1_bc", [128, D], F32)
            kvg_bc = sb(p1, "kvg_bc", [128, 128], F32)
            kig_bc = sb(p1, "kig_bc", [128, 64], F32)
            kib_bc = sb(p1, "kib_bc", [128, 64], F32)
            ic16 = sb(p1, "ic16", [128, 4, 16], F32)
            ckv1 = sb(p1, "ckv1", [128, NT, 130], BF16)
            ckvT = sb(p1, "ckvT", [128, T], BF16)
            kiT = sb(p1, "kiT", [128, T], BF16)
            widx = sb(p1, "widx", [128, NT, 8], F32)
            xt = [sb(p1, f"xt{i}", [128, D], F32) for i in range(2)]
            xb = [sb(p1, f"xb{i}", [128, D], BF16) for i in range(2)]
            xT = sb(p1, "xT", [128, 8, CH], BF16)
            ug = sb(p1, "ug", [128, 528], F32)
            halo = sb(p1, "halo", [128, 4, 16], F32)
            pA = sb(p1, "pA", [128, 528], F32)
            pB = sb(p1, "pB", [128, 528], F32)
            dT = sb(p1, "dT", [128, CH], BF16)
            qTs = [sb(p1, f"qT{i}", [128, 4, CH], BF16) for i in range(2)]
            qiT = sb(p1, "qiT", [128, 4, CH], BF16)
            mixTs = [sb(p1, f"mixT{i}", [128, 8, CH], BF16) for i in range(2)]
            SC = sb(p1, "SC", [128, T], F32)
            bmax = sb(p1, "bmax", [128, 1], F32)
            bmid = sb(p1, "bmid", [128, 1], F32)
            bcnt = sb(p1, "bcnt", [128, 1], F32)
            bd = sb(p1, "bd", [128, 1], F32)
            bnegl = sb(p1, "bnegl", [128, 1], F32)
            c256 = sb(p1, "c256", [128, 1], F32)
            pw2 = sb(p1, "pw2", [128, KBIS], F32)
            bsteps = sb(p1, "bsteps", [128, KBIS], F32)
            bnegh = sb(p1, "bnegh", [128, KBIS], F32)
            m128 = [sb(p1, f"m128_{i}", [128, 128], BF16) for i in range(2)]
            maskTs = [sb(p1, f"maskT{i}", [128, NT, CH], mybir.dt.uint8) for i in range(2)]
            rl = [sb(p1, f"rl{i}", [128, CH], F32) for i in range(2)]
            Eb = [sb(p1, f"Eb{i}", [128, CH], BF16) for i in range(2)]
            Pb = [sb(p1, f"Pb{i}", [128, CH], BF16) for i in range(2)]
            Eb.append(ug[:, 0:256].bitcast(BF16)); Pb.append(pB[:, 0:256].bitcast(BF16))
            qlat = [sb(p1, f"qlat{i}", [128, CH], BF16) for i in range(2)]
            olat = [sb(p1, f"olat{i}", [128, 128], BF16) for i in range(2)]
            rden = sb(p1, "rden", [128, 4], F32)
            ckn = sb(p1, "ckn", [128, 128], BF16)
            kn32 = sb(p1, "kn32", [128, 64], F32)
            kn2 = sb(p1, "kn2", [128, 128], BF16)
            tm = sb(p1, "tm", [128, 200], F32)
            olT2 = sb(p1, "olT2", [128, 2, CH], BF16)
            junk = sb(p1, "junk", [128, 128], F32)
            st1 = sb(p1, "st1", [128, 8], F32)
            bst = sb(p1, "bst", [128, 2, 6], F32)
            bmv = sb(p1, "bmv", [128, 2], F32)
            brs = sb(p1, "brs", [128, 1], F32)
            r1 = sb(p1, "r1", [128, D], F32)
            h1o = [r1, r1]
            mm = [ps(p1, f"mm{i}", [128, 512], F32) for i in range(2)]
            oacc = [ps(p1, f"oacc{i}", [128, 2, 512], F32) for i in range(2)]
            tp = [ps(p1, f"tp{i}", [128, 1024], BF16) for i in range(2)]
            mmr = Rot([0, 1]); tpr = Rot([0, 1])

            S.op("gpsimd", lambda e: e.memset(maskTs[1][:], 0), writes=["maskT1"])
            zsrc = maskTs[1][:].rearrange("p a b -> p (a b)").bitcast(BF16).rearrange("p (t d) -> p t d", d=D)
            for zi in range(NE * CAP // 1024):
                S.dma("scalar", lambda e, zi=zi: e.dma_start(out=xg_d[zi * 1024:(zi + 1) * 1024, :].rearrange("(t p) d -> p t d", p=128), in_=zsrc),
                      reads=["maskT1"], key="zf", serialize=False)
            S.lastw["xg_d"] = ("D:zf", S.cnt["D:zf"])
            S.dma("gpsimd", lambda e: e.dma_start(out=w_in_sb[:], in_=w_in_d.rearrange("(k p) n -> p k n", p=128)),
                  writes=["w_in_sb"], key="w0")
            S.dma("gpsimd", lambda e: e.dma_start(out=wpool_sb[:], in_=w_pool_d.rearrange("g c d -> c g d")),
                  writes=["wpool_sb"], key="w1")
            S.dma("gpsimd", lambda e: e.dma_start(out=wuk_sb[:], in_=w_uk_d.rearrange("(hp h2) d r -> (h2 d) hp r", h2=2)),
                  writes=["wuk_sb"], key="w2")
            S.op("vector", lambda e: e.memset(wuv_sb[:], 0.0), writes=["wuv_sb"])
            for h2 in range(2):
                S.dma("gpsimd", lambda e, h2=h2: e.dma_start(out=wuv_sb[:, :, h2, h2 * 64:(h2 + 1) * 64],
                                                             in_=w_uv_d.rearrange("(hp h2) r d -> h2 r hp d", h2=2)[h2]),
                      reads=["wuv_sb"], writes=["wuv_sb"], key=f"w3{h2}")
            S.dma("gpsimd", lambda e: e.dma_start(out=w_o_sb[:], in_=w_o_d.rearrange("(k p) n -> p k n", p=128)),
                  writes=["w_o_sb"], key="w4")
            S.dma("sync", lambda e: e.dma_start(out=pscale_sb[:], in_=pool_scale_d.rearrange("(g d) -> d g", d=128),
                                                allow_slow_non_contiguous=True),
                  writes=["pscale_sb"], key="c0")
            S.dma("sync", lambda e: e.dma_start(out=g1_bc[:], in_=bc(ln1g_d, D)), writes=["g1_bc"], key="c1")
            S.dma("sync", lambda e: e.dma_start(out=b1_bc[:], in_=bc(ln1b_d, D)), writes=["b1_bc"], key="c2")
            S.dma("sync", lambda e: e.dma_start(out=kvg_bc[:], in_=bc(kvg_d, 128)), writes=["kvg_bc"], key="c3")
            S.dma("sync", lambda e: e.dma_start(out=kig_bc[:], in_=bc(kig_d, 64)), writes=["kig_bc"], key="c4")
            S.dma("sync", lambda e: e.dma_start(out=kib_bc[:], in_=bc(kib_d, 64)), writes=["kib_bc"], key="c5")
            for g in range(4):
                w = 2 ** (g + 1)
                S.op("gpsimd", lambda e, g=g, w=w: e.memset(ic16[:, g, :], 1.0 / w), reads=["ic16"], writes=["ic16"])
                for t in range(w - 1):
                    S.op("gpsimd", lambda e, g=g, t=t: e.memset(ic16[:, g, t:t + 1], 1.0 / (t + 1)), reads=["ic16"], writes=["ic16"])
            S.op("gpsimd", lambda e: e.memset(halo[:], 0.0), writes=["halo"])
            S.op("gpsimd", lambda e: e.memset(c256[:], 256.0), writes=["c256"])
            for i_ in range(KBIS):
                S.op("gpsimd", lambda e, i_=i_: e.memset(pw2[:, i_:i_ + 1], 2.0 ** (-i_)), reads=["pw2"], writes=["pw2"])
            S.op("gpsimd", lambda e: e.memset(ckv1[:, :, 128:130], 1.0), writes=["ckv1"])

            def front(c):
                for t in range(4):
                    tg = 4 * c + t
                    b = tg % 2
                    S.dma("sync", lambda e, tg=tg, b=b: e.dma_start(out=xt[b][:], in_=x_d[tg * 128:(tg + 1) * 128, :]),
                          writes=[f"xt{b}"], key=f"x{b}")
                    S.op("scalar", lambda e, b=b: e.activation(out=xb[b][:], in_=xt[b][:], func=AF.Copy),
                         reads=[f"xt{b}"], writes=[f"xb{b}"])
                    for kh in range(2):
                        ti = tpr.next()
                        def tr(e, b=b, kh=kh, ti=ti):
                            ins = None
                            for kk in range(4):
                                k = kh * 4 + kk
                                ins = e.transpose(out=tp[ti][:, kk * 128:(kk + 1) * 128], in_=xb[b][:, k * 128:(k + 1) * 128],
                                                  identity=ident_b[:])
                            return ins
                        S.op("tensor", tr, reads=[f"xb{b}", "ident_b"], writes=[f"tp{ti}"])
                        S.op("vector", lambda e, kh=kh, ti=ti, t=t: e.tensor_copy(
                            out=xT[:, kh * 4:(kh + 1) * 4, t * 128:(t + 1) * 128],
                            in_=tp[ti][:, 0:512].rearrange("p (k n) -> p k n", k=4)),
                            reads=[f"tp{ti}"], writes=["xT"])

                def inproj_fm(col0):
                    bi = mmr.next()
                    def f(e, col0=col0, bi=bi):
                        ins = None
                        for k in range(8):
                            ins = e.matmul(mm[bi][:], lhsT=w_in_sb[:, k, col0:col0 + 128], rhs=xT[:, k, :],
                                           start=(k == 0), stop=(k == 7))
                        return ins
                    S.op("tensor", f, reads=["w_in_sb", "xT"], writes=[f"mm{bi}"])
                    return bi

                for g in range(4):
                    bi = inproj_fm(g * 128)
                    S.op("gpsimd", lambda e, g=g: e.tensor_copy(out=ug[:, 0:16], in_=halo[:, g, :]),
                         reads=["halo", "ug"], writes=["ug"])
                    S.op("scalar", lambda e, bi=bi: e.activation(out=ug[:, 16:528], in_=mm[bi][:], func=AF.Copy),
                         reads=[f"mm{bi}", "ug"], writes=["ug"])
                    S.op("gpsimd", lambda e, g=g: e.tensor_copy(out=halo[:, g, :], in_=ug[:, 512:528]),
                         reads=["ug"], writes=["halo"])
                    src, srcn = ug, "ug"
                    bufs = [(pA, "pA"), (pB, "pB")]
                    for lv in range(g + 1):
                        s = 2 ** lv
                        lo = 2 ** (lv + 1) - 1
                        dst, dstn = bufs[lv % 2]
                        S.op("gpsimd", lambda e, src=src, dst=dst, s=s, lo=lo: e.tensor_tensor(
                            out=dst[:, lo:528], in0=src[:, lo:528], in1=src[:, lo - s:528 - s], op=ALU.add),
                            reads=[srcn, dstn], writes=[dstn])
                        src, srcn = dst, dstn
                    w = 2 ** (g + 1)
                    S.op("vector", lambda e, src=src, w=w: e.scalar_tensor_tensor(
                        out=dT[:], in0=src[:, 16:528], scalar=1.0 / w, in1=ug[:, 16:528], op0=ALU.mult, op1=ALU.subtract),
                        reads=[srcn, "ug"], writes=["dT"])
                    if c == 0:
                        S.op("vector", lambda e, src=src, g=g: e.tensor_tensor(out=junk[:, 0:16], in0=src[:, 16:32], in1=ic16[:, g, :], op=ALU.mult),
                             reads=[srcn, "ic16"], writes=["junk"])
                        S.op("vector", lambda e: e.tensor_tensor(out=dT[:, 0:16], in0=junk[:, 0:16], in1=ug[:, 16:32], op=ALU.subtract),
                             reads=["junk", "ug", "dT"], writes=["dT"])
                    bi2 = mmr.next()
                    S.op("tensor", lambda e, g=g, bi2=bi2: e.matmul(mm[bi2][:], lhsT=wpool_sb[:, g, :], rhs=dT[:], start=True, stop=True),
                         reads=["wpool_sb", "dT"], writes=[f"mm{bi2}"])
                    S.op("scalar", lambda e, g=g, bi2=bi2: e.activation(out=mixTs[c % 2][:, g, :], in_=mm[bi2][:], func=AF.Copy, scale=pscale_sb[:, g:g + 1]),
                         reads=[f"mm{bi2}", "pscale_sb", f"mixT{c % 2}"], writes=[f"mixT{c % 2}"])
                for j in range(4):
                    bi = inproj_fm(512 + j * 128)
                    S.op("scalar", lambda e, j=j, bi=bi: e.activation(out=qTs[c % 2][:, j, :], in_=mm[bi][:], func=AF.Copy),
                         reads=[f"mm{bi}"], writes=[f"qT{c % 2}"])
                for j in range(4):
                    bi = inproj_fm(1152 + j * 128)
                    S.op("vector", lambda e, j=j, bi=bi: e.tensor_copy(out=qiT[:, j, :], in_=mm[bi][:]),
                         reads=[f"mm{bi}"], writes=["qiT"])
                tiA = tpr.next(); tiB = tpr.next()
                for t in range(4):
                    tg = 4 * c + t
                    bi = mmr.next()
                    def f(e, t=t, bi=bi):
                        ins = None
                        for k in range(8):
                            ins = e.matmul(mm[bi][:, 0:128], lhsT=xT[:, k, t * 128:(t + 1) * 128], rhs=w_in_sb[:, k, 1024:1152],
                                           start=(k == 0), stop=(k == 7))
                        for k in range(8):
                            ins = e.matmul(mm[bi][:, 128:200], lhsT=xT[:, k, t * 128:(t + 1) * 128], rhs=w_in_sb[:, k, 1664:1736],
                                           start=(k == 0), stop=(k == 7))
                        return ins
                    S.op("tensor", f, reads=["w_in_sb", "xT"], writes=[f"mm{bi}"])
                    S.op("scalar", lambda e, bi=bi: e.activation(out=tm[:], in_=mm[bi][:, 0:200], func=AF.Copy),
                         reads=[f"mm{bi}"], writes=["tm"])
                    S.op("scalar", lambda e: e.activation(out=junk[:], in_=tm[:, 0:128], func=AF.Square, accum_out=st1[:, 0:1]),
                         reads=["tm", "st1"], writes=["junk", "st1"])
                    ln_rstd(st1[:, 0:1], st1[:, 1:2], 1, 1.0 / 128, "st1", "st1")
                    S.op("vector", lambda e: e.scalar_tensor_tensor(out=ckn[:], in0=tm[:, 0:128], scalar=st1[:, 1:2], in1=kvg_bc[:],
                                                                     op0=ALU.mult, op1=ALU.mult),
                         reads=["tm", "st1", "kvg_bc"], writes=["ckn"])
                    S.op("gpsimd", lambda e, tg=tg: e.tensor_copy(out=ckv1[:, tg, 0:128], in_=ckn[:]), reads=["ckn", "ckv1"], writes=["ckv1"])
                    S.op("vector", lambda e: e.bn_stats(out=bst[:, 0, :], in_=tm[:, 128:192]), reads=["tm"], writes=["bst"])
                    S.op("vector", lambda e: e.bn_aggr(out=bmv[:], in_=bst[:, 0, :]), reads=["bst"], writes=["bmv"])
                    ln_rstd(bmv[:, 1:2], brs[:, 0:1], 0, 1.0, "bmv", "brs")
                    S.op("vector", lambda e: e.tensor_scalar(out=kn32[:], in0=tm[:, 128:192], scalar1=bmv[:, 0:1], scalar2=brs[:, 0:1],
                                                              op0=ALU.subtract, op1=ALU.mult),
                         reads=["tm", "bmv", "brs"], writes=["kn32"])
                    S.op("vector", lambda e, tg=tg: e.tensor_copy(out=widx[:, tg, :], in_=tm[:, 192:200]),
                         reads=["tm", "widx"], writes=["widx"])
                    S.op("gpsimd", lambda e: e.tensor_tensor(out=kn32[:], in0=kn32[:], in1=kig_bc[:], op=ALU.mult),
                         reads=["kn32", "kig_bc"], writes=["kn32"])
                    S.op("gpsimd", lambda e: e.tensor_tensor(out=kn2[:, 0:64], in0=kn32[:], in1=kib_bc[:], op=ALU.add),
                         reads=["kn32", "kib_bc", "kn2"], writes=["kn2"])
                    S.op("gpsimd", lambda e: e.tensor_copy(out=kn2[:, 64:128], in_=kn2[:, 0:64]), reads=["kn2"], writes=["kn2"])
                    S.op("tensor", lambda e, t=t, tiA=tiA: e.transpose(out=tp[tiA][:, t * 128:(t + 1) * 128], in_=ckn[:], identity=ident_b[:]),
                         reads=["ckn", "ident_b"], writes=[f"tp{tiA}"])
                    S.op("tensor", lambda e, t=t, tiB=tiB: e.transpose(out=tp[tiB][:, t * 128:(t + 1) * 128], in_=kn2[:], identity=ident_b[:]),
                         reads=["kn2", "ident_b"], writes=[f"tp{tiB}"])
                S.op("scalar", lambda e, c=c, tiA=tiA: e.activation(out=ckvT[:, c * CH:(c + 1) * CH], in_=tp[tiA][:, 0:512], func=AF.Copy),
                     reads=[f"tp{tiA}", "ckvT"], writes=["ckvT"])
                S.op("scalar", lambda e, c=c, tiB=tiB: e.activation(out=kiT[:, c * CH:(c + 1) * CH], in_=tp[tiB][:, 0:512], func=AF.Copy),
                     reads=[f"tp{tiB}", "kiT"], writes=["kiT"])


            def tile_sel(c, t):
                qt = 4 * c + t
                qt = 4 * c + t
                N = 128 * (qt + 1)
                nkc = (N + 511) // 512
                for kc in range(nkc):
                    k0 = kc * 512
                    kw = min(512, N - k0)
                    for h in range(8):
                        hp, h2 = h // 2, h % 2
                        bi = mmr.next()
                        S.op("tensor", lambda e, hp=hp, h2=h2, t=t, bi=bi, k0=k0, kw=kw: e.matmul(
                            mm[bi][:, 0:kw], lhsT=qiT[h2 * 64:(h2 + 1) * 64, hp, t * 128:(t + 1) * 128],
                            rhs=kiT[h2 * 64:(h2 + 1) * 64, k0:k0 + kw], start=True, stop=True),
                            reads=["qiT", "kiT"], writes=[f"mm{bi}"])
                        ri = h % 2
                        S.op("scalar", lambda e, bi=bi, ri=ri, kw=kw: e.activation(out=rl[ri][:, 0:kw], in_=mm[bi][:, 0:kw], func=AF.Relu),
                             reads=[f"mm{bi}"], writes=[f"rl{ri}"])
                        if h == 0:
                            S.op("vector", lambda e, ri=ri, k0=k0, kw=kw, qt=qt: e.tensor_scalar(
                                out=SC[:, k0:k0 + kw], in0=rl[ri][:, 0:kw], scalar1=widx[:, qt, 0:1], scalar2=None, op0=ALU.mult),
                                reads=[f"rl{ri}", "widx", "SC"], writes=["SC"])
                        else:
                            S.op("vector", lambda e, ri=ri, k0=k0, kw=kw, qt=qt, h=h: e.scalar_tensor_tensor(
                                out=SC[:, k0:k0 + kw], in0=rl[ri][:, 0:kw], scalar=widx[:, qt, h:h + 1], in1=SC[:, k0:k0 + kw],
                                op0=ALU.mult, op1=ALU.add),
                                reads=[f"rl{ri}", "widx", "SC"], writes=["SC"])
                if N > 256:
                    S.op("vector", lambda e, N=N: e.tensor_reduce(out=bmax[:], in_=SC[:, 0:N], axis=mybir.AxisListType.X, op=ALU.max,
                                                                   apply_absolute_value=True), reads=["SC"], writes=["bmax"])
                    S.op("vector", lambda e: e.tensor_scalar(out=bmax[:], in0=bmax[:], scalar1=1.0001, scalar2=1e-20, op0=ALU.mult, op1=ALU.add),
                         reads=["bmax"], writes=["bmax"])
                S.op("gpsimd", lambda e, qt=qt: e.affine_select(out=SC[:, qt * 128:(qt + 1) * 128], in_=SC[:, qt * 128:(qt + 1) * 128],
                                                                 pattern=[[-1, 128]], compare_op=ALU.is_ge, fill=NEG, base=0, channel_multiplier=1),
                     reads=["SC"], writes=["SC"])
                if N > 256:
                    S.op("vector", lambda e, N=N: e.tensor_scalar(out=bmid[:], in0=bmax[:], scalar1=0.0, scalar2=None, op0=ALU.mult),
                         reads=["bmax"], writes=["bmid"])
                    S.op("vector", lambda e: e.tensor_scalar(out=bsteps[:], in0=pw2[:], scalar1=bmax[:, 0:1], scalar2=None, op0=ALU.mult),
                         reads=["pw2", "bmax"], writes=["bsteps"])
                    S.op("vector", lambda e: e.tensor_scalar(out=bnegh[:], in0=bsteps[:], scalar1=-0.5, scalar2=None, op0=ALU.mult),
                         reads=["bsteps"], writes=["bnegh"])
                    for it_ in range(KBIS):
                        S.op("vector", lambda e, N=N: e.tensor_scalar(out=xT[:].rearrange("p k n -> p (k n)").bitcast(mybir.dt.uint8)[:, 0:N], in0=SC[:, 0:N], scalar1=bmid[:, 0:1], scalar2=0.0,
                                                                       op0=ALU.is_ge, op1=ALU.add, accum_out=bcnt[:, 0:1]),
                             reads=["SC", "bmid"], writes=["xT", "bcnt"])
                        S.op("vector", lambda e, it_=it_: e.tensor_scalar(out=bd[:], in0=bcnt[:], scalar1=c256[:, 0:1], scalar2=bsteps[:, it_:it_ + 1],
                                                                          op0=ALU.is_ge, op1=ALU.mult),
                             reads=["bcnt", "c256", "bsteps"], writes=["bd"])
                        sc2 = bnegh[:, it_:it_ + 1] if it_ < KBIS - 1 else bnegl[:, 0:1]
                        if it_ == KBIS - 1:
                            S.op("vector", lambda e, it_=it_: e.tensor_scalar(out=bnegl[:], in0=bsteps[:, it_:it_ + 1], scalar1=-1.0, scalar2=None, op0=ALU.mult),
                                 reads=["bsteps"], writes=["bnegl"])
                        S.op("vector", lambda e, sc2=sc2: e.tensor_scalar(out=bmid[:], in0=bmid[:], scalar1=bd[:, 0:1], scalar2=sc2,
                                                                          op0=ALU.add, op1=ALU.add),
                             reads=["bmid", "bd", "bnegh", "bnegl"], writes=["bmid"])

            def tile_mask(c, t):
                qt = 4 * c + t
                N = 128 * (qt + 1)
                for kt in range(qt + 1):
                    mi = kt % 2
                    if N > 256:
                        S.op("vector", lambda e, kt=kt, mi=mi: e.tensor_scalar(out=m128[mi][:], in0=SC[:, kt * 128:(kt + 1) * 128],
                                                                               scalar1=bmid[:, 0:1], scalar2=None, op0=ALU.is_ge),
                             reads=["SC", "bmid"], writes=[f"m128_{mi}"])
                    else:
                        S.op("vector", lambda e, kt=kt, mi=mi: e.tensor_scalar(out=m128[mi][:], in0=SC[:, kt * 128:(kt + 1) * 128],
                                                                               scalar1=-0.5e30, scalar2=None, op0=ALU.is_ge),
                             reads=["SC"], writes=[f"m128_{mi}"])
                    ti = tpr.next()
                    S.op("tensor", lambda e, mi=mi, ti=ti: e.transpose(out=tp[ti][:, 0:128], in_=m128[mi][:], identity=ident_b[:]),
                         reads=[f"m128_{mi}", "ident_b"], writes=[f"tp{ti}"])
                    S.op("scalar", lambda e, kt=kt, t=t, ti=ti: e.activation(out=maskTs[c % 2][:, kt, t * 128:(t + 1) * 128], in_=tp[ti][:, 0:128], func=AF.Copy),
                         reads=[f"tp{ti}", f"maskT{c % 2}"], writes=[f"maskT{c % 2}"])


            def att_head(c, h, use_dve=False, look=1):
                nkt = 4 * c + 4
                hp, h2 = h // 2, h % 2
                qi = h % 2
                oi = h % 2
                bi = mmr.next()
                S.op("tensor", lambda e, hp=hp, h2=h2, bi=bi: e.matmul(mm[bi][:], lhsT=wuk_sb[h2 * 64:(h2 + 1) * 64, hp, :],
                                                                    rhs=qTs[c % 2][h2 * 64:(h2 + 1) * 64, hp, :], start=True, stop=True),
                     reads=["wuk_sb", f"qT{c % 2}"], writes=[f"mm{bi}"])
                S.op("scalar", lambda e, bi=bi, qi=qi: e.activation(out=qlat[qi][:], in_=mm[bi][:], func=AF.Copy, scale=0.125),
                     reads=[f"mm{bi}"], writes=[f"qlat{qi}"])
                def qk(kt):
                    j0 = max(0, kt - 4 * c)
                    ncol = (4 - j0) * 128
                    c0 = j0 * 128
                    bi = mmr.next()
                    S.op("tensor", lambda e, kt=kt, bi=bi, qi=qi, c0=c0, ncol=ncol: e.matmul(
                        mm[bi][:, 0:ncol], lhsT=ckvT[:, kt * 128:(kt + 1) * 128], rhs=qlat[qi][:, c0:c0 + ncol], start=True, stop=True),
                        reads=["ckvT", f"qlat{qi}"], writes=[f"mm{bi}"])
                    return bi, j0, ncol, c0
                pend = [qk(k_) for k_ in range(min(look, nkt))]
                for kt in range(nkt):
                    bi, j0, ncol, c0 = pend.pop(0)
                    if kt + look < nkt:
                        pend.append(qk(kt + look))
                    ei = kt % (3 if look > 1 else 2)
                    S.op("scalar", lambda e, bi=bi, ei=ei, ncol=ncol: e.activation(out=Eb[ei][:, 0:ncol], in_=mm[bi][:, 0:ncol], func=AF.Exp),
                         reads=[f"mm{bi}"], writes=[f"Eb{ei}"])
                    S.op("vector" if (use_dve and kt % 2 == 0) else "gpsimd", lambda e, ei=ei, kt=kt, c0=c0, ncol=ncol, c=c: e.tensor_tensor(
                        out=Pb[ei][:, 0:ncol], in0=Eb[ei][:, 0:ncol], in1=maskTs[c % 2][:, kt, c0:c0 + ncol], op=ALU.mult),
                        reads=[f"Eb{ei}", f"maskT{c % 2}"], writes=[f"Pb{ei}"])
                    def pv(e, kt=kt, j0=j0, ei=ei, oi=oi, c=c):
                        ins = None
                        for j in range(j0, 4):
                            bank, off = (0, j * 129) if j < 3 else (1, 0)
                            ins = e.matmul(oacc[oi][:, bank, off:off + 129], lhsT=Pb[ei][:, (j - j0) * 128:(j - j0 + 1) * 128],
                                           rhs=ckv1[:, kt, 0:129], start=(kt == 0 and j in (0, 3)), stop=(kt == 4 * c + j),
                                           skip_group_check=True)
                        return ins
                    S.op("tensor", pv, reads=[f"Pb{ei}", "ckv1"], writes=[f"oacc{oi}"])
                ti = tpr.next()
                for j in range(4):
                    bank, off = (0, j * 129) if j < 3 else (1, 0)
                    li = j % 2
                    S.op("scalar", lambda e, oi=oi, bank=bank, off=off, j=j: e.activation(out=rden[:, j:j + 1], in_=oacc[oi][:, bank, off + 128:off + 129], func=AF.Ln),
                         reads=[f"oacc{oi}", "rden"], writes=["rden"])
                    S.op("scalar", lambda e, j=j: e.activation(out=rden[:, j:j + 1], in_=rden[:, j:j + 1], func=AF.Exp, scale=-1.0),
                         reads=["rden"], writes=["rden"])
                    S.op("scalar", lambda e, oi=oi, bank=bank, off=off, j=j, li=li: e.activation(
                        out=olat[li][:], in_=oacc[oi][:, bank, off:off + 128], func=AF.Copy, scale=rden[:, j:j + 1]),
                        reads=[f"oacc{oi}", "rden"], writes=[f"olat{li}"])
                    S.op("tensor", lambda e, li=li, ti=ti, j=j: e.transpose(out=tp[ti][:, j * 128:(j + 1) * 128], in_=olat[li][:], identity=ident_b[:]),
                         reads=[f"olat{li}", "ident_b"], writes=[f"tp{ti}"])
                S.op("scalar", lambda e, ti=ti, h2=h2: e.activation(out=olT2[:, h2, :], in_=tp[ti][:, 0:512], func=AF.Copy),
                     reads=[f"tp{ti}", "olT2"], writes=["olT2"])
                if h2 == 1:
                    bi = mmr.next()
                    def f(e, hp=hp, bi=bi):
                        e.matmul(mm[bi][:], lhsT=wuv_sb[:, hp, 0, :], rhs=olT2[:, 0, :], start=True, stop=False)
                        return e.matmul(mm[bi][:], lhsT=wuv_sb[:, hp, 1, :], rhs=olT2[:, 1, :], start=False, stop=True)
                    S.op("tensor", f, reads=["wuv_sb", "olT2"], writes=[f"mm{bi}"])
                    S.op("scalar", lambda e, hp=hp, bi=bi, c=c: e.activation(out=mixTs[c % 2][:, 4 + hp, :], in_=mm[bi][:], func=AF.Copy),
                         reads=[f"mm{bi}", f"mixT{c % 2}"], writes=[f"mixT{c % 2}"])


            def out_ln(c):
                if DEBUG == "1a" and c == 0:
                    dump(S, "qT", qTs[0][:].rearrange("p k n -> p (k n)"), [128, 4 * CH], BF16, ["qT0"])
                    dump(S, "qiT", qiT[:].rearrange("p k n -> p (k n)"), [128, 4 * CH], BF16, ["qiT"])
                    dump(S, "maskT", maskTs[0][:, 0:4, :].rearrange("p k n -> p (k n)"), [128, 4 * CH], mybir.dt.uint8, ["maskT0"])
                    dump(S, "olT2", olT2[:].rearrange("p k n -> p (k n)"), [128, 2 * CH], BF16, ["olT2"])
                    dump(S, "SC", SC[:, 0:512], [128, 512], F32, ["SC"])
                if DEBUG == "1a" and c == 7:
                    dump(S, "ckv1", ckv1[:].rearrange("p k n -> p (k n)"), [128, NT * 130], BF16, ["ckv1"])
                    dump(S, "ckvT", ckvT[:], [128, T], BF16, ["ckvT"])
                    dump(S, "kiT", kiT[:], [128, T], BF16, ["kiT"])
                    dump(S, "widx", widx[:].rearrange("p k n -> p (k n)"), [128, NT * 8], F32, ["widx"])
                if DEBUG == "1a":
                    S.dma("sync", lambda e, c=c: e.dma_start(out=dbg["mixT"][c], in_=mixTs[c % 2][:].rearrange("p k n -> p (k n)")),
                          reads=[f"mixT{c % 2}"], key="dbgm")

                for t in range(4):
                    tg = 4 * c + t
                    b = tg % 2
                    S.dma("sync", lambda e, tg=tg, b=b: e.dma_start(out=xt[b][:], in_=x_d[tg * 128:(tg + 1) * 128, :]),
                          writes=[f"xt{b}"], key=f"x{b}")
                    for hf in range(2):
                        bi = mmr.next()
                        def f(e, t=t, hf=hf, bi=bi):
                            ins = None
                            for k in range(8):
                                ins = e.matmul(mm[bi][:], lhsT=mixTs[c % 2][:, k, t * 128:(t + 1) * 128], rhs=w_o_sb[:, k, hf * 512:(hf + 1) * 512],
                                               start=(k == 0), stop=(k == 7))
                            return ins
                        S.op("tensor", f, reads=[f"mixT{c % 2}", "w_o_sb"], writes=[f"mm{bi}"])
                        S.op("vector", lambda e, b=b, hf=hf, bi=bi: e.scalar_tensor_tensor(
                            out=r1[:, hf * 512:(hf + 1) * 512], in0=xt[b][:, hf * 512:(hf + 1) * 512], scalar=ALPHA, in1=mm[bi][:],
                            op0=ALU.mult, op1=ALU.add),
                            reads=[f"xt{b}", f"mm{bi}", "r1"], writes=["r1"])
                    layer_norm_tile(r1[:], h1o[b][:], g1_bc, b1_bc, bst, bmv, brs, r1[:], ("r1", "r1", "r1"))
                    S.dma("sync", lambda e, tg=tg, b=b: e.dma_start(out=h1_d[tg * 128:(tg + 1) * 128, :], in_=h1o[b][:]),
                          reads=["r1"], key="h1s0")
                    if DEBUG == "1a":
                        S.dma("sync", lambda e, tg=tg, b=b: e.dma_start(out=dbg["h"][tg * 128:(tg + 1) * 128, :], in_=h1o[b][:]),
                              reads=["r1"], key="dbgh0")

            for c in range(NCH):
                front(c)
                for t in range(4):
                    tile_sel(c, t)
                    if c > 0:
                        att_head(c - 1, 2 * t)
                        att_head(c - 1, 2 * t + 1)
                    tile_mask(c, t)
                if c > 0:
                    out_ln(c - 1)
            S.full_barrier()
            mm.append(tp[1][:].bitcast(F32))
            mmr.items = [0, 1, 2]
            tpr.items = [0]
            for h in range(8):
                att_head(NCH - 1, h, use_dve=True, look=2)
            out_ln(NCH - 1)
            S.full_barrier()
            S.flush(nc)

        if DEBUG == "1a":
            return nc

        with ExitStack() as p2:
            w_mq_sb = sb(p2, "w_mq_sb", [128, 8, D], BF16)
            w_mo_sb = sb(p2, "w_mo_sb", [128, 8, D], BF16)
            w_mkv_sb = sb(p2, "w_mkv_sb", [128, 8, 2 * D], BF16)
            mb = sb(p2, "mb", [128, 2, D], BF16)
            memT = sb(p2, "memT", [128, 8, 256], BF16)
            KmT = sb(p2, "KmT", [128, 8, 256], BF16)
            Vm = sb(p2, "Vm", [128, 2, D], BF16)
            g2_bc = sb(p2, "g2_bc", [128, D], F32)
            b2_bc = sb(p2, "b2_bc", [128, D], F32)
            wr_sb = sb(p2, "wr_sb", [128, 8, NE], F32)
            br_bc = sb(p2, "br_bc", [128, NE], F32)
            ustr_f = sb(p2, "ustr_f", [128, 128], F32)
            ustr_b = sb(p2, "ustr_b", [128, 128], BF16)
            cb_i = sb(p2, "cb_i", [128, NE], I32)
            cbase = sb(p2, "cbase", [128, NE], F32)
            caphi = sb(p2, "caphi", [128, NE], F32)
            h1c = sb(p2, "h1c", [128, 4, D], F32)
            h1b = [sb(p2, f"h1b{i}", [128, D], BF16) for i in range(2)]
            hT = sb(p2, "hT", [128, 8, CH], BF16)
            qmT = sb(p2, "qmT", [128, 8, CH], BF16)
            Pm = [sb(p2, f"Pm{i}", [128, CH], BF16) for i in range(2)]
            rdn = sb(p2, "rdn", [128, CH], F32)
            omT = sb(p2, "omT", [128, 8, CH], BF16)
            r2 = sb(p2, "r2", [128, D], F32)
            tmpn2 = sb(p2, "tmpn2", [128, D], F32)
            h2o = [sb(p2, f"h2o{i}", [128, D], F32) for i in range(2)]
            h2b = [sb(p2, f"h2b{i}", [128, D], BF16) for i in range(2)]
            h2T = sb(p2, "h2T", [128, 8, 128], F32)
            lg = sb(p2, "lg", [128, NE], F32)
            mx8r = sb(p2, "mx8r", [128, 8], F32)
            negm = sb(p2, "negm", [128, 1], F32)
            ex4 = sb(p2, "ex4", [128, 4], F32)
            gsum = sb(p2, "gsum", [128, 1], F32)
            selb = sb(p2, "selb", [128, NE], BF16)
            slotm = sb(p2, "slotm", [128, NE], F32)
            ohp = sb(p2, "ohp", [128, NE], F32)
            slotf = sb(p2, "slotf", [128, 4], F32)
            bst2 = sb(p2, "bst2", [128, 2, 6], F32)
            bmv2 = sb(p2, "bmv2", [128, 2], F32)
            brs2 = sb(p2, "brs2", [128, 1], F32)
            mm = [ps(p2, f"mmB{i}", [128, 512], F32) for i in range(6)]
            tp = [ps(p2, f"tpB{i}", [128, 1024], BF16) for i in range(2)]
            mmr = Rot(list(range(6))); tpr = Rot([0, 1])

            S.dma("gpsimd", lambda e: e.dma_start(out=w_mkv_sb[:], in_=w_mkv_d.rearrange("(k p) n -> p k n", p=128)), writes=["w_mkv_sb"], key="w0")
            S.dma("gpsimd", lambda e: e.dma_start(out=mb[:], in_=mem_d.rearrange("(t p) d -> p t d", p=128)), writes=["mb"], key="w1")
            S.dma("gpsimd", lambda e: e.dma_start(out=w_mq_sb[:], in_=w_mq_d.rearrange("(k p) n -> p k n", p=128)), writes=["w_mq_sb"], key="w2")
            S.dma("gpsimd", lambda e: e.dma_start(out=w_mo_sb[:], in_=w_mo_d.rearrange("(k p) n -> p k n", p=128)), writes=["w_mo_sb"], key="w4")
            S.dma("sync", lambda e: e.dma_start(out=g2_bc[:], in_=bc(ln2g_d, D)), writes=["g2_bc"], key="c1")
            S.dma("sync", lambda e: e.dma_start(out=b2_bc[:], in_=bc(ln2b_d, D)), writes=["b2_bc"], key="c2")
            S.dma("sync", lambda e: e.dma_start(out=wr_sb[:], in_=w_r_d.rearrange("(k p) n -> p k n", p=128)), writes=["wr_sb"], key="c3")
            S.dma("sync", lambda e: e.dma_start(out=br_bc[:], in_=bc(b_r_d, NE)), writes=["br_bc"], key="c4")
            S.op("gpsimd", lambda e: e.memset(ustr_f[:], 1.0), writes=["ustr_f"])
            S.op("gpsimd", lambda e: e.affine_select(out=ustr_f[:], in_=ustr_f[:], pattern=[[1, 128]], compare_op=ALU.is_gt, fill=0.0,
                                                       base=0, channel_multiplier=-1), reads=["ustr_f"], writes=["ustr_f"])
            S.op("vector", lambda e: e.tensor_copy(out=ustr_b[:], in_=ustr_f[:]), reads=["ustr_f"], writes=["ustr_b"])
            S.op("gpsimd", lambda e: e.iota(out=cb_i[:], pattern=[[CAP, NE]], base=0, channel_multiplier=0), writes=["cb_i"])
            S.op("vector", lambda e: e.tensor_copy(out=cbase[:], in_=cb_i[:]), reads=["cb_i"], writes=["cbase"])
            S.op("vector", lambda e: e.tensor_scalar(out=caphi[:], in0=cbase[:], scalar1=float(CAP - 1), scalar2=None, op0=ALU.add),
                 reads=["cbase"], writes=["caphi"])
            for mt in range(2):
                for kh in range(2):
                    ti = tpr.next()
                    def tr(e, mt=mt, kh=kh, ti=ti):
                        ins = None
                        for kk in range(4):
                            k = kh * 4 + kk
                            ins = e.transpose(out=tp[ti][:, kk * 128:(kk + 1) * 128], in_=mb[:, mt, k * 128:(k + 1) * 128], identity=ident_b[:])
                        return ins
                    S.op("tensor", tr, reads=["mb", "ident_b"], writes=[f"tp{ti}"])
                    S.op("vector", lambda e, mt=mt, kh=kh, ti=ti: e.tensor_copy(
                        out=memT[:, kh * 4:(kh + 1) * 4, mt * 128:(mt + 1) * 128], in_=tp[ti][:, 0:512].rearrange("p (k n) -> p k n", k=4)),
                        reads=[f"tp{ti}", "memT"], writes=["memT"])
            for cc in range(8):
                bi = mmr.next()
                def f(e, cc=cc, bi=bi):
                    ins = None
                    for k in range(8):
                        ins = e.matmul(mm[bi][:, 0:256], lhsT=w_mkv_sb[:, k, cc * 128:(cc + 1) * 128], rhs=memT[:, k, :], start=(k == 0), stop=(k == 7))
                    return ins
                S.op("tensor", f, reads=["w_mkv_sb", "memT"], writes=[f"mm{bi}"])
                S.op("vector", lambda e, cc=cc, bi=bi: e.tensor_copy(out=KmT[:, cc, :], in_=mm[bi][:, 0:256]), reads=[f"mm{bi}", "KmT"], writes=["KmT"])
            for mt in range(2):
                for hf in range(2):
                    bi = mmr.next()
                    def f(e, mt=mt, hf=hf, bi=bi):
                        ins = None
                        for k in range(8):
                            ins = e.matmul(mm[bi][:], lhsT=memT[:, k, mt * 128:(mt + 1) * 128], rhs=w_mkv_sb[:, k, D + hf * 512:D + (hf + 1) * 512],
                                           start=(k == 0), stop=(k == 7))
                        return ins
                    S.op("tensor", f, reads=["w_mkv_sb", "memT"], writes=[f"mm{bi}"])
                    S.op("scalar", lambda e, mt=mt, hf=hf, bi=bi: e.activation(out=Vm[:, mt, hf * 512:(hf + 1) * 512], in_=mm[bi][:], func=AF.Copy),
                         reads=[f"mm{bi}", "Vm"], writes=["Vm"])

            for c in range(NCH):
                S.dma("sync", lambda e, c=c: e.dma_start(out=h1c[:], in_=h1_d[c * CH:(c + 1) * CH, :].rearrange("(t p) d -> p t d", p=128)),
                      writes=["h1c"], key="h1l")
                for t in range(4):
                    b = t % 2
                    S.op("scalar", lambda e, b=b, t=t: e.activation(out=h1b[b][:], in_=h1c[:, t, :], func=AF.Copy),
                         reads=["h1c"], writes=[f"h1b{b}"])
                    for kh in range(2):
                        ti = tpr.next()
                        def tr(e, b=b, kh=kh, ti=ti):
                            ins = None
                            for kk in range(4):
                                k = kh * 4 + kk
                                ins = e.transpose(out=tp[ti][:, kk * 128:(kk + 1) * 128], in_=h1b[b][:, k * 128:(k + 1) * 128], identity=ident_b[:])
                            return ins
                        S.op("tensor", tr, reads=[f"h1b{b}", "ident_b"], writes=[f"tp{ti}"])
                        S.op("vector", lambda e, kh=kh, ti=ti, t=t: e.tensor_copy(
                            out=hT[:, kh * 4:(kh + 1) * 4, t * 128:(t + 1) * 128], in_=tp[ti][:, 0:512].rearrange("p (k n) -> p k n", k=4)),
                            reads=[f"tp{ti}", "hT"], writes=["hT"])
                for cc in range(8):
                    bi = mmr.next()
                    def f(e, cc=cc, bi=bi):
                        ins = None
                        for k in range(8):
                            ins = e.matmul(mm[bi][:], lhsT=w_mq_sb[:, k, cc * 128:(cc + 1) * 128], rhs=hT[:, k, :], start=(k == 0), stop=(k == 7))
                        return ins
                    S.op("tensor", f, reads=["w_mq_sb", "hT"], writes=[f"mm{bi}"])
                    S.op("scalar", lambda e, cc=cc, bi=bi: e.activation(out=qmT[:, cc, :], in_=mm[bi][:], func=AF.Copy, scale=1.0 / 16),
                         reads=[f"mm{bi}", "qmT"], writes=["qmT"])
                for h in range(4):
                    for mt in range(2):
                        bi = mmr.next()
                        def f(e, h=h, mt=mt, bi=bi):
                            e.matmul(mm[bi][:], lhsT=KmT[:, 2 * h, mt * 128:(mt + 1) * 128], rhs=qmT[:, 2 * h, :], start=True, stop=False)
                            return e.matmul(mm[bi][:], lhsT=KmT[:, 2 * h + 1, mt * 128:(mt + 1) * 128], rhs=qmT[:, 2 * h + 1, :], start=False, stop=True)
                        S.op("tensor", f, reads=["KmT", "qmT"], writes=[f"mm{bi}"])
                        S.op("scalar", lambda e, mt=mt, bi=bi: e.activation(out=Pm[mt][:], in_=mm[bi][:], func=AF.Exp),
                             reads=[f"mm{bi}"], writes=[f"Pm{mt}"])
                    bi = mmr.next()
                    def f(e, bi=bi):
                        e.matmul(mm[bi][:], lhsT=ones_b[:], rhs=Pm[0][:], start=True, stop=False)
                        return e.matmul(mm[bi][:], lhsT=ones_b[:], rhs=Pm[1][:], start=False, stop=True)
                    S.op("tensor", f, reads=["ones_b", "Pm0", "Pm1"], writes=[f"mm{bi}"])
                    S.op("vector", lambda e, bi=bi: e.reciprocal(out=rdn[:], in_=mm[bi][:]), reads=[f"mm{bi}"], writes=["rdn"])
                    for dvc in range(2):
                        bi = mmr.next()
                        def f(e, h=h, dvc=dvc, bi=bi):
                            c0 = h * 256 + dvc * 128
                            e.matmul(mm[bi][:], lhsT=Vm[:, 0, c0:c0 + 128], rhs=Pm[0][:], start=True, stop=False)
                            return e.matmul(mm[bi][:], lhsT=Vm[:, 1, c0:c0 + 128], rhs=Pm[1][:], start=False, stop=True)
                        S.op("tensor", f, reads=["Vm", "Pm0", "Pm1"], writes=[f"mm{bi}"])
                        S.op("vector", lambda e, h=h, dvc=dvc, bi=bi: e.tensor_tensor(out=omT[:, 2 * h + dvc, :], in0=mm[bi][:], in1=rdn[:], op=ALU.mult),
                             reads=[f"mm{bi}", "rdn", "omT"], writes=["omT"])
                def outproj_ln(t):
                    tg = 4 * c + t
                    b = tg % 2
                    for hf in range(2):
                        bi = mmr.next()
                        def f(e, t=t, hf=hf, bi=bi):
                            ins = None
                            for k in range(8):
                                ins = e.matmul(mm[bi][:], lhsT=omT[:, k, t * 128:(t + 1) * 128], rhs=w_mo_sb[:, k, hf * 512:(hf + 1) * 512],
                                               start=(k == 0), stop=(k == 7))
                            return ins
                        S.op("tensor", f, reads=["omT", "w_mo_sb"], writes=[f"mm{bi}"])
                        S.op("vector", lambda e, t=t, hf=hf, bi=bi: e.scalar_tensor_tensor(
                            out=r2[:, hf * 512:(hf + 1) * 512], in0=h1c[:, t, hf * 512:(hf + 1) * 512], scalar=ALPHA, in1=mm[bi][:],
                            op0=ALU.mult, op1=ALU.add), reads=["h1c", f"mm{bi}", "r2"], writes=["r2"])
                    layer_norm_tile(r2[:], h2o[b][:], g2_bc, b2_bc, bst2, bmv2, brs2, tmpn2[:], ("r2", f"h2o{b}", "tmpn2"))
                    S.dma("sync", lambda e, tg=tg, b=b: e.dma_start(out=h2_d[tg * 128:(tg + 1) * 128, :], in_=h2o[b][:]),
                          reads=[f"h2o{b}"], key=f"h2s{b}")
                    if DEBUG == "1b":
                        S.dma("sync", lambda e, tg=tg, b=b: e.dma_start(out=dbg["h"][tg * 128:(tg + 1) * 128, :], in_=h2o[b][:]),
                              reads=[f"h2o{b}"], key=f"dbgh{b}")
                    S.op("scalar", lambda e, b=b: e.activation(out=h2b[b][:], in_=h2o[b][:], func=AF.Copy), reads=[f"h2o{b}"], writes=[f"h2b{b}"])
                def router(t):
                    tg = 4 * c + t
                    b = tg % 2
                    for kh in range(2):
                        bi = mmr.next()
                        def tr(e, b=b, kh=kh, bi=bi):
                            ins = None
                            for kk in range(4):
                                k = kh * 4 + kk
                                ins = e.transpose(out=mm[bi][:, kk * 128:(kk + 1) * 128], in_=h2o[b][:, k * 128:(k + 1) * 128], identity=ident_f[:])
                            return ins
                        S.op("tensor", tr, reads=[f"h2o{b}", "ident_f"], writes=[f"mm{bi}"])
                        S.op("vector", lambda e, kh=kh, bi=bi: e.tensor_copy(out=h2T[:, kh * 4:(kh + 1) * 4, :],
                                                                              in_=mm[bi][:].rearrange("p (k n) -> p k n", k=4)),
                             reads=[f"mm{bi}", "h2T"], writes=["h2T"])
                    bi = mmr.next()
                    def f(e, bi=bi):
                        ins = None
                        for k in range(8):
                            ins = e.matmul(mm[bi][:, 0:NE], lhsT=h2T[:, k, :], rhs=wr_sb[:, k, :], start=(k == 0), stop=(k == 7))
                        return ins
                    S.op("tensor", f, reads=["h2T", "wr_sb"], writes=[f"mm{bi}"])
                    S.op("vector", lambda e, bi=bi: e.tensor_tensor(out=lg[:], in0=mm[bi][:, 0:NE], in1=br_bc[:], op=ALU.add),
                         reads=[f"mm{bi}", "br_bc"], writes=["lg"])
                    S.op("vector", lambda e: e.max(out=mx8r[:], in_=lg[:]), reads=["lg"], writes=["mx8r"])
                    S.op("vector", lambda e: e.tensor_scalar(out=negm[:], in0=mx8r[:, 0:1], scalar1=-1.0, scalar2=None, op0=ALU.mult),
                         reads=["mx8r"], writes=["negm"])
                    S.op("scalar", lambda e: e.activation(out=ex4[:], in_=mx8r[:, 0:4], func=AF.Exp, bias=negm[:, 0:1], accum_out=gsum[:, 0:1]),
                         reads=["mx8r", "negm", "gsum"], writes=["ex4", "gsum"])
                    S.op("vector", lambda e: e.reciprocal(out=gsum[:], in_=gsum[:]), reads=["gsum"], writes=["gsum"])
                    S.op("vector", lambda e, tg=tg: e.tensor_scalar(out=gates_all[:, tg, :], in0=ex4[:], scalar1=gsum[:, 0:1], scalar2=None, op0=ALU.mult),
                         reads=["ex4", "gsum", "gates_all"], writes=["gates_all"])
                    S.op("vector", lambda e: e.tensor_scalar(out=selb[:], in0=lg[:], scalar1=mx8r[:, 3:4], scalar2=None, op0=ALU.is_ge),
                         reads=["lg", "mx8r"], writes=["selb"])
                    bi = mmr.next()
                    def f(e, bi=bi):
                        e.matmul(mm[bi][:, 0:NE], lhsT=ustr_b[:], rhs=selb[:], start=True, stop=True)
                        return e.matmul(mm[bi][:, 64:64 + NE], lhsT=ones_b[:], rhs=selb[:], start=True, stop=True)
                    S.op("tensor", f, reads=["ustr_b", "ones_b", "selb"], writes=[f"mm{bi}"])
                    S.op("vector", lambda e, bi=bi: e.tensor_tensor(out=slotm[:], in0=mm[bi][:, 0:NE], in1=cbase[:], op=ALU.add),
                         reads=[f"mm{bi}", "cbase"], writes=["slotm"])
                    S.op("vector", lambda e: e.tensor_tensor(out=slotm[:], in0=slotm[:], in1=caphi[:], op=ALU.min),
                         reads=["slotm", "caphi"], writes=["slotm"])
                    S.op("vector", lambda e, bi=bi: e.tensor_tensor(out=cbase[:], in0=mm[bi][:, 64:64 + NE], in1=cbase[:], op=ALU.add),
                         reads=[f"mm{bi}", "cbase"], writes=["cbase"])
                    for k in range(4):
                        S.op("vector", lambda e, k=k: e.scalar_tensor_tensor(out=ohp[:], in0=lg[:], scalar=mx8r[:, k:k + 1], in1=slotm[:],
                                                                             op0=ALU.is_equal, op1=ALU.mult),
                             reads=["lg", "mx8r", "slotm"], writes=["ohp"])
                        S.op("vector", lambda e, k=k: e.reduce_sum(out=slotf[:, k:k + 1], in_=ohp[:], axis=mybir.AxisListType.X),
                             reads=["ohp", "slotf"], writes=["slotf"])
                    S.op("vector", lambda e, tg=tg: e.tensor_copy(out=slots_all[:, tg, :], in_=slotf[:]), reads=["slotf", "slots_all"], writes=["slots_all"])
                    for k in range(4):
                        S.dma("gpsimd", lambda e, tg=tg, k=k, b=b: e.indirect_dma_start(
                            out=xg_d, out_offset=bass.IndirectOffsetOnAxis(ap=slots_all[:, tg, k:k + 1], axis=0), in_=h2b[b][:], in_offset=None),
                            reads=["slots_all", f"h2b{b}"], writes=["xg_d"], key=f"sc{k}")
                for t in range(4):
                    outproj_ln(t)
                    if t > 0:
                        router(t - 1)
                router(3)
            if DEBUG == "1b":
                dump(S, "slots", slots_all[:].rearrange("p a b -> p (a b)"), [128, NT * 4], I32, ["slots_all"])
                dump(S, "gates", gates_all[:].rearrange("p a b -> p (a b)"), [128, NT * 4], F32, ["gates_all"])
            S.full_barrier()
            S.flush(nc)
        if DEBUG == "1b":
            return nc

        BLKS = [(0, 512), (512, CAP - 512)]
        NTE = CAP // 128
        with ExitStack() as p3:
            wgu = [sb(p3, f"wgu{i}", [128, 8, 2 * D], BF16) for i in range(2)]
            wdn = [sb(p3, f"wdn{i}", [128, 8, D], BF16) for i in range(2)]
            bgr = sb(p3, "bgr", [NE, 2 * D], F32)
            bguT = sb(p3, "bguT", [128, 16, NE], F32)
            bgu7 = sb(p3, "bgu7", [128, 8, NE], F32)
            bdn = [sb(p3, f"bdn{i}", [128, D], F32) for i in range(2)]
            xg = [sb(p3, f"xg{i}", [128, NTE, D], BF16) for i in range(2)]
            xgT = sb(p3, "xgT", [128, 8, CAP], BF16)
            actT = sb(p3, "actT", [128, 8, CAP], BF16)
            s0 = [sb(p3, f"s0_{i}", [128, 512], F32) for i in range(2)]
            gcl = [sb(p3, f"gc{i}", [128, 512], F32) for i in range(2)]
            ucl = [sb(p3, f"uc{i}", [128, 512], F32) for i in range(2)]
            ysb = [sb(p3, f"ysb{i}", [128, D], F32) for i in range(2)]
            mm = [ps(p3, f"mmC{i}", [128, 512], F32) for i in range(6)]
            tp = [ps(p3, f"tpC{i}", [128, 1024], BF16) for i in range(2)]
            mmr = Rot(list(range(6))); tpr = Rot([0, 1])

            S.dma("sync", lambda e: e.dma_start(out=bgr[:], in_=b_gu_d), writes=["bgr"], key="c1")
            bi = mmr.next()
            def trb(e, bi=bi):
                ins = None
                for cidx in range(16):
                    ins = e.transpose(out=mm[bi][:, cidx * NE:(cidx + 1) * NE], in_=bgr[0:NE, cidx * 128:(cidx + 1) * 128], identity=ident_f[0:NE, 0:NE])
                return ins
            S.op("tensor", trb, reads=["bgr", "ident_f"], writes=[f"mm{bi}"])
            S.op("vector", lambda e, bi=bi: e.tensor_copy(out=bguT[:].rearrange("p a b -> p (a b)"), in_=mm[bi][:]), reads=[f"mm{bi}"], writes=["bguT"])
            S.op("vector", lambda e: e.tensor_scalar(out=bgu7[:], in0=bguT[:, 8:16, :], scalar1=7.0, scalar2=None, op0=ALU.add),
                 reads=["bguT"], writes=["bgu7"])

            def load_expert(ex):
                bf = ex % 2
                S.dma("gpsimd", lambda e: e.dma_start(out=wgu[bf][:], in_=w_gu_d[ex].rearrange("(k p) n -> p k n", p=128)),
                      writes=[f"wgu{bf}"], key=f"wg{bf}")
                S.dma("gpsimd", lambda e: e.dma_start(out=wdn[bf][:], in_=w_dn_d[ex].rearrange("(k p) n -> p k n", p=128)),
                      writes=[f"wdn{bf}"], key=f"wd{bf}")
                S.dma("sync", lambda e: e.dma_start(out=bdn[bf][:], in_=bc(b_dn_d[ex], D)), writes=[f"bdn{bf}"], key=f"bd{bf}")
                S.dma("sync", lambda e: e.dma_start(out=xg[bf][:], in_=xg_d[ex * CAP:(ex + 1) * CAP, :].rearrange("(t p) d -> p t d", p=128)),
                      reads=["xg_d"], writes=[f"xg{bf}"], key=f"xgl{bf}")

            load_expert(0)
            for ex in range(NE_RUN):
                bf = ex % 2
                if ex + 1 < NE_RUN:
                    load_expert(ex + 1)
                for t in range(NTE):
                    for kh in range(2):
                        ti = tpr.next()
                        def tr(e, bf=bf, t=t, kh=kh, ti=ti):
                            ins = None
                            for kk in range(4):
                                k = kh * 4 + kk
                                ins = e.transpose(out=tp[ti][:, kk * 128:(kk + 1) * 128], in_=xg[bf][:, t, k * 128:(k + 1) * 128], identity=ident_b[:])
                            return ins
                        S.op("tensor", tr, reads=[f"xg{bf}", "ident_b"], writes=[f"tp{ti}"])
                        S.op("scalar", lambda e, kh=kh, ti=ti, t=t: e.activation(
                            out=xgT[:, kh * 4:(kh + 1) * 4, t * 128:(t + 1) * 128], in_=tp[ti][:, 0:512].rearrange("p (k n) -> p k n", k=4), func=AF.Copy),
                            reads=[f"tp{ti}", "xgT"], writes=["xgT"])
                it = 0
                for j in range(8 if P2_STEPS >= 2 else 0):
                    for (b0, bw) in BLKS:
                        big = mmr.next(); biu = mmr.next()
                        def fg(e, bf=bf, j=j, b0=b0, bw=bw, big=big):
                            ins = None
                            for k in range(8):
                                ins = e.matmul(mm[big][:, 0:bw], lhsT=wgu[bf][:, k, j * 128:(j + 1) * 128], rhs=xgT[:, k, b0:b0 + bw],
                                               start=(k == 0), stop=(k == 7))
                            return ins
                        def fu(e, bf=bf, j=j, b0=b0, bw=bw, biu=biu):
                            ins = None
                            for k in range(8):
                                ins = e.matmul(mm[biu][:, 0:bw], lhsT=wgu[bf][:, k, D + j * 128:D + (j + 1) * 128], rhs=xgT[:, k, b0:b0 + bw],
                                               start=(k == 0), stop=(k == 7))
                            return ins
                        S.op("tensor", fg, reads=[f"wgu{bf}", "xgT"], writes=[f"mm{big}"])
                        S.op("tensor", fu, reads=[f"wgu{bf}", "xgT"], writes=[f"mm{biu}"])
                        i2 = it % 2; it += 1
                        S.op("vector", lambda e, big=big, bw=bw, j=j, ex=ex, i2=i2: e.tensor_scalar(
                            out=gcl[i2][:, 0:bw], in0=mm[big][:, 0:bw], scalar1=bguT[:, j, ex:ex + 1], scalar2=7.0, op0=ALU.add, op1=ALU.min),
                            reads=[f"mm{big}", "bguT"], writes=[f"gc{i2}"])
                        S.op("scalar", lambda e, bw=bw, i2=i2: e.activation(out=s0[i2][:, 0:bw], in_=gcl[i2][:, 0:bw], func=AF.Silu, scale=1.702),
                             reads=[f"gc{i2}"], writes=[f"s0_{i2}"])
                        S.op("scalar", lambda e, biu=biu, bw=bw, j=j, ex=ex, i2=i2: e.activation(
                            out=ucl[i2][:, 0:bw], in_=mm[biu][:, 0:bw], func=AF.Relu, bias=bgu7[:, j, ex:ex + 1]),
                            reads=[f"mm{biu}", "bgu7"], writes=[f"uc{i2}"])
                        S.op("vector", lambda e, bw=bw, i2=i2: e.tensor_scalar(
                            out=ucl[i2][:, 0:bw], in0=ucl[i2][:, 0:bw], scalar1=14.0, scalar2=-6.0, op0=ALU.min, op1=ALU.add),
                            reads=[f"uc{i2}"], writes=[f"uc{i2}"])
                        S.op("vector", lambda e, bw=bw, b0=b0, j=j, i2=i2: e.scalar_tensor_tensor(
                            out=actT[:, j, b0:b0 + bw], in0=s0[i2][:, 0:bw], scalar=1.0 / 1.702, in1=ucl[i2][:, 0:bw], op0=ALU.mult, op1=ALU.mult),
                            reads=[f"s0_{i2}", f"uc{i2}", "actT"], writes=["actT"])
                for t in range(NTE if P2_STEPS >= 4 else 0):
                    yb = t % 2
                    for hf in range(2):
                        bi = mmr.next()
                        def fd(e, bf=bf, t=t, hf=hf, bi=bi):
                            ins = None
                            for j in range(8):
                                ins = e.matmul(mm[bi][:], lhsT=actT[:, j, t * 128:(t + 1) * 128], rhs=wdn[bf][:, j, hf * 512:(hf + 1) * 512],
                                               start=(j == 0), stop=(j == 7))
                            return ins
                        S.op("tensor", fd, reads=["actT", f"wdn{bf}"], writes=[f"mm{bi}"])
                        S.op("vector", lambda e, bf=bf, yb=yb, hf=hf, bi=bi: e.tensor_tensor(
                            out=ysb[yb][:, hf * 512:(hf + 1) * 512], in0=mm[bi][:], in1=bdn[bf][:, hf * 512:(hf + 1) * 512], op=ALU.add),
                            reads=[f"mm{bi}", f"bdn{bf}", f"ysb{yb}"], writes=[f"ysb{yb}"])
                    r0 = ex * CAP + t * 128
                    S.dma("sync", lambda e, yb=yb, r0=r0: e.dma_start(out=ys_d[r0:r0 + 128, :], in_=ysb[yb][:]),
                          reads=[f"ysb{yb}"], writes=["ys_d"], key=f"yst{yb}")
            S.full_barrier()
            S.flush(nc)
        if DEBUG == "2":
            return nc

        with ExitStack() as p4:
            g3_bc = sb(p4, "g3_bc", [128, D], F32)
            b3_bc = sb(p4, "b3_bc", [128, D], F32)
            h2t = [sb(p4, f"h2t{i}", [128, D], F32) for i in range(2)]
            yk = [[sb(p4, f"yk{i}_{k}", [128, D], F32) for k in range(4)] for i in range(2)]
            acc = sb(p4, "acc", [128, D], F32)
            tmpn3 = sb(p4, "tmpn3", [128, D], F32)
            outt = [sb(p4, f"outt{i}", [128, D], F32) for i in range(2)]
            bst3 = sb(p4, "bst3", [128, 2, 6], F32)
            bmv3 = sb(p4, "bmv3", [128, 2], F32)
            brs3 = sb(p4, "brs3", [128, 1], F32)
            S.dma("sync", lambda e: e.dma_start(out=g3_bc[:], in_=bc(ln3g_d, D)), writes=["g3_bc"], key="c1")
            S.dma("sync", lambda e: e.dma_start(out=b3_bc[:], in_=bc(ln3b_d, D)), writes=["b3_bc"], key="c2")
            for tg in range(NT):
                b = tg % 2
                S.dma("sync", lambda e, tg=tg, b=b: e.dma_start(out=h2t[b][:], in_=h2_d[tg * 128:(tg + 1) * 128, :]),
                      writes=[f"h2t{b}"], key=f"h2l{b}")
                for k in range(4):
                    S.dma("gpsimd", lambda e, tg=tg, k=k, b=b: e.indirect_dma_start(
                        out=yk[b][k][:], out_offset=None, in_=ys_d, in_offset=bass.IndirectOffsetOnAxis(ap=slots_all[:, tg, k:k + 1], axis=0)),
                        reads=["ys_d", "slots_all"], writes=[f"yk{b}_{k}"], key=f"gk{b}{k}")
                S.op("vector", lambda e, b=b: e.tensor_scalar(out=acc[:], in0=h2t[b][:], scalar1=ALPHA, scalar2=None, op0=ALU.mult),
                     reads=[f"h2t{b}", "acc"], writes=["acc"])
                for k in range(4):
                    S.op("vector", lambda e, b=b, k=k, tg=tg: e.scalar_tensor_tensor(
                        out=acc[:], in0=yk[b][k][:], scalar=gates_all[:, tg, k:k + 1], in1=acc[:], op0=ALU.mult, op1=ALU.add),
                        reads=[f"yk{b}_{k}", "gates_all", "acc"], writes=["acc"])
                layer_norm_tile(acc[:], outt[b][:], g3_bc, b3_bc, bst3, bmv3, brs3, tmpn3[:], ("acc", f"outt{b}", "tmpn3"))
                S.dma("scalar", lambda e, tg=tg, b=b: e.dma_start(out=out_d[tg * 128:(tg + 1) * 128, :], in_=outt[b][:]),
                      reads=[f"outt{b}"], key=f"os{b}")
            S.full_barrier()
            S.flush(nc)
    return nc


_PROG = None


def kernel(**inputs):
    global _PROG
    if _PROG is None:
        _PROG = build_program()
    nc = _PROG
    B = inputs["x"].shape[0]
    in_maps = []
    for b in range(B):
        m = {}
        for k, v in inputs.items():
            a = np.asarray(v)
            if k in ("x", "mem"):
                m[k] = np.ascontiguousarray(a[b])
            else:
                m[k] = np.ascontiguousarray(a[0])
        in_maps.append(m)
    res = run_bass_kernel_spmd(nc, in_maps, core_ids=list(range(B)))
    return np.stack([np.asarray(r["out"]) for r in res.results], axis=0)
```

```python
import numpy as np
from contextlib import ExitStack
import concourse.bass as bass
import concourse.mybir as mybir
from concourse.bass_utils import run_bass_kernel_spmd

F32 = mybir.dt.float32
BF16 = mybir.dt.bfloat16
I32 = mybir.dt.int32
AF = mybir.ActivationFunctionType
ALU = mybir.AluOpType

T = 4096
NT = 32
D = 1024
CH = 512
NCH = 8
INW = 1736
CAP = 768
NE = 32
ALPHA = 2.0 ** 0.25
NEG = -1.0e30
NEG2 = -3.0e30
KBIS = 25
SIGMAX = float(1.0 / (1.0 + np.exp(-1.702 * 7.0)))

DEBUG = None
NE_RUN = NE
P2_STEPS = 9
P2_EW = 63


class Sched:
    EPOCH = 30000
    ENG = ("sync", "scalar", "vector", "gpsimd", "tensor")

    def __init__(self, sem_pool):
        self.ops = {e: [] for e in self.ENG}
        self.cnt = {}
        self.lastw = {}
        self.readers = {}
        self.seen = {e: {} for e in self.ENG}
        self.sem_pool = list(sem_pool)
        self.sems = {}

    def _sem(self, counter, val):
        if counter.startswith("E:"):
            ep = (val - 1) // self.EPOCH
            name, lv = f"{counter}#{ep}", val - ep * self.EPOCH
        else:
            name, lv = counter, val
        if name not in self.sems:
            self.sems[name] = self.sem_pool.pop()
        return self.sems[name], lv

    def _waits(self, eng, reads, writes, extra=()):
        need = {}
        def add(cv):
            c, v = cv
            if v > need.get(c, 0):
                need[c] = v
        for r in reads:
            if r in self.lastw:
                add(self.lastw[r])
        for w in writes:
            if w in self.lastw:
                add(self.lastw[w])
            for cv in self.readers.get(w, {}).items():
                add(cv)
        for cv in extra:
            add(cv)
        waits = []
        for c, v in need.items():
            if eng == "tensor" and c == "E:tensor":
                continue
            if self.seen[eng].get(c, 0) < v:
                self.seen[eng][c] = v
                waits.append(self._sem(c, v))
        return waits

    def _book(self, c, v, reads, writes):
        for r in reads:
            d = self.readers.setdefault(r, {})
            if d.get(c, 0) < v:
                d[c] = v
        for w in writes:
            self.lastw[w] = (c, v)
            self.readers[w] = {}

    def op(self, eng, fn, reads=(), writes=()):
        waits = self._waits(eng, reads, writes)
        c = "E:" + eng
        v = self.cnt.get(c, 0) + 1
        self.cnt[c] = v
        sem, _ = self._sem(c, v)
        self.ops[eng].append((waits, fn, sem, 1))
        self._book(c, v, reads, writes)

    def dma(self, eng, fn, reads=(), writes=(), key="d", serialize=True):
        c = "D:" + key
        prev = self.cnt.get(c, 0)
        waits = self._waits(eng, reads, writes, extra=[(c, prev)] if (prev and serialize) else [])
        v = prev + 16
        self.cnt[c] = v
        sem, _ = self._sem(c, v)
        self.ops[eng].append((waits, fn, sem, 16))
        self._book(c, v, reads, writes)

    def barrier_all(self, eng="sync"):
        waits = []
        for c, v in self.cnt.items():
            if v and self.seen[eng].get(c, 0) < v:
                self.seen[eng][c] = v
                waits.append(self._sem(c, v))
        self.ops[eng].append((waits, None, None, 0))

    def full_barrier(self):
        for e in self.ENG:
            self.barrier_all(e)

    def flush(self, nc):
        with nc.Block() as blk:
            for eng in self.ENG:
                lst = self.ops[eng]
                if not lst:
                    continue
                def body(e, lst=lst):
                    for waits, fn, sem, inc in lst:
                        for s, v in waits:
                            e.wait_ge(s, v)
                        if fn is not None:
                            ins = fn(e)
                            ins.then_inc(sem, inc)
                getattr(blk, eng)(body)
        self.ops = {e: [] for e in self.ENG}


class Rot:
    def __init__(self, items):
        self.items = items
        self.i = 0
    def next(self):
        it = self.items[self.i % len(self.items)]
        self.i += 1
        return it


def build_program():
    nc = bass.Bass("TRN2", target_bir_lowering=False)
    dt = lambda name, shape, dtype=F32, kind="ExternalInput": nc.dram_tensor(name, shape, dtype, kind=kind).ap()
    x_d = dt("x", [T, D])
    mem_d = dt("mem", [256, D])
    w_in_d = dt("w_in", [D, INW])
    w_pool_d = dt("w_pool", [4, 128, 128])
    pool_scale_d = dt("pool_scale", [512])
    kig_d = dt("idx_k_norm_g", [64])
    kib_d = dt("idx_k_norm_b", [64])
    kvg_d = dt("kv_norm_g", [128])
    w_uk_d = dt("w_uk", [8, 64, 128])
    w_uv_d = dt("w_uv", [8, 128, 64])
    w_o_d = dt("w_o", [D, D])
    ln1g_d = dt("ln1_g", [D]); ln1b_d = dt("ln1_b", [D])
    w_mq_d = dt("w_mq", [D, D])
    w_mkv_d = dt("w_mkv", [D, 2 * D])
    w_mo_d = dt("w_mo", [D, D])
    ln2g_d = dt("ln2_g", [D]); ln2b_d = dt("ln2_b", [D])
    w_r_d = dt("w_router", [D, NE])
    b_r_d = dt("b_router", [NE])
    w_gu_d = dt("w_gate_up", [NE, D, 2 * D])
    b_gu_d = dt("b_gate_up", [NE, 2 * D])
    w_dn_d = dt("w_down", [NE, D, D])
    b_dn_d = dt("b_down", [NE, D])
    ln3g_d = dt("ln3_g", [D]); ln3b_d = dt("ln3_b", [D])
    out_d = dt("out", [T, D], F32, "ExternalOutput")
    h1_d = dt("h1_scr", [T, D], F32, "Internal")
    h2_d = dt("h2_scr", [T, D], F32, "Internal")
    xg_d = dt("xg_scr", [NE * CAP, D], BF16, "Internal")
    ys_d = dt("ys_scr", [NE * CAP, D], F32, "Internal")
    dbg = {}
    if DEBUG:
        dbg["h"] = dt("dbg_h", [T, D], F32, "ExternalOutput")
        dbg["mixT"] = dt("dbg_mixT", [NCH, 128, 8 * CH], BF16, "ExternalOutput")

    dumps = []
    def dump(S, name, ap2d, shape, dtype, reads):
        if not DEBUG:
            return
        d = dt("dbg_" + name, shape, dtype, "ExternalOutput")
        S.dma("sync", lambda e: e.dma_start(out=d, in_=ap2d), reads=reads, key="dbgd")

    def bc(ap1d, n):
        return ap1d.rearrange("(o n) -> o n", o=1).to_broadcast([128, n])

    with ExitStack() as top:
        sem_pool = [top.enter_context(nc.semaphore(f"s{i}")) for i in range(96)]
        S = Sched(sem_pool)
        sb = lambda es, name, shape, dtype=F32: es.enter_context(nc.sbuf_tensor(name, shape, dtype))
        ps = lambda es, name, shape, dtype=F32: es.enter_context(nc.psum_tensor(name, shape, dtype))

        ident_b = sb(top, "ident_b", [128, 128], BF16)
        ident_f = sb(top, "ident_f", [128, 128], F32)
        ones_b = sb(top, "ones_b", [128, 128], BF16)
        slots_all = sb(top, "slots_all", [128, NT, 4], I32)
        gates_all = sb(top, "gates_all", [128, NT, 4], F32)

        S.op("gpsimd", lambda e: e.memset(ident_f[:], 0.0), writes=["ident_f"])
        S.op("gpsimd", lambda e: e.affine_select(out=ident_f[:], in_=ident_f[:], pattern=[[-1, 128]],
                                                   compare_op=ALU.not_equal, fill=1.0, base=0, channel_multiplier=1),
             reads=["ident_f"], writes=["ident_f"])
        S.op("vector", lambda e: e.tensor_copy(out=ident_b[:], in_=ident_f[:]), reads=["ident_f"], writes=["ident_b"])
        S.op("vector", lambda e: e.memset(ones_b[:], 1.0), writes=["ones_b"])

        def ln_rstd(var_ap, rstd_ap, tag, scale, rname, wname):
            S.op("scalar", lambda e: e.activation(out=rstd_ap, in_=var_ap, func=AF.Ln, bias=eps_tile[:, tag:tag + 1], scale=scale),
                 reads=[rname, "eps", wname], writes=[wname])
            S.op("scalar", lambda e: e.activation(out=rstd_ap, in_=rstd_ap, func=AF.Exp, scale=-0.5),
                 reads=[wname], writes=[wname])

        eps_tile = sb(top, "eps", [128, 2], F32)
        S.op("vector", lambda e: e.memset(eps_tile[:, 0:1], 1e-5), writes=["eps"])
        S.op("vector", lambda e: e.memset(eps_tile[:, 1:2], 1e-6), reads=["eps"], writes=["eps"])

        def layer_norm_tile(r_ap, out_ap, g_bc, b_bc, stats, mv, rstd, tmp_ap, names):
            rn, on, tn = names
            for hf in range(2):
                S.op("vector", lambda e, hf=hf: e.bn_stats(out=stats[:, hf, :], in_=r_ap[:, hf * 512:(hf + 1) * 512]),
                     reads=[rn], writes=[stats.name])
            S.op("vector", lambda e: e.bn_aggr(out=mv[:], in_=stats[:].rearrange("p a b -> p (a b)")),
                 reads=[stats.name], writes=[mv.name])
            ln_rstd(mv[:, 1:2], rstd[:, 0:1], 0, 1.0, mv.name, rstd.name)
            S.op("vector", lambda e: e.tensor_scalar(out=tmp_ap, in0=r_ap, scalar1=mv[:, 0:1], scalar2=rstd[:, 0:1],
                                                      op0=ALU.subtract, op1=ALU.mult),
                 reads=[rn, mv.name, rstd.name], writes=[tn])
            S.op("vector", lambda e: e.tensor_tensor(out=tmp_ap, in0=tmp_ap, in1=g_bc[:], op=ALU.mult),
                 reads=[tn, g_bc.name], writes=[tn])
            S.op("vector", lambda e: e.tensor_tensor(out=out_ap, in0=tmp_ap, in1=b_bc[:], op=ALU.add),
                 reads=[tn, b_bc.name], writes=[on])

        with ExitStack() as p1:
            w_in_sb = sb(p1, "w_in_sb", [128, 8, INW], BF16)
            w_o_sb = sb(p1, "w_o_sb", [128, 8, D], BF16)
            wpool_sb = sb(p1, "wpool_sb", [128, 4, 128], BF16)
            wuk_sb = sb(p1, "wuk_sb", [128, 4, 128], BF16)
            wuv_sb = sb(p1, "wuv_sb", [128, 4, 2, 128], BF16)
            pscale_sb = sb(p1, "pscale_sb", [128, 4], F32)
            g1_bc = sb(p1, "g1_bc", [128, D], F32)
            b1_bc = sb(p1, "b1_bc", [128, D], F32)
            kvg_bc = sb(p1, "kvg_bc", [128, 128], F32)
            kig_bc = sb(p1, "kig_bc", [128, 64], F32)
            kib_bc = sb(p1, "kib_bc", [128, 64], F32)
            ic16 = sb(p1, "ic16", [128, 4, 16], F32)
            ckv1 = sb(p1, "ckv1", [128, NT, 130], BF16)
            ckvT = sb(p1, "ckvT", [128, T], BF16)
            kiT = sb(p1, "kiT", [128, T], BF16)
            widx = sb(p1, "widx", [128, NT, 8], F32)
            xt = [sb(p1, f"xt{i}", [128, D], F32) for i in range(2)]
            xb = [sb(p1, f"xb{i}", [128, D], BF16) for i in range(2)]
            xT = sb(p1, "xT", [128, 8, CH], BF16)
            ug = sb(p1, "ug", [128, 528], F32)
            halo = sb(p1, "halo", [128, 4, 16], F32)
            pA = sb(p1, "pA", [128, 528], F32)
            pB = sb(p1, "pB", [128, 528], F32)
            dT = sb(p1, "dT", [128, CH], BF16)
            qTs = [sb(p1, f"qT{i}", [128, 4, CH], BF16) for i in range(2)]
            qiT = sb(p1, "qiT", [128, 4, CH], BF16)
            mixTs = [sb(p1, f"mixT{i}", [128, 8, CH], BF16) for i in range(2)]
            SC = sb(p1, "SC", [128, T], F32)
            bmax = sb(p1, "bmax", [128, 1], F32)
            bmid = sb(p1, "bmid", [128, 1], F32)
            bcnt = sb(p1, "bcnt", [128, 1], F32)
            bd = sb(p1, "bd", [128, 1], F32)
            bnegl = sb(p1, "bnegl", [128, 1], F32)
            c256 = sb(p1, "c256", [128, 1], F32)
            pw2 = sb(p1, "pw2", [128, KBIS], F32)
            bsteps = sb(p1, "bsteps", [128, KBIS], F32)
            bnegh = sb(p1, "bnegh", [128, KBIS], F32)
            m128 = [sb(p1, f"m128_{i}", [128, 128], BF16) for i in range(2)]
            maskTs = [sb(p1, f"maskT{i}", [128, NT, CH], mybir.dt.uint8) for i in range(2)]
            rl = [sb(p1, f"rl{i}", [128, CH], F32) for i in range(2)]
            Eb = [sb(p1, f"Eb{i}", [128, CH], BF16) for i in range(2)]
            Pb = [sb(p1, f"Pb{i}", [128, CH], BF16) for i in range(2)]
            Eb.append(ug[:, 0:256].bitcast(BF16)); Pb.append(pB[:, 0:256].bitcast(BF16))
            qlat = [sb(p1, f"qlat{i}", [128, CH], BF16) for i in range(2)]
            olat = [sb(p1, f"olat{i}", [128, 128], BF16) for i in range(2)]
            rden = sb(p1, "rden", [128, 4], F32)
            ckn = sb(p1, "ckn", [128, 128], BF16)
            kn32 = sb(p1, "kn32", [128, 64], F32)
            kn2 = sb(p1, "kn2", [128, 128], BF16)
            tm = sb(p1, "tm", [128, 200], F32)
            olT2 = sb(p1, "olT2", [128, 2, CH], BF16)
            junk = sb(p1, "junk", [128, 128], F32)
            st1 = sb(p1, "st1", [128, 8], F32)
            bst = sb(p1, "bst", [128, 2, 6], F32)
            bmv = sb(p1, "bmv", [128, 2], F32)
            brs = sb(p1, "brs", [128, 1], F32)
            r1 = sb(p1, "r1", [128, D], F32)
            h1o = [r1, r1]
            mm = [ps(p1, f"mm{i}", [128, 512], F32) for i in range(2)]
            oacc = [ps(p1, f"oacc{i}", [128, 2, 512], F32) for i in range(2)]
            tp = [ps(p1, f"tp{i}", [128, 1024], BF16) for i in range(2)]
            mmr = Rot([0, 1]); tpr = Rot([0, 1])

            S.op("gpsimd", lambda e: e.memset(maskTs[1][:], 0), writes=["maskT1"])
            zsrc = maskTs[1][:].rearrange("p a b -> p (a b)").bitcast(BF16).rearrange("p (t d) -> p t d", d=D)
            for zi in range(NE * CAP // 1024):
                S.dma("scalar", lambda e, zi=zi: e.dma_start(out=xg_d[zi * 1024:(zi + 1) * 1024, :].rearrange("(t p) d -> p t d", p=128), in_=zsrc),
                      reads=["maskT1"], key="zf", serialize=False)
            S.lastw["xg_d"] = ("D:zf", S.cnt["D:zf"])
            S.dma("gpsimd", lambda e: e.dma_start(out=w_in_sb[:], in_=w_in_d.rearrange("(k p) n -> p k n", p=128)),
                  writes=["w_in_sb"], key="w0")
            S.dma("gpsimd", lambda e: e.dma_start(out=wpool_sb[:], in_=w_pool_d.rearrange("g c d -> c g d")),
                  writes=["wpool_sb"], key="w1")
            S.dma("gpsimd", lambda e: e.dma_start(out=wuk_sb[:], in_=w_uk_d.rearrange("(hp h2) d r -> (h2 d) hp r", h2=2)),
                  writes=["wuk_sb"], key="w2")
            S.op("vector", lambda e: e.memset(wuv_sb[:], 0.0), writes=["wuv_sb"])
            for h2 in range(2):
                S.dma("gpsimd", lambda e, h2=h2: e.dma_start(out=wuv_sb[:, :, h2, h2 * 64:(h2 + 1) * 64],
                                                             in_=w_uv_d.rearrange("(hp h2) r d -> h2 r hp d", h2=2)[h2]),
                      reads=["wuv_sb"], writes=["wuv_sb"], key=f"w3{h2}")
            S.dma("gpsimd", lambda e: e.dma_start(out=w_o_sb[:], in_=w_o_d.rearrange("(k p) n -> p k n", p=128)),
                  writes=["w_o_sb"], key="w4")
            S.dma("sync", lambda e: e.dma_start(out=pscale_sb[:], in_=pool_scale_d.rearrange("(g d) -> d g", d=128),
                                                allow_slow_non_contiguous=True),
                  writes=["pscale_sb"], key="c0")
            S.dma("sync", lambda e: e.dma_start(out=g1_bc[:], in_=bc(ln1g_d, D)), writes=["g1_bc"], key="c1")
            S.dma("sync", lambda e: e.dma_start(out=b1_bc[:], in_=bc(ln1b_d, D)), writes=["b1_bc"], key="c2")
            S.dma("sync", lambda e: e.dma_start(out=kvg_bc[:], in_=bc(kvg_d, 128)), writes=["kvg_bc"], key="c3")
            S.dma("sync", lambda e: e.dma_start(out=kig_bc[:], in_=bc(kig_d, 64)), writes=["kig_bc"], key="c4")
            S.dma("sync", lambda e: e.dma_start(out=kib_bc[:], in_=bc(kib_d, 64)), writes=["kib_bc"], key="c5")
            for g in range(4):
                w = 2 ** (g + 1)
                S.op("gpsimd", lambda e, g=g, w=w: e.memset(ic16[:, g, :], 1.0 / w), reads=["ic16"], writes=["ic16"])
                for t in range(w - 1):
                    S.op("gpsimd", lambda e, g=g, t=t: e.memset(ic16[:, g, t:t + 1], 1.0 / (t + 1)), reads=["ic16"], writes=["ic16"])
            S.op("gpsimd", lambda e: e.memset(halo[:], 0.0), writes=["halo"])
            S.op("gpsimd", lambda e: e.memset(c256[:], 256.0), writes=["c256"])
            for i_ in range(KBIS):
                S.op("gpsimd", lambda e, i_=i_: e.memset(pw2[:, i_:i_ + 1], 2.0 ** (-i_)), reads=["pw2"], writes=["pw2"])
            S.op("gpsimd", lambda e: e.memset(ckv1[:, :, 128:130], 1.0), writes=["ckv1"])

            def front(c):
                for t in range(4):
                    tg = 4 * c + t
                    b = tg % 2
                    S.dma("sync", lambda e, tg=tg, b=b: e.dma_start(out=xt[b][:], in_=x_d[tg * 128:(tg + 1) * 128, :]),
                          writes=[f"xt{b}"], key=f"x{b}")
                    S.op("scalar", lambda e, b=b: e.activation(out=xb[b][:], in_=xt[b][:], func=AF.Copy),
                         reads=[f"xt{b}"], writes=[f"xb{b}"])
                    for kh in range(2):
                        ti = tpr.next()
                        def tr(e, b=b, kh=kh, ti=ti):
                            ins = None
                            for kk in range(4):
                                k = kh * 4 + kk
                                ins = e.transpose(out=tp[ti][:, kk * 128:(kk + 1) * 128], in_=xb[b][:, k * 128:(k + 1) * 128],
                                                  identity=ident_b[:])
                            return ins
                        S.op("tensor", tr, reads=[f"xb{b}", "ident_b"], writes=[f"tp{ti}"])
                        S.op("vector", lambda e, kh=kh, ti=ti, t=t: e.tensor_copy(
                            out=xT[:, kh * 4:(kh + 1) * 4, t * 128:(t + 1) * 128],
                            in_=tp[ti][:, 0:512].rearrange("p (k n) -> p k n", k=4)),
                            reads=[f"tp{ti}"], writes=["xT"])

                def inproj_fm(col0):
                    bi = mmr.next()
                    def f(e, col0=col0, bi=bi):
                        ins = None
                        for k in range(8):
                            ins = e.matmul(mm[bi][:], lhsT=w_in_sb[:, k, col0:col0 + 128], rhs=xT[:, k, :],
                                           start=(k == 0), stop=(k == 7))
                        return ins
                    S.op("tensor", f, reads=["w_in_sb", "xT"], writes=[f"mm{bi}"])
                    return bi

                for g in range(4):
                    bi = inproj_fm(g * 128)
                    S.op("gpsimd", lambda e, g=g: e.tensor_copy(out=ug[:, 0:16], in_=halo[:, g, :]),
                         reads=["halo", "ug"], writes=["ug"])
                    S.op("scalar", lambda e, bi=bi: e.activation(out=ug[:, 16:528], in_=mm[bi][:], func=AF.Copy),
                         reads=[f"mm{bi}", "ug"], writes=["ug"])
                    S.op("gpsimd", lambda e, g=g: e.tensor_copy(out=halo[:, g, :], in_=ug[:, 512:528]),
                         reads=["ug"], writes=["halo"])
                    src, srcn = ug, "ug"
                    bufs = [(pA, "pA"), (pB, "pB")]
                    for lv in range(g + 1):
                        s = 2 ** lv
                        lo = 2 ** (lv + 1) - 1
                        dst, dstn = bufs[lv % 2]
                        S.op("gpsimd", lambda e, src=src, dst=dst, s=s, lo=lo: e.tensor_tensor(
                            out=dst[:, lo:528], in0=src[:, lo:528], in1=src[:, lo - s:528 - s], op=ALU.add),
                            reads=[srcn, dstn], writes=[dstn])
                        src, srcn = dst, dstn
                    w = 2 ** (g + 1)
                    S.op("vector", lambda e, src=src, w=w: e.scalar_tensor_tensor(
                        out=dT[:], in0=src[:, 16:528], scalar=1.0 / w, in1=ug[:, 16:528], op0=ALU.mult, op1=ALU.subtract),
                        reads=[srcn, "ug"], writes=["dT"])
                    if c == 0:
                        S.op("vector", lambda e, src=src, g=g: e.tensor_tensor(out=junk[:, 0:16], in0=src[:, 16:32], in1=ic16[:, g, :], op=ALU.mult),
                             reads=[srcn, "ic16"], writes=["junk"])
                        S.op("vector", lambda e: e.tensor_tensor(out=dT[:, 0:16], in0=junk[:, 0:16], in1=ug[:, 16:32], op=ALU.subtract),
                             reads=["junk", "ug", "dT"], writes=["dT"])
                    bi2 = mmr.next()
                    S.op("tensor", lambda e, g=g, bi2=bi2: e.matmul(mm[bi2][:], lhsT=wpool_sb[:, g, :], rhs=dT[:], start=True, stop=True),
                         reads=["wpool_sb", "dT"], writes=[f"mm{bi2}"])
                    S.op("scalar", lambda e, g=g, bi2=bi2: e.activation(out=mixTs[c % 2][:, g, :], in_=mm[bi2][:], func=AF.Copy, scale=pscale_sb[:, g:g + 1]),
                         reads=[f"mm{bi2}", "pscale_sb", f"mixT{c % 2}"], writes=[f"mixT{c % 2}"])
                for j in range(4):
                    bi = inproj_fm(512 + j * 128)
                    S.op("scalar", lambda e, j=j, bi=bi: e.activation(out=qTs[c % 2][:, j, :], in_=mm[bi][:], func=AF.Copy),
                         reads=[f"mm{bi}"], writes=[f"qT{c % 2}"])
                for j in range(4):
                    bi = inproj_fm(1152 + j * 128)
                    S.op("vector", lambda e, j=j, bi=bi: e.tensor_copy(out=qiT[:, j, :], in_=mm[bi][:]),
                         reads=[f"mm{bi}"], writes=["qiT"])
                tiA = tpr.next(); tiB = tpr.next()
                for t in range(4):
                    tg = 4 * c + t
                    bi = mmr.next()
                    def f(e, t=t, bi=bi):
                        ins = None
                        for k in range(8):
                            ins = e.matmul(mm[bi][:, 0:128], lhsT=xT[:, k, t * 128:(t + 1) * 128], rhs=w_in_sb[:, k, 1024:1152],
                                           start=(k == 0), stop=(k == 7))
                        for k in range(8):
                            ins = e.matmul(mm[bi][:, 128:200], lhsT=xT[:, k, t * 128:(t + 1) * 128], rhs=w_in_sb[:, k, 1664:1736],
                                           start=(k == 0), stop=(k == 7))
                        return ins
                    S.op("tensor", f, reads=["w_in_sb", "xT"], writes=[f"mm{bi}"])
                    S.op("scalar", lambda e, bi=bi: e.activation(out=tm[:], in_=mm[bi][:, 0:200], func=AF.Copy),
                         reads=[f"mm{bi}"], writes=["tm"])
                    S.op("scalar", lambda e: e.activation(out=junk[:], in_=tm[:, 0:128], func=AF.Square, accum_out=st1[:, 0:1]),
                         reads=["tm", "st1"], writes=["junk", "st1"])
                    ln_rstd(st1[:, 0:1], st1[:, 1:2], 1, 1.0 / 128, "st1", "st1")
                    S.op("vector", lambda e: e.scalar_tensor_tensor(out=ckn[:], in0=tm[:, 0:128], scalar=st1[:, 1:2], in1=kvg_bc[:],
                                                                     op0=ALU.mult, op1=ALU.mult),
                         reads=["tm", "st1", "kvg_bc"], writes=["ckn"])
                    S.op("gpsimd", lambda e, tg=tg: e.tensor_copy(out=ckv1[:, tg, 0:128], in_=ckn[:]), reads=["ckn", "ckv1"], writes=["ckv1"])
                    S.op("vector", lambda e: e.bn_stats(out=bst[:, 0, :], in_=tm[:, 128:192]), reads=["tm"], writes=["bst"])
                    S.op("vector", lambda e: e.bn_aggr(out=bmv[:], in_=bst[:, 0, :]), reads=["bst"], writes=["bmv"])
                    ln_rstd(bmv[:, 1:2], brs[:, 0:1], 0, 1.0, "bmv", "brs")
                    S.op("vector", lambda e: e.tensor_scalar(out=kn32[:], in0=tm[:, 128:192], scalar1=bmv[:, 0:1], scalar2=brs[:, 0:1],
                                                              op0=ALU.subtract, op1=ALU.mult),
                         reads=["tm", "bmv", "brs"], writes=["kn32"])
                    S.op("vector", lambda e, tg=tg: e.tensor_copy(out=widx[:, tg, :], in_=tm[:, 192:200]),
                         reads=["tm", "widx"], writes=["widx"])
                    S.op("gpsimd", lambda e: e.tensor_tensor(out=kn32[:], in0=kn32[:], in1=kig_bc[:], op=ALU.mult),
                         reads=["kn32", "kig_bc"], writes=["kn32"])
                    S.op("gpsimd", lambda e: e.tensor_tensor(out=kn2[:, 0:64], in0=kn32[:], in1=kib_bc[:], op=ALU.add),
                         reads=["kn32", "kib_bc", "kn2"], writes=["kn2"])
                    S.op("gpsimd", lambda e: e.tensor_copy(out=kn2[:, 64:128], in_=kn2[:, 0:64]), reads=["kn2"], writes=["kn2"])
                    S.op("tensor", lambda e, t=t, tiA=tiA: e.transpose(out=tp[tiA][:, t * 128:(t + 1) * 128], in_=ckn[:], identity=ident_b[:]),
                         reads=["ckn", "ident_b"], writes=[f"tp{tiA}"])
                    S.op("tensor", lambda e, t=t, tiB=tiB: e.transpose(out=tp[tiB][:, t * 128:(t + 1) * 128], in_=kn2[:], identity=ident_b[:]),
                         reads=["kn2", "ident_b"], writes=[f"tp{tiB}"])
                S.op("scalar", lambda e, c=c, tiA=tiA: e.activation(out=ckvT[:, c * CH:(c + 1) * CH], in_=tp[tiA][:, 0:512], func=AF.Copy),
                     reads=[f"tp{tiA}", "ckvT"], writes=["ckvT"])
                S.op("scalar", lambda e, c=c, tiB=tiB: e.activation(out=kiT[:, c * CH:(c + 1) * CH], in_=tp[tiB][:, 0:512], func=AF.Copy),
                     reads=[f"tp{tiB}", "kiT"], writes=["kiT"])


            def tile_sel(c, t):
                qt = 4 * c + t
                qt = 4 * c + t
                N = 128 * (qt + 1)
                nkc = (N + 511) // 512
                for kc in range(nkc):
                    k0 = kc * 512
                    kw = min(512, N - k0)
                    for h in range(8):
                        hp, h2 = h // 2, h % 2
                        bi = mmr.next()
                        S.op("tensor", lambda e, hp=hp, h2=h2, t=t, bi=bi, k0=k0, kw=kw: e.matmul(
                            mm[bi][:, 0:kw], lhsT=qiT[h2 * 64:(h2 + 1) * 64, hp, t * 128:(t + 1) * 128],
                            rhs=kiT[h2 * 64:(h2 + 1) * 64, k0:k0 + kw], start=True, stop=True),
                            reads=["qiT", "kiT"], writes=[f"mm{bi}"])
                        ri = h % 2
                        S.op("scalar", lambda e, bi=bi, ri=ri, kw=kw: e.activation(out=rl[ri][:, 0:kw], in_=mm[bi][:, 0:kw], func=AF.Relu),
                             reads=[f"mm{bi}"], writes=[f"rl{ri}"])
                        if h == 0:
                            S.op("vector", lambda e, ri=ri, k0=k0, kw=kw, qt=qt: e.tensor_scalar(
                                out=SC[:, k0:k0 + kw], in0=rl[ri][:, 0:kw], scalar1=widx[:, qt, 0:1], scalar2=None, op0=ALU.mult),
                                reads=[f"rl{ri}", "widx", "SC"], writes=["SC"])
                        else:
                            S.op("vector", lambda e, ri=ri, k0=k0, kw=kw, qt=qt, h=h: e.scalar_tensor_tensor(
                                out=SC[:, k0:k0 + kw], in0=rl[ri][:, 0:kw], scalar=widx[:, qt, h:h + 1], in1=SC[:, k0:k0 + kw],
                                op0=ALU.mult, op1=ALU.add),
                                reads=[f"rl{ri}", "widx", "SC"], writes=["SC"])
                if N > 256:
                    S.op("vector", lambda e, N=N: e.tensor_reduce(out=bmax[:], in_=SC[:, 0:N], axis=mybir.AxisListType.X, op=ALU.max,
                                                                   apply_absolute_value=True), reads=["SC"], writes=["bmax"])
                    S.op("vector", lambda e: e.tensor_scalar(out=bmax[:], in0=bmax[:], scalar1=1.0001, scalar2=1e-20, op0=ALU.mult, op1=ALU.add),
                         reads=["bmax"], writes=["bmax"])
                S.op("gpsimd", lambda e, qt=qt: e.affine_select(out=SC[:, qt * 128:(qt + 1) * 128], in_=SC[:, qt * 128:(qt + 1) * 128],
                                                                 pattern=[[-1, 128]], compare_op=ALU.is_ge, fill=NEG, base=0, channel_multiplier=1),
                     reads=["SC"], writes=["SC"])
                if N > 256:
                    S.op("vector", lambda e, N=N: e.tensor_scalar(out=bmid[:], in0=bmax[:], scalar1=0.0, scalar2=None, op0=ALU.mult),
                         reads=["bmax"], writes=["bmid"])
                    S.op("vector", lambda e: e.tensor_scalar(out=bsteps[:], in0=pw2[:], scalar1=bmax[:, 0:1], scalar2=None, op0=ALU.mult),
                         reads=["pw2", "bmax"], writes=["bsteps"])
                    S.op("vector", lambda e: e.tensor_scalar(out=bnegh[:], in0=bsteps[:], scalar1=-0.5, scalar2=None, op0=ALU.mult),
                         reads=["bsteps"], writes=["bnegh"])
                    for it_ in range(KBIS):
                        S.op("vector", lambda e, N=N: e.tensor_scalar(out=xT[:].rearrange("p k n -> p (k n)").bitcast(mybir.dt.uint8)[:, 0:N], in0=SC[:, 0:N], scalar1=bmid[:, 0:1], scalar2=0.0,
                                                                       op0=ALU.is_ge, op1=ALU.add, accum_out=bcnt[:, 0:1]),
                             reads=["SC", "bmid"], writes=["xT", "bcnt"])
                        S.op("vector", lambda e, it_=it_: e.tensor_scalar(out=bd[:], in0=bcnt[:], scalar1=c256[:, 0:1], scalar2=bsteps[:, it_:it_ + 1],
                                                                          op0=ALU.is_ge, op1=ALU.mult),
                             reads=["bcnt", "c256", "bsteps"], writes=["bd"])
                        sc2 = bnegh[:, it_:it_ + 1] if it_ < KBIS - 1 else bnegl[:, 0:1]
                        if it_ == KBIS - 1:
                            S.op("vector", lambda e, it_=it_: e.tensor_scalar(out=bnegl[:], in0=bsteps[:, it_:it_ + 1], scalar1=-1.0, scalar2=None, op0=ALU.mult),
                                 reads=["bsteps"], writes=["bnegl"])
                        S.op("vector", lambda e, sc2=sc2: e.tensor_scalar(out=bmid[:], in0=bmid[:], scalar1=bd[:, 0:1], scalar2=sc2,
                                                                          op0=ALU.add, op1=ALU.add),
                             reads=["bmid", "bd", "bnegh", "bnegl"], writes=["bmid"])

            def tile_mask(c, t):
                qt = 4 * c + t
                N = 128 * (qt + 1)
                for kt in range(qt + 1):
                    mi = kt % 2
                    if N > 256:
                        S.op("vector", lambda e, kt=kt, mi=mi: e.tensor_scalar(out=m128[mi][:], in0=SC[:, kt * 128:(kt + 1) * 128],
                                                                               scalar1=bmid[:, 0:1], scalar2=None, op0=ALU.is_ge),
                             reads=["SC", "bmid"], writes=[f"m128_{mi}"])
                    else:
                        S.op("vector", lambda e, kt=kt, mi=mi: e.tensor_scalar(out=m128[mi][:], in0=SC[:, kt * 128:(kt + 1) * 128],
                                                                               scalar1=-0.5e30, scalar2=None, op0=ALU.is_ge),
                             reads=["SC"], writes=[f"m128_{mi}"])
                    ti = tpr.next()
                    S.op("tensor", lambda e, mi=mi, ti=ti: e.transpose(out=tp[ti][:, 0:128], in_=m128[mi][:], identity=ident_b[:]),
                         reads=[f"m128_{mi}", "ident_b"], writes=[f"tp{ti}"])
                    S.op("scalar", lambda e, kt=kt, t=t, ti=ti: e.activation(out=maskTs[c % 2][:, kt, t * 128:(t + 1) * 128], in_=tp[ti][:, 0:128], func=AF.Copy),
                         reads=[f"tp{ti}", f"maskT{c % 2}"], writes=[f"maskT{c % 2}"])


            def att_head(c, h, use_dve=False, look=1):
                nkt = 4 * c + 4
                hp, h2 = h // 2, h % 2
                qi = h % 2
                oi = h % 2
                bi = mmr.next()
                S.op("tensor", lambda e, hp=hp, h2=h2, bi=bi: e.matmul(mm[bi][:], lhsT=wuk_sb[h2 * 64:(h2 + 1) * 64, hp, :],
                                                                    rhs=qTs[c % 2][h2 * 64:(h2 + 1) * 64, hp, :], start=True, stop=True),
                     reads=["wuk_sb", f"qT{c % 2}"], writes=[f"mm{bi}"])
                S.op("scalar", lambda e, bi=bi, qi=qi: e.activation(out=qlat[qi][:], in_=mm[bi][:], func=AF.Copy, scale=0.125),
                     reads=[f"mm{bi}"], writes=[f"qlat{qi}"])
                def qk(kt):
                    j0 = max(0, kt - 4 * c)
                    ncol = (4 - j0) * 128
                    c0 = j0 * 128
                    bi = mmr.next()
                    S.op("tensor", lambda e, kt=kt, bi=bi, qi=qi, c0=c0, ncol=ncol: e.matmul(
                        mm[bi][:, 0:ncol], lhsT=ckvT[:, kt * 128:(kt + 1) * 128], rhs=qlat[qi][:, c0:c0 + ncol], start=True, stop=True),
                        reads=["ckvT", f"qlat{qi}"], writes=[f"mm{bi}"])
                    return bi, j0, ncol, c0
                pend = [qk(k_) for k_ in range(min(look, nkt))]
                for kt in range(nkt):
                    bi, j0, ncol, c0 = pend.pop(0)
                    if kt + look < nkt:
                        pend.append(qk(kt + look))
                    ei = kt % (3 if look > 1 else 2)
                    S.op("scalar", lambda e, bi=bi, ei=ei, ncol=ncol: e.activation(out=Eb[ei][:, 0:ncol], in_=mm[bi][:, 0:ncol], func=AF.Exp),
                         reads=[f"mm{bi}"], writes=[f"Eb{ei}"])
                    S.op("vector" if (use_dve and kt % 2 == 0) else "gpsimd", lambda e, ei=ei, kt=kt, c0=c0, ncol=ncol, c=c: e.tensor_tensor(
                        out=Pb[ei][:, 0:ncol], in0=Eb[ei][:, 0:ncol], in1=maskTs[c % 2][:, kt, c0:c0 + ncol], op=ALU.mult),
                        reads=[f"Eb{ei}", f"maskT{c % 2}"], writes=[f"Pb{ei}"])
                    def pv(e, kt=kt, j0=j0, ei=ei, oi=oi, c=c):
                        ins = None
                        for j in range(j0, 4):
                            bank, off = (0, j * 129) if j < 3 else (1, 0)
                            ins = e.matmul(oacc[oi][:, bank, off:off + 129], lhsT=Pb[ei][:, (j - j0) * 128:(j - j0 + 1) * 128],
                                           rhs=ckv1[:, kt, 0:129], start=(kt == 0 and j in (0, 3)), stop=(kt == 4 * c + j),
                                           skip_group_check=True)
                        return ins
                    S.op("tensor", pv, reads=[f"Pb{ei}", "ckv1"], writes=[f"oacc{oi}"])
                ti = tpr.next()
                for j in range(4):
                    bank, off = (0, j * 129) if j < 3 else (1, 0)
                    li = j % 2
                    S.op("scalar", lambda e, oi=oi, bank=bank, off=off, j=j: e.activation(out=rden[:, j:j + 1], in_=oacc[oi][:, bank, off + 128:off + 129], func=AF.Ln),
                         reads=[f"oacc{oi}", "rden"], writes=["rden"])
                    S.op("scalar", lambda e, j=j: e.activation(out=rden[:, j:j + 1], in_=rden[:, j:j + 1], func=AF.Exp, scale=-1.0),
                         reads=["rden"], writes=["rden"])
                    S.op("scalar", lambda e, oi=oi, bank=bank, off=off, j=j, li=li: e.activation(
                        out=olat[li][:], in_=oacc[oi][:, bank, off:off + 128], func=AF.Copy, scale=rden[:, j:j + 1]),
                        reads=[f"oacc{oi}", "rden"], writes=[f"olat{li}"])
                    S.op("tensor", lambda e, li=li, ti=ti, j=j: e.transpose(out=tp[ti][:, j * 128:(j + 1) * 128], in_=olat[li][:], identity=ident_b[:]),
                         reads=[f"olat{li}", "ident_b"], writes=[f"tp{ti}"])
                S.op("scalar", lambda e, ti=ti, h2=h2: e.activation(out=olT2[:, h2, :], in_=tp[ti][:, 0:512], func=AF.Copy),
                     reads=[f"tp{ti}", "olT2"], writes=["olT2"])
                if h2 == 1:
                    bi = mmr.next()
                    def f(e, hp=hp, bi=bi):
                        e.matmul(mm[bi][:], lhsT=wuv_sb[:, hp, 0, :], rhs=olT2[:, 0, :], start=True, stop=False)
                        return e.matmul(mm[bi][:], lhsT=wuv_sb[:, hp, 1, :], rhs=olT2[:, 1, :], start=False, stop=True)
                    S.op("tensor", f, reads=["wuv_sb", "olT2"], writes=[f"mm{bi}"])
                    S.op("scalar", lambda e, hp=hp, bi=bi, c=c: e.activation(out=mixTs[c % 2][:, 4 + hp, :], in_=mm[bi][:], func=AF.Copy),
                         reads=[f"mm{bi}", f"mixT{c % 2}"], writes=[f"mixT{c % 2}"])


            def out_ln(c):
                if DEBUG == "1a" and c == 0:
                    dump(S, "qT", qTs[0][:].rearrange("p k n -> p (k n)"), [128, 4 * CH], BF16, ["qT0"])
                    dump(S, "qiT", qiT[:].rearrange("p k n -> p (k n)"), [128, 4 * CH], BF16, ["qiT"])
                    dump(S, "maskT", maskTs[0][:, 0:4, :].rearrange("p k n -> p (k n)"), [128, 4 * CH], mybir.dt.uint8, ["maskT0"])
                    dump(S, "olT2", olT2[:].rearrange("p k n -> p (k n)"), [128, 2 * CH], BF16, ["olT2"])
                    dump(S, "SC", SC[:, 0:512], [128, 512], F32, ["SC"])
                if DEBUG == "1a" and c == 7:
                    dump(S, "ckv1", ckv1[:].rearrange("p k n -> p (k n)"), [128, NT * 130], BF16, ["ckv1"])
                    dump(S, "ckvT", ckvT[:], [128, T], BF16, ["ckvT"])
                    dump(S, "kiT", kiT[:], [128, T], BF16, ["kiT"])
                    dump(S, "widx", widx[:].rearrange("p k n -> p (k n)"), [128, NT * 8], F32, ["widx"])
                if DEBUG == "1a":
                    S.dma("sync", lambda e, c=c: e.dma_start(out=dbg["mixT"][c], in_=mixTs[c % 2][:].rearrange("p k n -> p (k n)")),
                          reads=[f"mixT{c % 2}"], key="dbgm")

                for t in range(4):
                    tg = 4 * c + t
                    b = tg % 2
                    S.dma("sync", lambda e, tg=tg, b=b: e.dma_start(out=xt[b][:], in_=x_d[tg * 128:(tg + 1) * 128, :]),
                          writes=[f"xt{b}"], key=f"x{b}")
                    for hf in range(2):
                        bi = mmr.next()
                        def f(e, t=t, hf=hf, bi=bi):
                            ins = None
                            for k in range(8):
                                ins = e.matmul(mm[bi][:], lhsT=mixTs[c % 2][:, k, t * 128:(t + 1) * 128], rhs=w_o_sb[:, k, hf * 512:(hf + 1) * 512],
                                               start=(k == 0), stop=(k == 7))
                            return ins
                        S.op("tensor", f, reads=[f"mixT{c % 2}", "w_o_sb"], writes=[f"mm{bi}"])
                        S.op("vector", lambda e, b=b, hf=hf, bi=bi: e.scalar_tensor_tensor(
                            out=r1[:, hf * 512:(hf + 1) * 512], in0=xt[b][:, hf * 512:(hf + 1) * 512], scalar=ALPHA, in1=mm[bi][:],
                            op0=ALU.mult, op1=ALU.add),
                            reads=[f"xt{b}", f"mm{bi}", "r1"], writes=["r1"])
                    layer_norm_tile(r1[:], h1o[b][:], g1_bc, b1_bc, bst, bmv, brs, r1[:], ("r1", "r1", "r1"))
                    S.dma("sync", lambda e, tg=tg, b=b: e.dma_start(out=h1_d[tg * 128:(tg + 1) * 128, :], in_=h1o[b][:]),
                          reads=["r1"], key="h1s0")
                    if DEBUG == "1a":
                        S.dma("sync", lambda e, tg=tg, b=b: e.dma_start(out=dbg["h"][tg * 128:(tg + 1) * 128, :], in_=h1o[b][:]),
                              reads=["r1"], key="dbgh0")

            for c in range(NCH):
                front(c)
                for t in range(4):
                    tile_sel(c, t)
                    if c > 0:
                        att_head(c - 1, 2 * t)
                        att_head(c - 1, 2 * t + 1)
                    tile_mask(c, t)
                if c > 0:
                    out_ln(c - 1)
            S.full_barrier()
            mm.append(tp[1][:].bitcast(F32))
            mmr.items = [0, 1, 2]
            tpr.items = [0]
            for h in range(8):
                att_head(NCH - 1, h, use_dve=True, look=2)
            out_ln(NCH - 1)
            S.full_barrier()
            S.flush(nc)

        if DEBUG == "1a":
            return nc

        with ExitStack() as p2:
            w_mq_sb = sb(p2, "w_mq_sb", [128, 8, D], BF16)
            w_mo_sb = sb(p2, "w_mo_sb", [128, 8, D], BF16)
            w_mkv_sb = sb(p2, "w_mkv_sb", [128, 8, 2 * D], BF16)
            mb = sb(p2, "mb", [128, 2, D], BF16)
            memT = sb(p2, "memT", [128, 8, 256], BF16)
            KmT = sb(p2, "KmT", [128, 8, 256], BF16)
            Vm = sb(p2, "Vm", [128, 2, D], BF16)
            g2_bc = sb(p2, "g2_bc", [128, D], F32)
            b2_bc = sb(p2, "b2_bc", [128, D], F32)
            wr_sb = sb(p2, "wr_sb", [128, 8, NE], F32)
            br_bc = sb(p2, "br_bc", [128, NE], F32)
            ustr_f = sb(p2, "ustr_f", [128, 128], F32)
            ustr_b = sb(p2, "ustr_b", [128, 128], BF16)
            cb_i = sb(p2, "cb_i", [128, NE], I32)
            cbase = sb(p2, "cbase", [128, NE], F32)
            caphi = sb(p2, "caphi", [128, NE], F32)
            h1c = sb(p2, "h1c", [128, 4, D], F32)
            h1b = [sb(p2, f"h1b{i}", [128, D], BF16) for i in range(2)]
            hT = sb(p2, "hT", [128, 8, CH], BF16)
            qmT = sb(p2, "qmT", [128, 8, CH], BF16)
            Pm = [sb(p2, f"Pm{i}", [128, CH], BF16) for i in range(2)]
            rdn = sb(p2, "rdn", [128, CH], F32)
            omT = sb(p2, "omT", [128, 8, CH], BF16)
            r2 = sb(p2, "r2", [128, D], F32)
            tmpn2 = sb(p2, "tmpn2", [128, D], F32)
            h2o = [sb(p2, f"h2o{i}", [128, D], F32) for i in range(2)]
            h2b = [sb(p2, f"h2b{i}", [128, D], BF16) for i in range(2)]
            h2T = sb(p2, "h2T", [128, 8, 128], F32)
            lg = sb(p2, "lg", [128, NE], F32)
            mx8r = sb(p2, "mx8r", [128, 8], F32)
            negm = sb(p2, "negm", [128, 1], F32)
            ex4 = sb(p2, "ex4", [128, 4], F32)
            gsum = sb(p2, "gsum", [128, 1], F32)
            selb = sb(p2, "selb", [128, NE], BF16)
            slotm = sb(p2, "slotm", [128, NE], F32)
            ohp = sb(p2, "ohp", [128, NE], F32)
            slotf = sb(p2, "slotf", [128, 4], F32)
            bst2 = sb(p2, "bst2", [128, 2, 6], F32)
            bmv2 = sb(p2, "bmv2", [128, 2], F32)
            brs2 = sb(p2, "brs2", [128, 1], F32)
            mm = [ps(p2, f"mmB{i}", [128, 512], F32) for i in range(6)]
            tp = [ps(p2, f"tpB{i}", [128, 1024], BF16) for i in range(2)]
            mmr = Rot(list(range(6))); tpr = Rot([0, 1])

            S.dma("gpsimd", lambda e: e.dma_start(out=w_mkv_sb[:], in_=w_mkv_d.rearrange("(k p) n -> p k n", p=128)), writes=["w_mkv_sb"], key="w0")
            S.dma("gpsimd", lambda e: e.dma_start(out=mb[:], in_=mem_d.rearrange("(t p) d -> p t d", p=128)), writes=["mb"], key="w1")
            S.dma("gpsimd", lambda e: e.dma_start(out=w_mq_sb[:], in_=w_mq_d.rearrange("(k p) n -> p k n", p=128)), writes=["w_mq_sb"], key="w2")
            S.dma("gpsimd", lambda e: e.dma_start(out=w_mo_sb[:], in_=w_mo_d.rearrange("(k p) n -> p k n", p=128)), writes=["w_mo_sb"], key="w4")
            S.dma("sync", lambda e: e.dma_start(out=g2_bc[:], in_=bc(ln2g_d, D)), writes=["g2_bc"], key="c1")
            S.dma("sync", lambda e: e.dma_start(out=b2_bc[:], in_=bc(ln2b_d, D)), writes=["b2_bc"], key="c2")
            S.dma("sync", lambda e: e.dma_start(out=wr_sb[:], in_=w_r_d.rearrange("(k p) n -> p k n", p=128)), writes=["wr_sb"], key="c3")
            S.dma("sync", lambda e: e.dma_start(out=br_bc[:], in_=bc(b_r_d, NE)), writes=["br_bc"], key="c4")
            S.op("gpsimd", lambda e: e.memset(ustr_f[:], 1.0), writes=["ustr_f"])
            S.op("gpsimd", lambda e: e.affine_select(out=ustr_f[:], in_=ustr_f[:], pattern=[[1, 128]], compare_op=ALU.is_gt, fill=0.0,
                                                       base=0, channel_multiplier=-1), reads=["ustr_f"], writes=["ustr_f"])
            S.op("vector", lambda e: e.tensor_copy(out=ustr_b[:], in_=ustr_f[:]), reads=["ustr_f"], writes=["ustr_b"])
            S.op("gpsimd", lambda e: e.iota(out=cb_i[:], pattern=[[CAP, NE]], base=0, channel_multiplier=0), writes=["cb_i"])
            S.op("vector", lambda e: e.tensor_copy(out=cbase[:], in_=cb_i[:]), reads=["cb_i"], writes=["cbase"])
            S.op("vector", lambda e: e.tensor_scalar(out=caphi[:], in0=cbase[:], scalar1=float(CAP - 1), scalar2=None, op0=ALU.add),
                 reads=["cbase"], writes=["caphi"])
            for mt in range(2):
                for kh in range(2):
                    ti = tpr.next()
                    def tr(e, mt=mt, kh=kh, ti=ti):
                        ins = None
                        for kk in range(4):
                            k = kh * 4 + kk
                            ins = e.transpose(out=tp[ti][:, kk * 128:(kk + 1) * 128], in_=mb[:, mt, k * 128:(k + 1) * 128], identity=ident_b[:])
                        return ins
                    S.op("tensor", tr, reads=["mb", "ident_b"], writes=[f"tp{ti}"])
                    S.op("vector", lambda e, mt=mt, kh=kh, ti=ti: e.tensor_copy(
                        out=memT[:, kh * 4:(kh + 1) * 4, mt * 128:(mt + 1) * 128], in_=tp[ti][:, 0:512].rearrange("p (k n) -> p k n", k=4)),
                        reads=[f"tp{ti}", "memT"], writes=["memT"])
            for cc in range(8):
                bi = mmr.next()
                def f(e, cc=cc, bi=bi):
                    ins = None
                    for k in range(8):
                        ins = e.matmul(mm[bi][:, 0:256], lhsT=w_mkv_sb[:, k, cc * 128:(cc + 1) * 128], rhs=memT[:, k, :], start=(k == 0), stop=(k == 7))
                    return ins
                S.op("tensor", f, reads=["w_mkv_sb", "memT"], writes=[f"mm{bi}"])
                S.op("vector", lambda e, cc=cc, bi=bi: e.tensor_copy(out=KmT[:, cc, :], in_=mm[bi][:, 0:256]), reads=[f"mm{bi}", "KmT"], writes=["KmT"])
            for mt in range(2):
                for hf in range(2):
                    bi = mmr.next()
                    def f(e, mt=mt, hf=hf, bi=bi):
                        ins = None
                        for k in range(8):
                            ins = e.matmul(mm[bi][:], lhsT=memT[:, k, mt * 128:(mt + 1) * 128], rhs=w_mkv_sb[:, k, D + hf * 512:D + (hf + 1) * 512],
                                           start=(k == 0), stop=(k == 7))
                        return ins
                    S.op("tensor", f, reads=["w_mkv_sb", "memT"], writes=[f"mm{bi}"])
                    S.op("scalar", lambda e, mt=mt, hf=hf, bi=bi: e.activation(out=Vm[:, mt, hf * 512:(hf + 1) * 512], in_=mm[bi][:], func=AF.Copy),
                         reads=[f"mm{bi}", "Vm"], writes=["Vm"])

            for c in range(NCH):
                S.dma("sync", lambda e, c=c: e.dma_start(out=h1c[:], in_=h1_d[c * CH:(c + 1) * CH, :].rearrange("(t p) d -> p t d", p=128)),
                      writes=["h1c"], key="h1l")
                for t in range(4):
                    b = t % 2
                    S.op("scalar", lambda e, b=b, t=t: e.activation(out=h1b[b][:], in_=h1c[:, t, :], func=AF.Copy),
                         reads=["h1c"], writes=[f"h1b{b}"])
                    for kh in range(2):
                        ti = tpr.next()
                        def tr(e, b=b, kh=kh, ti=ti):
                            ins = None
                            for kk in range(4):
                                k = kh * 4 + kk
                                ins = e.transpose(out=tp[ti][:, kk * 128:(kk + 1) * 128], in_=h1b[b][:, k * 128:(k + 1) * 128], identity=ident_b[:])
                            return ins
                        S.op("tensor", tr, reads=[f"h1b{b}", "ident_b"], writes=[f"tp{ti}"])
                        S.op("vector", lambda e, kh=kh, ti=ti, t=t: e.tensor_copy(
                            out=hT[:, kh * 4:(kh + 1) * 4, t * 128:(t + 1) * 128], in_=tp[ti][:, 0:512].rearrange("p (k n) -> p k n", k=4)),
                            reads=[f"tp{ti}", "hT"], writes=["hT"])
                for cc in range(8):
                    bi = mmr.next()
                    def f(e, cc=cc, bi=bi):
                        ins = None
                        for k in range(8):
                            ins = e.matmul(mm[bi][:], lhsT=w_mq_sb[:, k, cc * 128:(cc + 1) * 128], rhs=hT[:, k, :], start=(k == 0), stop=(k == 7))
                        return ins
                    S.op("tensor", f, reads=["w_mq_sb", "hT"], writes=[f"mm{bi}"])
                    S.op("scalar", lambda e, cc=cc, bi=bi: e.activation(out=qmT[:, cc, :], in_=mm[bi][:], func=AF.Copy, scale=1.0 / 16),
                         reads=[f"mm{bi}", "qmT"], writes=["qmT"])
                for h in range(4):
                    for mt in range(2):
                        bi = mmr.next()
                        def f(e, h=h, mt=mt, bi=bi):
                            e.matmul(mm[bi][:], lhsT=KmT[:, 2 * h, mt * 128:(mt + 1) * 128], rhs=qmT[:, 2 * h, :], start=True, stop=False)
                            return e.matmul(mm[bi][:], lhsT=KmT[:, 2 * h + 1, mt * 128:(mt + 1) * 128], rhs=qmT[:, 2 * h + 1, :], start=False, stop=True)
                        S.op("tensor", f, reads=["KmT", "qmT"], writes=[f"mm{bi}"])
                        S.op("scalar", lambda e, mt=mt, bi=bi: e.activation(out=Pm[mt][:], in_=mm[bi][:], func=AF.Exp),
                             reads=[f"mm{bi}"], writes=[f"Pm{mt}"])
                    bi = mmr.next()
                    def f(e, bi=bi):
                        e.matmul(mm[bi][:], lhsT=ones_b[:], rhs=Pm[0][:], start=True, stop=False)
                        return e.matmul(mm[bi][:], lhsT=ones_b[:], rhs=Pm[1][:], start=False, stop=True)
                    S.op("tensor", f, reads=["ones_b", "Pm0", "Pm1"], writes=[f"mm{bi}"])
                    S.op("vector", lambda e, bi=bi: e.reciprocal(out=rdn[:], in_=mm[bi][:]), reads=[f"mm{bi}"], writes=["rdn"])
                    for dvc in range(2):
                        bi = mmr.next()
                        def f(e, h=h, dvc=dvc, bi=bi):
                            c0 = h * 256 + dvc * 128
                            e.matmul(mm[bi][:], lhsT=Vm[:, 0, c0:c0 + 128], rhs=Pm[0][:], start=True, stop=False)
                            return e.matmul(mm[bi][:], lhsT=Vm[:, 1, c0:c0 + 128], rhs=Pm[1][:], start=False, stop=True)
                        S.op("tensor", f, reads=["Vm", "Pm0", "Pm1"], writes=[f"mm{bi}"])
                        S.op("vector", lambda e, h=h, dvc=dvc, bi=bi: e.tensor_tensor(out=omT[:, 2 * h + dvc, :], in0=mm[bi][:], in1=rdn[:], op=ALU.mult),
                             reads=[f"mm{bi}", "rdn", "omT"], writes=["omT"])
                def outproj_ln(t):
                    tg = 4 * c + t
                    b = tg % 2
                    for hf in range(2):
                        bi = mmr.next()
                        def f(e, t=t, hf=hf, bi=bi):
                            ins = None
                            for k in range(8):
                                ins = e.matmul(mm[bi][:], lhsT=omT[:, k, t * 128:(t + 1) * 128], rhs=w_mo_sb[:, k, hf * 512:(hf + 1) * 512],
                                               start=(k == 0), stop=(k == 7))
                            return ins
                        S.op("tensor", f, reads=["omT", "w_mo_sb"], writes=[f"mm{bi}"])
                        S.op("vector", lambda e, t=t, hf=hf, bi=bi: e.scalar_tensor_tensor(
                            out=r2[:, hf * 512:(hf + 1) * 512], in0=h1c[:, t, hf * 512:(hf + 1) * 512], scalar=ALPHA, in1=mm[bi][:],
                            op0=ALU.mult, op1=ALU.add), reads=["h1c", f"mm{bi}", "r2"], writes=["r2"])
                    layer_norm_tile(r2[:], h2o[b][:], g2_bc, b2_bc, bst2, bmv2, brs2, tmpn2[:], ("r2", f"h2o{b}", "tmpn2"))
                    S.dma("sync", lambda e, tg=tg, b=b: e.dma_start(out=h2_d[tg * 128:(tg + 1) * 128, :], in_=h2o[b][:]),
                          reads=[f"h2o{b}"], key=f"h2s{b}")
                    if DEBUG == "1b":
                        S.dma("sync", lambda e, tg=tg, b=b: e.dma_start(out=dbg["h"][tg * 128:(tg + 1) * 128, :], in_=h2o[b][:]),
                              reads=[f"h2o{b}"], key=f"dbgh{b}")
                    S.op("scalar", lambda e, b=b: e.activation(out=h2b[b][:], in_=h2o[b][:], func=AF.Copy), reads=[f"h2o{b}"], writes=[f"h2b{b}"])
                def router(t):
                    tg = 4 * c + t
                    b = tg % 2
                    for kh in range(2):
                        bi = mmr.next()
                        def tr(e, b=b, kh=kh, bi=bi):
                            ins = None
                            for kk in range(4):
                                k = kh * 4 + kk
                                ins = e.transpose(out=mm[bi][:, kk * 128:(kk + 1) * 128], in_=h2o[b][:, k * 128:(k + 1) * 128], identity=ident_f[:])
                            return ins
                        S.op("tensor", tr, reads=[f"h2o{b}", "ident_f"], writes=[f"mm{bi}"])
                        S.op("vector", lambda e, kh=kh, bi=bi: e.tensor_copy(out=h2T[:, kh * 4:(kh + 1) * 4, :],
                                                                              in_=mm[bi][:].rearrange("p (k n) -> p k n", k=4)),
                             reads=[f"mm{bi}", "h2T"], writes=["h2T"])
                    bi = mmr.next()
                    def f(e, bi=bi):
                        ins = None
                        for k in range(8):
                            ins = e.matmul(mm[bi][:, 0:NE], lhsT=h2T[:, k, :], rhs=wr_sb[:, k, :], start=(k == 0), stop=(k == 7))
                        return ins
                    S.op("tensor", f, reads=["h2T", "wr_sb"], writes=[f"mm{bi}"])
                    S.op("vector", lambda e, bi=bi: e.tensor_tensor(out=lg[:], in0=mm[bi][:, 0:NE], in1=br_bc[:], op=ALU.add),
                         reads=[f"mm{bi}", "br_bc"], writes=["lg"])
                    S.op("vector", lambda e: e.max(out=mx8r[:], in_=lg[:]), reads=["lg"], writes=["mx8r"])
                    S.op("vector", lambda e: e.tensor_scalar(out=negm[:], in0=mx8r[:, 0:1], scalar1=-1.0, scalar2=None, op0=ALU.mult),
                         reads=["mx8r"], writes=["negm"])
                    S.op("scalar", lambda e: e.activation(out=ex4[:], in_=mx8r[:, 0:4], func=AF.Exp, bias=negm[:, 0:1], accum_out=gsum[:, 0:1]),
                         reads=["mx8r", "negm", "gsum"], writes=["ex4", "gsum"])
                    S.op("vector", lambda e: e.reciprocal(out=gsum[:], in_=gsum[:]), reads=["gsum"], writes=["gsum"])
                    S.op("vector", lambda e, tg=tg: e.tensor_scalar(out=gates_all[:, tg, :], in0=ex4[:], scalar1=gsum[:, 0:1], scalar2=None, op0=ALU.mult),
                         reads=["ex4", "gsum", "gates_all"], writes=["gates_all"])
                    S.op("vector", lambda e: e.tensor_scalar(out=selb[:], in0=lg[:], scalar1=mx8r[:, 3:4], scalar2=None, op0=ALU.is_ge),
                         reads=["lg", "mx8r"], writes=["selb"])
                    bi = mmr.next()
                    def f(e, bi=bi):
                        e.matmul(mm[bi][:, 0:NE], lhsT=ustr_b[:], rhs=selb[:], start=True, stop=True)
                        return e.matmul(mm[bi][:, 64:64 + NE], lhsT=ones_b[:], rhs=selb[:], start=True, stop=True)
                    S.op("tensor", f, reads=["ustr_b", "ones_b", "selb"], writes=[f"mm{bi}"])
                    S.op("vector", lambda e, bi=bi: e.tensor_tensor(out=slotm[:], in0=mm[bi][:, 0:NE], in1=cbase[:], op=ALU.add),
                         reads=[f"mm{bi}", "cbase"], writes=["slotm"])
                    S.op("vector", lambda e: e.tensor_tensor(out=slotm[:], in0=slotm[:], in1=caphi[:], op=ALU.min),
                         reads=["slotm", "caphi"], writes=["slotm"])
                    S.op("vector", lambda e, bi=bi: e.tensor_tensor(out=cbase[:], in0=mm[bi][:, 64:64 + NE], in1=cbase[:], op=ALU.add),
                         reads=[f"mm{bi}", "cbase"], writes=["cbase"])
                    for k in range(4):
                        S.op("vector", lambda e, k=k: e.scalar_tensor_tensor(out=ohp[:], in0=lg[:], scalar=mx8r[:, k:k + 1], in1=slotm[:],
                                                                             op0=ALU.is_equal, op1=ALU.mult),
                             reads=["lg", "mx8r", "slotm"], writes=["ohp"])
                        S.op("vector", lambda e, k=k: e.reduce_sum(out=slotf[:, k:k + 1], in_=ohp[:], axis=mybir.AxisListType.X),
                             reads=["ohp", "slotf"], writes=["slotf"])
                    S.op("vector", lambda e, tg=tg: e.tensor_copy(out=slots_all[:, tg, :], in_=slotf[:]), reads=["slotf", "slots_all"], writes=["slots_all"])
                    for k in range(4):
                        S.dma("gpsimd", lambda e, tg=tg, k=k, b=b: e.indirect_dma_start(
                            out=xg_d, out_offset=bass.IndirectOffsetOnAxis(ap=slots_all[:, tg, k:k + 1], axis=0), in_=h2b[b][:], in_offset=None),
                            reads=["slots_all", f"h2b{b}"], writes=["xg_d"], key=f"sc{k}")
                for t in range(4):
                    outproj_ln(t)
                    if t > 0:
                        router(t - 1)
                router(3)
            if DEBUG == "1b":
                dump(S, "slots", slots_all[:].rearrange("p a b -> p (a b)"), [128, NT * 4], I32, ["slots_all"])
                dump(S, "gates", gates_all[:].rearrange("p a b -> p (a b)"), [128, NT * 4], F32, ["gates_all"])
            S.full_barrier()
            S.flush(nc)
        if DEBUG == "1b":
            return nc

        BLKS = [(0, 512), (512, CAP - 512)]
        NTE = CAP // 128
        with ExitStack() as p3:
            wgu = [sb(p3, f"wgu{i}", [128, 8, 2 * D], BF16) for i in range(2)]
            wdn = [sb(p3, f"wdn{i}", [128, 8, D], BF16) for i in range(2)]
            bgr = sb(p3, "bgr", [NE, 2 * D], F32)
            bguT = sb(p3, "bguT", [128, 16, NE], F32)
            bgu7 = sb(p3, "bgu7", [128, 8, NE], F32)
            bdn = [sb(p3, f"bdn{i}", [128, D], F32) for i in range(2)]
            xg = [sb(p3, f"xg{i}", [128, NTE, D], BF16) for i in range(2)]
            xgTs = [sb(p3, f"xgT{i}", [128, 8, CAP], BF16) for i in range(2)]
            actT = sb(p3, "actT", [128, 8, CAP], BF16)
            s0 = [sb(p3, f"s0_{i}", [128, 512], F32) for i in range(2)]
            gcl = [sb(p3, f"gc{i}", [128, 512], F32) for i in range(2)]
            ucl = [sb(p3, f"uc{i}", [128, 512], F32) for i in range(2)]
            ysb = [sb(p3, f"ysb{i}", [128, D], F32) for i in range(2)]
            mm = [ps(p3, f"mmC{i}", [128, 512], F32) for i in range(6)]
            tp = [ps(p3, f"tpC{i}", [128, 1024], BF16) for i in range(2)]
            mmr = Rot(list(range(6))); tpr = Rot([0, 1])

            S.dma("sync", lambda e: e.dma_start(out=bgr[:], in_=b_gu_d), writes=["bgr"], key="c1")
            bi = mmr.next()
            def trb(e, bi=bi):
                ins = None
                for cidx in range(16):
                    ins = e.transpose(out=mm[bi][:, cidx * NE:(cidx + 1) * NE], in_=bgr[0:NE, cidx * 128:(cidx + 1) * 128], identity=ident_f[0:NE, 0:NE])
                return ins
            S.op("tensor", trb, reads=["bgr", "ident_f"], writes=[f"mm{bi}"])
            S.op("vector", lambda e, bi=bi: e.tensor_copy(out=bguT[:].rearrange("p a b -> p (a b)"), in_=mm[bi][:]), reads=[f"mm{bi}"], writes=["bguT"])
            S.op("vector", lambda e: e.tensor_scalar(out=bgu7[:], in0=bguT[:, 8:16, :], scalar1=7.0, scalar2=None, op0=ALU.add),
                 reads=["bguT"], writes=["bgu7"])

            def load_expert(ex):
                bf = ex % 2
                S.dma("gpsimd", lambda e: e.dma_start(out=wgu[bf][:], in_=w_gu_d[ex].rearrange("(k p) n -> p k n", p=128)),
                      writes=[f"wgu{bf}"], key=f"wg{bf}")
                S.dma("gpsimd", lambda e: e.dma_start(out=wdn[bf][:], in_=w_dn_d[ex].rearrange("(k p) n -> p k n", p=128)),
                      writes=[f"wdn{bf}"], key=f"wd{bf}")
                S.dma("sync", lambda e: e.dma_start(out=bdn[bf][:], in_=bc(b_dn_d[ex], D)), writes=[f"bdn{bf}"], key=f"bd{bf}")
                S.dma("sync", lambda e: e.dma_start(out=xg[bf][:], in_=xg_d[ex * CAP:(ex + 1) * CAP, :].rearrange("(t p) d -> p t d", p=128)),
                      reads=["xg_d"], writes=[f"xg{bf}"], key=f"xgl{bf}")

            def prep_expert(exx):
                pb = exx % 2
                for t in range(NTE):
                    for kh in range(2):
                        ti = tpr.next()
                        def tr(e, pb=pb, t=t, kh=kh, ti=ti):
                            ins = None
                            for kk in range(4):
                                k = kh * 4 + kk
                                ins = e.transpose(out=tp[ti][:, kk * 128:(kk + 1) * 128], in_=xg[pb][:, t, k * 128:(k + 1) * 128], identity=ident_b[:])
                            return ins
                        S.op("tensor", tr, reads=[f"xg{pb}", "ident_b"], writes=[f"tp{ti}"])
                        S.op("scalar", lambda e, kh=kh, ti=ti, t=t, pb=pb: e.activation(
                            out=xgTs[pb][:, kh * 4:(kh + 1) * 4, t * 128:(t + 1) * 128], in_=tp[ti][:, 0:512].rearrange("p (k n) -> p k n", k=4), func=AF.Copy),
                            reads=[f"tp{ti}", f"xgT{pb}"], writes=[f"xgT{pb}"])

            load_expert(0)
            prep_expert(0)
            for ex in range(NE_RUN):
                bf = ex % 2
                if ex + 1 < NE_RUN:
                    load_expert(ex + 1)
                it = 0
                for j in range(8 if P2_STEPS >= 2 else 0):
                    for (b0, bw) in BLKS:
                        big = mmr.next(); biu = mmr.next()
                        def fg(e, bf=bf, j=j, b0=b0, bw=bw, big=big):
                            ins = None
                            for k in range(8):
                                ins = e.matmul(mm[big][:, 0:bw], lhsT=wgu[bf][:, k, j * 128:(j + 1) * 128], rhs=xgTs[bf][:, k, b0:b0 + bw],
                                               start=(k == 0), stop=(k == 7))
                            return ins
                        def fu(e, bf=bf, j=j, b0=b0, bw=bw, biu=biu):
                            ins = None
                            for k in range(8):
                                ins = e.matmul(mm[biu][:, 0:bw], lhsT=wgu[bf][:, k, D + j * 128:D + (j + 1) * 128], rhs=xgTs[bf][:, k, b0:b0 + bw],
                                               start=(k == 0), stop=(k == 7))
                            return ins
                        S.op("tensor", fg, reads=[f"wgu{bf}", f"xgT{bf}"], writes=[f"mm{big}"])
                        S.op("tensor", fu, reads=[f"wgu{bf}", f"xgT{bf}"], writes=[f"mm{biu}"])
                        i2 = it % 2; it += 1
                        S.op("vector", lambda e, big=big, bw=bw, j=j, ex=ex, i2=i2: e.tensor_scalar(
                            out=gcl[i2][:, 0:bw], in0=mm[big][:, 0:bw], scalar1=bguT[:, j, ex:ex + 1], scalar2=7.0, op0=ALU.add, op1=ALU.min),
                            reads=[f"mm{big}", "bguT"], writes=[f"gc{i2}"])
                        S.op("scalar", lambda e, bw=bw, i2=i2: e.activation(out=s0[i2][:, 0:bw], in_=gcl[i2][:, 0:bw], func=AF.Silu, scale=1.702),
                             reads=[f"gc{i2}"], writes=[f"s0_{i2}"])
                        S.op("scalar", lambda e, biu=biu, bw=bw, j=j, ex=ex, i2=i2: e.activation(
                            out=ucl[i2][:, 0:bw], in_=mm[biu][:, 0:bw], func=AF.Relu, bias=bgu7[:, j, ex:ex + 1]),
                            reads=[f"mm{biu}", "bgu7"], writes=[f"uc{i2}"])
                        S.op("vector", lambda e, bw=bw, i2=i2: e.tensor_scalar(
                            out=ucl[i2][:, 0:bw], in0=ucl[i2][:, 0:bw], scalar1=14.0, scalar2=-6.0, op0=ALU.min, op1=ALU.add),
                            reads=[f"uc{i2}"], writes=[f"uc{i2}"])
                        S.op("vector", lambda e, bw=bw, b0=b0, j=j, i2=i2: e.scalar_tensor_tensor(
                            out=actT[:, j, b0:b0 + bw], in0=s0[i2][:, 0:bw], scalar=1.0 / 1.702, in1=ucl[i2][:, 0:bw], op0=ALU.mult, op1=ALU.mult),
                            reads=[f"s0_{i2}", f"uc{i2}", "actT"], writes=["actT"])
                if ex + 1 < NE_RUN:
                    prep_expert(ex + 1)
                for t in range(NTE if P2_STEPS >= 4 else 0):
                    yb = t % 2
                    for hf in range(2):
                        bi = mmr.next()
                        def fd(e, bf=bf, t=t, hf=hf, bi=bi):
                            ins = None
                            for j in range(8):
                                ins = e.matmul(mm[bi][:], lhsT=actT[:, j, t * 128:(t + 1) * 128], rhs=wdn[bf][:, j, hf * 512:(hf + 1) * 512],
                                               start=(j == 0), stop=(j == 7))
                            return ins
                        S.op("tensor", fd, reads=["actT", f"wdn{bf}"], writes=[f"mm{bi}"])
                        S.op("vector", lambda e, bf=bf, yb=yb, hf=hf, bi=bi: e.tensor_tensor(
                            out=ysb[yb][:, hf * 512:(hf + 1) * 512], in0=mm[bi][:], in1=bdn[bf][:, hf * 512:(hf + 1) * 512], op=ALU.add),
                            reads=[f"mm{bi}", f"bdn{bf}", f"ysb{yb}"], writes=[f"ysb{yb}"])
                    r0 = ex * CAP + t * 128
                    S.dma("sync", lambda e, yb=yb, r0=r0: e.dma_start(out=ys_d[r0:r0 + 128, :], in_=ysb[yb][:]),
                          reads=[f"ysb{yb}"], writes=["ys_d"], key=f"yst{yb}")
            S.full_barrier()
            S.flush(nc)
        if DEBUG == "2":
            return nc

        with ExitStack() as p4:
            g3_bc = sb(p4, "g3_bc", [128, D], F32)
            b3_bc = sb(p4, "b3_bc", [128, D], F32)
            h2t = [sb(p4, f"h2t{i}", [128, D], F32) for i in range(2)]
            yk = [[sb(p4, f"yk{i}_{k}", [128, D], F32) for k in range(4)] for i in range(2)]
            acc = sb(p4, "acc", [128, D], F32)
            tmpn3 = sb(p4, "tmpn3", [128, D], F32)
            outt = [sb(p4, f"outt{i}", [128, D], F32) for i in range(2)]
            bst3 = sb(p4, "bst3", [128, 2, 6], F32)
            bmv3 = sb(p4, "bmv3", [128, 2], F32)
            brs3 = sb(p4, "brs3", [128, 1], F32)
            S.dma("sync", lambda e: e.dma_start(out=g3_bc[:], in_=bc(ln3g_d, D)), writes=["g3_bc"], key="c1")
            S.dma("sync", lambda e: e.dma_start(out=b3_bc[:], in_=bc(ln3b_d, D)), writes=["b3_bc"], key="c2")
            for tg in range(NT):
                b = tg % 2
                S.dma("sync", lambda e, tg=tg, b=b: e.dma_start(out=h2t[b][:], in_=h2_d[tg * 128:(tg + 1) * 128, :]),
                      writes=[f"h2t{b}"], key=f"h2l{b}")
                for k in range(4):
                    S.dma("gpsimd", lambda e, tg=tg, k=k, b=b: e.indirect_dma_start(
                        out=yk[b][k][:], out_offset=None, in_=ys_d, in_offset=bass.IndirectOffsetOnAxis(ap=slots_all[:, tg, k:k + 1], axis=0)),
                        reads=["ys_d", "slots_all"], writes=[f"yk{b}_{k}"], key=f"gk{b}{k}")
                S.op("vector", lambda e, b=b: e.tensor_scalar(out=acc[:], in0=h2t[b][:], scalar1=ALPHA, scalar2=None, op0=ALU.mult),
                     reads=[f"h2t{b}", "acc"], writes=["acc"])
                for k in range(4):
                    S.op("vector", lambda e, b=b, k=k, tg=tg: e.scalar_tensor_tensor(
                        out=acc[:], in0=yk[b][k][:], scalar=gates_all[:, tg, k:k + 1], in1=acc[:], op0=ALU.mult, op1=ALU.add),
                        reads=[f"yk{b}_{k}", "gates_all", "acc"], writes=["acc"])
                layer_norm_tile(acc[:], outt[b][:], g3_bc, b3_bc, bst3, bmv3, brs3, tmpn3[:], ("acc", f"outt{b}", "tmpn3"))
                S.dma("scalar", lambda e, tg=tg, b=b: e.dma_start(out=out_d[tg * 128:(tg + 1) * 128, :], in_=outt[b][:]),
                      reads=[f"outt{b}"], key=f"os{b}")
            S.full_barrier()
            S.flush(nc)
    return nc


_PROG = None


def kernel(**inputs):
    global _PROG
    if _PROG is None:
        _PROG = build_program()
    nc = _PROG
    B = inputs["x"].shape[0]
    in_maps = []
    for b in range(B):
        m = {}
        for k, v in inputs.items():
            a = np.asarray(v)
            if k in ("x", "mem"):
                m[k] = np.ascontiguousarray(a[b])
            else:
                m[k] = np.ascontiguousarray(a[0])
        in_maps.append(m)
    res = run_bass_kernel_spmd(nc, in_maps, core_ids=list(range(B)))
    return np.stack([np.asarray(r["out"]) for r in res.results], axis=0)
```

```python
import numpy as np
from contextlib import ExitStack
import concourse.bass as bass
import concourse.mybir as mybir
from concourse.bass_utils import run_bass_kernel_spmd

F32 = mybir.dt.float32
BF16 = mybir.dt.bfloat16
I32 = mybir.dt.int32
AF = mybir.ActivationFunctionType
ALU = mybir.AluOpType

T = 4096
NT = 32
D = 1024
CH = 512
NCH = 8
INW = 1736
CAP = 768
NE = 32
ALPHA = 2.0 ** 0.25
NEG = -1.0e30
NEG2 = -3.0e30
KBIS = 25
SIGMAX = float(1.0 / (1.0 + np.exp(-1.702 * 7.0)))

DEBUG = None
NE_RUN = NE
P2_STEPS = 9
P2_EW = 63


class Sched:
    EPOCH = 30000
    ENG = ("sync", "scalar", "vector", "gpsimd", "tensor")

    def __init__(self, sem_pool):
        self.ops = {e: [] for e in self.ENG}
        self.cnt = {}
        self.lastw = {}
        self.readers = {}
        self.seen = {e: {} for e in self.ENG}
        self.sem_pool = list(sem_pool)
        self.sems = {}

    def _sem(self, counter, val):
        if counter.startswith("E:"):
            ep = (val - 1) // self.EPOCH
            name, lv = f"{counter}#{ep}", val - ep * self.EPOCH
        else:
            name, lv = counter, val
        if name not in self.sems:
            self.sems[name] = self.sem_pool.pop()
        return self.sems[name], lv

    def _waits(self, eng, reads, writes, extra=()):
        need = {}
        def add(cv):
            c, v = cv
            if v > need.get(c, 0):
                need[c] = v
        for r in reads:
            if r in self.lastw:
                add(self.lastw[r])
        for w in writes:
            if w in self.lastw:
                add(self.lastw[w])
            for cv in self.readers.get(w, {}).items():
                add(cv)
        for cv in extra:
            add(cv)
        waits = []
        for c, v in need.items():
            if eng == "tensor" and c == "E:tensor":
                continue
            if self.seen[eng].get(c, 0) < v:
                self.seen[eng][c] = v
                waits.append(self._sem(c, v))
        return waits

    def _book(self, c, v, reads, writes):
        for r in reads:
            d = self.readers.setdefault(r, {})
            if d.get(c, 0) < v:
                d[c] = v
        for w in writes:
            self.lastw[w] = (c, v)
            self.readers[w] = {}

    def op(self, eng, fn, reads=(), writes=()):
        waits = self._waits(eng, reads, writes)
        c = "E:" + eng
        v = self.cnt.get(c, 0) + 1
        self.cnt[c] = v
        sem, _ = self._sem(c, v)
        self.ops[eng].append((waits, fn, sem, 1))
        self._book(c, v, reads, writes)

    def dma(self, eng, fn, reads=(), writes=(), key="d", serialize=True):
        c = "D:" + key
        prev = self.cnt.get(c, 0)
        waits = self._waits(eng, reads, writes, extra=[(c, prev)] if (prev and serialize) else [])
        v = prev + 16
        self.cnt[c] = v
        sem, _ = self._sem(c, v)
        self.ops[eng].append((waits, fn, sem, 16))
        self._book(c, v, reads, writes)

    def barrier_all(self, eng="sync"):
        waits = []
        for c, v in self.cnt.items():
            if v and self.seen[eng].get(c, 0) < v:
                self.seen[eng][c] = v
                waits.append(self._sem(c, v))
        self.ops[eng].append((waits, None, None, 0))

    def full_barrier(self):
        for e in self.ENG:
            self.barrier_all(e)

    def flush(self, nc):
        with nc.Block() as blk:
            for eng in self.ENG:
                lst = self.ops[eng]
                if not lst:
                    continue
                def body(e, lst=lst):
                    for waits, fn, sem, inc in lst:
                        for s, v in waits:
                            e.wait_ge(s, v)
                        if fn is not None:
                            ins = fn(e)
                            ins.then_inc(sem, inc)
                getattr(blk, eng)(body)
        self.ops = {e: [] for e in self.ENG}


class Rot:
    def __init__(self, items):
        self.items = items
        self.i = 0
    def next(self):
        it = self.items[self.i % len(self.items)]
        self.i += 1
        return it


def build_program():
    nc = bass.Bass("TRN2", target_bir_lowering=False)
    dt = lambda name, shape, dtype=F32, kind="ExternalInput": nc.dram_tensor(name, shape, dtype, kind=kind).ap()
    x_d = dt("x", [T, D])
    mem_d = dt("mem", [256, D])
    w_in_d = dt("w_in", [D, INW])
    w_pool_d = dt("w_pool", [4, 128, 128])
    pool_scale_d = dt("pool_scale", [512])
    kig_d = dt("idx_k_norm_g", [64])
    kib_d = dt("idx_k_norm_b", [64])
    kvg_d = dt("kv_norm_g", [128])
    w_uk_d = dt("w_uk", [8, 64, 128])
    w_uv_d = dt("w_uv", [8, 128, 64])
    w_o_d = dt("w_o", [D, D])
    ln1g_d = dt("ln1_g", [D]); ln1b_d = dt("ln1_b", [D])
    w_mq_d = dt("w_mq", [D, D])
    w_mkv_d = dt("w_mkv", [D, 2 * D])
    w_mo_d = dt("w_mo", [D, D])
    ln2g_d = dt("ln2_g", [D]); ln2b_d = dt("ln2_b", [D])
    w_r_d = dt("w_router", [D, NE])
    b_r_d = dt("b_router", [NE])
    w_gu_d = dt("w_gate_up", [NE, D, 2 * D])
    b_gu_d = dt("b_gate_up", [NE, 2 * D])
    w_dn_d = dt("w_down", [NE, D, D])
    b_dn_d = dt("b_down", [NE, D])
    ln3g_d = dt("ln3_g", [D]); ln3b_d = dt("ln3_b", [D])
    out_d = dt("out", [T, D], F32, "ExternalOutput")
    h1_d = dt("h1_scr", [T, D], F32, "Internal")
    h2_d = dt("h2_scr", [T, D], F32, "Internal")
    xg_d = dt("xg_scr", [NE * CAP, D], BF16, "Internal")
    ys_d = dt("ys_scr", [NE * CAP, D], F32, "Internal")
    dbg = {}
    if DEBUG:
        dbg["h"] = dt("dbg_h", [T, D], F32, "ExternalOutput")
        dbg["mixT"] = dt("dbg_mixT", [NCH, 128, 8 * CH], BF16, "ExternalOutput")

    dumps = []
    def dump(S, name, ap2d, shape, dtype, reads):
        if not DEBUG:
            return
        d = dt("dbg_" + name, shape, dtype, "ExternalOutput")
        S.dma("sync", lambda e: e.dma_start(out=d, in_=ap2d), reads=reads, key="dbgd")

    def bc(ap1d, n):
        return ap1d.rearrange("(o n) -> o n", o=1).to_broadcast([128, n])

    with ExitStack() as top:
        sem_pool = [top.enter_context(nc.semaphore(f"s{i}")) for i in range(96)]
        S = Sched(sem_pool)
        sb = lambda es, name, shape, dtype=F32: es.enter_context(nc.sbuf_tensor(name, shape, dtype))
        ps = lambda es, name, shape, dtype=F32: es.enter_context(nc.psum_tensor(name, shape, dtype))

        ident_b = sb(top, "ident_b", [128, 128], BF16)
        ident_f = sb(top, "ident_f", [128, 128], F32)
        ones_b = sb(top, "ones_b", [128, 128], BF16)
        slots_all = sb(top, "slots_all", [128, NT, 4], I32)
        gates_all = sb(top, "gates_all", [128, NT, 4], F32)

        S.op("gpsimd", lambda e: e.memset(ident_f[:], 0.0), writes=["ident_f"])
        S.op("gpsimd", lambda e: e.affine_select(out=ident_f[:], in_=ident_f[:], pattern=[[-1, 128]],
                                                   compare_op=ALU.not_equal, fill=1.0, base=0, channel_multiplier=1),
             reads=["ident_f"], writes=["ident_f"])
        S.op("vector", lambda e: e.tensor_copy(out=ident_b[:], in_=ident_f[:]), reads=["ident_f"], writes=["ident_b"])
        S.op("vector", lambda e: e.memset(ones_b[:], 1.0), writes=["ones_b"])

        def ln_rstd(var_ap, rstd_ap, tag, scale, rname, wname):
            S.op("scalar", lambda e: e.activation(out=rstd_ap, in_=var_ap, func=AF.Ln, bias=eps_tile[:, tag:tag + 1], scale=scale),
                 reads=[rname, "eps", wname], writes=[wname])
            S.op("scalar", lambda e: e.activation(out=rstd_ap, in_=rstd_ap, func=AF.Exp, scale=-0.5),
                 reads=[wname], writes=[wname])

        eps_tile = sb(top, "eps", [128, 2], F32)
        S.op("vector", lambda e: e.memset(eps_tile[:, 0:1], 1e-5), writes=["eps"])
        S.op("vector", lambda e: e.memset(eps_tile[:, 1:2], 1e-6), reads=["eps"], writes=["eps"])

        def layer_norm_tile(r_ap, out_ap, g_bc, b_bc, stats, mv, rstd, tmp_ap, names):
            rn, on, tn = names
            for hf in range(2):
                S.op("vector", lambda e, hf=hf: e.bn_stats(out=stats[:, hf, :], in_=r_ap[:, hf * 512:(hf + 1) * 512]),
                     reads=[rn], writes=[stats.name])
            S.op("vector", lambda e: e.bn_aggr(out=mv[:], in_=stats[:].rearrange("p a b -> p (a b)")),
                 reads=[stats.name], writes=[mv.name])
            ln_rstd(mv[:, 1:2], rstd[:, 0:1], 0, 1.0, mv.name, rstd.name)
            S.op("vector", lambda e: e.tensor_scalar(out=tmp_ap, in0=r_ap, scalar1=mv[:, 0:1], scalar2=rstd[:, 0:1],
                                                      op0=ALU.subtract, op1=ALU.mult),
                 reads=[rn, mv.name, rstd.name], writes=[tn])
            S.op("vector", lambda e: e.tensor_tensor(out=tmp_ap, in0=tmp_ap, in1=g_bc[:], op=ALU.mult),
                 reads=[tn, g_bc.name], writes=[tn])
            S.op("vector", lambda e: e.tensor_tensor(out=out_ap, in0=tmp_ap, in1=b_bc[:], op=ALU.add),
                 reads=[tn, b_bc.name], writes=[on])

        with ExitStack() as p1:
            w_in_sb = sb(p1, "w_in_sb", [128, 8, INW], BF16)
            w_o_sb = sb(p1, "w_o_sb", [128, 8, D], BF16)
            wpool_sb = sb(p1, "wpool_sb", [128, 4, 128], BF16)
            wuk_sb = sb(p1, "wuk_sb", [128, 4, 128], BF16)
            wuv_sb = sb(p1, "wuv_sb", [128, 4, 2, 128], BF16)
            pscale_sb = sb(p1, "pscale_sb", [128, 4], F32)
            g1_bc = sb(p1, "g1_bc", [128, D], F32)
            b1_bc = sb(p1, "b1_bc", [128, D], F32)
            kvg_bc = sb(p1, "kvg_bc", [128, 128], F32)
            kig_bc = sb(p1, "kig_bc", [128, 64], F32)
            kib_bc = sb(p1, "kib_bc", [128, 64], F32)
            ic16 = sb(p1, "ic16", [128, 4, 16], F32)
            ckv1 = sb(p1, "ckv1", [128, NT, 130], BF16)
            ckvT = sb(p1, "ckvT", [128, T], BF16)
            kiT = sb(p1, "kiT", [128, T], BF16)
            widx = sb(p1, "widx", [128, NT, 8], F32)
            xt = [sb(p1, f"xt{i}", [128, D], F32) for i in range(2)]
            xb = [sb(p1, f"xb{i}", [128, D], BF16) for i in range(2)]
            xT = sb(p1, "xT", [128, 8, CH], BF16)
            ug = sb(p1, "ug", [128, 528], F32)
            halo = sb(p1, "halo", [128, 4, 16], F32)
            pA = sb(p1, "pA", [128, 528], F32)
            pB = sb(p1, "pB", [128, 528], F32)
            dT = sb(p1, "dT", [128, CH], BF16)
            qTs = [sb(p1, f"qT{i}", [128, 4, CH], BF16) for i in range(2)]
            qiT = sb(p1, "qiT", [128, 4, CH], BF16)
            mixTs = [sb(p1, f"mixT{i}", [128, 8, CH], BF16) for i in range(2)]
            SC = sb(p1, "SC", [128, T], F32)
            bmax = sb(p1, "bmax", [128, 1], F32)
            bmid = sb(p1, "bmid", [128, 1], F32)
            bcnt = sb(p1, "bcnt", [128, 1], F32)
            bd = sb(p1, "bd", [128, 1], F32)
            bnegl = sb(p1, "bnegl", [128, 1], F32)
            c256 = sb(p1, "c256", [128, 1], F32)
            pw2 = sb(p1, "pw2", [128, KBIS], F32)
            bsteps = sb(p1, "bsteps", [128, KBIS], F32)
            bnegh = sb(p1, "bnegh", [128, KBIS], F32)
            m128 = [sb(p1, f"m128_{i}", [128, 128], BF16) for i in range(2)]
            maskTs = [sb(p1, f"maskT{i}", [128, NT, CH], mybir.dt.uint8) for i in range(2)]
            rl = [sb(p1, f"rl{i}", [128, CH], F32) for i in range(2)]
            Eb = [sb(p1, f"Eb{i}", [128, CH], BF16) for i in range(2)]
            Pb = [sb(p1, f"Pb{i}", [128, CH], BF16) for i in range(2)]
            Eb.append(ug[:, 0:256].bitcast(BF16)); Pb.append(pB[:, 0:256].bitcast(BF16))
            qlat = [sb(p1, f"qlat{i}", [128, CH], BF16) for i in range(2)]
            olat = [sb(p1, f"olat{i}", [128, 128], BF16) for i in range(2)]
            rden = sb(p1, "rden", [128, 4], F32)
            ckn = sb(p1, "ckn", [128, 128], BF16)
            kn32 = sb(p1, "kn32", [128, 64], F32)
            kn2 = sb(p1, "kn2", [128, 128], BF16)
            tm = sb(p1, "tm", [128, 200], F32)
            olT2 = sb(p1, "olT2", [128, 2, CH], BF16)
            junk = sb(p1, "junk", [128, 128], F32)
            st1 = sb(p1, "st1", [128, 8], F32)
            bst = sb(p1, "bst", [128, 2, 6], F32)
            bmv = sb(p1, "bmv", [128, 2], F32)
            brs = sb(p1, "brs", [128, 1], F32)
            r1 = sb(p1, "r1", [128, D], F32)
            h1o = [r1, r1]
            mm = [ps(p1, f"mm{i}", [128, 512], F32) for i in range(2)]
            oacc = [ps(p1, f"oacc{i}", [128, 2, 512], F32) for i in range(2)]
            tp = [ps(p1, f"tp{i}", [128, 1024], BF16) for i in range(2)]
            mmr = Rot([0, 1]); tpr = Rot([0, 1])

            S.op("gpsimd", lambda e: e.memset(maskTs[1][:], 0), writes=["maskT1"])
            zsrc = maskTs[1][:].rearrange("p a b -> p (a b)").bitcast(BF16).rearrange("p (t d) -> p t d", d=D)
            for zi in range(NE * CAP // 1024):
                S.dma("scalar", lambda e, zi=zi: e.dma_start(out=xg_d[zi * 1024:(zi + 1) * 1024, :].rearrange("(t p) d -> p t d", p=128), in_=zsrc),
                      reads=["maskT1"], key="zf", serialize=False)
            S.lastw["xg_d"] = ("D:zf", S.cnt["D:zf"])
            S.dma("gpsimd", lambda e: e.dma_start(out=w_in_sb[:], in_=w_in_d.rearrange("(k p) n -> p k n", p=128)),
                  writes=["w_in_sb"], key="w0")
            S.dma("gpsimd", lambda e: e.dma_start(out=wpool_sb[:], in_=w_pool_d.rearrange("g c d -> c g d")),
                  writes=["wpool_sb"], key="w1")
            S.dma("gpsimd", lambda e: e.dma_start(out=wuk_sb[:], in_=w_uk_d.rearrange("(hp h2) d r -> (h2 d) hp r", h2=2)),
                  writes=["wuk_sb"], key="w2")
            S.op("vector", lambda e: e.memset(wuv_sb[:], 0.0), writes=["wuv_sb"])
            for h2 in range(2):
                S.dma("gpsimd", lambda e, h2=h2: e.dma_start(out=wuv_sb[:, :, h2, h2 * 64:(h2 + 1) * 64],
                                                             in_=w_uv_d.rearrange("(hp h2) r d -> h2 r hp d", h2=2)[h2]),
                      reads=["wuv_sb"], writes=["wuv_sb"], key=f"w3{h2}")
            S.dma("gpsimd", lambda e: e.dma_start(out=w_o_sb[:], in_=w_o_d.rearrange("(k p) n -> p k n", p=128)),
                  writes=["w_o_sb"], key="w4")
            S.dma("sync", lambda e: e.dma_start(out=pscale_sb[:], in_=pool_scale_d.rearrange("(g d) -> d g", d=128),
                                                allow_slow_non_contiguous=True),
                  writes=["pscale_sb"], key="c0")
            S.dma("sync", lambda e: e.dma_start(out=g1_bc[:], in_=bc(ln1g_d, D)), writes=["g1_bc"], key="c1")
            S.dma("sync", lambda e: e.dma_start(out=b1_bc[:], in_=bc(ln1b_d, D)), writes=["b1_bc"], key="c2")
            S.dma("sync", lambda e: e.dma_start(out=kvg_bc[:], in_=bc(kvg_d, 128)), writes=["kvg_bc"], key="c3")
            S.dma("sync", lambda e: e.dma_start(out=kig_bc[:], in_=bc(kig_d, 64)), writes=["kig_bc"], key="c4")
            S.dma("sync", lambda e: e.dma_start(out=kib_bc[:], in_=bc(kib_d, 64)), writes=["kib_bc"], key="c5")
            for g in range(4):
                w = 2 ** (g + 1)
                S.op("gpsimd", lambda e, g=g, w=w: e.memset(ic16[:, g, :], 1.0 / w), reads=["ic16"], writes=["ic16"])
                for t in range(w - 1):
                    S.op("gpsimd", lambda e, g=g, t=t: e.memset(ic16[:, g, t:t + 1], 1.0 / (t + 1)), reads=["ic16"], writes=["ic16"])
            S.op("gpsimd", lambda e: e.memset(halo[:], 0.0), writes=["halo"])
            S.op("gpsimd", lambda e: e.memset(c256[:], 256.0), writes=["c256"])
            for i_ in range(KBIS):
                S.op("gpsimd", lambda e, i_=i_: e.memset(pw2[:, i_:i_ + 1], 2.0 ** (-i_)), reads=["pw2"], writes=["pw2"])
            S.op("gpsimd", lambda e: e.memset(ckv1[:, :, 128:130], 1.0), writes=["ckv1"])

            def front(c):
                for t in range(4):
                    tg = 4 * c + t
                    b = tg % 2
                    S.dma("sync", lambda e, tg=tg, b=b: e.dma_start(out=xt[b][:], in_=x_d[tg * 128:(tg + 1) * 128, :]),
                          writes=[f"xt{b}"], key=f"x{b}")
                    S.op("scalar", lambda e, b=b: e.activation(out=xb[b][:], in_=xt[b][:], func=AF.Copy),
                         reads=[f"xt{b}"], writes=[f"xb{b}"])
                    for kh in range(2):
                        ti = tpr.next()
                        def tr(e, b=b, kh=kh, ti=ti):
                            ins = None
                            for kk in range(4):
                                k = kh * 4 + kk
                                ins = e.transpose(out=tp[ti][:, kk * 128:(kk + 1) * 128], in_=xb[b][:, k * 128:(k + 1) * 128],
                                                  identity=ident_b[:])
                            return ins
                        S.op("tensor", tr, reads=[f"xb{b}", "ident_b"], writes=[f"tp{ti}"])
                        S.op("vector", lambda e, kh=kh, ti=ti, t=t: e.tensor_copy(
                            out=xT[:, kh * 4:(kh + 1) * 4, t * 128:(t + 1) * 128],
                            in_=tp[ti][:, 0:512].rearrange("p (k n) -> p k n", k=4)),
                            reads=[f"tp{ti}"], writes=["xT"])

                def inproj_fm(col0):
                    bi = mmr.next()
                    def f(e, col0=col0, bi=bi):
                        ins = None
                        for k in range(8):
                            ins = e.matmul(mm[bi][:], lhsT=w_in_sb[:, k, col0:col0 + 128], rhs=xT[:, k, :],
                                           start=(k == 0), stop=(k == 7))
                        return ins
                    S.op("tensor", f, reads=["w_in_sb", "xT"], writes=[f"mm{bi}"])
                    return bi

                for g in range(4):
                    bi = inproj_fm(g * 128)
                    S.op("gpsimd", lambda e, g=g: e.tensor_copy(out=ug[:, 0:16], in_=halo[:, g, :]),
                         reads=["halo", "ug"], writes=["ug"])
                    S.op("scalar", lambda e, bi=bi: e.activation(out=ug[:, 16:528], in_=mm[bi][:], func=AF.Copy),
                         reads=[f"mm{bi}", "ug"], writes=["ug"])
                    S.op("gpsimd", lambda e, g=g: e.tensor_copy(out=halo[:, g, :], in_=ug[:, 512:528]),
                         reads=["ug"], writes=["halo"])
                    src, srcn = ug, "ug"
                    bufs = [(pA, "pA"), (pB, "pB")]
                    for lv in range(g + 1):
                        s = 2 ** lv
                        lo = 2 ** (lv + 1) - 1
                        dst, dstn = bufs[lv % 2]
                        S.op("gpsimd", lambda e, src=src, dst=dst, s=s, lo=lo: e.tensor_tensor(
                            out=dst[:, lo:528], in0=src[:, lo:528], in1=src[:, lo - s:528 - s], op=ALU.add),
                            reads=[srcn, dstn], writes=[dstn])
                        src, srcn = dst, dstn
                    w = 2 ** (g + 1)
                    S.op("vector", lambda e, src=src, w=w: e.scalar_tensor_tensor(
                        out=dT[:], in0=src[:, 16:528], scalar=1.0 / w, in1=ug[:, 16:528], op0=ALU.mult, op1=ALU.subtract),
                        reads=[srcn, "ug"], writes=["dT"])
                    if c == 0:
                        S.op("vector", lambda e, src=src, g=g: e.tensor_tensor(out=junk[:, 0:16], in0=src[:, 16:32], in1=ic16[:, g, :], op=ALU.mult),
                             reads=[srcn, "ic16"], writes=["junk"])
                        S.op("vector", lambda e: e.tensor_tensor(out=dT[:, 0:16], in0=junk[:, 0:16], in1=ug[:, 16:32], op=ALU.subtract),
                             reads=["junk", "ug", "dT"], writes=["dT"])
                    bi2 = mmr.next()
                    S.op("tensor", lambda e, g=g, bi2=bi2: e.matmul(mm[bi2][:], lhsT=wpool_sb[:, g, :], rhs=dT[:], start=True, stop=True),
                         reads=["wpool_sb", "dT"], writes=[f"mm{bi2}"])
                    S.op("scalar", lambda e, g=g, bi2=bi2: e.activation(out=mixTs[c % 2][:, g, :], in_=mm[bi2][:], func=AF.Copy, scale=pscale_sb[:, g:g + 1]),
                         reads=[f"mm{bi2}", "pscale_sb", f"mixT{c % 2}"], writes=[f"mixT{c % 2}"])
                for j in range(4):
                    bi = inproj_fm(512 + j * 128)
                    S.op("scalar", lambda e, j=j, bi=bi: e.activation(out=qTs[c % 2][:, j, :], in_=mm[bi][:], func=AF.Copy),
                         reads=[f"mm{bi}"], writes=[f"qT{c % 2}"])
                for j in range(4):
                    bi = inproj_fm(1152 + j * 128)
                    S.op("vector", lambda e, j=j, bi=bi: e.tensor_copy(out=qiT[:, j, :], in_=mm[bi][:]),
                         reads=[f"mm{bi}"], writes=["qiT"])
                tiA = tpr.next(); tiB = tpr.next()
                for t in range(4):
                    tg = 4 * c + t
                    bi = mmr.next()
                    def f(e, t=t, bi=bi):
                        ins = None
                        for k in range(8):
                            ins = e.matmul(mm[bi][:, 0:128], lhsT=xT[:, k, t * 128:(t + 1) * 128], rhs=w_in_sb[:, k, 1024:1152],
                                           start=(k == 0), stop=(k == 7))
                        for k in range(8):
                            ins = e.matmul(mm[bi][:, 128:200], lhsT=xT[:, k, t * 128:(t + 1) * 128], rhs=w_in_sb[:, k, 1664:1736],
                                           start=(k == 0), stop=(k == 7))
                        return ins
                    S.op("tensor", f, reads=["w_in_sb", "xT"], writes=[f"mm{bi}"])
                    S.op("scalar", lambda e, bi=bi: e.activation(out=tm[:], in_=mm[bi][:, 0:200], func=AF.Copy),
                         reads=[f"mm{bi}"], writes=["tm"])
                    S.op("scalar", lambda e: e.activation(out=junk[:], in_=tm[:, 0:128], func=AF.Square, accum_out=st1[:, 0:1]),
                         reads=["tm", "st1"], writes=["junk", "st1"])
                    ln_rstd(st1[:, 0:1], st1[:, 1:2], 1, 1.0 / 128, "st1", "st1")
                    S.op("vector", lambda e: e.scalar_tensor_tensor(out=ckn[:], in0=tm[:, 0:128], scalar=st1[:, 1:2], in1=kvg_bc[:],
                                                                     op0=ALU.mult, op1=ALU.mult),
                         reads=["tm", "st1", "kvg_bc"], writes=["ckn"])
                    S.op("gpsimd", lambda e, tg=tg: e.tensor_copy(out=ckv1[:, tg, 0:128], in_=ckn[:]), reads=["ckn", "ckv1"], writes=["ckv1"])
                    S.op("vector", lambda e: e.bn_stats(out=bst[:, 0, :], in_=tm[:, 128:192]), reads=["tm"], writes=["bst"])
                    S.op("vector", lambda e: e.bn_aggr(out=bmv[:], in_=bst[:, 0, :]), reads=["bst"], writes=["bmv"])
                    ln_rstd(bmv[:, 1:2], brs[:, 0:1], 0, 1.0, "bmv", "brs")
                    S.op("vector", lambda e: e.tensor_scalar(out=kn32[:], in0=tm[:, 128:192], scalar1=bmv[:, 0:1], scalar2=brs[:, 0:1],
                                                              op0=ALU.subtract, op1=ALU.mult),
                         reads=["tm", "bmv", "brs"], writes=["kn32"])
                    S.op("vector", lambda e, tg=tg: e.tensor_copy(out=widx[:, tg, :], in_=tm[:, 192:200]),
                         reads=["tm", "widx"], writes=["widx"])
                    S.op("gpsimd", lambda e: e.tensor_tensor(out=kn32[:], in0=kn32[:], in1=kig_bc[:], op=ALU.mult),
                         reads=["kn32", "kig_bc"], writes=["kn32"])
                    S.op("gpsimd", lambda e: e.tensor_tensor(out=kn2[:, 0:64], in0=kn32[:], in1=kib_bc[:], op=ALU.add),
                         reads=["kn32", "kib_bc", "kn2"], writes=["kn2"])
                    S.op("gpsimd", lambda e: e.tensor_copy(out=kn2[:, 64:128], in_=kn2[:, 0:64]), reads=["kn2"], writes=["kn2"])
                    S.op("tensor", lambda e, t=t, tiA=tiA: e.transpose(out=tp[tiA][:, t * 128:(t + 1) * 128], in_=ckn[:], identity=ident_b[:]),
                         reads=["ckn", "ident_b"], writes=[f"tp{tiA}"])
                    S.op("tensor", lambda e, t=t, tiB=tiB: e.transpose(out=tp[tiB][:, t * 128:(t + 1) * 128], in_=kn2[:], identity=ident_b[:]),
                         reads=["kn2", "ident_b"], writes=[f"tp{tiB}"])
                S.op("scalar", lambda e, c=c, tiA=tiA: e.activation(out=ckvT[:, c * CH:(c + 1) * CH], in_=tp[tiA][:, 0:512], func=AF.Copy),
                     reads=[f"tp{tiA}", "ckvT"], writes=["ckvT"])
                S.op("scalar", lambda e, c=c, tiB=tiB: e.activation(out=kiT[:, c * CH:(c + 1) * CH], in_=tp[tiB][:, 0:512], func=AF.Copy),
                     reads=[f"tp{tiB}", "kiT"], writes=["kiT"])


            def tile_sel(c, t):
                qt = 4 * c + t
                qt = 4 * c + t
                N = 128 * (qt + 1)
                nkc = (N + 511) // 512
                for kc in range(nkc):
                    k0 = kc * 512
                    kw = min(512, N - k0)
                    for h in range(8):
                        hp, h2 = h // 2, h % 2
                        bi = mmr.next()
                        S.op("tensor", lambda e, hp=hp, h2=h2, t=t, bi=bi, k0=k0, kw=kw: e.matmul(
                            mm[bi][:, 0:kw], lhsT=qiT[h2 * 64:(h2 + 1) * 64, hp, t * 128:(t + 1) * 128],
                            rhs=kiT[h2 * 64:(h2 + 1) * 64, k0:k0 + kw], start=True, stop=True),
                            reads=["qiT", "kiT"], writes=[f"mm{bi}"])
                        ri = h % 2
                        S.op("scalar", lambda e, bi=bi, ri=ri, kw=kw: e.activation(out=rl[ri][:, 0:kw], in_=mm[bi][:, 0:kw], func=AF.Relu),
                             reads=[f"mm{bi}"], writes=[f"rl{ri}"])
                        if h == 0:
                            S.op("vector", lambda e, ri=ri, k0=k0, kw=kw, qt=qt: e.tensor_scalar(
                                out=SC[:, k0:k0 + kw], in0=rl[ri][:, 0:kw], scalar1=widx[:, qt, 0:1], scalar2=None, op0=ALU.mult),
                                reads=[f"rl{ri}", "widx", "SC"], writes=["SC"])
                        else:
                            S.op("vector", lambda e, ri=ri, k0=k0, kw=kw, qt=qt, h=h: e.scalar_tensor_tensor(
                                out=SC[:, k0:k0 + kw], in0=rl[ri][:, 0:kw], scalar=widx[:, qt, h:h + 1], in1=SC[:, k0:k0 + kw],
                                op0=ALU.mult, op1=ALU.add),
                                reads=[f"rl{ri}", "widx", "SC"], writes=["SC"])
                if N > 256:
                    S.op("vector", lambda e, N=N: e.tensor_reduce(out=bmax[:], in_=SC[:, 0:N], axis=mybir.AxisListType.X, op=ALU.max,
                                                                   apply_absolute_value=True), reads=["SC"], writes=["bmax"])
                    S.op("vector", lambda e: e.tensor_scalar(out=bmax[:], in0=bmax[:], scalar1=1.0001, scalar2=1e-20, op0=ALU.mult, op1=ALU.add),
                         reads=["bmax"], writes=["bmax"])
                S.op("gpsimd", lambda e, qt=qt: e.affine_select(out=SC[:, qt * 128:(qt + 1) * 128], in_=SC[:, qt * 128:(qt + 1) * 128],
                                                                 pattern=[[-1, 128]], compare_op=ALU.is_ge, fill=NEG, base=0, channel_multiplier=1),
                     reads=["SC"], writes=["SC"])
                if N > 256:
                    S.op("vector", lambda e, N=N: e.tensor_scalar(out=bmid[:], in0=bmax[:], scalar1=0.0, scalar2=None, op0=ALU.mult),
                         reads=["bmax"], writes=["bmid"])
                    S.op("vector", lambda e: e.tensor_scalar(out=bsteps[:], in0=pw2[:], scalar1=bmax[:, 0:1], scalar2=None, op0=ALU.mult),
                         reads=["pw2", "bmax"], writes=["bsteps"])
                    S.op("vector", lambda e: e.tensor_scalar(out=bnegh[:], in0=bsteps[:], scalar1=-0.5, scalar2=None, op0=ALU.mult),
                         reads=["bsteps"], writes=["bnegh"])
                    for it_ in range(KBIS):
                        S.op("vector", lambda e, N=N: e.tensor_scalar(out=xT[:].rearrange("p k n -> p (k n)").bitcast(mybir.dt.uint8)[:, 0:N], in0=SC[:, 0:N], scalar1=bmid[:, 0:1], scalar2=0.0,
                                                                       op0=ALU.is_ge, op1=ALU.add, accum_out=bcnt[:, 0:1]),
                             reads=["SC", "bmid"], writes=["xT", "bcnt"])
                        S.op("vector", lambda e, it_=it_: e.tensor_scalar(out=bd[:], in0=bcnt[:], scalar1=c256[:, 0:1], scalar2=bsteps[:, it_:it_ + 1],
                                                                          op0=ALU.is_ge, op1=ALU.mult),
                             reads=["bcnt", "c256", "bsteps"], writes=["bd"])
                        sc2 = bnegh[:, it_:it_ + 1] if it_ < KBIS - 1 else bnegl[:, 0:1]
                        if it_ == KBIS - 1:
                            S.op("vector", lambda e, it_=it_: e.tensor_scalar(out=bnegl[:], in0=bsteps[:, it_:it_ + 1], scalar1=-1.0, scalar2=None, op0=ALU.mult),
                                 reads=["bsteps"], writes=["bnegl"])
                        S.op("vector", lambda e, sc2=sc2: e.tensor_scalar(out=bmid[:], in0=bmid[:], scalar1=bd[:, 0:1], scalar2=sc2,
                                                                          op0=ALU.add, op1=ALU.add),
                             reads=["bmid", "bd", "bnegh", "bnegl"], writes=["bmid"])

            def tile_mask(c, t):
                qt = 4 * c + t
                N = 128 * (qt + 1)
                for kt in range(qt + 1):
                    mi = kt % 2
                    if N > 256:
                        S.op("vector", lambda e, kt=kt, mi=mi: e.tensor_scalar(out=m128[mi][:], in0=SC[:, kt * 128:(kt + 1) * 128],
                                                                               scalar1=bmid[:, 0:1], scalar2=None, op0=ALU.is_ge),
                             reads=["SC", "bmid"], writes=[f"m128_{mi}"])
                    else:
                        S.op("vector", lambda e, kt=kt, mi=mi: e.tensor_scalar(out=m128[mi][:], in0=SC[:, kt * 128:(kt + 1) * 128],
                                                                               scalar1=-0.5e30, scalar2=None, op0=ALU.is_ge),
                             reads=["SC"], writes=[f"m128_{mi}"])
                    ti = tpr.next()
                    S.op("tensor", lambda e, mi=mi, ti=ti: e.transpose(out=tp[ti][:, 0:128], in_=m128[mi][:], identity=ident_b[:]),
                         reads=[f"m128_{mi}", "ident_b"], writes=[f"tp{ti}"])
                    S.op("scalar", lambda e, kt=kt, t=t, ti=ti: e.activation(out=maskTs[c % 2][:, kt, t * 128:(t + 1) * 128], in_=tp[ti][:, 0:128], func=AF.Copy),
                         reads=[f"tp{ti}", f"maskT{c % 2}"], writes=[f"maskT{c % 2}"])


            def att_head(c, h, use_dve=False, look=1):
                nkt = 4 * c + 4
                hp, h2 = h // 2, h % 2
                qi = h % 2
                oi = h % 2
                bi = mmr.next()
                S.op("tensor", lambda e, hp=hp, h2=h2, bi=bi: e.matmul(mm[bi][:], lhsT=wuk_sb[h2 * 64:(h2 + 1) * 64, hp, :],
                                                                    rhs=qTs[c % 2][h2 * 64:(h2 + 1) * 64, hp, :], start=True, stop=True),
                     reads=["wuk_sb", f"qT{c % 2}"], writes=[f"mm{bi}"])
                S.op("scalar", lambda e, bi=bi, qi=qi: e.activation(out=qlat[qi][:], in_=mm[bi][:], func=AF.Copy, scale=0.125),
                     reads=[f"mm{bi}"], writes=[f"qlat{qi}"])
                def qk(kt):
                    j0 = max(0, kt - 4 * c)
                    ncol = (4 - j0) * 128
                    c0 = j0 * 128
                    bi = mmr.next()
                    S.op("tensor", lambda e, kt=kt, bi=bi, qi=qi, c0=c0, ncol=ncol: e.matmul(
                        mm[bi][:, 0:ncol], lhsT=ckvT[:, kt * 128:(kt + 1) * 128], rhs=qlat[qi][:, c0:c0 + ncol], start=True, stop=True),
                        reads=["ckvT", f"qlat{qi}"], writes=[f"mm{bi}"])
                    return bi, j0, ncol, c0
                pend = [qk(k_) for k_ in range(min(look, nkt))]
                for kt in range(nkt):
                    bi, j0, ncol, c0 = pend.pop(0)
                    if kt + look < nkt:
                        pend.append(qk(kt + look))
                    ei = kt % (3 if look > 1 else 2)
                    S.op("scalar", lambda e, bi=bi, ei=ei, ncol=ncol: e.activation(out=Eb[ei][:, 0:ncol], in_=mm[bi][:, 0:ncol], func=AF.Exp),
                         reads=[f"mm{bi}"], writes=[f"Eb{ei}"])
                    S.op("vector" if (use_dve and kt % 2 == 0) else "gpsimd", lambda e, ei=ei, kt=kt, c0=c0, ncol=ncol, c=c: e.tensor_tensor(
                        out=Pb[ei][:, 0:ncol], in0=Eb[ei][:, 0:ncol], in1=maskTs[c % 2][:, kt, c0:c0 + ncol], op=ALU.mult),
                        reads=[f"Eb{ei}", f"maskT{c % 2}"], writes=[f"Pb{ei}"])
                    def pv(e, kt=kt, j0=j0, ei=ei, oi=oi, c=c):
                        ins = None
                        for j in range(j0, 4):
                            bank, off = (0, j * 129) if j < 3 else (1, 0)
                            ins = e.matmul(oacc[oi][:, bank, off:off + 129], lhsT=Pb[ei][:, (j - j0) * 128:(j - j0 + 1) * 128],
                                           rhs=ckv1[:, kt, 0:129], start=(kt == 0 and j in (0, 3)), stop=(kt == 4 * c + j),
                                           skip_group_check=True)
                        return ins
                    S.op("tensor", pv, reads=[f"Pb{ei}", "ckv1"], writes=[f"oacc{oi}"])
                ti = tpr.next()
                for j in range(4):
                    bank, off = (0, j * 129) if j < 3 else (1, 0)
                    li = j % 2
                    S.op("scalar", lambda e, oi=oi, bank=bank, off=off, j=j: e.activation(out=rden[:, j:j + 1], in_=oacc[oi][:, bank, off + 128:off + 129], func=AF.Ln),
                         reads=[f"oacc{oi}", "rden"], writes=["rden"])
                    S.op("scalar", lambda e, j=j: e.activation(out=rden[:, j:j + 1], in_=rden[:, j:j + 1], func=AF.Exp, scale=-1.0),
                         reads=["rden"], writes=["rden"])
                    S.op("scalar", lambda e, oi=oi, bank=bank, off=off, j=j, li=li: e.activation(
                        out=olat[li][:], in_=oacc[oi][:, bank, off:off + 128], func=AF.Copy, scale=rden[:, j:j + 1]),
                        reads=[f"oacc{oi}", "rden"], writes=[f"olat{li}"])
                    S.op("tensor", lambda e, li=li, ti=ti, j=j: e.transpose(out=tp[ti][:, j * 128:(j + 1) * 128], in_=olat[li][:], identity=ident_b[:]),
                         reads=[f"olat{li}", "ident_b"], writes=[f"tp{ti}"])
                S.op("scalar", lambda e, ti=ti, h2=h2: e.activation(out=olT2[:, h2, :], in_=tp[ti][:, 0:512], func=AF.Copy),
                     reads=[f"tp{ti}", "olT2"], writes=["olT2"])
                if h2 == 1:
                    bi = mmr.next()
                    def f(e, hp=hp, bi=bi):
                        e.matmul(mm[bi][:], lhsT=wuv_sb[:, hp, 0, :], rhs=olT2[:, 0, :], start=True, stop=False)
                        return e.matmul(mm[bi][:], lhsT=wuv_sb[:, hp, 1, :], rhs=olT2[:, 1, :], start=False, stop=True)
                    S.op("tensor", f, reads=["wuv_sb", "olT2"], writes=[f"mm{bi}"])
                    S.op("scalar", lambda e, hp=hp, bi=bi, c=c: e.activation(out=mixTs[c % 2][:, 4 + hp, :], in_=mm[bi][:], func=AF.Copy),
                         reads=[f"mm{bi}", f"mixT{c % 2}"], writes=[f"mixT{c % 2}"])


            def out_ln(c):
                if DEBUG == "1a" and c == 0:
                    dump(S, "qT", qTs[0][:].rearrange("p k n -> p (k n)"), [128, 4 * CH], BF16, ["qT0"])
                    dump(S, "qiT", qiT[:].rearrange("p k n -> p (k n)"), [128, 4 * CH], BF16, ["qiT"])
                    dump(S, "maskT", maskTs[0][:, 0:4, :].rearrange("p k n -> p (k n)"), [128, 4 * CH], mybir.dt.uint8, ["maskT0"])
                    dump(S, "olT2", olT2[:].rearrange("p k n -> p (k n)"), [128, 2 * CH], BF16, ["olT2"])
                    dump(S, "SC", SC[:, 0:512], [128, 512], F32, ["SC"])
                if DEBUG == "1a" and c == 7:
                    dump(S, "ckv1", ckv1[:].rearrange("p k n -> p (k n)"), [128, NT * 130], BF16, ["ckv1"])
                    dump(S, "ckvT", ckvT[:], [128, T], BF16, ["ckvT"])
                    dump(S, "kiT", kiT[:], [128, T], BF16, ["kiT"])
                    dump(S, "widx", widx[:].rearrange("p k n -> p (k n)"), [128, NT * 8], F32, ["widx"])
                if DEBUG == "1a":
                    S.dma("sync", lambda e, c=c: e.dma_start(out=dbg["mixT"][c], in_=mixTs[c % 2][:].rearrange("p k n -> p (k n)")),
                          reads=[f"mixT{c % 2}"], key="dbgm")

                for t in range(4):
                    tg = 4 * c + t
                    b = tg % 2
                    S.dma("sync", lambda e, tg=tg, b=b: e.dma_start(out=xt[b][:], in_=x_d[tg * 128:(tg + 1) * 128, :]),
                          writes=[f"xt{b}"], key=f"x{b}")
                    for hf in range(2):
                        bi = mmr.next()
                        def f(e, t=t, hf=hf, bi=bi):
                            ins = None
                            for k in range(8):
                                ins = e.matmul(mm[bi][:], lhsT=mixTs[c % 2][:, k, t * 128:(t + 1) * 128], rhs=w_o_sb[:, k, hf * 512:(hf + 1) * 512],
                                               start=(k == 0), stop=(k == 7))
                            return ins
                        S.op("tensor", f, reads=[f"mixT{c % 2}", "w_o_sb"], writes=[f"mm{bi}"])
                        S.op("vector", lambda e, b=b, hf=hf, bi=bi: e.scalar_tensor_tensor(
                            out=r1[:, hf * 512:(hf + 1) * 512], in0=xt[b][:, hf * 512:(hf + 1) * 512], scalar=ALPHA, in1=mm[bi][:],
                            op0=ALU.mult, op1=ALU.add),
                            reads=[f"xt{b}", f"mm{bi}", "r1"], writes=["r1"])
                    layer_norm_tile(r1[:], h1o[b][:], g1_bc, b1_bc, bst, bmv, brs, r1[:], ("r1", "r1", "r1"))
                    S.dma("sync", lambda e, tg=tg, b=b: e.dma_start(out=h1_d[tg * 128:(tg + 1) * 128, :], in_=h1o[b][:]),
                          reads=["r1"], key="h1s0")
                    if DEBUG == "1a":
                        S.dma("sync", lambda e, tg=tg, b=b: e.dma_start(out=dbg["h"][tg * 128:(tg + 1) * 128, :], in_=h1o[b][:]),
                              reads=["r1"], key="dbgh0")

            for c in range(NCH):
                front(c)
                for t in range(4):
                    tile_sel(c, t)
                    if c > 0:
                        att_head(c - 1, 2 * t)
                        att_head(c - 1, 2 * t + 1)
                    tile_mask(c, t)
                if c > 0:
                    out_ln(c - 1)
            S.full_barrier()
            mm.append(tp[1][:].bitcast(F32))
            mmr.items = [0, 1, 2]
            tpr.items = [0]
            for h in range(8):
                att_head(NCH - 1, h, use_dve=True, look=2)
            out_ln(NCH - 1)
            S.full_barrier()
            S.flush(nc)

        if DEBUG == "1a":
            return nc

        with ExitStack() as p2:
            w_mq_sb = sb(p2, "w_mq_sb", [128, 8, D], BF16)
            w_mo_sb = sb(p2, "w_mo_sb", [128, 8, D], BF16)
            w_mkv_sb = sb(p2, "w_mkv_sb", [128, 8, 2 * D], BF16)
            mb = sb(p2, "mb", [128, 2, D], BF16)
            memT = sb(p2, "memT", [128, 8, 256], BF16)
            KmT = sb(p2, "KmT", [128, 8, 256], BF16)
            Vm = sb(p2, "Vm", [128, 2, D], BF16)
            g2_bc = sb(p2, "g2_bc", [128, D], F32)
            b2_bc = sb(p2, "b2_bc", [128, D], F32)
            wr_sb = sb(p2, "wr_sb", [128, 8, NE], F32)
            br_bc = sb(p2, "br_bc", [128, NE], F32)
            ustr_f = sb(p2, "ustr_f", [128, 128], F32)
            ustr_b = sb(p2, "ustr_b", [128, 128], BF16)
            cb_i = sb(p2, "cb_i", [128, NE], I32)
            cbase = sb(p2, "cbase", [128, NE], F32)
            caphi = sb(p2, "caphi", [128, NE], F32)
            h1cs = [sb(p2, f"h1c{i}", [128, 4, D], F32) for i in range(2)]
            h1b = [sb(p2, f"h1b{i}", [128, D], BF16) for i in range(2)]
            hTs = [sb(p2, f"hT{i}", [128, 8, CH], BF16) for i in range(2)]
            qmTs = [sb(p2, f"qmT{i}", [128, 8, CH], BF16) for i in range(2)]
            Pm = [sb(p2, f"Pm{i}", [128, CH], BF16) for i in range(2)]
            rdn = sb(p2, "rdn", [128, CH], F32)
            omTs = [sb(p2, f"omT{i}", [128, 8, CH], BF16) for i in range(2)]
            r2 = sb(p2, "r2", [128, D], F32)
            tmpn2 = sb(p2, "tmpn2", [128, D], F32)
            h2o = [sb(p2, f"h2o{i}", [128, D], F32) for i in range(2)]
            h2b = [sb(p2, f"h2b{i}", [128, D], BF16) for i in range(2)]
            h2T = sb(p2, "h2T", [128, 8, 128], F32)
            lg = sb(p2, "lg", [128, NE], F32)
            mx8r = sb(p2, "mx8r", [128, 8], F32)
            negm = sb(p2, "negm", [128, 1], F32)
            ex4 = sb(p2, "ex4", [128, 4], F32)
            gsum = sb(p2, "gsum", [128, 1], F32)
            selb = sb(p2, "selb", [128, NE], BF16)
            slotm = sb(p2, "slotm", [128, NE], F32)
            ohp = sb(p2, "ohp", [128, NE], F32)
            slotf = sb(p2, "slotf", [128, 4], F32)
            bst2 = sb(p2, "bst2", [128, 2, 6], F32)
            bmv2 = sb(p2, "bmv2", [128, 2], F32)
            brs2 = sb(p2, "brs2", [128, 1], F32)
            mm = [ps(p2, f"mmB{i}", [128, 512], F32) for i in range(6)]
            tp = [ps(p2, f"tpB{i}", [128, 1024], BF16) for i in range(2)]
            mmr = Rot(list(range(6))); tpr = Rot([0, 1])

            S.dma("gpsimd", lambda e: e.dma_start(out=w_mkv_sb[:], in_=w_mkv_d.rearrange("(k p) n -> p k n", p=128)), writes=["w_mkv_sb"], key="w0")
            S.dma("gpsimd", lambda e: e.dma_start(out=mb[:], in_=mem_d.rearrange("(t p) d -> p t d", p=128)), writes=["mb"], key="w1")
            S.dma("gpsimd", lambda e: e.dma_start(out=w_mq_sb[:], in_=w_mq_d.rearrange("(k p) n -> p k n", p=128)), writes=["w_mq_sb"], key="w2")
            S.dma("gpsimd", lambda e: e.dma_start(out=w_mo_sb[:], in_=w_mo_d.rearrange("(k p) n -> p k n", p=128)), writes=["w_mo_sb"], key="w4")
            S.dma("sync", lambda e: e.dma_start(out=g2_bc[:], in_=bc(ln2g_d, D)), writes=["g2_bc"], key="c1")
            S.dma("sync", lambda e: e.dma_start(out=b2_bc[:], in_=bc(ln2b_d, D)), writes=["b2_bc"], key="c2")
            S.dma("sync", lambda e: e.dma_start(out=wr_sb[:], in_=w_r_d.rearrange("(k p) n -> p k n", p=128)), writes=["wr_sb"], key="c3")
            S.dma("sync", lambda e: e.dma_start(out=br_bc[:], in_=bc(b_r_d, NE)), writes=["br_bc"], key="c4")
            S.op("gpsimd", lambda e: e.memset(ustr_f[:], 1.0), writes=["ustr_f"])
            S.op("gpsimd", lambda e: e.affine_select(out=ustr_f[:], in_=ustr_f[:], pattern=[[1, 128]], compare_op=ALU.is_gt, fill=0.0,
                                                       base=0, channel_multiplier=-1), reads=["ustr_f"], writes=["ustr_f"])
            S.op("vector", lambda e: e.tensor_copy(out=ustr_b[:], in_=ustr_f[:]), reads=["ustr_f"], writes=["ustr_b"])
            S.op("gpsimd", lambda e: e.iota(out=cb_i[:], pattern=[[CAP, NE]], base=0, channel_multiplier=0), writes=["cb_i"])
            S.op("vector", lambda e: e.tensor_copy(out=cbase[:], in_=cb_i[:]), reads=["cb_i"], writes=["cbase"])
            S.op("vector", lambda e: e.tensor_scalar(out=caphi[:], in0=cbase[:], scalar1=float(CAP - 1), scalar2=None, op0=ALU.add),
                 reads=["cbase"], writes=["caphi"])
            for mt in range(2):
                for kh in range(2):
                    ti = tpr.next()
                    def tr(e, mt=mt, kh=kh, ti=ti):
                        ins = None
                        for kk in range(4):
                            k = kh * 4 + kk
                            ins = e.transpose(out=tp[ti][:, kk * 128:(kk + 1) * 128], in_=mb[:, mt, k * 128:(k + 1) * 128], identity=ident_b[:])
                        return ins
                    S.op("tensor", tr, reads=["mb", "ident_b"], writes=[f"tp{ti}"])
                    S.op("vector", lambda e, mt=mt, kh=kh, ti=ti: e.tensor_copy(
                        out=memT[:, kh * 4:(kh + 1) * 4, mt * 128:(mt + 1) * 128], in_=tp[ti][:, 0:512].rearrange("p (k n) -> p k n", k=4)),
                        reads=[f"tp{ti}", "memT"], writes=["memT"])
            for cc in range(8):
                bi = mmr.next()
                def f(e, cc=cc, bi=bi):
                    ins = None
                    for k in range(8):
                        ins = e.matmul(mm[bi][:, 0:256], lhsT=w_mkv_sb[:, k, cc * 128:(cc + 1) * 128], rhs=memT[:, k, :], start=(k == 0), stop=(k == 7))
                    return ins
                S.op("tensor", f, reads=["w_mkv_sb", "memT"], writes=[f"mm{bi}"])
                S.op("vector", lambda e, cc=cc, bi=bi: e.tensor_copy(out=KmT[:, cc, :], in_=mm[bi][:, 0:256]), reads=[f"mm{bi}", "KmT"], writes=["KmT"])
            for mt in range(2):
                for hf in range(2):
                    bi = mmr.next()
                    def f(e, mt=mt, hf=hf, bi=bi):
                        ins = None
                        for k in range(8):
                            ins = e.matmul(mm[bi][:], lhsT=memT[:, k, mt * 128:(mt + 1) * 128], rhs=w_mkv_sb[:, k, D + hf * 512:D + (hf + 1) * 512],
                                           start=(k == 0), stop=(k == 7))
                        return ins
                    S.op("tensor", f, reads=["w_mkv_sb", "memT"], writes=[f"mm{bi}"])
                    S.op("scalar", lambda e, mt=mt, hf=hf, bi=bi: e.activation(out=Vm[:, mt, hf * 512:(hf + 1) * 512], in_=mm[bi][:], func=AF.Copy),
                         reads=[f"mm{bi}", "Vm"], writes=["Vm"])

            def stageA(c, part):
                p_ = c % 2
                h1c, hT, qmT, omT = h1cs[p_], hTs[p_], qmTs[p_], omTs[p_]
                h1n, hTn, qmn, omn = f"h1c{p_}", f"hT{p_}", f"qmT{p_}", f"omT{p_}"
                if part == 0:
                    S.dma("sync", lambda e, c=c, h1c=h1c: e.dma_start(out=h1c[:], in_=h1_d[c * CH:(c + 1) * CH, :].rearrange("(t p) d -> p t d", p=128)),
                          writes=[h1n], key=f"h1l{p_}")
                for t in ([0, 1] if part == 0 else [2, 3] if part == 1 else []):
                    b = t % 2
                    S.op("scalar", lambda e, b=b, t=t: e.activation(out=h1b[b][:], in_=h1c[:, t, :], func=AF.Copy),
                         reads=[h1n], writes=[f"h1b{b}"])
                    for kh in range(2):
                        ti = tpr.next()
                        def tr(e, b=b, kh=kh, ti=ti):
                            ins = None
                            for kk in range(4):
                                k = kh * 4 + kk
                                ins = e.transpose(out=tp[ti][:, kk * 128:(kk + 1) * 128], in_=h1b[b][:, k * 128:(k + 1) * 128], identity=ident_b[:])
                            return ins
                        S.op("tensor", tr, reads=[f"h1b{b}", "ident_b"], writes=[f"tp{ti}"])
                        S.op("vector", lambda e, kh=kh, ti=ti, t=t: e.tensor_copy(
                            out=hT[:, kh * 4:(kh + 1) * 4, t * 128:(t + 1) * 128], in_=tp[ti][:, 0:512].rearrange("p (k n) -> p k n", k=4)),
                            reads=[f"tp{ti}", hTn], writes=[hTn])
                for cc in (range(8) if part == 2 else []):
                    bi = mmr.next()
                    def f(e, cc=cc, bi=bi):
                        ins = None
                        for k in range(8):
                            ins = e.matmul(mm[bi][:], lhsT=w_mq_sb[:, k, cc * 128:(cc + 1) * 128], rhs=hT[:, k, :], start=(k == 0), stop=(k == 7))
                        return ins
                    S.op("tensor", f, reads=["w_mq_sb", hTn], writes=[f"mm{bi}"])
                    S.op("scalar", lambda e, cc=cc, bi=bi: e.activation(out=qmT[:, cc, :], in_=mm[bi][:], func=AF.Copy, scale=1.0 / 16),
                         reads=[f"mm{bi}", qmn], writes=[qmn])
                for h in (range(4) if part == 3 else []):
                    for mt in range(2):
                        bi = mmr.next()
                        def f(e, h=h, mt=mt, bi=bi):
                            e.matmul(mm[bi][:], lhsT=KmT[:, 2 * h, mt * 128:(mt + 1) * 128], rhs=qmT[:, 2 * h, :], start=True, stop=False)
                            return e.matmul(mm[bi][:], lhsT=KmT[:, 2 * h + 1, mt * 128:(mt + 1) * 128], rhs=qmT[:, 2 * h + 1, :], start=False, stop=True)
                        S.op("tensor", f, reads=["KmT", qmn], writes=[f"mm{bi}"])
                        S.op("scalar", lambda e, mt=mt, bi=bi: e.activation(out=Pm[mt][:], in_=mm[bi][:], func=AF.Exp),
                             reads=[f"mm{bi}"], writes=[f"Pm{mt}"])
                    bi = mmr.next()
                    def f(e, bi=bi):
                        e.matmul(mm[bi][:], lhsT=ones_b[:], rhs=Pm[0][:], start=True, stop=False)
                        return e.matmul(mm[bi][:], lhsT=ones_b[:], rhs=Pm[1][:], start=False, stop=True)
                    S.op("tensor", f, reads=["ones_b", "Pm0", "Pm1"], writes=[f"mm{bi}"])
                    S.op("vector", lambda e, bi=bi: e.reciprocal(out=rdn[:], in_=mm[bi][:]), reads=[f"mm{bi}"], writes=["rdn"])
                    for dvc in range(2):
                        bi = mmr.next()
                        def f(e, h=h, dvc=dvc, bi=bi):
                            c0 = h * 256 + dvc * 128
                            e.matmul(mm[bi][:], lhsT=Vm[:, 0, c0:c0 + 128], rhs=Pm[0][:], start=True, stop=False)
                            return e.matmul(mm[bi][:], lhsT=Vm[:, 1, c0:c0 + 128], rhs=Pm[1][:], start=False, stop=True)
                        S.op("tensor", f, reads=["Vm", "Pm0", "Pm1"], writes=[f"mm{bi}"])
                        S.op("vector", lambda e, h=h, dvc=dvc, bi=bi: e.tensor_tensor(out=omT[:, 2 * h + dvc, :], in0=mm[bi][:], in1=rdn[:], op=ALU.mult),
                             reads=[f"mm{bi}", "rdn", omn], writes=[omn])

            def outproj_ln(c, t):
                p_ = c % 2
                h1c, hT, qmT, omT = h1cs[p_], hTs[p_], qmTs[p_], omTs[p_]
                h1n, hTn, qmn, omn = f"h1c{p_}", f"hT{p_}", f"qmT{p_}", f"omT{p_}"
                tg = 4 * c + t
                b = tg % 2
                for hf in range(2):
                    bi = mmr.next()
                    def f(e, t=t, hf=hf, bi=bi):
                        ins = None
                        for k in range(8):
                            ins = e.matmul(mm[bi][:], lhsT=omT[:, k, t * 128:(t + 1) * 128], rhs=w_mo_sb[:, k, hf * 512:(hf + 1) * 512],
                                           start=(k == 0), stop=(k == 7))
                        return ins
                    S.op("tensor", f, reads=[omn, "w_mo_sb"], writes=[f"mm{bi}"])
                    S.op("vector", lambda e, t=t, hf=hf, bi=bi: e.scalar_tensor_tensor(
                        out=r2[:, hf * 512:(hf + 1) * 512], in0=h1c[:, t, hf * 512:(hf + 1) * 512], scalar=ALPHA, in1=mm[bi][:],
                        op0=ALU.mult, op1=ALU.add), reads=[h1n, f"mm{bi}", "r2"], writes=["r2"])
                layer_norm_tile(r2[:], h2o[b][:], g2_bc, b2_bc, bst2, bmv2, brs2, tmpn2[:], ("r2", f"h2o{b}", "tmpn2"))
                S.dma("sync", lambda e, tg=tg, b=b: e.dma_start(out=h2_d[tg * 128:(tg + 1) * 128, :], in_=h2o[b][:]),
                      reads=[f"h2o{b}"], key=f"h2s{b}")
                if DEBUG == "1b":
                    S.dma("sync", lambda e, tg=tg, b=b: e.dma_start(out=dbg["h"][tg * 128:(tg + 1) * 128, :], in_=h2o[b][:]),
                          reads=[f"h2o{b}"], key=f"dbgh{b}")
                S.op("scalar", lambda e, b=b: e.activation(out=h2b[b][:], in_=h2o[b][:], func=AF.Copy), reads=[f"h2o{b}"], writes=[f"h2b{b}"])

            def router(c, t):
                p_ = c % 2
                h1c, hT, qmT, omT = h1cs[p_], hTs[p_], qmTs[p_], omTs[p_]
                h1n, hTn, qmn, omn = f"h1c{p_}", f"hT{p_}", f"qmT{p_}", f"omT{p_}"
                tg = 4 * c + t
                b = tg % 2
                for kh in range(2):
                    bi = mmr.next()
                    def tr(e, b=b, kh=kh, bi=bi):
                        ins = None
                        for kk in range(4):
                            k = kh * 4 + kk
                            ins = e.transpose(out=mm[bi][:, kk * 128:(kk + 1) * 128], in_=h2o[b][:, k * 128:(k + 1) * 128], identity=ident_f[:])
                        return ins
                    S.op("tensor", tr, reads=[f"h2o{b}", "ident_f"], writes=[f"mm{bi}"])
                    S.op("vector", lambda e, kh=kh, bi=bi: e.tensor_copy(out=h2T[:, kh * 4:(kh + 1) * 4, :],
                                                                          in_=mm[bi][:].rearrange("p (k n) -> p k n", k=4)),
                         reads=[f"mm{bi}", "h2T"], writes=["h2T"])
                bi = mmr.next()
                def f(e, bi=bi):
                    ins = None
                    for k in range(8):
                        ins = e.matmul(mm[bi][:, 0:NE], lhsT=h2T[:, k, :], rhs=wr_sb[:, k, :], start=(k == 0), stop=(k == 7))
                    return ins
                S.op("tensor", f, reads=["h2T", "wr_sb"], writes=[f"mm{bi}"])
                S.op("vector", lambda e, bi=bi: e.tensor_tensor(out=lg[:], in0=mm[bi][:, 0:NE], in1=br_bc[:], op=ALU.add),
                     reads=[f"mm{bi}", "br_bc"], writes=["lg"])
                S.op("vector", lambda e: e.max(out=mx8r[:], in_=lg[:]), reads=["lg"], writes=["mx8r"])
                S.op("vector", lambda e: e.tensor_scalar(out=negm[:], in0=mx8r[:, 0:1], scalar1=-1.0, scalar2=None, op0=ALU.mult),
                     reads=["mx8r"], writes=["negm"])
                S.op("scalar", lambda e: e.activation(out=ex4[:], in_=mx8r[:, 0:4], func=AF.Exp, bias=negm[:, 0:1], accum_out=gsum[:, 0:1]),
                     reads=["mx8r", "negm", "gsum"], writes=["ex4", "gsum"])
                S.op("vector", lambda e: e.reciprocal(out=gsum[:], in_=gsum[:]), reads=["gsum"], writes=["gsum"])
                S.op("vector", lambda e, tg=tg: e.tensor_scalar(out=gates_all[:, tg, :], in0=ex4[:], scalar1=gsum[:, 0:1], scalar2=None, op0=ALU.mult),
                     reads=["ex4", "gsum", "gates_all"], writes=["gates_all"])
                S.op("vector", lambda e: e.tensor_scalar(out=selb[:], in0=lg[:], scalar1=mx8r[:, 3:4], scalar2=None, op0=ALU.is_ge),
                     reads=["lg", "mx8r"], writes=["selb"])
                bi = mmr.next()
                def f(e, bi=bi):
                    e.matmul(mm[bi][:, 0:NE], lhsT=ustr_b[:], rhs=selb[:], start=True, stop=True)
                    return e.matmul(mm[bi][:, 64:64 + NE], lhsT=ones_b[:], rhs=selb[:], start=True, stop=True)
                S.op("tensor", f, reads=["ustr_b", "ones_b", "selb"], writes=[f"mm{bi}"])
                S.op("vector", lambda e, bi=bi: e.tensor_tensor(out=slotm[:], in0=mm[bi][:, 0:NE], in1=cbase[:], op=ALU.add),
                     reads=[f"mm{bi}", "cbase"], writes=["slotm"])
                S.op("vector", lambda e: e.tensor_tensor(out=slotm[:], in0=slotm[:], in1=caphi[:], op=ALU.min),
                     reads=["slotm", "caphi"], writes=["slotm"])
                S.op("vector", lambda e, bi=bi: e.tensor_tensor(out=cbase[:], in0=mm[bi][:, 64:64 + NE], in1=cbase[:], op=ALU.add),
                     reads=[f"mm{bi}", "cbase"], writes=["cbase"])
                for k in range(4):
                    S.op("vector", lambda e, k=k: e.scalar_tensor_tensor(out=ohp[:], in0=lg[:], scalar=mx8r[:, k:k + 1], in1=slotm[:],
                                                                         op0=ALU.is_equal, op1=ALU.mult),
                         reads=["lg", "mx8r", "slotm"], writes=["ohp"])
                    S.op("vector", lambda e, k=k: e.reduce_sum(out=slotf[:, k:k + 1], in_=ohp[:], axis=mybir.AxisListType.X),
                         reads=["ohp", "slotf"], writes=["slotf"])
                S.op("vector", lambda e, tg=tg: e.tensor_copy(out=slots_all[:, tg, :], in_=slotf[:]), reads=["slotf", "slots_all"], writes=["slots_all"])
                for k in range(4):
                    S.dma("gpsimd", lambda e, tg=tg, k=k, b=b: e.indirect_dma_start(
                        out=xg_d, out_offset=bass.IndirectOffsetOnAxis(ap=slots_all[:, tg, k:k + 1], axis=0), in_=h2b[b][:], in_offset=None),
                        reads=["slots_all", f"h2b{b}"], writes=["xg_d"], key=f"sc{k}")

            for part in range(4):
                stageA(0, part)
            for c in range(NCH):
                for t in range(4):
                    outproj_ln(c, t)
                    if t > 0:
                        router(c, t - 1)
                    if c + 1 < NCH:
                        stageA(c + 1, t)
                router(c, 3)
            if DEBUG == "1b":
                dump(S, "slots", slots_all[:].rearrange("p a b -> p (a b)"), [128, NT * 4], I32, ["slots_all"])
                dump(S, "gates", gates_all[:].rearrange("p a b -> p (a b)"), [128, NT * 4], F32, ["gates_all"])
            S.full_barrier()
            S.flush(nc)
        if DEBUG == "1b":
            return nc

        BLKS = [(0, 512), (512, CAP - 512)]
        NTE = CAP // 128
        with ExitStack() as p3:
            wgu = [sb(p3, f"wgu{i}", [128, 8, 2 * D], BF16) for i in range(2)]
            wdn = [sb(p3, f"wdn{i}", [128, 8, D], BF16) for i in range(2)]
            bgr = sb(p3, "bgr", [NE, 2 * D], F32)
            bguT = sb(p3, "bguT", [128, 16, NE], F32)
            bgu7 = sb(p3, "bgu7", [128, 8, NE], F32)
            bdn = [sb(p3, f"bdn{i}", [128, D], F32) for i in range(2)]
            xg = [sb(p3, f"xg{i}", [128, NTE, D], BF16) for i in range(2)]
            xgTs = [sb(p3, f"xgT{i}", [128, 8, CAP], BF16) for i in range(2)]
            actT = sb(p3, "actT", [128, 8, CAP], BF16)
            s0 = [sb(p3, f"s0_{i}", [128, 512], F32) for i in range(2)]
            gcl = [sb(p3, f"gc{i}", [128, 512], F32) for i in range(2)]
            ucl = [sb(p3, f"uc{i}", [128, 512], F32) for i in range(2)]
            ysb = [sb(p3, f"ysb{i}", [128, D], F32) for i in range(2)]
            mm = [ps(p3, f"mmC{i}", [128, 512], F32) for i in range(6)]
            tp = [ps(p3, f"tpC{i}", [128, 1024], BF16) for i in range(2)]
            mmr = Rot(list(range(6))); tpr = Rot([0, 1])

            S.dma("sync", lambda e: e.dma_start(out=bgr[:], in_=b_gu_d), writes=["bgr"], key="c1")
            bi = mmr.next()
            def trb(e, bi=bi):
                ins = None
                for cidx in range(16):
                    ins = e.transpose(out=mm[bi][:, cidx * NE:(cidx + 1) * NE], in_=bgr[0:NE, cidx * 128:(cidx + 1) * 128], identity=ident_f[0:NE, 0:NE])
                return ins
            S.op("tensor", trb, reads=["bgr", "ident_f"], writes=[f"mm{bi}"])
            S.op("vector", lambda e, bi=bi: e.tensor_copy(out=bguT[:].rearrange("p a b -> p (a b)"), in_=mm[bi][:]), reads=[f"mm{bi}"], writes=["bguT"])
            S.op("vector", lambda e: e.tensor_scalar(out=bgu7[:], in0=bguT[:, 8:16, :], scalar1=7.0, scalar2=None, op0=ALU.add),
                 reads=["bguT"], writes=["bgu7"])

            def load_expert(ex):
                bf = ex % 2
                S.dma("gpsimd", lambda e: e.dma_start(out=wgu[bf][:], in_=w_gu_d[ex].rearrange("(k p) n -> p k n", p=128)),
                      writes=[f"wgu{bf}"], key=f"wg{bf}")
                S.dma("gpsimd", lambda e: e.dma_start(out=wdn[bf][:], in_=w_dn_d[ex].rearrange("(k p) n -> p k n", p=128)),
                      writes=[f"wdn{bf}"], key=f"wd{bf}")
                S.dma("sync", lambda e: e.dma_start(out=bdn[bf][:], in_=bc(b_dn_d[ex], D)), writes=[f"bdn{bf}"], key=f"bd{bf}")
                S.dma("sync", lambda e: e.dma_start(out=xg[bf][:], in_=xg_d[ex * CAP:(ex + 1) * CAP, :].rearrange("(t p) d -> p t d", p=128)),
                      reads=["xg_d"], writes=[f"xg{bf}"], key=f"xgl{bf}")

            def prep_expert(exx):
                pb = exx % 2
                for t in range(NTE):
                    for kh in range(2):
                        ti = tpr.next()
                        def tr(e, pb=pb, t=t, kh=kh, ti=ti):
                            ins = None
                            for kk in range(4):
                                k = kh * 4 + kk
                                ins = e.transpose(out=tp[ti][:, kk * 128:(kk + 1) * 128], in_=xg[pb][:, t, k * 128:(k + 1) * 128], identity=ident_b[:])
                            return ins
                        S.op("tensor", tr, reads=[f"xg{pb}", "ident_b"], writes=[f"tp{ti}"])
                        S.op("scalar", lambda e, kh=kh, ti=ti, t=t, pb=pb: e.activation(
                            out=xgTs[pb][:, kh * 4:(kh + 1) * 4, t * 128:(t + 1) * 128], in_=tp[ti][:, 0:512].rearrange("p (k n) -> p k n", k=4), func=AF.Copy),
                            reads=[f"tp{ti}", f"xgT{pb}"], writes=[f"xgT{pb}"])

            load_expert(0)
            prep_expert(0)
            for ex in range(NE_RUN):
                bf = ex % 2
                if ex + 1 < NE_RUN:
                    load_expert(ex + 1)
                it = 0
                for j in range(8 if P2_STEPS >= 2 else 0):
                    for (b0, bw) in BLKS:
                        big = mmr.next(); biu = mmr.next()
                        def fg(e, bf=bf, j=j, b0=b0, bw=bw, big=big):
                            ins = None
                            for k in range(8):
                                ins = e.matmul(mm[big][:, 0:bw], lhsT=wgu[bf][:, k, j * 128:(j + 1) * 128], rhs=xgTs[bf][:, k, b0:b0 + bw],
                                               start=(k == 0), stop=(k == 7))
                            return ins
                        def fu(e, bf=bf, j=j, b0=b0, bw=bw, biu=biu):
                            ins = None
                            for k in range(8):
                                ins = e.matmul(mm[biu][:, 0:bw], lhsT=wgu[bf][:, k, D + j * 128:D + (j + 1) * 128], rhs=xgTs[bf][:, k, b0:b0 + bw],
                                               start=(k == 0), stop=(k == 7))
                            return ins
                        S.op("tensor", fg, reads=[f"wgu{bf}", f"xgT{bf}"], writes=[f"mm{big}"])
                        S.op("tensor", fu, reads=[f"wgu{bf}", f"xgT{bf}"], writes=[f"mm{biu}"])
                        i2 = it % 2; it += 1
                        S.op("vector", lambda e, big=big, bw=bw, j=j, ex=ex, i2=i2: e.tensor_scalar(
                            out=gcl[i2][:, 0:bw], in0=mm[big][:, 0:bw], scalar1=bguT[:, j, ex:ex + 1], scalar2=7.0, op0=ALU.add, op1=ALU.min),
                            reads=[f"mm{big}", "bguT"], writes=[f"gc{i2}"])
                        S.op("scalar", lambda e, bw=bw, i2=i2: e.activation(out=s0[i2][:, 0:bw], in_=gcl[i2][:, 0:bw], func=AF.Silu, scale=1.702),
                             reads=[f"gc{i2}"], writes=[f"s0_{i2}"])
                        S.op("scalar", lambda e, biu=biu, bw=bw, j=j, ex=ex, i2=i2: e.activation(
                            out=ucl[i2][:, 0:bw], in_=mm[biu][:, 0:bw], func=AF.Relu, bias=bgu7[:, j, ex:ex + 1]),
                            reads=[f"mm{biu}", "bgu7"], writes=[f"uc{i2}"])
                        S.op("vector", lambda e, bw=bw, i2=i2: e.tensor_scalar(
                            out=ucl[i2][:, 0:bw], in0=ucl[i2][:, 0:bw], scalar1=14.0, scalar2=-6.0, op0=ALU.min, op1=ALU.add),
                            reads=[f"uc{i2}"], writes=[f"uc{i2}"])
                        S.op("vector", lambda e, bw=bw, b0=b0, j=j, i2=i2: e.scalar_tensor_tensor(
                            out=actT[:, j, b0:b0 + bw], in0=s0[i2][:, 0:bw], scalar=1.0 / 1.702, in1=ucl[i2][:, 0:bw], op0=ALU.mult, op1=ALU.mult),
                            reads=[f"s0_{i2}", f"uc{i2}", "actT"], writes=["actT"])
                if ex + 1 < NE_RUN:
                    prep_expert(ex + 1)
                for t in range(NTE if P2_STEPS >= 4 else 0):
                    yb = t % 2
                    for hf in range(2):
                        bi = mmr.next()
                        def fd(e, bf=bf, t=t, hf=hf, bi=bi):
                            ins = None
                            for j in range(8):
                                ins = e.matmul(mm[bi][:], lhsT=actT[:, j, t * 128:(t + 1) * 128], rhs=wdn[bf][:, j, hf * 512:(hf + 1) * 512],
                                               start=(j == 0), stop=(j == 7))
                            return ins
                        S.op("tensor", fd, reads=["actT", f"wdn{bf}"], writes=[f"mm{bi}"])
                        S.op("vector", lambda e, bf=bf, yb=yb, hf=hf, bi=bi: e.tensor_tensor(
                            out=ysb[yb][:, hf * 512:(hf + 1) * 512], in0=mm[bi][:], in1=bdn[bf][:, hf * 512:(hf + 1) * 512], op=ALU.add),
                            reads=[f"mm{bi}", f"bdn{bf}", f"ysb{yb}"], writes=[f"ysb{yb}"])
                    r0 = ex * CAP + t * 128
                    S.dma("sync", lambda e, yb=yb, r0=r0: e.dma_start(out=ys_d[r0:r0 + 128, :], in_=ysb[yb][:]),
                          reads=[f"ysb{yb}"], writes=["ys_d"], key=f"yst{yb}")
            S.full_barrier()
            S.flush(nc)
        if DEBUG == "2":
            return nc

        with ExitStack() as p4:
            g3_bc = sb(p4, "g3_bc", [128, D], F32)
            b3_bc = sb(p4, "b3_bc", [128, D], F32)
            h2t = [sb(p4, f"h2t{i}", [128, D], F32) for i in range(2)]
            yk = [[sb(p4, f"yk{i}_{k}", [128, D], F32) for k in range(4)] for i in range(2)]
            acc = sb(p4, "acc", [128, D], F32)
            tmpn3 = sb(p4, "tmpn3", [128, D], F32)
            outt = [sb(p4, f"outt{i}", [128, D], F32) for i in range(2)]
            bst3 = sb(p4, "bst3", [128, 2, 6], F32)
            bmv3 = sb(p4, "bmv3", [128, 2], F32)
            brs3 = sb(p4, "brs3", [128, 1], F32)
            S.dma("sync", lambda e: e.dma_start(out=g3_bc[:], in_=bc(ln3g_d, D)), writes=["g3_bc"], key="c1")
            S.dma("sync", lambda e: e.dma_start(out=b3_bc[:], in_=bc(ln3b_d, D)), writes=["b3_bc"], key="c2")
            for tg in range(NT):
                b = tg % 2
                S.dma("sync", lambda e, tg=tg, b=b: e.dma_start(out=h2t[b][:], in_=h2_d[tg * 128:(tg + 1) * 128, :]),
                      writes=[f"h2t{b}"], key=f"h2l{b}")
                for k in range(4):
                    S.dma("gpsimd", lambda e, tg=tg, k=k, b=b: e.indirect_dma_start(
                        out=yk[b][k][:], out_offset=None, in_=ys_d, in_offset=bass.IndirectOffsetOnAxis(ap=slots_all[:, tg, k:k + 1], axis=0)),
                        reads=["ys_d", "slots_all"], writes=[f"yk{b}_{k}"], key=f"gk{b}{k}")
                S.op("vector", lambda e, b=b: e.tensor_scalar(out=acc[:], in0=h2t[b][:], scalar1=ALPHA, scalar2=None, op0=ALU.mult),
                     reads=[f"h2t{b}", "acc"], writes=["acc"])
                for k in range(4):
                    S.op("vector", lambda e, b=b, k=k, tg=tg: e.scalar_tensor_tensor(
                        out=acc[:], in0=yk[b][k][:], scalar=gates_all[:, tg, k:k + 1], in1=acc[:], op0=ALU.mult, op1=ALU.add),
                        reads=[f"yk{b}_{k}", "gates_all", "acc"], writes=["acc"])
                layer_norm_tile(acc[:], outt[b][:], g3_bc, b3_bc, bst3, bmv3, brs3, tmpn3[:], ("acc", f"outt{b}", "tmpn3"))
                S.dma("scalar", lambda e, tg=tg, b=b: e.dma_start(out=out_d[tg * 128:(tg + 1) * 128, :], in_=outt[b][:]),
                      reads=[f"outt{b}"], key=f"os{b}")
            S.full_barrier()
            S.flush(nc)
    return nc


_PROG = None


def kernel(**inputs):
    global _PROG
    if _PROG is None:
        _PROG = build_program()
    nc = _PROG
    B = inputs["x"].shape[0]
    in_maps = []
    for b in range(B):
        m = {}
        for k, v in inputs.items():
            a = np.asarray(v)
            if k in ("x", "mem"):
                m[k] = np.ascontiguousarray(a[b])
            else:
                m[k] = np.ascontiguousarray(a[0])
        in_maps.append(m)
    res = run_bass_kernel_spmd(nc, in_maps, core_ids=list(range(B)))
    return np.stack([np.asarray(r["out"]) for r in res.results], axis=0)
```

```python
import numpy as np
from contextlib import ExitStack
import concourse.bass as bass
import concourse.mybir as mybir
from concourse.bass_utils import run_bass_kernel_spmd

F32 = mybir.dt.float32
BF16 = mybir.dt.bfloat16
I32 = mybir.dt.int32
AF = mybir.ActivationFunctionType
ALU = mybir.AluOpType

T = 4096
NT = 32
D = 1024
CH = 512
NCH = 8
INW = 1736
CAP = 768
NE = 32
ALPHA = 2.0 ** 0.25
NEG = -1.0e30
NEG2 = -3.0e30
KBIS = 25
SIGMAX = float(1.0 / (1.0 + np.exp(-1.702 * 7.0)))

DEBUG = None
NE_RUN = NE
P2_STEPS = 9
P2_EW = 63


class Sched:
    EPOCH = 30000
    ENG = ("sync", "scalar", "vector", "gpsimd", "tensor")

    def __init__(self, sem_pool):
        self.ops = {e: [] for e in self.ENG}
        self.cnt = {}
        self.lastw = {}
        self.readers = {}
        self.seen = {e: {} for e in self.ENG}
        self.sem_pool = list(sem_pool)
        self.sems = {}

    def _sem(self, counter, val):
        if counter.startswith("E:"):
            ep = (val - 1) // self.EPOCH
            name, lv = f"{counter}#{ep}", val - ep * self.EPOCH
        else:
            name, lv = counter, val
        if name not in self.sems:
            self.sems[name] = self.sem_pool.pop()
        return self.sems[name], lv

    def _waits(self, eng, reads, writes, extra=()):
        need = {}
        def add(cv):
            c, v = cv
            if v > need.get(c, 0):
                need[c] = v
        for r in reads:
            if r in self.lastw:
                add(self.lastw[r])
        for w in writes:
            if w in self.lastw:
                add(self.lastw[w])
            for cv in self.readers.get(w, {}).items():
                add(cv)
        for cv in extra:
            add(cv)
        waits = []
        for c, v in need.items():
            if eng == "tensor" and c == "E:tensor":
                continue
            if self.seen[eng].get(c, 0) < v:
                self.seen[eng][c] = v
                waits.append(self._sem(c, v))
        return waits

    def _book(self, c, v, reads, writes):
        for r in reads:
            d = self.readers.setdefault(r, {})
            if d.get(c, 0) < v:
                d[c] = v
        for w in writes:
            self.lastw[w] = (c, v)
            self.readers[w] = {}

    def op(self, eng, fn, reads=(), writes=()):
        waits = self._waits(eng, reads, writes)
        c = "E:" + eng
        v = self.cnt.get(c, 0) + 1
        self.cnt[c] = v
        sem, _ = self._sem(c, v)
        self.ops[eng].append((waits, fn, sem, 1))
        self._book(c, v, reads, writes)

    def dma(self, eng, fn, reads=(), writes=(), key="d", serialize=True):
        c = "D:" + key
        prev = self.cnt.get(c, 0)
        waits = self._waits(eng, reads, writes, extra=[(c, prev)] if (prev and serialize) else [])
        v = prev + 16
        self.cnt[c] = v
        sem, _ = self._sem(c, v)
        self.ops[eng].append((waits, fn, sem, 16))
        self._book(c, v, reads, writes)

    def barrier_all(self, eng="sync"):
        waits = []
        for c, v in self.cnt.items():
            if v and self.seen[eng].get(c, 0) < v:
                self.seen[eng][c] = v
                waits.append(self._sem(c, v))
        self.ops[eng].append((waits, None, None, 0))

    def full_barrier(self):
        for e in self.ENG:
            self.barrier_all(e)

    def flush(self, nc):
        with nc.Block() as blk:
            for eng in self.ENG:
                lst = self.ops[eng]
                if not lst:
                    continue
                def body(e, lst=lst):
                    for waits, fn, sem, inc in lst:
                        for s, v in waits:
                            e.wait_ge(s, v)
                        if fn is not None:
                            ins = fn(e)
                            ins.then_inc(sem, inc)
                getattr(blk, eng)(body)
        self.ops = {e: [] for e in self.ENG}


class Rot:
    def __init__(self, items):
        self.items = items
        self.i = 0
    def next(self):
        it = self.items[self.i % len(self.items)]
        self.i += 1
        return it


def build_program():
    nc = bass.Bass("TRN2", target_bir_lowering=False)
    dt = lambda name, shape, dtype=F32, kind="ExternalInput": nc.dram_tensor(name, shape, dtype, kind=kind).ap()
    x_d = dt("x", [T, D])
    mem_d = dt("mem", [256, D])
    w_in_d = dt("w_in", [D, INW])
    w_pool_d = dt("w_pool", [4, 128, 128])
    pool_scale_d = dt("pool_scale", [512])
    kig_d = dt("idx_k_norm_g", [64])
    kib_d = dt("idx_k_norm_b", [64])
    kvg_d = dt("kv_norm_g", [128])
    w_uk_d = dt("w_uk", [8, 64, 128])
    w_uv_d = dt("w_uv", [8, 128, 64])
    w_o_d = dt("w_o", [D, D])
    ln1g_d = dt("ln1_g", [D]); ln1b_d = dt("ln1_b", [D])
    w_mq_d = dt("w_mq", [D, D])
    w_mkv_d = dt("w_mkv", [D, 2 * D])
    w_mo_d = dt("w_mo", [D, D])
    ln2g_d = dt("ln2_g", [D]); ln2b_d = dt("ln2_b", [D])
    w_r_d = dt("w_router", [D, NE])
    b_r_d = dt("b_router", [NE])
    w_gu_d = dt("w_gate_up", [NE, D, 2 * D])
    b_gu_d = dt("b_gate_up", [NE, 2 * D])
    w_dn_d = dt("w_down", [NE, D, D])
    b_dn_d = dt("b_down", [NE, D])
    ln3g_d = dt("ln3_g", [D]); ln3b_d = dt("ln3_b", [D])
    out_d = dt("out", [T, D], F32, "ExternalOutput")
    h1_d = dt("h1_scr", [T, D], F32, "Internal")
    h2_d = dt("h2_scr", [T, D], F32, "Internal")
    xg_d = dt("xg_scr", [NE * CAP, D], BF16, "Internal")
    ys_d = dt("ys_scr", [NE * CAP, D], F32, "Internal")
    dbg = {}
    if DEBUG:
        dbg["h"] = dt("dbg_h", [T, D], F32, "ExternalOutput")
        dbg["mixT"] = dt("dbg_mixT", [NCH, 128, 8 * CH], BF16, "ExternalOutput")

    dumps = []
    def dump(S, name, ap2d, shape, dtype, reads):
        if not DEBUG:
            return
        d = dt("dbg_" + name, shape, dtype, "ExternalOutput")
        S.dma("sync", lambda e: e.dma_start(out=d, in_=ap2d), reads=reads, key="dbgd")

    def bc(ap1d, n):
        return ap1d.rearrange("(o n) -> o n", o=1).to_broadcast([128, n])

    with ExitStack() as top:
        sem_pool = [top.enter_context(nc.semaphore(f"s{i}")) for i in range(96)]
        S = Sched(sem_pool)
        sb = lambda es, name, shape, dtype=F32: es.enter_context(nc.sbuf_tensor(name, shape, dtype))
        ps = lambda es, name, shape, dtype=F32: es.enter_context(nc.psum_tensor(name, shape, dtype))

        ident_b = sb(top, "ident_b", [128, 128], BF16)
        ident_f = sb(top, "ident_f", [128, 128], F32)
        ones_b = sb(top, "ones_b", [128, 128], BF16)
        slots_all = sb(top, "slots_all", [128, NT, 4], I32)
        gates_all = sb(top, "gates_all", [128, NT, 4], F32)

        S.op("gpsimd", lambda e: e.memset(ident_f[:], 0.0), writes=["ident_f"])
        S.op("gpsimd", lambda e: e.affine_select(out=ident_f[:], in_=ident_f[:], pattern=[[-1, 128]],
                                                   compare_op=ALU.not_equal, fill=1.0, base=0, channel_multiplier=1),
             reads=["ident_f"], writes=["ident_f"])
        S.op("vector", lambda e: e.tensor_copy(out=ident_b[:], in_=ident_f[:]), reads=["ident_f"], writes=["ident_b"])
        S.op("vector", lambda e: e.memset(ones_b[:], 1.0), writes=["ones_b"])

        def ln_rstd(var_ap, rstd_ap, tag, scale, rname, wname):
            S.op("scalar", lambda e: e.activation(out=rstd_ap, in_=var_ap, func=AF.Ln, bias=eps_tile[:, tag:tag + 1], scale=scale),
                 reads=[rname, "eps", wname], writes=[wname])
            S.op("scalar", lambda e: e.activation(out=rstd_ap, in_=rstd_ap, func=AF.Exp, scale=-0.5),
                 reads=[wname], writes=[wname])

        eps_tile = sb(top, "eps", [128, 2], F32)
        S.op("vector", lambda e: e.memset(eps_tile[:, 0:1], 1e-5), writes=["eps"])
        S.op("vector", lambda e: e.memset(eps_tile[:, 1:2], 1e-6), reads=["eps"], writes=["eps"])

        def layer_norm_tile(r_ap, out_ap, g_bc, b_bc, stats, mv, rstd, tmp_ap, names):
            rn, on, tn = names
            for hf in range(2):
                S.op("vector", lambda e, hf=hf: e.bn_stats(out=stats[:, hf, :], in_=r_ap[:, hf * 512:(hf + 1) * 512]),
                     reads=[rn], writes=[stats.name])
            S.op("vector", lambda e: e.bn_aggr(out=mv[:], in_=stats[:].rearrange("p a b -> p (a b)")),
                 reads=[stats.name], writes=[mv.name])
            ln_rstd(mv[:, 1:2], rstd[:, 0:1], 0, 1.0, mv.name, rstd.name)
            S.op("vector", lambda e: e.tensor_scalar(out=tmp_ap, in0=r_ap, scalar1=mv[:, 0:1], scalar2=rstd[:, 0:1],
                                                      op0=ALU.subtract, op1=ALU.mult),
                 reads=[rn, mv.name, rstd.name], writes=[tn])
            S.op("vector", lambda e: e.tensor_tensor(out=tmp_ap, in0=tmp_ap, in1=g_bc[:], op=ALU.mult),
                 reads=[tn, g_bc.name], writes=[tn])
            S.op("vector", lambda e: e.tensor_tensor(out=out_ap, in0=tmp_ap, in1=b_bc[:], op=ALU.add),
                 reads=[tn, b_bc.name], writes=[on])

        with ExitStack() as p1:
            w_in_sb = sb(p1, "w_in_sb", [128, 8, INW], BF16)
            w_o_sb = sb(p1, "w_o_sb", [128, 8, D], BF16)
            wpool_sb = sb(p1, "wpool_sb", [128, 4, 128], BF16)
            wuk_sb = sb(p1, "wuk_sb", [128, 4, 128], BF16)
            wuv_sb = sb(p1, "wuv_sb", [128, 4, 2, 128], BF16)
            pscale_sb = sb(p1, "pscale_sb", [128, 4], F32)
            g1_bc = sb(p1, "g1_bc", [128, D], F32)
            b1_bc = sb(p1, "b1_bc", [128, D], F32)
            kvg_bc = sb(p1, "kvg_bc", [128, 128], F32)
            kig_bc = sb(p1, "kig_bc", [128, 64], F32)
            kib_bc = sb(p1, "kib_bc", [128, 64], F32)
            ic16 = sb(p1, "ic16", [128, 4, 16], F32)
            ckv1 = sb(p1, "ckv1", [128, NT, 130], BF16)
            ckvT = sb(p1, "ckvT", [128, T], BF16)
            kiT = sb(p1, "kiT", [128, T], BF16)
            widx = sb(p1, "widx", [128, NT, 8], F32)
            xt = [sb(p1, f"xt{i}", [128, D], F32) for i in range(2)]
            xb = [sb(p1, f"xb{i}", [128, D], BF16) for i in range(2)]
            xT = sb(p1, "xT", [128, 8, CH], BF16)
            ug = sb(p1, "ug", [128, 528], F32)
            halo = sb(p1, "halo", [128, 4, 16], F32)
            pA = sb(p1, "pA", [128, 528], F32)
            pB = sb(p1, "pB", [128, 528], F32)
            dT = sb(p1, "dT", [128, CH], BF16)
            qTs = [sb(p1, f"qT{i}", [128, 4, CH], BF16) for i in range(2)]
            qiT = sb(p1, "qiT", [128, 4, CH], BF16)
            mixTs = [sb(p1, f"mixT{i}", [128, 8, CH], BF16) for i in range(2)]
            SC = sb(p1, "SC", [128, T], F32)
            bmax = sb(p1, "bmax", [128, 1], F32)
            bmid = sb(p1, "bmid", [128, 1], F32)
            bcnt = sb(p1, "bcnt", [128, 1], F32)
            bd = sb(p1, "bd", [128, 1], F32)
            bnegl = sb(p1, "bnegl", [128, 1], F32)
            c256 = sb(p1, "c256", [128, 1], F32)
            pw2 = sb(p1, "pw2", [128, KBIS], F32)
            bsteps = sb(p1, "bsteps", [128, KBIS], F32)
            bnegh = sb(p1, "bnegh", [128, KBIS], F32)
            m128 = [sb(p1, f"m128_{i}", [128, 128], BF16) for i in range(2)]
            maskTs = [sb(p1, f"maskT{i}", [128, NT, CH], mybir.dt.uint8) for i in range(2)]
            rl = [sb(p1, f"rl{i}", [128, CH], F32) for i in range(2)]
            Eb = [sb(p1, f"Eb{i}", [128, CH], BF16) for i in range(2)]
            Pb = [sb(p1, f"Pb{i}", [128, CH], BF16) for i in range(2)]
            Eb.append(ug[:, 0:256].bitcast(BF16)); Pb.append(pB[:, 0:256].bitcast(BF16))
            qlat = [sb(p1, f"qlat{i}", [128, CH], BF16) for i in range(2)]
            olat = [sb(p1, f"olat{i}", [128, 128], BF16) for i in range(2)]
            rden = sb(p1, "rden", [128, 4], F32)
            ckn = sb(p1, "ckn", [128, 128], BF16)
            kn32 = sb(p1, "kn32", [128, 64], F32)
            kn2 = sb(p1, "kn2", [128, 128], BF16)
            tm = sb(p1, "tm", [128, 200], F32)
            olT2 = sb(p1, "olT2", [128, 2, CH], BF16)
            junk = sb(p1, "junk", [128, 128], F32)
            st1 = sb(p1, "st1", [128, 8], F32)
            bst = sb(p1, "bst", [128, 2, 6], F32)
            bmv = sb(p1, "bmv", [128, 2], F32)
            brs = sb(p1, "brs", [128, 1], F32)
            r1 = sb(p1, "r1", [128, D], F32)
            h1o = [r1, r1]
            mm = [ps(p1, f"mm{i}", [128, 512], F32) for i in range(2)]
            oacc = [ps(p1, f"oacc{i}", [128, 2, 512], F32) for i in range(2)]
            tp = [ps(p1, f"tp{i}", [128, 1024], BF16) for i in range(2)]
            mmr = Rot([0, 1]); tpr = Rot([0, 1])

            S.op("gpsimd", lambda e: e.memset(maskTs[1][:].rearrange("p a b -> p (a b)").bitcast(I32), 0), writes=["maskT1"])
            zsrc = maskTs[1][:].rearrange("p a b -> p (a b)").bitcast(BF16).rearrange("p (t d) -> p t d", d=D)
            S.dma("gpsimd", lambda e: e.dma_start(out=w_in_sb[:], in_=w_in_d.rearrange("(k p) n -> p k n", p=128)),
                  writes=["w_in_sb"], key="w0")
            S.dma("gpsimd", lambda e: e.dma_start(out=wpool_sb[:], in_=w_pool_d.rearrange("g c d -> c g d")),
                  writes=["wpool_sb"], key="w1")
            S.dma("gpsimd", lambda e: e.dma_start(out=wuk_sb[:], in_=w_uk_d.rearrange("(hp h2) d r -> (h2 d) hp r", h2=2)),
                  writes=["wuk_sb"], key="w2")
            S.op("vector", lambda e: e.memset(wuv_sb[:], 0.0), writes=["wuv_sb"])
            for h2 in range(2):
                S.dma("gpsimd", lambda e, h2=h2: e.dma_start(out=wuv_sb[:, :, h2, h2 * 64:(h2 + 1) * 64],
                                                             in_=w_uv_d.rearrange("(hp h2) r d -> h2 r hp d", h2=2)[h2]),
                      reads=["wuv_sb"], writes=["wuv_sb"], key=f"w3{h2}")
            S.dma("gpsimd", lambda e: e.dma_start(out=w_o_sb[:], in_=w_o_d.rearrange("(k p) n -> p k n", p=128)),
                  writes=["w_o_sb"], key="w4")
            S.dma("sync", lambda e: e.dma_start(out=pscale_sb[:], in_=pool_scale_d.rearrange("(g d) -> d g", d=128),
                                                allow_slow_non_contiguous=True),
                  writes=["pscale_sb"], key="c0")
            S.dma("sync", lambda e: e.dma_start(out=g1_bc[:], in_=bc(ln1g_d, D)), writes=["g1_bc"], key="c1")
            S.dma("sync", lambda e: e.dma_start(out=b1_bc[:], in_=bc(ln1b_d, D)), writes=["b1_bc"], key="c2")
            S.dma("sync", lambda e: e.dma_start(out=kvg_bc[:], in_=bc(kvg_d, 128)), writes=["kvg_bc"], key="c3")
            S.dma("sync", lambda e: e.dma_start(out=kig_bc[:], in_=bc(kig_d, 64)), writes=["kig_bc"], key="c4")
            S.dma("sync", lambda e: e.dma_start(out=kib_bc[:], in_=bc(kib_d, 64)), writes=["kib_bc"], key="c5")
            for g in range(4):
                w = 2 ** (g + 1)
                S.op("gpsimd", lambda e, g=g, w=w: e.memset(ic16[:, g, :], 1.0 / w), reads=["ic16"], writes=["ic16"])
                for t in range(w - 1):
                    S.op("gpsimd", lambda e, g=g, t=t: e.memset(ic16[:, g, t:t + 1], 1.0 / (t + 1)), reads=["ic16"], writes=["ic16"])
            S.op("gpsimd", lambda e: e.memset(halo[:], 0.0), writes=["halo"])
            S.op("gpsimd", lambda e: e.memset(c256[:], 256.0), writes=["c256"])
            for i_ in range(KBIS):
                S.op("gpsimd", lambda e, i_=i_: e.memset(pw2[:, i_:i_ + 1], 2.0 ** (-i_)), reads=["pw2"], writes=["pw2"])
            S.op("gpsimd", lambda e: e.memset(ckv1[:, :, 128:130], 1.0), writes=["ckv1"])

            def front(c):
                for t in range(4):
                    tg = 4 * c + t
                    b = tg % 2
                    S.dma("sync", lambda e, tg=tg, b=b: e.dma_start(out=xt[b][:], in_=x_d[tg * 128:(tg + 1) * 128, :]),
                          writes=[f"xt{b}"], key=f"x{b}")
                    S.op("scalar", lambda e, b=b: e.activation(out=xb[b][:], in_=xt[b][:], func=AF.Copy),
                         reads=[f"xt{b}"], writes=[f"xb{b}"])
                    for kh in range(2):
                        ti = tpr.next()
                        def tr(e, b=b, kh=kh, ti=ti):
                            ins = None
                            for kk in range(4):
                                k = kh * 4 + kk
                                ins = e.transpose(out=tp[ti][:, kk * 128:(kk + 1) * 128], in_=xb[b][:, k * 128:(k + 1) * 128],
                                                  identity=ident_b[:])
                            return ins
                        S.op("tensor", tr, reads=[f"xb{b}", "ident_b"], writes=[f"tp{ti}"])
                        S.op("vector", lambda e, kh=kh, ti=ti, t=t: e.tensor_copy(
                            out=xT[:, kh * 4:(kh + 1) * 4, t * 128:(t + 1) * 128],
                            in_=tp[ti][:, 0:512].rearrange("p (k n) -> p k n", k=4)),
                            reads=[f"tp{ti}"], writes=["xT"])

                def inproj_fm(col0):
                    bi = mmr.next()
                    def f(e, col0=col0, bi=bi):
                        ins = None
                        for k in range(8):
                            ins = e.matmul(mm[bi][:], lhsT=w_in_sb[:, k, col0:col0 + 128], rhs=xT[:, k, :],
                                           start=(k == 0), stop=(k == 7))
                        return ins
                    S.op("tensor", f, reads=["w_in_sb", "xT"], writes=[f"mm{bi}"])
                    return bi

                for g in range(4):
                    bi = inproj_fm(g * 128)
                    S.op("gpsimd", lambda e, g=g: e.tensor_copy(out=ug[:, 0:16], in_=halo[:, g, :]),
                         reads=["halo", "ug"], writes=["ug"])
                    S.op("scalar", lambda e, bi=bi: e.activation(out=ug[:, 16:528], in_=mm[bi][:], func=AF.Copy),
                         reads=[f"mm{bi}", "ug"], writes=["ug"])
                    S.op("gpsimd", lambda e, g=g: e.tensor_copy(out=halo[:, g, :], in_=ug[:, 512:528]),
                         reads=["ug"], writes=["halo"])
                    src, srcn = ug, "ug"
                    bufs = [(pA, "pA"), (pB, "pB")]
                    for lv in range(g + 1):
                        s = 2 ** lv
                        lo = 2 ** (lv + 1) - 1
                        dst, dstn = bufs[lv % 2]
                        S.op("gpsimd", lambda e, src=src, dst=dst, s=s, lo=lo: e.tensor_tensor(
                            out=dst[:, lo:528], in0=src[:, lo:528], in1=src[:, lo - s:528 - s], op=ALU.add),
                            reads=[srcn, dstn], writes=[dstn])
                        src, srcn = dst, dstn
                    w = 2 ** (g + 1)
                    S.op("vector", lambda e, src=src, w=w: e.scalar_tensor_tensor(
                        out=dT[:], in0=src[:, 16:528], scalar=1.0 / w, in1=ug[:, 16:528], op0=ALU.mult, op1=ALU.subtract),
                        reads=[srcn, "ug"], writes=["dT"])
                    if c == 0:
                        S.op("vector", lambda e, src=src, g=g: e.tensor_tensor(out=junk[:, 0:16], in0=src[:, 16:32], in1=ic16[:, g, :], op=ALU.mult),
                             reads=[srcn, "ic16"], writes=["junk"])
                        S.op("vector", lambda e: e.tensor_tensor(out=dT[:, 0:16], in0=junk[:, 0:16], in1=ug[:, 16:32], op=ALU.subtract),
                             reads=["junk", "ug", "dT"], writes=["dT"])
                    bi2 = mmr.next()
                    S.op("tensor", lambda e, g=g, bi2=bi2: e.matmul(mm[bi2][:], lhsT=wpool_sb[:, g, :], rhs=dT[:], start=True, stop=True),
                         reads=["wpool_sb", "dT"], writes=[f"mm{bi2}"])
                    S.op("scalar", lambda e, g=g, bi2=bi2: e.activation(out=mixTs[c % 2][:, g, :], in_=mm[bi2][:], func=AF.Copy, scale=pscale_sb[:, g:g + 1]),
                         reads=[f"mm{bi2}", "pscale_sb", f"mixT{c % 2}"], writes=[f"mixT{c % 2}"])
                for j in range(4):
                    bi = inproj_fm(512 + j * 128)
                    S.op("scalar", lambda e, j=j, bi=bi: e.activation(out=qTs[c % 2][:, j, :], in_=mm[bi][:], func=AF.Copy),
                         reads=[f"mm{bi}"], writes=[f"qT{c % 2}"])
                for j in range(4):
                    bi = inproj_fm(1152 + j * 128)
                    S.op("vector", lambda e, j=j, bi=bi: e.tensor_copy(out=qiT[:, j, :], in_=mm[bi][:]),
                         reads=[f"mm{bi}"], writes=["qiT"])
                tiA = tpr.next(); tiB = tpr.next()
                for t in range(4):
                    tg = 4 * c + t
                    bi = mmr.next()
                    def f(e, t=t, bi=bi):
                        ins = None
                        for k in range(8):
                            ins = e.matmul(mm[bi][:, 0:128], lhsT=xT[:, k, t * 128:(t + 1) * 128], rhs=w_in_sb[:, k, 1024:1152],
                                           start=(k == 0), stop=(k == 7))
                        for k in range(8):
                            ins = e.matmul(mm[bi][:, 128:200], lhsT=xT[:, k, t * 128:(t + 1) * 128], rhs=w_in_sb[:, k, 1664:1736],
                                           start=(k == 0), stop=(k == 7))
                        return ins
                    S.op("tensor", f, reads=["w_in_sb", "xT"], writes=[f"mm{bi}"])
                    S.op("scalar", lambda e, bi=bi: e.activation(out=tm[:], in_=mm[bi][:, 0:200], func=AF.Copy),
                         reads=[f"mm{bi}"], writes=["tm"])
                    S.op("scalar", lambda e: e.activation(out=junk[:], in_=tm[:, 0:128], func=AF.Square, accum_out=st1[:, 0:1]),
                         reads=["tm", "st1"], writes=["junk", "st1"])
                    ln_rstd(st1[:, 0:1], st1[:, 1:2], 1, 1.0 / 128, "st1", "st1")
                    S.op("vector", lambda e: e.scalar_tensor_tensor(out=ckn[:], in0=tm[:, 0:128], scalar=st1[:, 1:2], in1=kvg_bc[:],
                                                                     op0=ALU.mult, op1=ALU.mult),
                         reads=["tm", "st1", "kvg_bc"], writes=["ckn"])
                    S.op("gpsimd", lambda e, tg=tg: e.tensor_copy(out=ckv1[:, tg, 0:128], in_=ckn[:]), reads=["ckn", "ckv1"], writes=["ckv1"])
                    S.op("vector", lambda e: e.bn_stats(out=bst[:, 0, :], in_=tm[:, 128:192]), reads=["tm"], writes=["bst"])
                    S.op("vector", lambda e: e.bn_aggr(out=bmv[:], in_=bst[:, 0, :]), reads=["bst"], writes=["bmv"])
                    ln_rstd(bmv[:, 1:2], brs[:, 0:1], 0, 1.0, "bmv", "brs")
                    S.op("vector", lambda e: e.tensor_scalar(out=kn32[:], in0=tm[:, 128:192], scalar1=bmv[:, 0:1], scalar2=brs[:, 0:1],
                                                              op0=ALU.subtract, op1=ALU.mult),
                         reads=["tm", "bmv", "brs"], writes=["kn32"])
                    S.op("vector", lambda e, tg=tg: e.tensor_copy(out=widx[:, tg, :], in_=tm[:, 192:200]),
                         reads=["tm", "widx"], writes=["widx"])
                    S.op("gpsimd", lambda e: e.tensor_tensor(out=kn32[:], in0=kn32[:], in1=kig_bc[:], op=ALU.mult),
                         reads=["kn32", "kig_bc"], writes=["kn32"])
                    S.op("gpsimd", lambda e: e.tensor_tensor(out=kn2[:, 0:64], in0=kn32[:], in1=kib_bc[:], op=ALU.add),
                         reads=["kn32", "kib_bc", "kn2"], writes=["kn2"])
                    S.op("gpsimd", lambda e: e.tensor_copy(out=kn2[:, 64:128], in_=kn2[:, 0:64]), reads=["kn2"], writes=["kn2"])
                    S.op("tensor", lambda e, t=t, tiA=tiA: e.transpose(out=tp[tiA][:, t * 128:(t + 1) * 128], in_=ckn[:], identity=ident_b[:]),
                         reads=["ckn", "ident_b"], writes=[f"tp{tiA}"])
                    S.op("tensor", lambda e, t=t, tiB=tiB: e.transpose(out=tp[tiB][:, t * 128:(t + 1) * 128], in_=kn2[:], identity=ident_b[:]),
                         reads=["kn2", "ident_b"], writes=[f"tp{tiB}"])
                S.op("scalar", lambda e, c=c, tiA=tiA: e.activation(out=ckvT[:, c * CH:(c + 1) * CH], in_=tp[tiA][:, 0:512], func=AF.Copy),
                     reads=[f"tp{tiA}", "ckvT"], writes=["ckvT"])
                S.op("scalar", lambda e, c=c, tiB=tiB: e.activation(out=kiT[:, c * CH:(c + 1) * CH], in_=tp[tiB][:, 0:512], func=AF.Copy),
                     reads=[f"tp{tiB}", "kiT"], writes=["kiT"])


            def tile_sel(c, t):
                qt = 4 * c + t
                qt = 4 * c + t
                N = 128 * (qt + 1)
                nkc = (N + 511) // 512
                for kc in range(nkc):
                    k0 = kc * 512
                    kw = min(512, N - k0)
                    for h in range(8):
                        hp, h2 = h // 2, h % 2
                        bi = mmr.next()
                        S.op("tensor", lambda e, hp=hp, h2=h2, t=t, bi=bi, k0=k0, kw=kw: e.matmul(
                            mm[bi][:, 0:kw], lhsT=qiT[h2 * 64:(h2 + 1) * 64, hp, t * 128:(t + 1) * 128],
                            rhs=kiT[h2 * 64:(h2 + 1) * 64, k0:k0 + kw], start=True, stop=True),
                            reads=["qiT", "kiT"], writes=[f"mm{bi}"])
                        ri = h % 2
                        S.op("scalar", lambda e, bi=bi, ri=ri, kw=kw: e.activation(out=rl[ri][:, 0:kw], in_=mm[bi][:, 0:kw], func=AF.Relu),
                             reads=[f"mm{bi}"], writes=[f"rl{ri}"])
                        if h == 0:
                            S.op("vector", lambda e, ri=ri, k0=k0, kw=kw, qt=qt: e.tensor_scalar(
                                out=SC[:, k0:k0 + kw], in0=rl[ri][:, 0:kw], scalar1=widx[:, qt, 0:1], scalar2=None, op0=ALU.mult),
                                reads=[f"rl{ri}", "widx", "SC"], writes=["SC"])
                        else:
                            S.op("vector", lambda e, ri=ri, k0=k0, kw=kw, qt=qt, h=h: e.scalar_tensor_tensor(
                                out=SC[:, k0:k0 + kw], in0=rl[ri][:, 0:kw], scalar=widx[:, qt, h:h + 1], in1=SC[:, k0:k0 + kw],
                                op0=ALU.mult, op1=ALU.add),
                                reads=[f"rl{ri}", "widx", "SC"], writes=["SC"])
                if N > 256:
                    S.op("vector", lambda e, N=N: e.tensor_reduce(out=bmax[:], in_=SC[:, 0:N], axis=mybir.AxisListType.X, op=ALU.max,
                                                                   apply_absolute_value=True), reads=["SC"], writes=["bmax"])
                    S.op("vector", lambda e: e.tensor_scalar(out=bmax[:], in0=bmax[:], scalar1=1.0001, scalar2=1e-20, op0=ALU.mult, op1=ALU.add),
                         reads=["bmax"], writes=["bmax"])
                S.op("gpsimd", lambda e, qt=qt: e.affine_select(out=SC[:, qt * 128:(qt + 1) * 128], in_=SC[:, qt * 128:(qt + 1) * 128],
                                                                 pattern=[[-1, 128]], compare_op=ALU.is_ge, fill=NEG, base=0, channel_multiplier=1),
                     reads=["SC"], writes=["SC"])
                if N > 256:
                    S.op("vector", lambda e, N=N: e.tensor_scalar(out=bmid[:], in0=bmax[:], scalar1=0.0, scalar2=None, op0=ALU.mult),
                         reads=["bmax"], writes=["bmid"])
                    S.op("vector", lambda e: e.tensor_scalar(out=bsteps[:], in0=pw2[:], scalar1=bmax[:, 0:1], scalar2=None, op0=ALU.mult),
                         reads=["pw2", "bmax"], writes=["bsteps"])
                    S.op("vector", lambda e: e.tensor_scalar(out=bnegh[:], in0=bsteps[:], scalar1=-0.5, scalar2=None, op0=ALU.mult),
                         reads=["bsteps"], writes=["bnegh"])
                    for it_ in range(KBIS):
                        S.op("vector", lambda e, N=N: e.tensor_scalar(out=xT[:].rearrange("p k n -> p (k n)").bitcast(mybir.dt.uint8)[:, 0:N], in0=SC[:, 0:N], scalar1=bmid[:, 0:1], scalar2=0.0,
                                                                       op0=ALU.is_ge, op1=ALU.add, accum_out=bcnt[:, 0:1]),
                             reads=["SC", "bmid"], writes=["xT", "bcnt"])
                        S.op("vector", lambda e, it_=it_: e.tensor_scalar(out=bd[:], in0=bcnt[:], scalar1=c256[:, 0:1], scalar2=bsteps[:, it_:it_ + 1],
                                                                          op0=ALU.is_ge, op1=ALU.mult),
                             reads=["bcnt", "c256", "bsteps"], writes=["bd"])
                        sc2 = bnegh[:, it_:it_ + 1] if it_ < KBIS - 1 else bnegl[:, 0:1]
                        if it_ == KBIS - 1:
                            S.op("vector", lambda e, it_=it_: e.tensor_scalar(out=bnegl[:], in0=bsteps[:, it_:it_ + 1], scalar1=-1.0, scalar2=None, op0=ALU.mult),
                                 reads=["bsteps"], writes=["bnegl"])
                        S.op("vector", lambda e, sc2=sc2: e.tensor_scalar(out=bmid[:], in0=bmid[:], scalar1=bd[:, 0:1], scalar2=sc2,
                                                                          op0=ALU.add, op1=ALU.add),
                             reads=["bmid", "bd", "bnegh", "bnegl"], writes=["bmid"])

            def tile_mask(c, t):
                qt = 4 * c + t
                N = 128 * (qt + 1)
                for kt in range(qt + 1):
                    mi = kt % 2
                    if N > 256:
                        S.op("vector", lambda e, kt=kt, mi=mi: e.tensor_scalar(out=m128[mi][:], in0=SC[:, kt * 128:(kt + 1) * 128],
                                                                               scalar1=bmid[:, 0:1], scalar2=None, op0=ALU.is_ge),
                             reads=["SC", "bmid"], writes=[f"m128_{mi}"])
                    else:
                        S.op("vector", lambda e, kt=kt, mi=mi: e.tensor_scalar(out=m128[mi][:], in0=SC[:, kt * 128:(kt + 1) * 128],
                                                                               scalar1=-0.5e30, scalar2=None, op0=ALU.is_ge),
                             reads=["SC"], writes=[f"m128_{mi}"])
                    ti = tpr.next()
                    S.op("tensor", lambda e, mi=mi, ti=ti: e.transpose(out=tp[ti][:, 0:128], in_=m128[mi][:], identity=ident_b[:]),
                         reads=[f"m128_{mi}", "ident_b"], writes=[f"tp{ti}"])
                    S.op("scalar", lambda e, kt=kt, t=t, ti=ti: e.activation(out=maskTs[c % 2][:, kt, t * 128:(t + 1) * 128], in_=tp[ti][:, 0:128], func=AF.Copy),
                         reads=[f"tp{ti}", f"maskT{c % 2}"], writes=[f"maskT{c % 2}"])


            def att_head(c, h, use_dve=False, look=1):
                nkt = 4 * c + 4
                hp, h2 = h // 2, h % 2
                qi = h % 2
                oi = h % 2
                bi = mmr.next()
                S.op("tensor", lambda e, hp=hp, h2=h2, bi=bi: e.matmul(mm[bi][:], lhsT=wuk_sb[h2 * 64:(h2 + 1) * 64, hp, :],
                                                                    rhs=qTs[c % 2][h2 * 64:(h2 + 1) * 64, hp, :], start=True, stop=True),
                     reads=["wuk_sb", f"qT{c % 2}"], writes=[f"mm{bi}"])
                S.op("scalar", lambda e, bi=bi, qi=qi: e.activation(out=qlat[qi][:], in_=mm[bi][:], func=AF.Copy, scale=0.125),
                     reads=[f"mm{bi}"], writes=[f"qlat{qi}"])
                def qk(kt):
                    j0 = max(0, kt - 4 * c)
                    ncol = (4 - j0) * 128
                    c0 = j0 * 128
                    bi = mmr.next()
                    S.op("tensor", lambda e, kt=kt, bi=bi, qi=qi, c0=c0, ncol=ncol: e.matmul(
                        mm[bi][:, 0:ncol], lhsT=ckvT[:, kt * 128:(kt + 1) * 128], rhs=qlat[qi][:, c0:c0 + ncol], start=True, stop=True),
                        reads=["ckvT", f"qlat{qi}"], writes=[f"mm{bi}"])
                    return bi, j0, ncol, c0
                pend = [qk(k_) for k_ in range(min(look, nkt))]
                for kt in range(nkt):
                    bi, j0, ncol, c0 = pend.pop(0)
                    if kt + look < nkt:
                        pend.append(qk(kt + look))
                    ei = kt % (3 if look > 1 else 2)
                    S.op("scalar", lambda e, bi=bi, ei=ei, ncol=ncol: e.activation(out=Eb[ei][:, 0:ncol], in_=mm[bi][:, 0:ncol], func=AF.Exp),
                         reads=[f"mm{bi}"], writes=[f"Eb{ei}"])
                    S.op("vector" if (use_dve and kt % 2 == 0) else "gpsimd", lambda e, ei=ei, kt=kt, c0=c0, ncol=ncol, c=c: e.tensor_tensor(
                        out=Pb[ei][:, 0:ncol], in0=Eb[ei][:, 0:ncol], in1=maskTs[c % 2][:, kt, c0:c0 + ncol], op=ALU.mult),
                        reads=[f"Eb{ei}", f"maskT{c % 2}"], writes=[f"Pb{ei}"])
                    def pv(e, kt=kt, j0=j0, ei=ei, oi=oi, c=c):
                        ins = None
                        for j in range(j0, 4):
                            bank, off = (0, j * 129) if j < 3 else (1, 0)
                            ins = e.matmul(oacc[oi][:, bank, off:off + 129], lhsT=Pb[ei][:, (j - j0) * 128:(j - j0 + 1) * 128],
                                           rhs=ckv1[:, kt, 0:129], start=(kt == 0 and j in (0, 3)), stop=(kt == 4 * c + j),
                                           skip_group_check=True)
                        return ins
                    S.op("tensor", pv, reads=[f"Pb{ei}", "ckv1"], writes=[f"oacc{oi}"])
                ti = tpr.next()
                for j in range(4):
                    bank, off = (0, j * 129) if j < 3 else (1, 0)
                    li = j % 2
                    S.op("scalar", lambda e, oi=oi, bank=bank, off=off, j=j: e.activation(out=rden[:, j:j + 1], in_=oacc[oi][:, bank, off + 128:off + 129], func=AF.Ln),
                         reads=[f"oacc{oi}", "rden"], writes=["rden"])
                    S.op("scalar", lambda e, j=j: e.activation(out=rden[:, j:j + 1], in_=rden[:, j:j + 1], func=AF.Exp, scale=-1.0),
                         reads=["rden"], writes=["rden"])
                    S.op("scalar", lambda e, oi=oi, bank=bank, off=off, j=j, li=li: e.activation(
                        out=olat[li][:], in_=oacc[oi][:, bank, off:off + 128], func=AF.Copy, scale=rden[:, j:j + 1]),
                        reads=[f"oacc{oi}", "rden"], writes=[f"olat{li}"])
                    S.op("tensor", lambda e, li=li, ti=ti, j=j: e.transpose(out=tp[ti][:, j * 128:(j + 1) * 128], in_=olat[li][:], identity=ident_b[:]),
                         reads=[f"olat{li}", "ident_b"], writes=[f"tp{ti}"])
                S.op("scalar", lambda e, ti=ti, h2=h2: e.activation(out=olT2[:, h2, :], in_=tp[ti][:, 0:512], func=AF.Copy),
                     reads=[f"tp{ti}", "olT2"], writes=["olT2"])
                if h2 == 1:
                    bi = mmr.next()
                    def f(e, hp=hp, bi=bi):
                        e.matmul(mm[bi][:], lhsT=wuv_sb[:, hp, 0, :], rhs=olT2[:, 0, :], start=True, stop=False)
                        return e.matmul(mm[bi][:], lhsT=wuv_sb[:, hp, 1, :], rhs=olT2[:, 1, :], start=False, stop=True)
                    S.op("tensor", f, reads=["wuv_sb", "olT2"], writes=[f"mm{bi}"])
                    S.op("scalar", lambda e, hp=hp, bi=bi, c=c: e.activation(out=mixTs[c % 2][:, 4 + hp, :], in_=mm[bi][:], func=AF.Copy),
                         reads=[f"mm{bi}", f"mixT{c % 2}"], writes=[f"mixT{c % 2}"])


            def out_ln(c):
                if DEBUG == "1a" and c == 0:
                    dump(S, "qT", qTs[0][:].rearrange("p k n -> p (k n)"), [128, 4 * CH], BF16, ["qT0"])
                    dump(S, "qiT", qiT[:].rearrange("p k n -> p (k n)"), [128, 4 * CH], BF16, ["qiT"])
                    dump(S, "maskT", maskTs[0][:, 0:4, :].rearrange("p k n -> p (k n)"), [128, 4 * CH], mybir.dt.uint8, ["maskT0"])
                    dump(S, "olT2", olT2[:].rearrange("p k n -> p (k n)"), [128, 2 * CH], BF16, ["olT2"])
                    dump(S, "SC", SC[:, 0:512], [128, 512], F32, ["SC"])
                if DEBUG == "1a" and c == 7:
                    dump(S, "ckv1", ckv1[:].rearrange("p k n -> p (k n)"), [128, NT * 130], BF16, ["ckv1"])
                    dump(S, "ckvT", ckvT[:], [128, T], BF16, ["ckvT"])
                    dump(S, "kiT", kiT[:], [128, T], BF16, ["kiT"])
                    dump(S, "widx", widx[:].rearrange("p k n -> p (k n)"), [128, NT * 8], F32, ["widx"])
                if DEBUG == "1a":
                    S.dma("sync", lambda e, c=c: e.dma_start(out=dbg["mixT"][c], in_=mixTs[c % 2][:].rearrange("p k n -> p (k n)")),
                          reads=[f"mixT{c % 2}"], key="dbgm")

                for t in range(4):
                    tg = 4 * c + t
                    b = tg % 2
                    S.dma("sync", lambda e, tg=tg, b=b: e.dma_start(out=xt[b][:], in_=x_d[tg * 128:(tg + 1) * 128, :]),
                          writes=[f"xt{b}"], key=f"x{b}")
                    for hf in range(2):
                        bi = mmr.next()
                        def f(e, t=t, hf=hf, bi=bi):
                            ins = None
                            for k in range(8):
                                ins = e.matmul(mm[bi][:], lhsT=mixTs[c % 2][:, k, t * 128:(t + 1) * 128], rhs=w_o_sb[:, k, hf * 512:(hf + 1) * 512],
                                               start=(k == 0), stop=(k == 7))
                            return ins
                        S.op("tensor", f, reads=[f"mixT{c % 2}", "w_o_sb"], writes=[f"mm{bi}"])
                        S.op("vector", lambda e, b=b, hf=hf, bi=bi: e.scalar_tensor_tensor(
                            out=r1[:, hf * 512:(hf + 1) * 512], in0=xt[b][:, hf * 512:(hf + 1) * 512], scalar=ALPHA, in1=mm[bi][:],
                            op0=ALU.mult, op1=ALU.add),
                            reads=[f"xt{b}", f"mm{bi}", "r1"], writes=["r1"])
                    layer_norm_tile(r1[:], h1o[b][:], g1_bc, b1_bc, bst, bmv, brs, r1[:], ("r1", "r1", "r1"))
                    S.dma("sync", lambda e, tg=tg, b=b: e.dma_start(out=h1_d[tg * 128:(tg + 1) * 128, :], in_=h1o[b][:]),
                          reads=["r1"], key="h1s0")
                    if DEBUG == "1a":
                        S.dma("sync", lambda e, tg=tg, b=b: e.dma_start(out=dbg["h"][tg * 128:(tg + 1) * 128, :], in_=h1o[b][:]),
                              reads=["r1"], key="dbgh0")

            for c in range(NCH):
                front(c)
                if c == 0:
                    for zi in range(NE * CAP // 1024):
                        S.dma("sync", lambda e, zi=zi: e.dma_start(out=xg_d[zi * 1024:(zi + 1) * 1024, :].rearrange("(t p) d -> p t d", p=128), in_=zsrc),
                              reads=["maskT1"], key="zf", serialize=False)
                    S.lastw["xg_d"] = ("D:zf", S.cnt["D:zf"])
                for t in range(4):
                    tile_sel(c, t)
                    if c > 0:
                        att_head(c - 1, 2 * t)
                        att_head(c - 1, 2 * t + 1)
                    tile_mask(c, t)
                if c > 0:
                    out_ln(c - 1)
            S.full_barrier()
            mm.append(tp[1][:].bitcast(F32))
            mmr.items = [0, 1, 2]
            tpr.items = [0]
            for h in range(8):
                att_head(NCH - 1, h, use_dve=True, look=2)
            out_ln(NCH - 1)
            S.full_barrier()
            S.flush(nc)

        if DEBUG == "1a":
            return nc

        with ExitStack() as p2:
            w_mq_sb = sb(p2, "w_mq_sb", [128, 8, D], BF16)
            w_mo_sb = sb(p2, "w_mo_sb", [128, 8, D], BF16)
            w_mkv_sb = sb(p2, "w_mkv_sb", [128, 8, 2 * D], BF16)
            mb = sb(p2, "mb", [128, 2, D], BF16)
            memT = sb(p2, "memT", [128, 8, 256], BF16)
            KmT = sb(p2, "KmT", [128, 8, 256], BF16)
            Vm = sb(p2, "Vm", [128, 2, D], BF16)
            g2_bc = sb(p2, "g2_bc", [128, D], F32)
            b2_bc = sb(p2, "b2_bc", [128, D], F32)
            wr_sb = sb(p2, "wr_sb", [128, 8, NE], F32)
            br_bc = sb(p2, "br_bc", [128, NE], F32)
            ustr_f = sb(p2, "ustr_f", [128, 128], F32)
            ustr_b = sb(p2, "ustr_b", [128, 128], BF16)
            cb_i = sb(p2, "cb_i", [128, NE], I32)
            cbase = sb(p2, "cbase", [128, NE], F32)
            caphi = sb(p2, "caphi", [128, NE], F32)
            h1cs = [sb(p2, f"h1c{i}", [128, 4, D], F32) for i in range(2)]
            h1b = [sb(p2, f"h1b{i}", [128, D], BF16) for i in range(2)]
            hTs = [sb(p2, f"hT{i}", [128, 8, CH], BF16) for i in range(2)]
            qmTs = [sb(p2, f"qmT{i}", [128, 8, CH], BF16) for i in range(2)]
            Pm = [sb(p2, f"Pm{i}", [128, CH], BF16) for i in range(2)]
            rdn = sb(p2, "rdn", [128, CH], F32)
            omTs = [sb(p2, f"omT{i}", [128, 8, CH], BF16) for i in range(2)]
            r2 = sb(p2, "r2", [128, D], F32)
            tmpn2 = sb(p2, "tmpn2", [128, D], F32)
            h2o = [sb(p2, f"h2o{i}", [128, D], F32) for i in range(2)]
            h2b = [sb(p2, f"h2b{i}", [128, D], BF16) for i in range(2)]
            h2T = sb(p2, "h2T", [128, 8, 128], F32)
            lg = sb(p2, "lg", [128, NE], F32)
            mx8r = sb(p2, "mx8r", [128, 8], F32)
            negm = sb(p2, "negm", [128, 1], F32)
            ex4 = sb(p2, "ex4", [128, 4], F32)
            gsum = sb(p2, "gsum", [128, 1], F32)
            selb = sb(p2, "selb", [128, NE], BF16)
            slotm = sb(p2, "slotm", [128, NE], F32)
            ohp = sb(p2, "ohp", [128, NE], F32)
            slotf = sb(p2, "slotf", [128, 4], F32)
            bst2 = sb(p2, "bst2", [128, 2, 6], F32)
            bmv2 = sb(p2, "bmv2", [128, 2], F32)
            brs2 = sb(p2, "brs2", [128, 1], F32)
            mm = [ps(p2, f"mmB{i}", [128, 512], F32) for i in range(6)]
            tp = [ps(p2, f"tpB{i}", [128, 1024], BF16) for i in range(2)]
            mmr = Rot(list(range(6))); tpr = Rot([0, 1])

            S.dma("gpsimd", lambda e: e.dma_start(out=w_mkv_sb[:], in_=w_mkv_d.rearrange("(k p) n -> p k n", p=128)), writes=["w_mkv_sb"], key="w0")
            S.dma("gpsimd", lambda e: e.dma_start(out=mb[:], in_=mem_d.rearrange("(t p) d -> p t d", p=128)), writes=["mb"], key="w1")
            S.dma("gpsimd", lambda e: e.dma_start(out=w_mq_sb[:], in_=w_mq_d.rearrange("(k p) n -> p k n", p=128)), writes=["w_mq_sb"], key="w2")
            S.dma("gpsimd", lambda e: e.dma_start(out=w_mo_sb[:], in_=w_mo_d.rearrange("(k p) n -> p k n", p=128)), writes=["w_mo_sb"], key="w4")
            S.dma("sync", lambda e: e.dma_start(out=g2_bc[:], in_=bc(ln2g_d, D)), writes=["g2_bc"], key="c1")
            S.dma("sync", lambda e: e.dma_start(out=b2_bc[:], in_=bc(ln2b_d, D)), writes=["b2_bc"], key="c2")
            S.dma("sync", lambda e: e.dma_start(out=wr_sb[:], in_=w_r_d.rearrange("(k p) n -> p k n", p=128)), writes=["wr_sb"], key="c3")
            S.dma("sync", lambda e: e.dma_start(out=br_bc[:], in_=bc(b_r_d, NE)), writes=["br_bc"], key="c4")
            S.op("gpsimd", lambda e: e.memset(ustr_f[:], 1.0), writes=["ustr_f"])
            S.op("gpsimd", lambda e: e.affine_select(out=ustr_f[:], in_=ustr_f[:], pattern=[[1, 128]], compare_op=ALU.is_gt, fill=0.0,
                                                       base=0, channel_multiplier=-1), reads=["ustr_f"], writes=["ustr_f"])
            S.op("vector", lambda e: e.tensor_copy(out=ustr_b[:], in_=ustr_f[:]), reads=["ustr_f"], writes=["ustr_b"])
            S.op("gpsimd", lambda e: e.iota(out=cb_i[:], pattern=[[CAP, NE]], base=0, channel_multiplier=0), writes=["cb_i"])
            S.op("vector", lambda e: e.tensor_copy(out=cbase[:], in_=cb_i[:]), reads=["cb_i"], writes=["cbase"])
            S.op("vector", lambda e: e.tensor_scalar(out=caphi[:], in0=cbase[:], scalar1=float(CAP - 1), scalar2=None, op0=ALU.add),
                 reads=["cbase"], writes=["caphi"])
            for mt in range(2):
                for kh in range(2):
                    ti = tpr.next()
                    def tr(e, mt=mt, kh=kh, ti=ti):
                        ins = None
                        for kk in range(4):
                            k = kh * 4 + kk
                            ins = e.transpose(out=tp[ti][:, kk * 128:(kk + 1) * 128], in_=mb[:, mt, k * 128:(k + 1) * 128], identity=ident_b[:])
                        return ins
                    S.op("tensor", tr, reads=["mb", "ident_b"], writes=[f"tp{ti}"])
                    S.op("vector", lambda e, mt=mt, kh=kh, ti=ti: e.tensor_copy(
                        out=memT[:, kh * 4:(kh + 1) * 4, mt * 128:(mt + 1) * 128], in_=tp[ti][:, 0:512].rearrange("p (k n) -> p k n", k=4)),
                        reads=[f"tp{ti}", "memT"], writes=["memT"])
            for cc in range(8):
                bi = mmr.next()
                def f(e, cc=cc, bi=bi):
                    ins = None
                    for k in range(8):
                        ins = e.matmul(mm[bi][:, 0:256], lhsT=w_mkv_sb[:, k, cc * 128:(cc + 1) * 128], rhs=memT[:, k, :], start=(k == 0), stop=(k == 7))
                    return ins
                S.op("tensor", f, reads=["w_mkv_sb", "memT"], writes=[f"mm{bi}"])
                S.op("vector", lambda e, cc=cc, bi=bi: e.tensor_copy(out=KmT[:, cc, :], in_=mm[bi][:, 0:256]), reads=[f"mm{bi}", "KmT"], writes=["KmT"])
            for mt in range(2):
                for hf in range(2):
                    bi = mmr.next()
                    def f(e, mt=mt, hf=hf, bi=bi):
                        ins = None
                        for k in range(8):
                            ins = e.matmul(mm[bi][:], lhsT=memT[:, k, mt * 128:(mt + 1) * 128], rhs=w_mkv_sb[:, k, D + hf * 512:D + (hf + 1) * 512],
                                           start=(k == 0), stop=(k == 7))
                        return ins
                    S.op("tensor", f, reads=["w_mkv_sb", "memT"], writes=[f"mm{bi}"])
                    S.op("scalar", lambda e, mt=mt, hf=hf, bi=bi: e.activation(out=Vm[:, mt, hf * 512:(hf + 1) * 512], in_=mm[bi][:], func=AF.Copy),
                         reads=[f"mm{bi}", "Vm"], writes=["Vm"])

            def stageA(c, part):
                p_ = c % 2
                h1c, hT, qmT, omT = h1cs[p_], hTs[p_], qmTs[p_], omTs[p_]
                h1n, hTn, qmn, omn = f"h1c{p_}", f"hT{p_}", f"qmT{p_}", f"omT{p_}"
                if part == 0:
                    S.dma("sync", lambda e, c=c, h1c=h1c: e.dma_start(out=h1c[:], in_=h1_d[c * CH:(c + 1) * CH, :].rearrange("(t p) d -> p t d", p=128)),
                          writes=[h1n], key=f"h1l{p_}")
                for t in ([0, 1] if part == 0 else [2, 3] if part == 1 else []):
                    b = t % 2
                    S.op("scalar", lambda e, b=b, t=t: e.activation(out=h1b[b][:], in_=h1c[:, t, :], func=AF.Copy),
                         reads=[h1n], writes=[f"h1b{b}"])
                    for kh in range(2):
                        ti = tpr.next()
                        def tr(e, b=b, kh=kh, ti=ti):
                            ins = None
                            for kk in range(4):
                                k = kh * 4 + kk
                                ins = e.transpose(out=tp[ti][:, kk * 128:(kk + 1) * 128], in_=h1b[b][:, k * 128:(k + 1) * 128], identity=ident_b[:])
                            return ins
                        S.op("tensor", tr, reads=[f"h1b{b}", "ident_b"], writes=[f"tp{ti}"])
                        S.op("vector", lambda e, kh=kh, ti=ti, t=t: e.tensor_copy(
                            out=hT[:, kh * 4:(kh + 1) * 4, t * 128:(t + 1) * 128], in_=tp[ti][:, 0:512].rearrange("p (k n) -> p k n", k=4)),
                            reads=[f"tp{ti}", hTn], writes=[hTn])
                for cc in (range(8) if part == 2 else []):
                    bi = mmr.next()
                    def f(e, cc=cc, bi=bi):
                        ins = None
                        for k in range(8):
                            ins = e.matmul(mm[bi][:], lhsT=w_mq_sb[:, k, cc * 128:(cc + 1) * 128], rhs=hT[:, k, :], start=(k == 0), stop=(k == 7))
                        return ins
                    S.op("tensor", f, reads=["w_mq_sb", hTn], writes=[f"mm{bi}"])
                    S.op("scalar", lambda e, cc=cc, bi=bi: e.activation(out=qmT[:, cc, :], in_=mm[bi][:], func=AF.Copy, scale=1.0 / 16),
                         reads=[f"mm{bi}", qmn], writes=[qmn])
                for h in (range(4) if part == 3 else []):
                    for mt in range(2):
                        bi = mmr.next()
                        def f(e, h=h, mt=mt, bi=bi):
                            e.matmul(mm[bi][:], lhsT=KmT[:, 2 * h, mt * 128:(mt + 1) * 128], rhs=qmT[:, 2 * h, :], start=True, stop=False)
                            return e.matmul(mm[bi][:], lhsT=KmT[:, 2 * h + 1, mt * 128:(mt + 1) * 128], rhs=qmT[:, 2 * h + 1, :], start=False, stop=True)
                        S.op("tensor", f, reads=["KmT", qmn], writes=[f"mm{bi}"])
                        S.op("scalar", lambda e, mt=mt, bi=bi: e.activation(out=Pm[mt][:], in_=mm[bi][:], func=AF.Exp),
                             reads=[f"mm{bi}"], writes=[f"Pm{mt}"])
                    bi = mmr.next()
                    def f(e, bi=bi):
                        e.matmul(mm[bi][:], lhsT=ones_b[:], rhs=Pm[0][:], start=True, stop=False)
                        return e.matmul(mm[bi][:], lhsT=ones_b[:], rhs=Pm[1][:], start=False, stop=True)
                    S.op("tensor", f, reads=["ones_b", "Pm0", "Pm1"], writes=[f"mm{bi}"])
                    S.op("vector", lambda e, bi=bi: e.reciprocal(out=rdn[:], in_=mm[bi][:]), reads=[f"mm{bi}"], writes=["rdn"])
                    for dvc in range(2):
                        bi = mmr.next()
                        def f(e, h=h, dvc=dvc, bi=bi):
                            c0 = h * 256 + dvc * 128
                            e.matmul(mm[bi][:], lhsT=Vm[:, 0, c0:c0 + 128], rhs=Pm[0][:], start=True, stop=False)
                            return e.matmul(mm[bi][:], lhsT=Vm[:, 1, c0:c0 + 128], rhs=Pm[1][:], start=False, stop=True)
                        S.op("tensor", f, reads=["Vm", "Pm0", "Pm1"], writes=[f"mm{bi}"])
                        S.op("vector", lambda e, h=h, dvc=dvc, bi=bi: e.tensor_tensor(out=omT[:, 2 * h + dvc, :], in0=mm[bi][:], in1=rdn[:], op=ALU.mult),
                             reads=[f"mm{bi}", "rdn", omn], writes=[omn])

            def outproj_ln(c, t):
                p_ = c % 2
                h1c, hT, qmT, omT = h1cs[p_], hTs[p_], qmTs[p_], omTs[p_]
                h1n, hTn, qmn, omn = f"h1c{p_}", f"hT{p_}", f"qmT{p_}", f"omT{p_}"
                tg = 4 * c + t
                b = tg % 2
                for hf in range(2):
                    bi = mmr.next()
                    def f(e, t=t, hf=hf, bi=bi):
                        ins = None
                        for k in range(8):
                            ins = e.matmul(mm[bi][:], lhsT=omT[:, k, t * 128:(t + 1) * 128], rhs=w_mo_sb[:, k, hf * 512:(hf + 1) * 512],
                                           start=(k == 0), stop=(k == 7))
                        return ins
                    S.op("tensor", f, reads=[omn, "w_mo_sb"], writes=[f"mm{bi}"])
                    S.op("vector", lambda e, t=t, hf=hf, bi=bi: e.scalar_tensor_tensor(
                        out=r2[:, hf * 512:(hf + 1) * 512], in0=h1c[:, t, hf * 512:(hf + 1) * 512], scalar=ALPHA, in1=mm[bi][:],
                        op0=ALU.mult, op1=ALU.add), reads=[h1n, f"mm{bi}", "r2"], writes=["r2"])
                layer_norm_tile(r2[:], h2o[b][:], g2_bc, b2_bc, bst2, bmv2, brs2, tmpn2[:], ("r2", f"h2o{b}", "tmpn2"))
                S.dma("sync", lambda e, tg=tg, b=b: e.dma_start(out=h2_d[tg * 128:(tg + 1) * 128, :], in_=h2o[b][:]),
                      reads=[f"h2o{b}"], key=f"h2s{b}")
                if DEBUG == "1b":
                    S.dma("sync", lambda e, tg=tg, b=b: e.dma_start(out=dbg["h"][tg * 128:(tg + 1) * 128, :], in_=h2o[b][:]),
                          reads=[f"h2o{b}"], key=f"dbgh{b}")
                S.op("scalar", lambda e, b=b: e.activation(out=h2b[b][:], in_=h2o[b][:], func=AF.Copy), reads=[f"h2o{b}"], writes=[f"h2b{b}"])

            def router(c, t):
                p_ = c % 2
                h1c, hT, qmT, omT = h1cs[p_], hTs[p_], qmTs[p_], omTs[p_]
                h1n, hTn, qmn, omn = f"h1c{p_}", f"hT{p_}", f"qmT{p_}", f"omT{p_}"
                tg = 4 * c + t
                b = tg % 2
                for kh in range(2):
                    bi = mmr.next()
                    def tr(e, b=b, kh=kh, bi=bi):
                        ins = None
                        for kk in range(4):
                            k = kh * 4 + kk
                            ins = e.transpose(out=mm[bi][:, kk * 128:(kk + 1) * 128], in_=h2o[b][:, k * 128:(k + 1) * 128], identity=ident_f[:])
                        return ins
                    S.op("tensor", tr, reads=[f"h2o{b}", "ident_f"], writes=[f"mm{bi}"])
                    S.op("vector", lambda e, kh=kh, bi=bi: e.tensor_copy(out=h2T[:, kh * 4:(kh + 1) * 4, :],
                                                                          in_=mm[bi][:].rearrange("p (k n) -> p k n", k=4)),
                         reads=[f"mm{bi}", "h2T"], writes=["h2T"])
                bi = mmr.next()
                def f(e, bi=bi):
                    ins = None
                    for k in range(8):
                        ins = e.matmul(mm[bi][:, 0:NE], lhsT=h2T[:, k, :], rhs=wr_sb[:, k, :], start=(k == 0), stop=(k == 7))
                    return ins
                S.op("tensor", f, reads=["h2T", "wr_sb"], writes=[f"mm{bi}"])
                S.op("vector", lambda e, bi=bi: e.tensor_tensor(out=lg[:], in0=mm[bi][:, 0:NE], in1=br_bc[:], op=ALU.add),
                     reads=[f"mm{bi}", "br_bc"], writes=["lg"])
                S.op("vector", lambda e: e.max(out=mx8r[:], in_=lg[:]), reads=["lg"], writes=["mx8r"])
                S.op("vector", lambda e: e.tensor_scalar(out=negm[:], in0=mx8r[:, 0:1], scalar1=-1.0, scalar2=None, op0=ALU.mult),
                     reads=["mx8r"], writes=["negm"])
                S.op("scalar", lambda e: e.activation(out=ex4[:], in_=mx8r[:, 0:4], func=AF.Exp, bias=negm[:, 0:1], accum_out=gsum[:, 0:1]),
                     reads=["mx8r", "negm", "gsum"], writes=["ex4", "gsum"])
                S.op("vector", lambda e: e.reciprocal(out=gsum[:], in_=gsum[:]), reads=["gsum"], writes=["gsum"])
                S.op("vector", lambda e, tg=tg: e.tensor_scalar(out=gates_all[:, tg, :], in0=ex4[:], scalar1=gsum[:, 0:1], scalar2=None, op0=ALU.mult),
                     reads=["ex4", "gsum", "gates_all"], writes=["gates_all"])
                S.op("vector", lambda e: e.tensor_scalar(out=selb[:], in0=lg[:], scalar1=mx8r[:, 3:4], scalar2=None, op0=ALU.is_ge),
                     reads=["lg", "mx8r"], writes=["selb"])
                bi = mmr.next()
                def f(e, bi=bi):
                    e.matmul(mm[bi][:, 0:NE], lhsT=ustr_b[:], rhs=selb[:], start=True, stop=True)
                    return e.matmul(mm[bi][:, 64:64 + NE], lhsT=ones_b[:], rhs=selb[:], start=True, stop=True)
                S.op("tensor", f, reads=["ustr_b", "ones_b", "selb"], writes=[f"mm{bi}"])
                S.op("vector", lambda e, bi=bi: e.tensor_tensor(out=slotm[:], in0=mm[bi][:, 0:NE], in1=cbase[:], op=ALU.add),
                     reads=[f"mm{bi}", "cbase"], writes=["slotm"])
                S.op("vector", lambda e: e.tensor_tensor(out=slotm[:], in0=slotm[:], in1=caphi[:], op=ALU.min),
                     reads=["slotm", "caphi"], writes=["slotm"])
                S.op("vector", lambda e, bi=bi: e.tensor_tensor(out=cbase[:], in0=mm[bi][:, 64:64 + NE], in1=cbase[:], op=ALU.add),
                     reads=[f"mm{bi}", "cbase"], writes=["cbase"])
                for k in range(4):
                    S.op("vector", lambda e, k=k: e.scalar_tensor_tensor(out=ohp[:], in0=lg[:], scalar=mx8r[:, k:k + 1], in1=slotm[:],
                                                                         op0=ALU.is_equal, op1=ALU.mult),
                         reads=["lg", "mx8r", "slotm"], writes=["ohp"])
                    S.op("vector", lambda e, k=k: e.reduce_sum(out=slotf[:, k:k + 1], in_=ohp[:], axis=mybir.AxisListType.X),
                         reads=["ohp", "slotf"], writes=["slotf"])
                S.op("vector", lambda e, tg=tg: e.tensor_copy(out=slots_all[:, tg, :], in_=slotf[:]), reads=["slotf", "slots_all"], writes=["slots_all"])
                for k in range(4):
                    S.dma("gpsimd", lambda e, tg=tg, k=k, b=b: e.indirect_dma_start(
                        out=xg_d, out_offset=bass.IndirectOffsetOnAxis(ap=slots_all[:, tg, k:k + 1], axis=0), in_=h2b[b][:], in_offset=None),
                        reads=["slots_all", f"h2b{b}"], writes=["xg_d"], key=f"sc{k}")

            for part in range(4):
                stageA(0, part)
            for c in range(NCH):
                for t in range(4):
                    outproj_ln(c, t)
                    if t > 0:
                        router(c, t - 1)
                    if c + 1 < NCH:
                        stageA(c + 1, t)
                router(c, 3)
            if DEBUG == "1b":
                dump(S, "slots", slots_all[:].rearrange("p a b -> p (a b)"), [128, NT * 4], I32, ["slots_all"])
                dump(S, "gates", gates_all[:].rearrange("p a b -> p (a b)"), [128, NT * 4], F32, ["gates_all"])
            S.full_barrier()
            S.flush(nc)
        if DEBUG == "1b":
            return nc

        BLKS = [(0, 512), (512, CAP - 512)]
        NTE = CAP // 128
        with ExitStack() as p3:
            wgu = [sb(p3, f"wgu{i}", [128, 8, 2 * D], BF16) for i in range(2)]
            wdn = [sb(p3, f"wdn{i}", [128, 8, D], BF16) for i in range(2)]
            bgr = sb(p3, "bgr", [NE, 2 * D], F32)
            bguT = sb(p3, "bguT", [128, 16, NE], F32)
            bgu7 = sb(p3, "bgu7", [128, 8, NE], F32)
            bdn = [sb(p3, f"bdn{i}", [128, D], F32) for i in range(2)]
            xg = [sb(p3, f"xg{i}", [128, NTE, D], BF16) for i in range(2)]
            xgTs = [sb(p3, f"xgT{i}", [128, 8, CAP], BF16) for i in range(2)]
            actT = sb(p3, "actT", [128, 8, CAP], BF16)
            s0 = [sb(p3, f"s0_{i}", [128, 512], F32) for i in range(2)]
            gcl = [sb(p3, f"gc{i}", [128, 512], F32) for i in range(2)]
            ucl = [sb(p3, f"uc{i}", [128, 512], F32) for i in range(2)]
            ysb = [sb(p3, f"ysb{i}", [128, D], F32) for i in range(2)]
            mm = [ps(p3, f"mmC{i}", [128, 512], F32) for i in range(6)]
            tp = [ps(p3, f"tpC{i}", [128, 1024], BF16) for i in range(2)]
            mmr = Rot(list(range(6))); tpr = Rot([0, 1])

            S.dma("sync", lambda e: e.dma_start(out=bgr[:], in_=b_gu_d), writes=["bgr"], key="c1")
            bi = mmr.next()
            def trb(e, bi=bi):
                ins = None
                for cidx in range(16):
                    ins = e.transpose(out=mm[bi][:, cidx * NE:(cidx + 1) * NE], in_=bgr[0:NE, cidx * 128:(cidx + 1) * 128], identity=ident_f[0:NE, 0:NE])
                return ins
            S.op("tensor", trb, reads=["bgr", "ident_f"], writes=[f"mm{bi}"])
            S.op("vector", lambda e, bi=bi: e.tensor_copy(out=bguT[:].rearrange("p a b -> p (a b)"), in_=mm[bi][:]), reads=[f"mm{bi}"], writes=["bguT"])
            S.op("vector", lambda e: e.tensor_scalar(out=bgu7[:], in0=bguT[:, 8:16, :], scalar1=7.0, scalar2=None, op0=ALU.add),
                 reads=["bguT"], writes=["bgu7"])

            def load_expert(ex):
                bf = ex % 2
                S.dma("gpsimd", lambda e: e.dma_start(out=wgu[bf][:], in_=w_gu_d[ex].rearrange("(k p) n -> p k n", p=128)),
                      writes=[f"wgu{bf}"], key=f"wg{bf}")
                S.dma("gpsimd", lambda e: e.dma_start(out=wdn[bf][:], in_=w_dn_d[ex].rearrange("(k p) n -> p k n", p=128)),
                      writes=[f"wdn{bf}"], key=f"wd{bf}")
                S.dma("sync", lambda e: e.dma_start(out=bdn[bf][:], in_=bc(b_dn_d[ex], D)), writes=[f"bdn{bf}"], key=f"bd{bf}")
                S.dma("sync", lambda e: e.dma_start(out=xg[bf][:], in_=xg_d[ex * CAP:(ex + 1) * CAP, :].rearrange("(t p) d -> p t d", p=128)),
                      reads=["xg_d"], writes=[f"xg{bf}"], key=f"xgl{bf}")

            def prep_expert(exx):
                pb = exx % 2
                for t in range(NTE):
                    for kh in range(2):
                        ti = tpr.next()
                        def tr(e, pb=pb, t=t, kh=kh, ti=ti):
                            ins = None
                            for kk in range(4):
                                k = kh * 4 + kk
                                ins = e.transpose(out=tp[ti][:, kk * 128:(kk + 1) * 128], in_=xg[pb][:, t, k * 128:(k + 1) * 128], identity=ident_b[:])
                            return ins
                        S.op("tensor", tr, reads=[f"xg{pb}", "ident_b"], writes=[f"tp{ti}"])
                        S.op("scalar", lambda e, kh=kh, ti=ti, t=t, pb=pb: e.activation(
                            out=xgTs[pb][:, kh * 4:(kh + 1) * 4, t * 128:(t + 1) * 128], in_=tp[ti][:, 0:512].rearrange("p (k n) -> p k n", k=4), func=AF.Copy),
                            reads=[f"tp{ti}", f"xgT{pb}"], writes=[f"xgT{pb}"])

            load_expert(0)
            prep_expert(0)
            for ex in range(NE_RUN):
                bf = ex % 2
                if ex + 1 < NE_RUN:
                    load_expert(ex + 1)
                it = 0
                for j in range(8 if P2_STEPS >= 2 else 0):
                    for (b0, bw) in BLKS:
                        big = mmr.next(); biu = mmr.next()
                        def fg(e, bf=bf, j=j, b0=b0, bw=bw, big=big):
                            ins = None
                            for k in range(8):
                                ins = e.matmul(mm[big][:, 0:bw], lhsT=wgu[bf][:, k, j * 128:(j + 1) * 128], rhs=xgTs[bf][:, k, b0:b0 + bw],
                                               start=(k == 0), stop=(k == 7))
                            return ins
                        def fu(e, bf=bf, j=j, b0=b0, bw=bw, biu=biu):
                            ins = None
                            for k in range(8):
                                ins = e.matmul(mm[biu][:, 0:bw], lhsT=wgu[bf][:, k, D + j * 128:D + (j + 1) * 128], rhs=xgTs[bf][:, k, b0:b0 + bw],
                                               start=(k == 0), stop=(k == 7))
                            return ins
                        S.op("tensor", fg, reads=[f"wgu{bf}", f"xgT{bf}"], writes=[f"mm{big}"])
                        S.op("tensor", fu, reads=[f"wgu{bf}", f"xgT{bf}"], writes=[f"mm{biu}"])
                        i2 = it % 2; it += 1
                        S.op("vector", lambda e, big=big, bw=bw, j=j, ex=ex, i2=i2: e.tensor_scalar(
                            out=gcl[i2][:, 0:bw], in0=mm[big][:, 0:bw], scalar1=bguT[:, j, ex:ex + 1], scalar2=7.0, op0=ALU.add, op1=ALU.min),
                            reads=[f"mm{big}", "bguT"], writes=[f"gc{i2}"])
                        S.op("scalar", lambda e, bw=bw, i2=i2: e.activation(out=s0[i2][:, 0:bw], in_=gcl[i2][:, 0:bw], func=AF.Silu, scale=1.702),
                             reads=[f"gc{i2}"], writes=[f"s0_{i2}"])
                        S.op("scalar", lambda e, biu=biu, bw=bw, j=j, ex=ex, i2=i2: e.activation(
                            out=ucl[i2][:, 0:bw], in_=mm[biu][:, 0:bw], func=AF.Relu, bias=bgu7[:, j, ex:ex + 1]),
                            reads=[f"mm{biu}", "bgu7"], writes=[f"uc{i2}"])
                        S.op("vector", lambda e, bw=bw, i2=i2: e.tensor_scalar(
                            out=ucl[i2][:, 0:bw], in0=ucl[i2][:, 0:bw], scalar1=14.0, scalar2=-6.0, op0=ALU.min, op1=ALU.add),
                            reads=[f"uc{i2}"], writes=[f"uc{i2}"])
                        S.op("vector", lambda e, bw=bw, b0=b0, j=j, i2=i2: e.scalar_tensor_tensor(
                            out=actT[:, j, b0:b0 + bw], in0=s0[i2][:, 0:bw], scalar=1.0 / 1.702, in1=ucl[i2][:, 0:bw], op0=ALU.mult, op1=ALU.mult),
                            reads=[f"s0_{i2}", f"uc{i2}", "actT"], writes=["actT"])
                if ex + 1 < NE_RUN:
                    prep_expert(ex + 1)
                for t in range(NTE if P2_STEPS >= 4 else 0):
                    yb = t % 2
                    for hf in range(2):
                        bi = mmr.next()
                        def fd(e, bf=bf, t=t, hf=hf, bi=bi):
                            ins = None
                            for j in range(8):
                                ins = e.matmul(mm[bi][:], lhsT=actT[:, j, t * 128:(t + 1) * 128], rhs=wdn[bf][:, j, hf * 512:(hf + 1) * 512],
                                               start=(j == 0), stop=(j == 7))
                            return ins
                        S.op("tensor", fd, reads=["actT", f"wdn{bf}"], writes=[f"mm{bi}"])
                        S.op("vector", lambda e, bf=bf, yb=yb, hf=hf, bi=bi: e.tensor_tensor(
                            out=ysb[yb][:, hf * 512:(hf + 1) * 512], in0=mm[bi][:], in1=bdn[bf][:, hf * 512:(hf + 1) * 512], op=ALU.add),
                            reads=[f"mm{bi}", f"bdn{bf}", f"ysb{yb}"], writes=[f"ysb{yb}"])
                    r0 = ex * CAP + t * 128
                    S.dma("sync", lambda e, yb=yb, r0=r0: e.dma_start(out=ys_d[r0:r0 + 128, :], in_=ysb[yb][:]),
                          reads=[f"ysb{yb}"], writes=["ys_d"], key=f"yst{yb}")
            S.full_barrier()
            S.flush(nc)
        if DEBUG == "2":
            return nc

        with ExitStack() as p4:
            g3_bc = sb(p4, "g3_bc", [128, D], F32)
            b3_bc = sb(p4, "b3_bc", [128, D], F32)
            h2t = [sb(p4, f"h2t{i}", [128, D], F32) for i in range(2)]
            yk = [[sb(p4, f"yk{i}_{k}", [128, D], F32) for k in range(4)] for i in range(2)]
            acc = sb(p4, "acc", [128, D], F32)
            tmpn3 = sb(p4, "tmpn3", [128, D], F32)
            outt = [sb(p4, f"outt{i}", [128, D], F32) for i in range(2)]
            bst3 = sb(p4, "bst3", [128, 2, 6], F32)
            bmv3 = sb(p4, "bmv3", [128, 2], F32)
            brs3 = sb(p4, "brs3", [128, 1], F32)
            S.dma("sync", lambda e: e.dma_start(out=g3_bc[:], in_=bc(ln3g_d, D)), writes=["g3_bc"], key="c1")
            S.dma("sync", lambda e: e.dma_start(out=b3_bc[:], in_=bc(ln3b_d, D)), writes=["b3_bc"], key="c2")
            for tg in range(NT):
                b = tg % 2
                S.dma("sync", lambda e, tg=tg, b=b: e.dma_start(out=h2t[b][:], in_=h2_d[tg * 128:(tg + 1) * 128, :]),
                      writes=[f"h2t{b}"], key=f"h2l{b}")
                for k in range(4):
                    S.dma("gpsimd", lambda e, tg=tg, k=k, b=b: e.indirect_dma_start(
                        out=yk[b][k][:], out_offset=None, in_=ys_d, in_offset=bass.IndirectOffsetOnAxis(ap=slots_all[:, tg, k:k + 1], axis=0)),
                        reads=["ys_d", "slots_all"], writes=[f"yk{b}_{k}"], key=f"gk{b}{k}")
                S.op("vector", lambda e, b=b: e.tensor_scalar(out=acc[:], in0=h2t[b][:], scalar1=ALPHA, scalar2=None, op0=ALU.mult),
                     reads=[f"h2t{b}", "acc"], writes=["acc"])
                for k in range(4):
                    S.op("vector", lambda e, b=b, k=k, tg=tg: e.scalar_tensor_tensor(
                        out=acc[:], in0=yk[b][k][:], scalar=gates_all[:, tg, k:k + 1], in1=acc[:], op0=ALU.mult, op1=ALU.add),
                        reads=[f"yk{b}_{k}", "gates_all", "acc"], writes=["acc"])
                layer_norm_tile(acc[:], outt[b][:], g3_bc, b3_bc, bst3, bmv3, brs3, tmpn3[:], ("acc", f"outt{b}", "tmpn3"))
                S.dma("scalar", lambda e, tg=tg, b=b: e.dma_start(out=out_d[tg * 128:(tg + 1) * 128, :], in_=outt[b][:]),
                      reads=[f"outt{b}"], key=f"os{b}")
            S.full_barrier()
            S.flush(nc)
    return nc


_PROG = None


def kernel(**inputs):
    global _PROG
    if _PROG is None:
        _PROG = build_program()
    nc = _PROG
    B = inputs["x"].shape[0]
    in_maps = []
    for b in range(B):
        m = {}
        for k, v in inputs.items():
            a = np.asarray(v)
            if k in ("x", "mem"):
                m[k] = np.ascontiguousarray(a[b])
            else:
                m[k] = np.ascontiguousarray(a[0])
        in_maps.append(m)
    res = run_bass_kernel_spmd(nc, in_maps, core_ids=list(range(B)))
    return np.stack([np.asarray(r["out"]) for r in res.results], axis=0)
```

```python
import numpy as np
from contextlib import ExitStack
import concourse.bass as bass
import concourse.mybir as mybir
from concourse.bass_utils import run_bass_kernel_spmd

F32 = mybir.dt.float32
BF16 = mybir.dt.bfloat16
I32 = mybir.dt.int32
AF = mybir.ActivationFunctionType
ALU = mybir.AluOpType

T = 4096
NT = 32
D = 1024
CH = 512
NCH = 8
INW = 1736
CAP = 768
NE = 32
ALPHA = 2.0 ** 0.25
NEG = -1.0e30
NEG2 = -3.0e30
KBIS = 25
SIGMAX = float(1.0 / (1.0 + np.exp(-1.702 * 7.0)))

DEBUG = None
NE_RUN = NE
P2_STEPS = 9
P2_EW = 63


class Sched:
    EPOCH = 30000
    ENG = ("sync", "scalar", "vector", "gpsimd", "tensor")

    def __init__(self, sem_pool):
        self.ops = {e: [] for e in self.ENG}
        self.cnt = {}
        self.lastw = {}
        self.readers = {}
        self.seen = {e: {} for e in self.ENG}
        self.sem_pool = list(sem_pool)
        self.sems = {}

    def _sem(self, counter, val):
        if counter.startswith("E:"):
            ep = (val - 1) // self.EPOCH
            name, lv = f"{counter}#{ep}", val - ep * self.EPOCH
        else:
            name, lv = counter, val
        if name not in self.sems:
            self.sems[name] = self.sem_pool.pop()
        return self.sems[name], lv

    def _waits(self, eng, reads, writes, extra=()):
        need = {}
        def add(cv):
            c, v = cv
            if v > need.get(c, 0):
                need[c] = v
        for r in reads:
            if r in self.lastw:
                add(self.lastw[r])
        for w in writes:
            if w in self.lastw:
                add(self.lastw[w])
            for cv in self.readers.get(w, {}).items():
                add(cv)
        for cv in extra:
            add(cv)
        waits = []
        for c, v in need.items():
            if eng == "tensor" and c == "E:tensor":
                continue
            if self.seen[eng].get(c, 0) < v:
                self.seen[eng][c] = v
                waits.append(self._sem(c, v))
        return waits

    def _book(self, c, v, reads, writes):
        for r in reads:
            d = self.readers.setdefault(r, {})
            if d.get(c, 0) < v:
                d[c] = v
        for w in writes:
            self.lastw[w] = (c, v)
            self.readers[w] = {}

    def op(self, eng, fn, reads=(), writes=()):
        waits = self._waits(eng, reads, writes)
        c = "E:" + eng
        v = self.cnt.get(c, 0) + 1
        self.cnt[c] = v
        sem, _ = self._sem(c, v)
        self.ops[eng].append((waits, fn, sem, 1))
        self._book(c, v, reads, writes)

    def dma(self, eng, fn, reads=(), writes=(), key="d", serialize=True):
        c = "D:" + key
        prev = self.cnt.get(c, 0)
        waits = self._waits(eng, reads, writes, extra=[(c, prev)] if (prev and serialize) else [])
        v = prev + 16
        self.cnt[c] = v
        sem, _ = self._sem(c, v)
        self.ops[eng].append((waits, fn, sem, 16))
        self._book(c, v, reads, writes)

    def barrier_all(self, eng="sync"):
        waits = []
        for c, v in self.cnt.items():
            if v and self.seen[eng].get(c, 0) < v:
                self.seen[eng][c] = v
                waits.append(self._sem(c, v))
        self.ops[eng].append((waits, None, None, 0))

    def full_barrier(self):
        for e in self.ENG:
            self.barrier_all(e)

    def flush(self, nc):
        with nc.Block() as blk:
            for eng in self.ENG:
                lst = self.ops[eng]
                if not lst:
                    continue
                def body(e, lst=lst):
                    for waits, fn, sem, inc in lst:
                        for s, v in waits:
                            e.wait_ge(s, v)
                        if fn is not None:
                            ins = fn(e)
                            ins.then_inc(sem, inc)
                getattr(blk, eng)(body)
        self.ops = {e: [] for e in self.ENG}


class Rot:
    def __init__(self, items):
        self.items = items
        self.i = 0
    def next(self):
        it = self.items[self.i % len(self.items)]
        self.i += 1
        return it


def build_program():
    nc = bass.Bass("TRN2", target_bir_lowering=False)
    dt = lambda name, shape, dtype=F32, kind="ExternalInput": nc.dram_tensor(name, shape, dtype, kind=kind).ap()
    x_d = dt("x", [T, D])
    mem_d = dt("mem", [256, D])
    w_in_d = dt("w_in", [D, INW])
    w_pool_d = dt("w_pool", [4, 128, 128])
    pool_scale_d = dt("pool_scale", [512])
    kig_d = dt("idx_k_norm_g", [64])
    kib_d = dt("idx_k_norm_b", [64])
    kvg_d = dt("kv_norm_g", [128])
    w_uk_d = dt("w_uk", [8, 64, 128])
    w_uv_d = dt("w_uv", [8, 128, 64])
    w_o_d = dt("w_o", [D, D])
    ln1g_d = dt("ln1_g", [D]); ln1b_d = dt("ln1_b", [D])
    w_mq_d = dt("w_mq", [D, D])
    w_mkv_d = dt("w_mkv", [D, 2 * D])
    w_mo_d = dt("w_mo", [D, D])
    ln2g_d = dt("ln2_g", [D]); ln2b_d = dt("ln2_b", [D])
    w_r_d = dt("w_router", [D, NE])
    b_r_d = dt("b_router", [NE])
    w_gu_d = dt("w_gate_up", [NE, D, 2 * D])
    b_gu_d = dt("b_gate_up", [NE, 2 * D])
    w_dn_d = dt("w_down", [NE, D, D])
    b_dn_d = dt("b_down", [NE, D])
    ln3g_d = dt("ln3_g", [D]); ln3b_d = dt("ln3_b", [D])
    out_d = dt("out", [T, D], F32, "ExternalOutput")
    h1_d = dt("h1_scr", [T, D], F32, "Internal")
    h2_d = dt("h2_scr", [T, D], F32, "Internal")
    xg_d = dt("xg_scr", [NE * CAP, D], BF16, "Internal")
    ys_d = dt("ys_scr", [NE * CAP, D], F32, "Internal")
    dbg = {}
    if DEBUG:
        dbg["h"] = dt("dbg_h", [T, D], F32, "ExternalOutput")
        dbg["mixT"] = dt("dbg_mixT", [NCH, 128, 8 * CH], BF16, "ExternalOutput")

    dumps = []
    def dump(S, name, ap2d, shape, dtype, reads):
        if not DEBUG:
            return
        d = dt("dbg_" + name, shape, dtype, "ExternalOutput")
        S.dma("sync", lambda e: e.dma_start(out=d, in_=ap2d), reads=reads, key="dbgd")

    def bc(ap1d, n):
        return ap1d.rearrange("(o n) -> o n", o=1).to_broadcast([128, n])

    with ExitStack() as top:
        sem_pool = [top.enter_context(nc.semaphore(f"s{i}")) for i in range(96)]
        S = Sched(sem_pool)
        sb = lambda es, name, shape, dtype=F32: es.enter_context(nc.sbuf_tensor(name, shape, dtype))
        ps = lambda es, name, shape, dtype=F32: es.enter_context(nc.psum_tensor(name, shape, dtype))

        ident_b = sb(top, "ident_b", [128, 128], BF16)
        ident_f = sb(top, "ident_f", [128, 128], F32)
        ones_b = sb(top, "ones_b", [128, 128], BF16)
        slots_all = sb(top, "slots_all", [128, NT, 4], I32)
        gates_all = sb(top, "gates_all", [128, NT, 4], F32)

        S.op("gpsimd", lambda e: e.memset(ident_f[:], 0.0), writes=["ident_f"])
        S.op("gpsimd", lambda e: e.affine_select(out=ident_f[:], in_=ident_f[:], pattern=[[-1, 128]],
                                                   compare_op=ALU.not_equal, fill=1.0, base=0, channel_multiplier=1),
             reads=["ident_f"], writes=["ident_f"])
        S.op("vector", lambda e: e.tensor_copy(out=ident_b[:], in_=ident_f[:]), reads=["ident_f"], writes=["ident_b"])
        S.op("vector", lambda e: e.memset(ones_b[:], 1.0), writes=["ones_b"])

        def ln_rstd(var_ap, rstd_ap, tag, scale, rname, wname):
            S.op("scalar", lambda e: e.activation(out=rstd_ap, in_=var_ap, func=AF.Ln, bias=eps_tile[:, tag:tag + 1], scale=scale),
                 reads=[rname, "eps", wname], writes=[wname])
            S.op("scalar", lambda e: e.activation(out=rstd_ap, in_=rstd_ap, func=AF.Exp, scale=-0.5),
                 reads=[wname], writes=[wname])

        eps_tile = sb(top, "eps", [128, 2], F32)
        S.op("vector", lambda e: e.memset(eps_tile[:, 0:1], 1e-5), writes=["eps"])
        S.op("vector", lambda e: e.memset(eps_tile[:, 1:2], 1e-6), reads=["eps"], writes=["eps"])

        def layer_norm_tile(r_ap, out_ap, g_bc, b_bc, stats, mv, rstd, tmp_ap, names):
            rn, on, tn = names
            for hf in range(2):
                S.op("vector", lambda e, hf=hf: e.bn_stats(out=stats[:, hf, :], in_=r_ap[:, hf * 512:(hf + 1) * 512]),
                     reads=[rn], writes=[stats.name])
            S.op("vector", lambda e: e.bn_aggr(out=mv[:], in_=stats[:].rearrange("p a b -> p (a b)")),
                 reads=[stats.name], writes=[mv.name])
            ln_rstd(mv[:, 1:2], rstd[:, 0:1], 0, 1.0, mv.name, rstd.name)
            S.op("vector", lambda e: e.tensor_scalar(out=tmp_ap, in0=r_ap, scalar1=mv[:, 0:1], scalar2=rstd[:, 0:1],
                                                      op0=ALU.subtract, op1=ALU.mult),
                 reads=[rn, mv.name, rstd.name], writes=[tn])
            S.op("vector", lambda e: e.tensor_tensor(out=tmp_ap, in0=tmp_ap, in1=g_bc[:], op=ALU.mult),
                 reads=[tn, g_bc.name], writes=[tn])
            S.op("vector", lambda e: e.tensor_tensor(out=out_ap, in0=tmp_ap, in1=b_bc[:], op=ALU.add),
                 reads=[tn, b_bc.name], writes=[on])

        with ExitStack() as p1:
            w_in_sb = sb(p1, "w_in_sb", [128, 8, INW], BF16)
            w_o_sb = sb(p1, "w_o_sb", [128, 8, D], BF16)
            wpool_sb = sb(p1, "wpool_sb", [128, 4, 128], BF16)
            wuk_sb = sb(p1, "wuk_sb", [128, 4, 128], BF16)
            wuv_sb = sb(p1, "wuv_sb", [128, 4, 2, 128], BF16)
            pscale_sb = sb(p1, "pscale_sb", [128, 4], F32)
            g1_bc = sb(p1, "g1_bc", [128, D], F32)
            b1_bc = sb(p1, "b1_bc", [128, D], F32)
            kvg_bc = sb(p1, "kvg_bc", [128, 128], F32)
            kig_bc = sb(p1, "kig_bc", [128, 64], F32)
            kib_bc = sb(p1, "kib_bc", [128, 64], F32)
            ic16 = sb(p1, "ic16", [128, 4, 16], F32)
            ckv1 = sb(p1, "ckv1", [128, NT, 130], BF16)
            ckvT = sb(p1, "ckvT", [128, T], BF16)
            kiT = sb(p1, "kiT", [128, T], BF16)
            widx = sb(p1, "widx", [128, NT, 8], F32)
            xt = [sb(p1, f"xt{i}", [128, D], F32) for i in range(2)]
            xb = [sb(p1, f"xb{i}", [128, D], BF16) for i in range(2)]
            xT = sb(p1, "xT", [128, 8, CH], BF16)
            ug = sb(p1, "ug", [128, 528], F32)
            halo = sb(p1, "halo", [128, 4, 16], F32)
            pA = sb(p1, "pA", [128, 528], F32)
            pB = sb(p1, "pB", [128, 528], F32)
            dT = sb(p1, "dT", [128, CH], BF16)
            qTs = [sb(p1, f"qT{i}", [128, 4, CH], BF16) for i in range(2)]
            qiT = sb(p1, "qiT", [128, 4, CH], BF16)
            mixTs = [sb(p1, f"mixT{i}", [128, 8, CH], BF16) for i in range(2)]
            SC = sb(p1, "SC", [128, T], F32)
            bmax = sb(p1, "bmax", [128, 1], F32)
            bmid = sb(p1, "bmid", [128, 1], F32)
            bcnt = sb(p1, "bcnt", [128, 1], F32)
            bd = sb(p1, "bd", [128, 1], F32)
            bnegl = sb(p1, "bnegl", [128, 1], F32)
            c256 = sb(p1, "c256", [128, 1], F32)
            pw2 = sb(p1, "pw2", [128, KBIS], F32)
            bsteps = sb(p1, "bsteps", [128, KBIS], F32)
            bnegh = sb(p1, "bnegh", [128, KBIS], F32)
            m128 = [sb(p1, f"m128_{i}", [128, 128], BF16) for i in range(2)]
            maskTs = [sb(p1, f"maskT{i}", [128, NT, CH], mybir.dt.uint8) for i in range(2)]
            rl = [sb(p1, f"rl{i}", [128, CH], F32) for i in range(2)]
            Eb = [sb(p1, f"Eb{i}", [128, CH], BF16) for i in range(2)]
            Pb = [sb(p1, f"Pb{i}", [128, CH], BF16) for i in range(2)]
            Eb.append(ug[:, 0:256].bitcast(BF16)); Pb.append(pB[:, 0:256].bitcast(BF16))
            qlat = [sb(p1, f"qlat{i}", [128, CH], BF16) for i in range(2)]
            olat = [sb(p1, f"olat{i}", [128, 128], BF16) for i in range(2)]
            rden = sb(p1, "rden", [128, 4], F32)
            ckn = sb(p1, "ckn", [128, 128], BF16)
            kn32 = sb(p1, "kn32", [128, 64], F32)
            kn2 = sb(p1, "kn2", [128, 128], BF16)
            tm = sb(p1, "tm", [128, 200], F32)
            olT2 = sb(p1, "olT2", [128, 2, CH], BF16)
            junk = sb(p1, "junk", [128, 128], F32)
            st1 = sb(p1, "st1", [128, 8], F32)
            bst = sb(p1, "bst", [128, 2, 6], F32)
            bmv = sb(p1, "bmv", [128, 2], F32)
            brs = sb(p1, "brs", [128, 1], F32)
            r1 = sb(p1, "r1", [128, D], F32)
            h1o = [r1, r1]
            mm = [ps(p1, f"mm{i}", [128, 512], F32) for i in range(2)]
            oacc = [ps(p1, f"oacc{i}", [128, 2, 512], F32) for i in range(2)]
            tp = [ps(p1, f"tp{i}", [128, 1024], BF16) for i in range(2)]
            mmr = Rot([0, 1]); tpr = Rot([0, 1])

            S.op("gpsimd", lambda e: e.memset(maskTs[1][:].rearrange("p a b -> p (a b)").bitcast(I32), 0), writes=["maskT1"])
            zsrc = maskTs[1][:].rearrange("p a b -> p (a b)").bitcast(BF16).rearrange("p (t d) -> p t d", d=D)
            S.dma("gpsimd", lambda e: e.dma_start(out=w_in_sb[:], in_=w_in_d.rearrange("(k p) n -> p k n", p=128)),
                  writes=["w_in_sb"], key="w0")
            S.dma("gpsimd", lambda e: e.dma_start(out=wpool_sb[:], in_=w_pool_d.rearrange("g c d -> c g d")),
                  writes=["wpool_sb"], key="w1")
            S.dma("gpsimd", lambda e: e.dma_start(out=wuk_sb[:], in_=w_uk_d.rearrange("(hp h2) d r -> (h2 d) hp r", h2=2)),
                  writes=["wuk_sb"], key="w2")
            S.op("vector", lambda e: e.memset(wuv_sb[:], 0.0), writes=["wuv_sb"])
            for h2 in range(2):
                S.dma("gpsimd", lambda e, h2=h2: e.dma_start(out=wuv_sb[:, :, h2, h2 * 64:(h2 + 1) * 64],
                                                             in_=w_uv_d.rearrange("(hp h2) r d -> h2 r hp d", h2=2)[h2]),
                      reads=["wuv_sb"], writes=["wuv_sb"], key=f"w3{h2}")
            S.dma("gpsimd", lambda e: e.dma_start(out=w_o_sb[:], in_=w_o_d.rearrange("(k p) n -> p k n", p=128)),
                  writes=["w_o_sb"], key="w4")
            S.dma("sync", lambda e: e.dma_start(out=pscale_sb[:], in_=pool_scale_d.rearrange("(g d) -> d g", d=128),
                                                allow_slow_non_contiguous=True),
                  writes=["pscale_sb"], key="c0")
            S.dma("sync", lambda e: e.dma_start(out=g1_bc[:], in_=bc(ln1g_d, D)), writes=["g1_bc"], key="c1")
            S.dma("sync", lambda e: e.dma_start(out=b1_bc[:], in_=bc(ln1b_d, D)), writes=["b1_bc"], key="c2")
            S.dma("sync", lambda e: e.dma_start(out=kvg_bc[:], in_=bc(kvg_d, 128)), writes=["kvg_bc"], key="c3")
            S.dma("sync", lambda e: e.dma_start(out=kig_bc[:], in_=bc(kig_d, 64)), writes=["kig_bc"], key="c4")
            S.dma("sync", lambda e: e.dma_start(out=kib_bc[:], in_=bc(kib_d, 64)), writes=["kib_bc"], key="c5")
            for g in range(4):
                w = 2 ** (g + 1)
                S.op("gpsimd", lambda e, g=g, w=w: e.memset(ic16[:, g, :], 1.0 / w), reads=["ic16"], writes=["ic16"])
                for t in range(w - 1):
                    S.op("gpsimd", lambda e, g=g, t=t: e.memset(ic16[:, g, t:t + 1], 1.0 / (t + 1)), reads=["ic16"], writes=["ic16"])
            S.op("gpsimd", lambda e: e.memset(halo[:], 0.0), writes=["halo"])
            S.op("gpsimd", lambda e: e.memset(c256[:], 256.0), writes=["c256"])
            for i_ in range(KBIS):
                S.op("gpsimd", lambda e, i_=i_: e.memset(pw2[:, i_:i_ + 1], 2.0 ** (-i_)), reads=["pw2"], writes=["pw2"])
            S.op("gpsimd", lambda e: e.memset(ckv1[:, :, 128:130], 1.0), writes=["ckv1"])

            def front(c):
                for t in range(4):
                    tg = 4 * c + t
                    b = tg % 2
                    S.dma("sync", lambda e, tg=tg, b=b: e.dma_start(out=xt[b][:], in_=x_d[tg * 128:(tg + 1) * 128, :]),
                          writes=[f"xt{b}"], key=f"x{b}")
                    S.op("scalar", lambda e, b=b: e.activation(out=xb[b][:], in_=xt[b][:], func=AF.Copy),
                         reads=[f"xt{b}"], writes=[f"xb{b}"])
                    for kh in range(2):
                        ti = tpr.next()
                        def tr(e, b=b, kh=kh, ti=ti):
                            ins = None
                            for kk in range(4):
                                k = kh * 4 + kk
                                ins = e.transpose(out=tp[ti][:, kk * 128:(kk + 1) * 128], in_=xb[b][:, k * 128:(k + 1) * 128],
                                                  identity=ident_b[:])
                            return ins
                        S.op("tensor", tr, reads=[f"xb{b}", "ident_b"], writes=[f"tp{ti}"])
                        S.op("vector", lambda e, kh=kh, ti=ti, t=t: e.tensor_copy(
                            out=xT[:, kh * 4:(kh + 1) * 4, t * 128:(t + 1) * 128],
                            in_=tp[ti][:, 0:512].rearrange("p (k n) -> p k n", k=4)),
                            reads=[f"tp{ti}"], writes=["xT"])

                def inproj_fm(col0):
                    bi = mmr.next()
                    def f(e, col0=col0, bi=bi):
                        ins = None
                        for k in range(8):
                            ins = e.matmul(mm[bi][:], lhsT=w_in_sb[:, k, col0:col0 + 128], rhs=xT[:, k, :],
                                           start=(k == 0), stop=(k == 7))
                        return ins
                    S.op("tensor", f, reads=["w_in_sb", "xT"], writes=[f"mm{bi}"])
                    return bi

                for g in range(4):
                    bi = inproj_fm(g * 128)
                    S.op("gpsimd", lambda e, g=g: e.tensor_copy(out=ug[:, 0:16], in_=halo[:, g, :]),
                         reads=["halo", "ug"], writes=["ug"])
                    S.op("scalar", lambda e, bi=bi: e.activation(out=ug[:, 16:528], in_=mm[bi][:], func=AF.Copy),
                         reads=[f"mm{bi}", "ug"], writes=["ug"])
                    S.op("gpsimd", lambda e, g=g: e.tensor_copy(out=halo[:, g, :], in_=ug[:, 512:528]),
                         reads=["ug"], writes=["halo"])
                    src, srcn = ug, "ug"
                    bufs = [(pA, "pA"), (pB, "pB")]
                    for lv in range(g + 1):
                        s = 2 ** lv
                        lo = 2 ** (lv + 1) - 1
                        dst, dstn = bufs[lv % 2]
                        S.op("gpsimd", lambda e, src=src, dst=dst, s=s, lo=lo: e.tensor_tensor(
                            out=dst[:, lo:528], in0=src[:, lo:528], in1=src[:, lo - s:528 - s], op=ALU.add),
                            reads=[srcn, dstn], writes=[dstn])
                        src, srcn = dst, dstn
                    w = 2 ** (g + 1)
                    S.op("vector", lambda e, src=src, w=w: e.scalar_tensor_tensor(
                        out=dT[:], in0=src[:, 16:528], scalar=1.0 / w, in1=ug[:, 16:528], op0=ALU.mult, op1=ALU.subtract),
                        reads=[srcn, "ug"], writes=["dT"])
                    if c == 0:
                        S.op("vector", lambda e, src=src, g=g: e.tensor_tensor(out=junk[:, 0:16], in0=src[:, 16:32], in1=ic16[:, g, :], op=ALU.mult),
                             reads=[srcn, "ic16"], writes=["junk"])
                        S.op("vector", lambda e: e.tensor_tensor(out=dT[:, 0:16], in0=junk[:, 0:16], in1=ug[:, 16:32], op=ALU.subtract),
                             reads=["junk", "ug", "dT"], writes=["dT"])
                    bi2 = mmr.next()
                    S.op("tensor", lambda e, g=g, bi2=bi2: e.matmul(mm[bi2][:], lhsT=wpool_sb[:, g, :], rhs=dT[:], start=True, stop=True),
                         reads=["wpool_sb", "dT"], writes=[f"mm{bi2}"])
                    S.op("scalar", lambda e, g=g, bi2=bi2: e.activation(out=mixTs[c % 2][:, g, :], in_=mm[bi2][:], func=AF.Copy, scale=pscale_sb[:, g:g + 1]),
                         reads=[f"mm{bi2}", "pscale_sb", f"mixT{c % 2}"], writes=[f"mixT{c % 2}"])
                for j in range(4):
                    bi = inproj_fm(512 + j * 128)
                    S.op("scalar", lambda e, j=j, bi=bi: e.activation(out=qTs[c % 2][:, j, :], in_=mm[bi][:], func=AF.Copy),
                         reads=[f"mm{bi}"], writes=[f"qT{c % 2}"])
                for j in range(4):
                    bi = inproj_fm(1152 + j * 128)
                    S.op("vector", lambda e, j=j, bi=bi: e.tensor_copy(out=qiT[:, j, :], in_=mm[bi][:]),
                         reads=[f"mm{bi}"], writes=["qiT"])
                tiA = tpr.next(); tiB = tpr.next()
                for t in range(4):
                    tg = 4 * c + t
                    bi = mmr.next()
                    def f(e, t=t, bi=bi):
                        ins = None
                        for k in range(8):
                            ins = e.matmul(mm[bi][:, 0:128], lhsT=xT[:, k, t * 128:(t + 1) * 128], rhs=w_in_sb[:, k, 1024:1152],
                                           start=(k == 0), stop=(k == 7))
                        for k in range(8):
                            ins = e.matmul(mm[bi][:, 128:200], lhsT=xT[:, k, t * 128:(t + 1) * 128], rhs=w_in_sb[:, k, 1664:1736],
                                           start=(k == 0), stop=(k == 7))
                        return ins
                    S.op("tensor", f, reads=["w_in_sb", "xT"], writes=[f"mm{bi}"])
                    S.op("scalar", lambda e, bi=bi: e.activation(out=tm[:], in_=mm[bi][:, 0:200], func=AF.Copy),
                         reads=[f"mm{bi}"], writes=["tm"])
                    S.op("scalar", lambda e: e.activation(out=junk[:], in_=tm[:, 0:128], func=AF.Square, accum_out=st1[:, 0:1]),
                         reads=["tm", "st1"], writes=["junk", "st1"])
                    ln_rstd(st1[:, 0:1], st1[:, 1:2], 1, 1.0 / 128, "st1", "st1")
                    S.op("vector", lambda e: e.scalar_tensor_tensor(out=ckn[:], in0=tm[:, 0:128], scalar=st1[:, 1:2], in1=kvg_bc[:],
                                                                     op0=ALU.mult, op1=ALU.mult),
                         reads=["tm", "st1", "kvg_bc"], writes=["ckn"])
                    S.op("gpsimd", lambda e, tg=tg: e.tensor_copy(out=ckv1[:, tg, 0:128], in_=ckn[:]), reads=["ckn", "ckv1"], writes=["ckv1"])
                    S.op("vector", lambda e: e.bn_stats(out=bst[:, 0, :], in_=tm[:, 128:192]), reads=["tm"], writes=["bst"])
                    S.op("vector", lambda e: e.bn_aggr(out=bmv[:], in_=bst[:, 0, :]), reads=["bst"], writes=["bmv"])
                    ln_rstd(bmv[:, 1:2], brs[:, 0:1], 0, 1.0, "bmv", "brs")
                    S.op("vector", lambda e: e.tensor_scalar(out=kn32[:], in0=tm[:, 128:192], scalar1=bmv[:, 0:1], scalar2=brs[:, 0:1],
                                                              op0=ALU.subtract, op1=ALU.mult),
                         reads=["tm", "bmv", "brs"], writes=["kn32"])
                    S.op("vector", lambda e, tg=tg: e.tensor_copy(out=widx[:, tg, :], in_=tm[:, 192:200]),
                         reads=["tm", "widx"], writes=["widx"])
                    S.op("gpsimd", lambda e: e.tensor_tensor(out=kn32[:], in0=kn32[:], in1=kig_bc[:], op=ALU.mult),
                         reads=["kn32", "kig_bc"], writes=["kn32"])
                    S.op("gpsimd", lambda e: e.tensor_tensor(out=kn2[:, 0:64], in0=kn32[:], in1=kib_bc[:], op=ALU.add),
                         reads=["kn32", "kib_bc", "kn2"], writes=["kn2"])
                    S.op("gpsimd", lambda e: e.tensor_copy(out=kn2[:, 64:128], in_=kn2[:, 0:64]), reads=["kn2"], writes=["kn2"])
                    S.op("tensor", lambda e, t=t, tiA=tiA: e.transpose(out=tp[tiA][:, t * 128:(t + 1) * 128], in_=ckn[:], identity=ident_b[:]),
                         reads=["ckn", "ident_b"], writes=[f"tp{tiA}"])
                    S.op("tensor", lambda e, t=t, tiB=tiB: e.transpose(out=tp[tiB][:, t * 128:(t + 1) * 128], in_=kn2[:], identity=ident_b[:]),
                         reads=["kn2", "ident_b"], writes=[f"tp{tiB}"])
                S.op("scalar", lambda e, c=c, tiA=tiA: e.activation(out=ckvT[:, c * CH:(c + 1) * CH], in_=tp[tiA][:, 0:512], func=AF.Copy),
                     reads=[f"tp{tiA}", "ckvT"], writes=["ckvT"])
                S.op("scalar", lambda e, c=c, tiB=tiB: e.activation(out=kiT[:, c * CH:(c + 1) * CH], in_=tp[tiB][:, 0:512], func=AF.Copy),
                     reads=[f"tp{tiB}", "kiT"], writes=["kiT"])


            def tile_sel(c, t):
                qt = 4 * c + t
                qt = 4 * c + t
                N = 128 * (qt + 1)
                nkc = (N + 511) // 512
                for kc in range(nkc):
                    k0 = kc * 512
                    kw = min(512, N - k0)
                    for h in range(8):
                        hp, h2 = h // 2, h % 2
                        bi = mmr.next()
                        S.op("tensor", lambda e, hp=hp, h2=h2, t=t, bi=bi, k0=k0, kw=kw: e.matmul(
                            mm[bi][:, 0:kw], lhsT=qiT[h2 * 64:(h2 + 1) * 64, hp, t * 128:(t + 1) * 128],
                            rhs=kiT[h2 * 64:(h2 + 1) * 64, k0:k0 + kw], start=True, stop=True),
                            reads=["qiT", "kiT"], writes=[f"mm{bi}"])
                        ri = h % 2
                        S.op("scalar", lambda e, bi=bi, ri=ri, kw=kw: e.activation(out=rl[ri][:, 0:kw], in_=mm[bi][:, 0:kw], func=AF.Relu),
                             reads=[f"mm{bi}"], writes=[f"rl{ri}"])
                        if h == 0:
                            S.op("vector", lambda e, ri=ri, k0=k0, kw=kw, qt=qt: e.tensor_scalar(
                                out=SC[:, k0:k0 + kw], in0=rl[ri][:, 0:kw], scalar1=widx[:, qt, 0:1], scalar2=None, op0=ALU.mult),
                                reads=[f"rl{ri}", "widx", "SC"], writes=["SC"])
                        else:
                            S.op("vector", lambda e, ri=ri, k0=k0, kw=kw, qt=qt, h=h: e.scalar_tensor_tensor(
                                out=SC[:, k0:k0 + kw], in0=rl[ri][:, 0:kw], scalar=widx[:, qt, h:h + 1], in1=SC[:, k0:k0 + kw],
                                op0=ALU.mult, op1=ALU.add),
                                reads=[f"rl{ri}", "widx", "SC"], writes=["SC"])
                if N > 256:
                    S.op("vector", lambda e, N=N: e.tensor_reduce(out=bmax[:], in_=SC[:, 0:N], axis=mybir.AxisListType.X, op=ALU.max,
                                                                   apply_absolute_value=True), reads=["SC"], writes=["bmax"])
                    S.op("vector", lambda e: e.tensor_scalar(out=bmax[:], in0=bmax[:], scalar1=1.0001, scalar2=1e-20, op0=ALU.mult, op1=ALU.add),
                         reads=["bmax"], writes=["bmax"])
                S.op("gpsimd", lambda e, qt=qt: e.affine_select(out=SC[:, qt * 128:(qt + 1) * 128], in_=SC[:, qt * 128:(qt + 1) * 128],
                                                                 pattern=[[-1, 128]], compare_op=ALU.is_ge, fill=NEG, base=0, channel_multiplier=1),
                     reads=["SC"], writes=["SC"])
                if N > 256:
                    S.op("vector", lambda e, N=N: e.tensor_scalar(out=bmid[:], in0=bmax[:], scalar1=0.0, scalar2=None, op0=ALU.mult),
                         reads=["bmax"], writes=["bmid"])
                    S.op("vector", lambda e: e.tensor_scalar(out=bsteps[:], in0=pw2[:], scalar1=bmax[:, 0:1], scalar2=None, op0=ALU.mult),
                         reads=["pw2", "bmax"], writes=["bsteps"])
                    S.op("vector", lambda e: e.tensor_scalar(out=bnegh[:], in0=bsteps[:], scalar1=-0.5, scalar2=None, op0=ALU.mult),
                         reads=["bsteps"], writes=["bnegh"])
                    for it_ in range(KBIS):
                        S.op("vector", lambda e, N=N: e.tensor_scalar(out=xT[:].rearrange("p k n -> p (k n)").bitcast(mybir.dt.uint8)[:, 0:N], in0=SC[:, 0:N], scalar1=bmid[:, 0:1], scalar2=0.0,
                                                                       op0=ALU.is_ge, op1=ALU.add, accum_out=bcnt[:, 0:1]),
                             reads=["SC", "bmid"], writes=["xT", "bcnt"])
                        S.op("vector", lambda e, it_=it_: e.tensor_scalar(out=bd[:], in0=bcnt[:], scalar1=c256[:, 0:1], scalar2=bsteps[:, it_:it_ + 1],
                                                                          op0=ALU.is_ge, op1=ALU.mult),
                             reads=["bcnt", "c256", "bsteps"], writes=["bd"])
                        sc2 = bnegh[:, it_:it_ + 1] if it_ < KBIS - 1 else bnegl[:, 0:1]
                        if it_ == KBIS - 1:
                            S.op("vector", lambda e, it_=it_: e.tensor_scalar(out=bnegl[:], in0=bsteps[:, it_:it_ + 1], scalar1=-1.0, scalar2=None, op0=ALU.mult),
                                 reads=["bsteps"], writes=["bnegl"])
                        S.op("vector", lambda e, sc2=sc2: e.tensor_scalar(out=bmid[:], in0=bmid[:], scalar1=bd[:, 0:1], scalar2=sc2,
                                                                          op0=ALU.add, op1=ALU.add),
                             reads=["bmid", "bd", "bnegh", "bnegl"], writes=["bmid"])

            def tile_mask(c, t):
                qt = 4 * c + t
                N = 128 * (qt + 1)
                for kt in range(qt + 1):
                    mi = kt % 2
                    if N > 256:
                        S.op("vector", lambda e, kt=kt, mi=mi: e.tensor_scalar(out=m128[mi][:], in0=SC[:, kt * 128:(kt + 1) * 128],
                                                                               scalar1=bmid[:, 0:1], scalar2=None, op0=ALU.is_ge),
                             reads=["SC", "bmid"], writes=[f"m128_{mi}"])
                    else:
                        S.op("vector", lambda e, kt=kt, mi=mi: e.tensor_scalar(out=m128[mi][:], in0=SC[:, kt * 128:(kt + 1) * 128],
                                                                               scalar1=-0.5e30, scalar2=None, op0=ALU.is_ge),
                             reads=["SC"], writes=[f"m128_{mi}"])
                    ti = tpr.next()
                    S.op("tensor", lambda e, mi=mi, ti=ti: e.transpose(out=tp[ti][:, 0:128], in_=m128[mi][:], identity=ident_b[:]),
                         reads=[f"m128_{mi}", "ident_b"], writes=[f"tp{ti}"])
                    S.op("scalar", lambda e, kt=kt, t=t, ti=ti: e.activation(out=maskTs[c % 2][:, kt, t * 128:(t + 1) * 128], in_=tp[ti][:, 0:128], func=AF.Copy),
                         reads=[f"tp{ti}", f"maskT{c % 2}"], writes=[f"maskT{c % 2}"])


            def att_head(c, h, use_dve=False, look=1):
                nkt = 4 * c + 4
                hp, h2 = h // 2, h % 2
                qi = h % 2
                oi = h % 2
                bi = mmr.next()
                S.op("tensor", lambda e, hp=hp, h2=h2, bi=bi: e.matmul(mm[bi][:], lhsT=wuk_sb[h2 * 64:(h2 + 1) * 64, hp, :],
                                                                    rhs=qTs[c % 2][h2 * 64:(h2 + 1) * 64, hp, :], start=True, stop=True),
                     reads=["wuk_sb", f"qT{c % 2}"], writes=[f"mm{bi}"])
                S.op("scalar", lambda e, bi=bi, qi=qi: e.activation(out=qlat[qi][:], in_=mm[bi][:], func=AF.Copy, scale=0.125),
                     reads=[f"mm{bi}"], writes=[f"qlat{qi}"])
                def qk(kt):
                    j0 = max(0, kt - 4 * c)
                    ncol = (4 - j0) * 128
                    c0 = j0 * 128
                    bi = mmr.next()
                    S.op("tensor", lambda e, kt=kt, bi=bi, qi=qi, c0=c0, ncol=ncol: e.matmul(
                        mm[bi][:, 0:ncol], lhsT=ckvT[:, kt * 128:(kt + 1) * 128], rhs=qlat[qi][:, c0:c0 + ncol], start=True, stop=True),
                        reads=["ckvT", f"qlat{qi}"], writes=[f"mm{bi}"])
                    return bi, j0, ncol, c0
                pend = [qk(k_) for k_ in range(min(look, nkt))]
                for kt in range(nkt):
                    bi, j0, ncol, c0 = pend.pop(0)
                    if kt + look < nkt:
                        pend.append(qk(kt + look))
                    ei = kt % (3 if look > 1 else 2)
                    S.op("scalar", lambda e, bi=bi, ei=ei, ncol=ncol: e.activation(out=Eb[ei][:, 0:ncol], in_=mm[bi][:, 0:ncol], func=AF.Exp),
                         reads=[f"mm{bi}"], writes=[f"Eb{ei}"])
                    S.op("vector" if (use_dve and kt % 2 == 0) else "gpsimd", lambda e, ei=ei, kt=kt, c0=c0, ncol=ncol, c=c: e.tensor_tensor(
                        out=Pb[ei][:, 0:ncol], in0=Eb[ei][:, 0:ncol], in1=maskTs[c % 2][:, kt, c0:c0 + ncol], op=ALU.mult),
                        reads=[f"Eb{ei}", f"maskT{c % 2}"], writes=[f"Pb{ei}"])
                    def pv(e, kt=kt, j0=j0, ei=ei, oi=oi, c=c):
                        ins = None
                        for j in range(j0, 4):
                            bank, off = (0, j * 129) if j < 3 else (1, 0)
                            ins = e.matmul(oacc[oi][:, bank, off:off + 129], lhsT=Pb[ei][:, (j - j0) * 128:(j - j0 + 1) * 128],
                                           rhs=ckv1[:, kt, 0:129], start=(kt == 0 and j in (0, 3)), stop=(kt == 4 * c + j),
                                           skip_group_check=True)
                        return ins
                    S.op("tensor", pv, reads=[f"Pb{ei}", "ckv1"], writes=[f"oacc{oi}"])
                ti = tpr.next()
                for j in range(4):
                    bank, off = (0, j * 129) if j < 3 else (1, 0)
                    li = j % 2
                    S.op("scalar", lambda e, oi=oi, bank=bank, off=off, j=j: e.activation(out=rden[:, j:j + 1], in_=oacc[oi][:, bank, off + 128:off + 129], func=AF.Ln),
                         reads=[f"oacc{oi}", "rden"], writes=["rden"])
                    S.op("scalar", lambda e, j=j: e.activation(out=rden[:, j:j + 1], in_=rden[:, j:j + 1], func=AF.Exp, scale=-1.0),
                         reads=["rden"], writes=["rden"])
                    S.op("scalar", lambda e, oi=oi, bank=bank, off=off, j=j, li=li: e.activation(
                        out=olat[li][:], in_=oacc[oi][:, bank, off:off + 128], func=AF.Copy, scale=rden[:, j:j + 1]),
                        reads=[f"oacc{oi}", "rden"], writes=[f"olat{li}"])
                    S.op("tensor", lambda e, li=li, ti=ti, j=j: e.transpose(out=tp[ti][:, j * 128:(j + 1) * 128], in_=olat[li][:], identity=ident_b[:]),
                         reads=[f"olat{li}", "ident_b"], writes=[f"tp{ti}"])
                S.op("scalar", lambda e, ti=ti, h2=h2: e.activation(out=olT2[:, h2, :], in_=tp[ti][:, 0:512], func=AF.Copy),
                     reads=[f"tp{ti}", "olT2"], writes=["olT2"])
                if h2 == 1:
                    bi = mmr.next()
                    def f(e, hp=hp, bi=bi):
                        e.matmul(mm[bi][:], lhsT=wuv_sb[:, hp, 0, :], rhs=olT2[:, 0, :], start=True, stop=False)
                        return e.matmul(mm[bi][:], lhsT=wuv_sb[:, hp, 1, :], rhs=olT2[:, 1, :], start=False, stop=True)
                    S.op("tensor", f, reads=["wuv_sb", "olT2"], writes=[f"mm{bi}"])
                    S.op("scalar", lambda e, hp=hp, bi=bi, c=c: e.activation(out=mixTs[c % 2][:, 4 + hp, :], in_=mm[bi][:], func=AF.Copy),
                         reads=[f"mm{bi}", f"mixT{c % 2}"], writes=[f"mixT{c % 2}"])


            def out_ln(c):
                if DEBUG == "1a" and c == 0:
                    dump(S, "qT", qTs[0][:].rearrange("p k n -> p (k n)"), [128, 4 * CH], BF16, ["qT0"])
                    dump(S, "qiT", qiT[:].rearrange("p k n -> p (k n)"), [128, 4 * CH], BF16, ["qiT"])
                    dump(S, "maskT", maskTs[0][:, 0:4, :].rearrange("p k n -> p (k n)"), [128, 4 * CH], mybir.dt.uint8, ["maskT0"])
                    dump(S, "olT2", olT2[:].rearrange("p k n -> p (k n)"), [128, 2 * CH], BF16, ["olT2"])
                    dump(S, "SC", SC[:, 0:512], [128, 512], F32, ["SC"])
                if DEBUG == "1a" and c == 7:
                    dump(S, "ckv1", ckv1[:].rearrange("p k n -> p (k n)"), [128, NT * 130], BF16, ["ckv1"])
                    dump(S, "ckvT", ckvT[:], [128, T], BF16, ["ckvT"])
                    dump(S, "kiT", kiT[:], [128, T], BF16, ["kiT"])
                    dump(S, "widx", widx[:].rearrange("p k n -> p (k n)"), [128, NT * 8], F32, ["widx"])
                if DEBUG == "1a":
                    S.dma("sync", lambda e, c=c: e.dma_start(out=dbg["mixT"][c], in_=mixTs[c % 2][:].rearrange("p k n -> p (k n)")),
                          reads=[f"mixT{c % 2}"], key="dbgm")

                def xload(t_):
                    tg_ = 4 * c + t_
                    b_ = tg_ % 2
                    S.dma("sync", lambda e: e.dma_start(out=xt[b_][:], in_=x_d[tg_ * 128:(tg_ + 1) * 128, :]),
                          writes=[f"xt{b_}"], key=f"x{b_}")
                xload(0)
                for t in range(4):
                    tg = 4 * c + t
                    b = tg % 2
                    if t < 3:
                        xload(t + 1)
                    for hf in range(2):
                        bi = mmr.next()
                        def f(e, t=t, hf=hf, bi=bi):
                            ins = None
                            for k in range(8):
                                ins = e.matmul(mm[bi][:], lhsT=mixTs[c % 2][:, k, t * 128:(t + 1) * 128], rhs=w_o_sb[:, k, hf * 512:(hf + 1) * 512],
                                               start=(k == 0), stop=(k == 7))
                            return ins
                        S.op("tensor", f, reads=[f"mixT{c % 2}", "w_o_sb"], writes=[f"mm{bi}"])
                        S.op("vector", lambda e, b=b, hf=hf, bi=bi: e.scalar_tensor_tensor(
                            out=r1[:, hf * 512:(hf + 1) * 512], in0=xt[b][:, hf * 512:(hf + 1) * 512], scalar=ALPHA, in1=mm[bi][:],
                            op0=ALU.mult, op1=ALU.add),
                            reads=[f"xt{b}", f"mm{bi}", "r1"], writes=["r1"])
                    layer_norm_tile(r1[:], h1o[b][:], g1_bc, b1_bc, bst, bmv, brs, r1[:], ("r1", "r1", "r1"))
                    S.dma("sync", lambda e, tg=tg, b=b: e.dma_start(out=h1_d[tg * 128:(tg + 1) * 128, :], in_=h1o[b][:]),
                          reads=["r1"], key="h1s0")
                    if DEBUG == "1a":
                        S.dma("sync", lambda e, tg=tg, b=b: e.dma_start(out=dbg["h"][tg * 128:(tg + 1) * 128, :], in_=h1o[b][:]),
                              reads=["r1"], key="dbgh0")

            for c in range(NCH):
                front(c)
                if c == 0:
                    for zi in range(NE * CAP // 1024):
                        S.dma("sync", lambda e, zi=zi: e.dma_start(out=xg_d[zi * 1024:(zi + 1) * 1024, :].rearrange("(t p) d -> p t d", p=128), in_=zsrc),
                              reads=["maskT1"], key="zf", serialize=False)
                    S.lastw["xg_d"] = ("D:zf", S.cnt["D:zf"])
                for t in range(4):
                    tile_sel(c, t)
                    if c > 0:
                        att_head(c - 1, 2 * t)
                        att_head(c - 1, 2 * t + 1)
                    tile_mask(c, t)
                if c > 0:
                    out_ln(c - 1)
            S.full_barrier()
            mm.append(tp[1][:].bitcast(F32))
            mmr.items = [0, 1, 2]
            tpr.items = [0]
            for h in range(8):
                att_head(NCH - 1, h, use_dve=True, look=2)
            out_ln(NCH - 1)
            S.full_barrier()
            S.flush(nc)

        if DEBUG == "1a":
            return nc

        with ExitStack() as p2:
            w_mq_sb = sb(p2, "w_mq_sb", [128, 8, D], BF16)
            w_mo_sb = sb(p2, "w_mo_sb", [128, 8, D], BF16)
            w_mkv_sb = sb(p2, "w_mkv_sb", [128, 8, 2 * D], BF16)
            mb = sb(p2, "mb", [128, 2, D], BF16)
            memT = sb(p2, "memT", [128, 8, 256], BF16)
            KmT = sb(p2, "KmT", [128, 8, 256], BF16)
            Vm = sb(p2, "Vm", [128, 2, D], BF16)
            g2_bc = sb(p2, "g2_bc", [128, D], F32)
            b2_bc = sb(p2, "b2_bc", [128, D], F32)
            wr_sb = sb(p2, "wr_sb", [128, 8, NE], F32)
            br_bc = sb(p2, "br_bc", [128, NE], F32)
            ustr_f = sb(p2, "ustr_f", [128, 128], F32)
            ustr_b = sb(p2, "ustr_b", [128, 128], BF16)
            cb_i = sb(p2, "cb_i", [128, NE], I32)
            cbase = sb(p2, "cbase", [128, NE], F32)
            caphi = sb(p2, "caphi", [128, NE], F32)
            h1cs = [sb(p2, f"h1c{i}", [128, 4, D], F32) for i in range(2)]
            h1b = [sb(p2, f"h1b{i}", [128, D], BF16) for i in range(2)]
            hTs = [sb(p2, f"hT{i}", [128, 8, CH], BF16) for i in range(2)]
            qmTs = [sb(p2, f"qmT{i}", [128, 8, CH], BF16) for i in range(2)]
            Pm = [sb(p2, f"Pm{i}", [128, CH], BF16) for i in range(2)]
            rdn = sb(p2, "rdn", [128, CH], F32)
            omTs = [sb(p2, f"omT{i}", [128, 8, CH], BF16) for i in range(2)]
            r2 = sb(p2, "r2", [128, D], F32)
            tmpn2 = sb(p2, "tmpn2", [128, D], F32)
            h2o = [sb(p2, f"h2o{i}", [128, D], F32) for i in range(2)]
            h2b = [sb(p2, f"h2b{i}", [128, D], BF16) for i in range(2)]
            h2T = sb(p2, "h2T", [128, 8, 128], F32)
            lg = sb(p2, "lg", [128, NE], F32)
            mx8r = sb(p2, "mx8r", [128, 8], F32)
            negm = sb(p2, "negm", [128, 1], F32)
            ex4 = sb(p2, "ex4", [128, 4], F32)
            gsum = sb(p2, "gsum", [128, 1], F32)
            selb = sb(p2, "selb", [128, NE], BF16)
            slotm = sb(p2, "slotm", [128, NE], F32)
            ohp = sb(p2, "ohp", [128, NE], F32)
            slotf = sb(p2, "slotf", [128, 4], F32)
            bst2 = sb(p2, "bst2", [128, 2, 6], F32)
            bmv2 = sb(p2, "bmv2", [128, 2], F32)
            brs2 = sb(p2, "brs2", [128, 1], F32)
            mm = [ps(p2, f"mmB{i}", [128, 512], F32) for i in range(6)]
            tp = [ps(p2, f"tpB{i}", [128, 1024], BF16) for i in range(2)]
            mmr = Rot(list(range(6))); tpr = Rot([0, 1])

            S.dma("gpsimd", lambda e: e.dma_start(out=w_mkv_sb[:], in_=w_mkv_d.rearrange("(k p) n -> p k n", p=128)), writes=["w_mkv_sb"], key="w0")
            S.dma("gpsimd", lambda e: e.dma_start(out=mb[:], in_=mem_d.rearrange("(t p) d -> p t d", p=128)), writes=["mb"], key="w1")
            S.dma("gpsimd", lambda e: e.dma_start(out=w_mq_sb[:], in_=w_mq_d.rearrange("(k p) n -> p k n", p=128)), writes=["w_mq_sb"], key="w2")
            S.dma("gpsimd", lambda e: e.dma_start(out=w_mo_sb[:], in_=w_mo_d.rearrange("(k p) n -> p k n", p=128)), writes=["w_mo_sb"], key="w4")
            S.dma("sync", lambda e: e.dma_start(out=g2_bc[:], in_=bc(ln2g_d, D)), writes=["g2_bc"], key="c1")
            S.dma("sync", lambda e: e.dma_start(out=b2_bc[:], in_=bc(ln2b_d, D)), writes=["b2_bc"], key="c2")
            S.dma("sync", lambda e: e.dma_start(out=wr_sb[:], in_=w_r_d.rearrange("(k p) n -> p k n", p=128)), writes=["wr_sb"], key="c3")
            S.dma("sync", lambda e: e.dma_start(out=br_bc[:], in_=bc(b_r_d, NE)), writes=["br_bc"], key="c4")
            S.op("gpsimd", lambda e: e.memset(ustr_f[:], 1.0), writes=["ustr_f"])
            S.op("gpsimd", lambda e: e.affine_select(out=ustr_f[:], in_=ustr_f[:], pattern=[[1, 128]], compare_op=ALU.is_gt, fill=0.0,
                                                       base=0, channel_multiplier=-1), reads=["ustr_f"], writes=["ustr_f"])
            S.op("vector", lambda e: e.tensor_copy(out=ustr_b[:], in_=ustr_f[:]), reads=["ustr_f"], writes=["ustr_b"])
            S.op("gpsimd", lambda e: e.iota(out=cb_i[:], pattern=[[CAP, NE]], base=0, channel_multiplier=0), writes=["cb_i"])
            S.op("vector", lambda e: e.tensor_copy(out=cbase[:], in_=cb_i[:]), reads=["cb_i"], writes=["cbase"])
            S.op("vector", lambda e: e.tensor_scalar(out=caphi[:], in0=cbase[:], scalar1=float(CAP - 1), scalar2=None, op0=ALU.add),
                 reads=["cbase"], writes=["caphi"])
            for mt in range(2):
                for kh in range(2):
                    ti = tpr.next()
                    def tr(e, mt=mt, kh=kh, ti=ti):
                        ins = None
                        for kk in range(4):
                            k = kh * 4 + kk
                            ins = e.transpose(out=tp[ti][:, kk * 128:(kk + 1) * 128], in_=mb[:, mt, k * 128:(k + 1) * 128], identity=ident_b[:])
                        return ins
                    S.op("tensor", tr, reads=["mb", "ident_b"], writes=[f"tp{ti}"])
                    S.op("vector", lambda e, mt=mt, kh=kh, ti=ti: e.tensor_copy(
                        out=memT[:, kh * 4:(kh + 1) * 4, mt * 128:(mt + 1) * 128], in_=tp[ti][:, 0:512].rearrange("p (k n) -> p k n", k=4)),
                        reads=[f"tp{ti}", "memT"], writes=["memT"])
            for cc in range(8):
                bi = mmr.next()
                def f(e, cc=cc, bi=bi):
                    ins = None
                    for k in range(8):
                        ins = e.matmul(mm[bi][:, 0:256], lhsT=w_mkv_sb[:, k, cc * 128:(cc + 1) * 128], rhs=memT[:, k, :], start=(k == 0), stop=(k == 7))
                    return ins
                S.op("tensor", f, reads=["w_mkv_sb", "memT"], writes=[f"mm{bi}"])
                S.op("vector", lambda e, cc=cc, bi=bi: e.tensor_copy(out=KmT[:, cc, :], in_=mm[bi][:, 0:256]), reads=[f"mm{bi}", "KmT"], writes=["KmT"])
            for mt in range(2):
                for hf in range(2):
                    bi = mmr.next()
                    def f(e, mt=mt, hf=hf, bi=bi):
                        ins = None
                        for k in range(8):
                            ins = e.matmul(mm[bi][:], lhsT=memT[:, k, mt * 128:(mt + 1) * 128], rhs=w_mkv_sb[:, k, D + hf * 512:D + (hf + 1) * 512],
                                           start=(k == 0), stop=(k == 7))
                        return ins
                    S.op("tensor", f, reads=["w_mkv_sb", "memT"], writes=[f"mm{bi}"])
                    S.op("scalar", lambda e, mt=mt, hf=hf, bi=bi: e.activation(out=Vm[:, mt, hf * 512:(hf + 1) * 512], in_=mm[bi][:], func=AF.Copy),
                         reads=[f"mm{bi}", "Vm"], writes=["Vm"])

            def stageA(c, part):
                p_ = c % 2
                h1c, hT, qmT, omT = h1cs[p_], hTs[p_], qmTs[p_], omTs[p_]
                h1n, hTn, qmn, omn = f"h1c{p_}", f"hT{p_}", f"qmT{p_}", f"omT{p_}"
                if part == 0:
                    S.dma("sync", lambda e, c=c, h1c=h1c: e.dma_start(out=h1c[:], in_=h1_d[c * CH:(c + 1) * CH, :].rearrange("(t p) d -> p t d", p=128)),
                          writes=[h1n], key=f"h1l{p_}")
                for t in ([0, 1] if part == 0 else [2, 3] if part == 1 else []):
                    b = t % 2
                    S.op("scalar", lambda e, b=b, t=t: e.activation(out=h1b[b][:], in_=h1c[:, t, :], func=AF.Copy),
                         reads=[h1n], writes=[f"h1b{b}"])
                    for kh in range(2):
                        ti = tpr.next()
                        def tr(e, b=b, kh=kh, ti=ti):
                            ins = None
                            for kk in range(4):
                                k = kh * 4 + kk
                                ins = e.transpose(out=tp[ti][:, kk * 128:(kk + 1) * 128], in_=h1b[b][:, k * 128:(k + 1) * 128], identity=ident_b[:])
                            return ins
                        S.op("tensor", tr, reads=[f"h1b{b}", "ident_b"], writes=[f"tp{ti}"])
                        S.op("vector", lambda e, kh=kh, ti=ti, t=t: e.tensor_copy(
                            out=hT[:, kh * 4:(kh + 1) * 4, t * 128:(t + 1) * 128], in_=tp[ti][:, 0:512].rearrange("p (k n) -> p k n", k=4)),
                            reads=[f"tp{ti}", hTn], writes=[hTn])
                for cc in (range(8) if part == 2 else []):
                    bi = mmr.next()
                    def f(e, cc=cc, bi=bi):
                        ins = None
                        for k in range(8):
                            ins = e.matmul(mm[bi][:], lhsT=w_mq_sb[:, k, cc * 128:(cc + 1) * 128], rhs=hT[:, k, :], start=(k == 0), stop=(k == 7))
                        return ins
                    S.op("tensor", f, reads=["w_mq_sb", hTn], writes=[f"mm{bi}"])
                    S.op("scalar", lambda e, cc=cc, bi=bi: e.activation(out=qmT[:, cc, :], in_=mm[bi][:], func=AF.Copy, scale=1.0 / 16),
                         reads=[f"mm{bi}", qmn], writes=[qmn])
                for h in (range(4) if part == 3 else []):
                    for mt in range(2):
                        bi = mmr.next()
                        def f(e, h=h, mt=mt, bi=bi):
                            e.matmul(mm[bi][:], lhsT=KmT[:, 2 * h, mt * 128:(mt + 1) * 128], rhs=qmT[:, 2 * h, :], start=True, stop=False)
                            return e.matmul(mm[bi][:], lhsT=KmT[:, 2 * h + 1, mt * 128:(mt + 1) * 128], rhs=qmT[:, 2 * h + 1, :], start=False, stop=True)
                        S.op("tensor", f, reads=["KmT", qmn], writes=[f"mm{bi}"])
                        S.op("scalar", lambda e, mt=mt, bi=bi: e.activation(out=Pm[mt][:], in_=mm[bi][:], func=AF.Exp),
                             reads=[f"mm{bi}"], writes=[f"Pm{mt}"])
                    bi = mmr.next()
                    def f(e, bi=bi):
                        e.matmul(mm[bi][:], lhsT=ones_b[:], rhs=Pm[0][:], start=True, stop=False)
                        return e.matmul(mm[bi][:], lhsT=ones_b[:], rhs=Pm[1][:], start=False, stop=True)
                    S.op("tensor", f, reads=["ones_b", "Pm0", "Pm1"], writes=[f"mm{bi}"])
                    S.op("vector", lambda e, bi=bi: e.reciprocal(out=rdn[:], in_=mm[bi][:]), reads=[f"mm{bi}"], writes=["rdn"])
                    for dvc in range(2):
                        bi = mmr.next()
                        def f(e, h=h, dvc=dvc, bi=bi):
                            c0 = h * 256 + dvc * 128
                            e.matmul(mm[bi][:], lhsT=Vm[:, 0, c0:c0 + 128], rhs=Pm[0][:], start=True, stop=False)
                            return e.matmul(mm[bi][:], lhsT=Vm[:, 1, c0:c0 + 128], rhs=Pm[1][:], start=False, stop=True)
                        S.op("tensor", f, reads=["Vm", "Pm0", "Pm1"], writes=[f"mm{bi}"])
                        S.op("vector", lambda e, h=h, dvc=dvc, bi=bi: e.tensor_tensor(out=omT[:, 2 * h + dvc, :], in0=mm[bi][:], in1=rdn[:], op=ALU.mult),
                             reads=[f"mm{bi}", "rdn", omn], writes=[omn])

            def outproj_ln(c, t):
                p_ = c % 2
                h1c, hT, qmT, omT = h1cs[p_], hTs[p_], qmTs[p_], omTs[p_]
                h1n, hTn, qmn, omn = f"h1c{p_}", f"hT{p_}", f"qmT{p_}", f"omT{p_}"
                tg = 4 * c + t
                b = tg % 2
                for hf in range(2):
                    bi = mmr.next()
                    def f(e, t=t, hf=hf, bi=bi):
                        ins = None
                        for k in range(8):
                            ins = e.matmul(mm[bi][:], lhsT=omT[:, k, t * 128:(t + 1) * 128], rhs=w_mo_sb[:, k, hf * 512:(hf + 1) * 512],
                                           start=(k == 0), stop=(k == 7))
                        return ins
                    S.op("tensor", f, reads=[omn, "w_mo_sb"], writes=[f"mm{bi}"])
                    S.op("vector", lambda e, t=t, hf=hf, bi=bi: e.scalar_tensor_tensor(
                        out=r2[:, hf * 512:(hf + 1) * 512], in0=h1c[:, t, hf * 512:(hf + 1) * 512], scalar=ALPHA, in1=mm[bi][:],
                        op0=ALU.mult, op1=ALU.add), reads=[h1n, f"mm{bi}", "r2"], writes=["r2"])
                layer_norm_tile(r2[:], h2o[b][:], g2_bc, b2_bc, bst2, bmv2, brs2, tmpn2[:], ("r2", f"h2o{b}", "tmpn2"))
                S.dma("sync", lambda e, tg=tg, b=b: e.dma_start(out=h2_d[tg * 128:(tg + 1) * 128, :], in_=h2o[b][:]),
                      reads=[f"h2o{b}"], key=f"h2s{b}")
                if DEBUG == "1b":
                    S.dma("sync", lambda e, tg=tg, b=b: e.dma_start(out=dbg["h"][tg * 128:(tg + 1) * 128, :], in_=h2o[b][:]),
                          reads=[f"h2o{b}"], key=f"dbgh{b}")
                S.op("scalar", lambda e, b=b: e.activation(out=h2b[b][:], in_=h2o[b][:], func=AF.Copy), reads=[f"h2o{b}"], writes=[f"h2b{b}"])

            def router(c, t):
                p_ = c % 2
                h1c, hT, qmT, omT = h1cs[p_], hTs[p_], qmTs[p_], omTs[p_]
                h1n, hTn, qmn, omn = f"h1c{p_}", f"hT{p_}", f"qmT{p_}", f"omT{p_}"
                tg = 4 * c + t
                b = tg % 2
                for kh in range(2):
                    bi = mmr.next()
                    def tr(e, b=b, kh=kh, bi=bi):
                        ins = None
                        for kk in range(4):
                            k = kh * 4 + kk
                            ins = e.transpose(out=mm[bi][:, kk * 128:(kk + 1) * 128], in_=h2o[b][:, k * 128:(k + 1) * 128], identity=ident_f[:])
                        return ins
                    S.op("tensor", tr, reads=[f"h2o{b}", "ident_f"], writes=[f"mm{bi}"])
                    S.op("vector", lambda e, kh=kh, bi=bi: e.tensor_copy(out=h2T[:, kh * 4:(kh + 1) * 4, :],
                                                                          in_=mm[bi][:].rearrange("p (k n) -> p k n", k=4)),
                         reads=[f"mm{bi}", "h2T"], writes=["h2T"])
                bi = mmr.next()
                def f(e, bi=bi):
                    ins = None
                    for k in range(8):
                        ins = e.matmul(mm[bi][:, 0:NE], lhsT=h2T[:, k, :], rhs=wr_sb[:, k, :], start=(k == 0), stop=(k == 7))
                    return ins
                S.op("tensor", f, reads=["h2T", "wr_sb"], writes=[f"mm{bi}"])
                S.op("vector", lambda e, bi=bi: e.tensor_tensor(out=lg[:], in0=mm[bi][:, 0:NE], in1=br_bc[:], op=ALU.add),
                     reads=[f"mm{bi}", "br_bc"], writes=["lg"])
                S.op("vector", lambda e: e.max(out=mx8r[:], in_=lg[:]), reads=["lg"], writes=["mx8r"])
                S.op("vector", lambda e: e.tensor_scalar(out=negm[:], in0=mx8r[:, 0:1], scalar1=-1.0, scalar2=None, op0=ALU.mult),
                     reads=["mx8r"], writes=["negm"])
                S.op("scalar", lambda e: e.activation(out=ex4[:], in_=mx8r[:, 0:4], func=AF.Exp, bias=negm[:, 0:1], accum_out=gsum[:, 0:1]),
                     reads=["mx8r", "negm", "gsum"], writes=["ex4", "gsum"])
                S.op("vector", lambda e: e.reciprocal(out=gsum[:], in_=gsum[:]), reads=["gsum"], writes=["gsum"])
                S.op("vector", lambda e, tg=tg: e.tensor_scalar(out=gates_all[:, tg, :], in0=ex4[:], scalar1=gsum[:, 0:1], scalar2=None, op0=ALU.mult),
                     reads=["ex4", "gsum", "gates_all"], writes=["gates_all"])
                S.op("vector", lambda e: e.tensor_scalar(out=selb[:], in0=lg[:], scalar1=mx8r[:, 3:4], scalar2=None, op0=ALU.is_ge),
                     reads=["lg", "mx8r"], writes=["selb"])
                bi = mmr.next()
                def f(e, bi=bi):
                    e.matmul(mm[bi][:, 0:NE], lhsT=ustr_b[:], rhs=selb[:], start=True, stop=True)
                    return e.matmul(mm[bi][:, 64:64 + NE], lhsT=ones_b[:], rhs=selb[:], start=True, stop=True)
                S.op("tensor", f, reads=["ustr_b", "ones_b", "selb"], writes=[f"mm{bi}"])
                S.op("vector", lambda e, bi=bi: e.tensor_tensor(out=slotm[:], in0=mm[bi][:, 0:NE], in1=cbase[:], op=ALU.add),
                     reads=[f"mm{bi}", "cbase"], writes=["slotm"])
                S.op("vector", lambda e: e.tensor_tensor(out=slotm[:], in0=slotm[:], in1=caphi[:], op=ALU.min),
                     reads=["slotm", "caphi"], writes=["slotm"])
                S.op("vector", lambda e, bi=bi: e.tensor_tensor(out=cbase[:], in0=mm[bi][:, 64:64 + NE], in1=cbase[:], op=ALU.add),
                     reads=[f"mm{bi}", "cbase"], writes=["cbase"])
                for k in range(4):
                    S.op("vector", lambda e, k=k: e.scalar_tensor_tensor(out=ohp[:], in0=lg[:], scalar=mx8r[:, k:k + 1], in1=slotm[:],
                                                                         op0=ALU.is_equal, op1=ALU.mult),
                         reads=["lg", "mx8r", "slotm"], writes=["ohp"])
                    S.op("vector", lambda e, k=k: e.reduce_sum(out=slotf[:, k:k + 1], in_=ohp[:], axis=mybir.AxisListType.X),
                         reads=["ohp", "slotf"], writes=["slotf"])
                S.op("vector", lambda e, tg=tg: e.tensor_copy(out=slots_all[:, tg, :], in_=slotf[:]), reads=["slotf", "slots_all"], writes=["slots_all"])
                for k in range(4):
                    S.dma("gpsimd", lambda e, tg=tg, k=k, b=b: e.indirect_dma_start(
                        out=xg_d, out_offset=bass.IndirectOffsetOnAxis(ap=slots_all[:, tg, k:k + 1], axis=0), in_=h2b[b][:], in_offset=None),
                        reads=["slots_all", f"h2b{b}"], writes=["xg_d"], key=f"sc{k}")

            for part in range(4):
                stageA(0, part)
            for c in range(NCH):
                for t in range(4):
                    outproj_ln(c, t)
                    if t > 0:
                        router(c, t - 1)
                    if c + 1 < NCH:
                        stageA(c + 1, t)
                router(c, 3)
            if DEBUG == "1b":
                dump(S, "slots", slots_all[:].rearrange("p a b -> p (a b)"), [128, NT * 4], I32, ["slots_all"])
                dump(S, "gates", gates_all[:].rearrange("p a b -> p (a b)"), [128, NT * 4], F32, ["gates_all"])
            S.full_barrier()
            S.flush(nc)
        if DEBUG == "1b":
            return nc

        BLKS = [(0, 512), (512, CAP - 512)]
        NTE = CAP // 128
        with ExitStack() as p3:
            wgu = [sb(p3, f"wgu{i}", [128, 8, 2 * D], BF16) for i in range(2)]
            wdn = [sb(p3, f"wdn{i}", [128, 8, D], BF16) for i in range(2)]
            bgr = sb(p3, "bgr", [NE, 2 * D], F32)
            bguT = sb(p3, "bguT", [128, 16, NE], F32)
            bgu7 = sb(p3, "bgu7", [128, 8, NE], F32)
            bdn = [sb(p3, f"bdn{i}", [128, D], F32) for i in range(2)]
            xg = [sb(p3, f"xg{i}", [128, NTE, D], BF16) for i in range(2)]
            xgTs = [sb(p3, f"xgT{i}", [128, 8, CAP], BF16) for i in range(2)]
            actT = sb(p3, "actT", [128, 8, CAP], BF16)
            s0 = [sb(p3, f"s0_{i}", [128, 512], F32) for i in range(2)]
            gcl = [sb(p3, f"gc{i}", [128, 512], F32) for i in range(2)]
            ucl = [sb(p3, f"uc{i}", [128, 512], F32) for i in range(2)]
            ysb = [sb(p3, f"ysb{i}", [128, D], F32) for i in range(2)]
            mm = [ps(p3, f"mmC{i}", [128, 512], F32) for i in range(6)]
            tp = [ps(p3, f"tpC{i}", [128, 1024], BF16) for i in range(2)]
            mmr = Rot(list(range(6))); tpr = Rot([0, 1])

            S.dma("sync", lambda e: e.dma_start(out=bgr[:], in_=b_gu_d), writes=["bgr"], key="c1")
            bi = mmr.next()
            def trb(e, bi=bi):
                ins = None
                for cidx in range(16):
                    ins = e.transpose(out=mm[bi][:, cidx * NE:(cidx + 1) * NE], in_=bgr[0:NE, cidx * 128:(cidx + 1) * 128], identity=ident_f[0:NE, 0:NE])
                return ins
            S.op("tensor", trb, reads=["bgr", "ident_f"], writes=[f"mm{bi}"])
            S.op("vector", lambda e, bi=bi: e.tensor_copy(out=bguT[:].rearrange("p a b -> p (a b)"), in_=mm[bi][:]), reads=[f"mm{bi}"], writes=["bguT"])
            S.op("vector", lambda e: e.tensor_scalar(out=bgu7[:], in0=bguT[:, 8:16, :], scalar1=7.0, scalar2=None, op0=ALU.add),
                 reads=["bguT"], writes=["bgu7"])

            def load_expert(ex):
                bf = ex % 2
                S.dma("gpsimd", lambda e: e.dma_start(out=wgu[bf][:], in_=w_gu_d[ex].rearrange("(k p) n -> p k n", p=128)),
                      writes=[f"wgu{bf}"], key=f"wg{bf}")
                S.dma("gpsimd", lambda e: e.dma_start(out=wdn[bf][:], in_=w_dn_d[ex].rearrange("(k p) n -> p k n", p=128)),
                      writes=[f"wdn{bf}"], key=f"wd{bf}")
                S.dma("sync", lambda e: e.dma_start(out=bdn[bf][:], in_=bc(b_dn_d[ex], D)), writes=[f"bdn{bf}"], key=f"bd{bf}")
                S.dma("sync", lambda e: e.dma_start(out=xg[bf][:], in_=xg_d[ex * CAP:(ex + 1) * CAP, :].rearrange("(t p) d -> p t d", p=128)),
                      reads=["xg_d"], writes=[f"xg{bf}"], key=f"xgl{bf}")

            def prep_expert(exx):
                pb = exx % 2
                for t in range(NTE):
                    for kh in range(2):
                        ti = tpr.next()
                        def tr(e, pb=pb, t=t, kh=kh, ti=ti):
                            ins = None
                            for kk in range(4):
                                k = kh * 4 + kk
                                ins = e.transpose(out=tp[ti][:, kk * 128:(kk + 1) * 128], in_=xg[pb][:, t, k * 128:(k + 1) * 128], identity=ident_b[:])
                            return ins
                        S.op("tensor", tr, reads=[f"xg{pb}", "ident_b"], writes=[f"tp{ti}"])
                        S.op("scalar", lambda e, kh=kh, ti=ti, t=t, pb=pb: e.activation(
                            out=xgTs[pb][:, kh * 4:(kh + 1) * 4, t * 128:(t + 1) * 128], in_=tp[ti][:, 0:512].rearrange("p (k n) -> p k n", k=4), func=AF.Copy),
                            reads=[f"tp{ti}", f"xgT{pb}"], writes=[f"xgT{pb}"])

            load_expert(0)
            prep_expert(0)
            for ex in range(NE_RUN):
                bf = ex % 2
                if ex + 1 < NE_RUN:
                    load_expert(ex + 1)
                it = 0
                for j in range(8 if P2_STEPS >= 2 else 0):
                    for (b0, bw) in BLKS:
                        big = mmr.next(); biu = mmr.next()
                        def fg(e, bf=bf, j=j, b0=b0, bw=bw, big=big):
                            ins = None
                            for k in range(8):
                                ins = e.matmul(mm[big][:, 0:bw], lhsT=wgu[bf][:, k, j * 128:(j + 1) * 128], rhs=xgTs[bf][:, k, b0:b0 + bw],
                                               start=(k == 0), stop=(k == 7))
                            return ins
                        def fu(e, bf=bf, j=j, b0=b0, bw=bw, biu=biu):
                            ins = None
                            for k in range(8):
                                ins = e.matmul(mm[biu][:, 0:bw], lhsT=wgu[bf][:, k, D + j * 128:D + (j + 1) * 128], rhs=xgTs[bf][:, k, b0:b0 + bw],
                                               start=(k == 0), stop=(k == 7))
                            return ins
                        S.op("tensor", fg, reads=[f"wgu{bf}", f"xgT{bf}"], writes=[f"mm{big}"])
                        S.op("tensor", fu, reads=[f"wgu{bf}", f"xgT{bf}"], writes=[f"mm{biu}"])
                        i2 = it % 2; it += 1
                        S.op("vector", lambda e, big=big, bw=bw, j=j, ex=ex, i2=i2: e.tensor_scalar(
                            out=gcl[i2][:, 0:bw], in0=mm[big][:, 0:bw], scalar1=bguT[:, j, ex:ex + 1], scalar2=7.0, op0=ALU.add, op1=ALU.min),
                            reads=[f"mm{big}", "bguT"], writes=[f"gc{i2}"])
                        S.op("scalar", lambda e, bw=bw, i2=i2: e.activation(out=s0[i2][:, 0:bw], in_=gcl[i2][:, 0:bw], func=AF.Silu, scale=1.702),
                             reads=[f"gc{i2}"], writes=[f"s0_{i2}"])
                        S.op("scalar", lambda e, biu=biu, bw=bw, j=j, ex=ex, i2=i2: e.activation(
                            out=ucl[i2][:, 0:bw], in_=mm[biu][:, 0:bw], func=AF.Relu, bias=bgu7[:, j, ex:ex + 1]),
                            reads=[f"mm{biu}", "bgu7"], writes=[f"uc{i2}"])
                        S.op("vector", lambda e, bw=bw, i2=i2: e.tensor_scalar(
                            out=ucl[i2][:, 0:bw], in0=ucl[i2][:, 0:bw], scalar1=14.0, scalar2=-6.0, op0=ALU.min, op1=ALU.add),
                            reads=[f"uc{i2}"], writes=[f"uc{i2}"])
                        S.op("vector", lambda e, bw=bw, b0=b0, j=j, i2=i2: e.scalar_tensor_tensor(
                            out=actT[:, j, b0:b0 + bw], in0=s0[i2][:, 0:bw], scalar=1.0 / 1.702, in1=ucl[i2][:, 0:bw], op0=ALU.mult, op1=ALU.mult),
                            reads=[f"s0_{i2}", f"uc{i2}", "actT"], writes=["actT"])
                if ex + 1 < NE_RUN:
                    prep_expert(ex + 1)
                for t in range(NTE if P2_STEPS >= 4 else 0):
                    yb = t % 2
                    for hf in range(2):
                        bi = mmr.next()
                        def fd(e, bf=bf, t=t, hf=hf, bi=bi):
                            ins = None
                            for j in range(8):
                                ins = e.matmul(mm[bi][:], lhsT=actT[:, j, t * 128:(t + 1) * 128], rhs=wdn[bf][:, j, hf * 512:(hf + 1) * 512],
                                               start=(j == 0), stop=(j == 7))
                            return ins
                        S.op("tensor", fd, reads=["actT", f"wdn{bf}"], writes=[f"mm{bi}"])
                        S.op("vector", lambda e, bf=bf, yb=yb, hf=hf, bi=bi: e.tensor_tensor(
                            out=ysb[yb][:, hf * 512:(hf + 1) * 512], in0=mm[bi][:], in1=bdn[bf][:, hf * 512:(hf + 1) * 512], op=ALU.add),
                            reads=[f"mm{bi}", f"bdn{bf}", f"ysb{yb}"], writes=[f"ysb{yb}"])
                    r0 = ex * CAP + t * 128
                    S.dma("sync", lambda e, yb=yb, r0=r0: e.dma_start(out=ys_d[r0:r0 + 128, :], in_=ysb[yb][:]),
                          reads=[f"ysb{yb}"], writes=["ys_d"], key=f"yst{yb}")
            S.full_barrier()
            S.flush(nc)
        if DEBUG == "2":
            return nc

        with ExitStack() as p4:
            g3_bc = sb(p4, "g3_bc", [128, D], F32)
            b3_bc = sb(p4, "b3_bc", [128, D], F32)
            h2t = [sb(p4, f"h2t{i}", [128, D], F32) for i in range(2)]
            yk = [[sb(p4, f"yk{i}_{k}", [128, D], F32) for k in range(4)] for i in range(2)]
            acc = sb(p4, "acc", [128, D], F32)
            tmpn3 = sb(p4, "tmpn3", [128, D], F32)
            outt = [sb(p4, f"outt{i}", [128, D], F32) for i in range(2)]
            bst3 = sb(p4, "bst3", [128, 2, 6], F32)
            bmv3 = sb(p4, "bmv3", [128, 2], F32)
            brs3 = sb(p4, "brs3", [128, 1], F32)
            S.dma("sync", lambda e: e.dma_start(out=g3_bc[:], in_=bc(ln3g_d, D)), writes=["g3_bc"], key="c1")
            S.dma("sync", lambda e: e.dma_start(out=b3_bc[:], in_=bc(ln3b_d, D)), writes=["b3_bc"], key="c2")
            for tg in range(NT):
                b = tg % 2
                S.dma("sync", lambda e, tg=tg, b=b: e.dma_start(out=h2t[b][:], in_=h2_d[tg * 128:(tg + 1) * 128, :]),
                      writes=[f"h2t{b}"], key=f"h2l{b}")
                for k in range(4):
                    S.dma("gpsimd", lambda e, tg=tg, k=k, b=b: e.indirect_dma_start(
                        out=yk[b][k][:], out_offset=None, in_=ys_d, in_offset=bass.IndirectOffsetOnAxis(ap=slots_all[:, tg, k:k + 1], axis=0)),
                        reads=["ys_d", "slots_all"], writes=[f"yk{b}_{k}"], key=f"gk{b}{k}")
                S.op("vector", lambda e, b=b: e.tensor_scalar(out=acc[:], in0=h2t[b][:], scalar1=ALPHA, scalar2=None, op0=ALU.mult),
                     reads=[f"h2t{b}", "acc"], writes=["acc"])
                for k in range(4):
                    S.op("vector", lambda e, b=b, k=k, tg=tg: e.scalar_tensor_tensor(
                        out=acc[:], in0=yk[b][k][:], scalar=gates_all[:, tg, k:k + 1], in1=acc[:], op0=ALU.mult, op1=ALU.add),
                        reads=[f"yk{b}_{k}", "gates_all", "acc"], writes=["acc"])
                layer_norm_tile(acc[:], outt[b][:], g3_bc, b3_bc, bst3, bmv3, brs3, tmpn3[:], ("acc", f"outt{b}", "tmpn3"))
                S.dma("scalar", lambda e, tg=tg, b=b: e.dma_start(out=out_d[tg * 128:(tg + 1) * 128, :], in_=outt[b][:]),
                      reads=[f"outt{b}"], key=f"os{b}")
            S.full_barrier()
            S.flush(nc)
    return nc


_PROG = None


def kernel(**inputs):
    global _PROG
    if _PROG is None:
        _PROG = build_program()
    nc = _PROG
    B = inputs["x"].shape[0]
    in_maps = []
    for b in range(B):
        m = {}
        for k, v in inputs.items():
            a = np.asarray(v)
            if k in ("x", "mem"):
                m[k] = np.ascontiguousarray(a[b])
            else:
                m[k] = np.ascontiguousarray(a[0])
        in_maps.append(m)
    res = run_bass_kernel_spmd(nc, in_maps, core_ids=list(range(B)))
    return np.stack([np.asarray(r["out"]) for r in res.results], axis=0)
```
